# Optimizing a Trainium2 kernel written in Bass

```python
import jax, jax.numpy as jnp
from jax import lax
import numpy as np

D_MODEL = 1024
BATCH = 8
SEQ = 4096
DEPTH = 2

GRID_W = 64
CTX_LEN = 256
EPS = 1e-6
ROPE_BASE = 10000.0

MLA_HEADS = 8
MLA_Q_RANK = D_MODEL // 4
MLA_KV_RANK = D_MODEL // 8
MLA_NOPE = 64
MLA_ROPE = 32
MLA_V = 64
MLA_IN = MLA_Q_RANK + MLA_KV_RANK + MLA_ROPE
MLA_SCALE = (MLA_NOPE + MLA_ROPE) ** -0.5
Q_BLOCK = 128

CONV_DIM = D_MODEL // 2
CONV_WIDTH = 3
EVEN_IN = MLA_IN + 3 * CONV_DIM
EVEN_MIX = MLA_HEADS * MLA_V + CONV_DIM

ML_HEADS = 4
ML_DQK = D_MODEL // 8
ML_DV = D_MODEL // 4
ML_CHUNK = 64
ML_QKVG = ML_HEADS * (2 * ML_DQK + ML_DV) + 4 * ML_HEADS
ML_MIX = ML_HEADS * ML_DV
ODD_IN = ML_QKVG + ML_MIX
F_BIAS = 3.0

PEER_HEADS = 8
PEER_NKEYS = 128
PEER_N = PEER_NKEYS * PEER_NKEYS
PEER_DKEY = 256
PEER_DHALF = PEER_DKEY // 2
PEER_TOPK = 16
PEER_BLOCK = 128

kernel_name = 'hybrid_mla_conv_mlstm_peer_dit'


def rmsnorm(x, g):
    x32 = x.astype(jnp.float32)
    y = x32 * lax.rsqrt(jnp.mean(x32 * x32, axis=-1, keepdims=True) + EPS)
    return (y * g.astype(jnp.float32)).astype(x.dtype)


def modulate(h, shift, scale):
    return h * (1 + scale) + shift


def axial_rope(rows, dim):
    n_freq = dim // 4
    inv = ROPE_BASE ** (-jnp.arange(n_freq, dtype=jnp.float32) / n_freq)
    row = jnp.repeat(jnp.arange(rows, dtype=jnp.float32), GRID_W)
    col = jnp.tile(jnp.arange(GRID_W, dtype=jnp.float32), rows)
    ang = jnp.concatenate([row[:, None] * inv, col[:, None] * inv], axis=-1)
    return jnp.cos(ang), jnp.sin(ang)


def apply_rope(x, cos, sin):
    half = x.shape[-1] // 2
    x32 = x.astype(jnp.float32)
    x1, x2 = x32[..., :half], x32[..., half:]
    return jnp.concatenate([x1 * cos - x2 * sin, x1 * sin + x2 * cos], axis=-1).astype(x.dtype)


def mla_q(p_q, q_norm, w_uq, cos, sin):
    B, T, _ = p_q.shape
    q = (rmsnorm(p_q, q_norm) @ w_uq).reshape(B, T, MLA_HEADS, MLA_NOPE + MLA_ROPE)
    q_n, q_r = q[..., :MLA_NOPE], q[..., MLA_NOPE:]
    if cos is not None:
        q_r = apply_rope(q_r, cos[:, None, :], sin[:, None, :])
    return q_n, q_r


def mla_kv(p_kv, kv_norm, w_ukv, cos, sin):
    B, T, _ = p_kv.shape
    kv = (rmsnorm(p_kv[..., :MLA_KV_RANK], kv_norm) @ w_ukv).reshape(B, T, MLA_HEADS, MLA_NOPE + MLA_V)
    k_r = p_kv[..., MLA_KV_RANK:]
    if cos is not None:
        k_r = apply_rope(k_r, cos, sin)
    return kv[..., :MLA_NOPE], k_r, kv[..., MLA_NOPE:]


def mla_attend(q_n, q_r, k_n, k_r, v):
    s = jnp.einsum('bqhd,bkhd->bhqk', q_n, k_n) + jnp.einsum('bqhr,bkr->bhqk', q_r, k_r)
    p = jax.nn.softmax(s.astype(jnp.float32) * MLA_SCALE, axis=-1).astype(v.dtype)
    out = jnp.einsum('bhqk,bkhd->bqhd', p, v)
    return out.reshape(out.shape[0], out.shape[1], MLA_HEADS * MLA_V)


def short_conv(p, conv_w):
    T = p.shape[1]
    b_gate, c_gate, u = p[..., :CONV_DIM], p[..., CONV_DIM:2 * CONV_DIM], p[..., 2 * CONV_DIM:]
    z = jnp.pad(c_gate * u, ((0, 0), (1, 1), (0, 0)))
    y = conv_w[0] * z[:, :T] + conv_w[1] * z[:, 1:T + 1] + conv_w[2] * z[:, 2:]
    return b_gate * y


def even_mixer(nc, nl, w_in, q_norm, kv_norm, w_uq, w_ukv, conv_w, w_out, cos, sin, need_ctx):
    B, S, _ = nl.shape
    pl = nl @ w_in
    if need_ctx:
        pc = nc @ w_in
        pc_kv = pc[..., MLA_Q_RANK:MLA_IN]
    else:
        pc_kv = nc @ w_in[:, MLA_Q_RANK:MLA_IN]
    qn_l, qr_l = mla_q(pl[..., :MLA_Q_RANK], q_norm, w_uq, cos, sin)
    kn_l, kr_l, v_l = mla_kv(pl[..., MLA_Q_RANK:MLA_IN], kv_norm, w_ukv, cos, sin)
    kn_c, kr_c, v_c = mla_kv(pc_kv, kv_norm, w_ukv, None, None)
    kn = jnp.concatenate([kn_l, kn_c], axis=1)
    kr = jnp.concatenate([kr_l, kr_c], axis=1)
    vv = jnp.concatenate([v_l, v_c], axis=1)
    blocks = lambda a: jnp.moveaxis(a.reshape((B, S // Q_BLOCK, Q_BLOCK) + a.shape[2:]), 1, 0)
    ol = lax.map(lambda qb: mla_attend(qb[0], qb[1], kn, kr, vv), (blocks(qn_l), blocks(qr_l)))
    ol = jnp.moveaxis(ol, 0, 1).reshape(B, S, MLA_HEADS * MLA_V)
    yl = jnp.concatenate([ol, short_conv(pl[..., MLA_IN:], conv_w)], axis=-1) @ w_out
    yc = None
    if need_ctx:
        qn_c, qr_c = mla_q(pc[..., :MLA_Q_RANK], q_norm, w_uq, None, None)
        oc = mla_attend(qn_c, qr_c, kn_c, kr_c, v_c)
        yc = jnp.concatenate([oc, short_conv(pc[..., MLA_IN:], conv_w)], axis=-1) @ w_out
    return yc, yl


def mlstm_inputs(p, gate_b):
    B, T, _ = p.shape
    qk, vd = ML_HEADS * ML_DQK, ML_HEADS * ML_DV
    heads = lambda a, d: a.reshape(B, T, ML_HEADS, d).transpose(0, 2, 1, 3)
    q = heads(p[..., :qk], ML_DQK) * (ML_DQK ** -0.5)
    k = heads(p[..., qk:2 * qk], ML_DQK)
    v = heads(p[..., 2 * qk:2 * qk + vd], ML_DV)
    g = p[..., 2 * qk + vd:ML_QKVG].astype(jnp.float32) + gate_b.astype(jnp.float32)
    g = g.reshape(B, T, 4, ML_HEADS).transpose(2, 0, 3, 1)
    return q, k, v, g


def mlstm_scan(q, k, v, ig, lf, state, with_output):
    B, H, T, _ = q.shape
    L = ML_CHUNK
    nc = T // L
    chunks = lambda a: jnp.moveaxis(a.reshape(a.shape[:2] + (nc, L) + a.shape[3:]), 2, 0)
    xs = (chunks(q), chunks(k), chunks(v), chunks(ig), chunks(lf))
    causal = jnp.tril(jnp.ones((L, L), dtype=bool))

    def step(carry, xc):
        C, n, m = carry
        qc, kc, vc, ic, fc = xc
        b = jnp.cumsum(fc, axis=-1)
        b_end = b[..., -1]
        a = ic + b_end[..., None] - b
        m_new = jnp.maximum(b_end + m, jnp.max(a, axis=-1))
        w_s = jnp.exp(a - m_new[..., None])
        decay = jnp.exp(b_end + m - m_new)
        C_new = decay[..., None, None] * C + jnp.einsum('bhs,bhsk,bhsv->bhkv', w_s, kc, vc)
        n_new = decay[..., None] * n + jnp.einsum('bhs,bhsk->bhk', w_s, kc)
        if with_output:
            dmat = jnp.where(causal, b[..., :, None] - b[..., None, :] + ic[..., None, :], -jnp.inf)
            m_t = jnp.maximum(b + m[..., None], jnp.max(dmat, axis=-1))
            inter = jnp.exp(b + m[..., None] - m_t)
            sc = jnp.einsum('bhtk,bhsk->bhts', qc, kc) * jnp.exp(dmat - m_t[..., None])
            num = jnp.einsum('bhts,bhsv->bhtv', sc, vc) + inter[..., None] * jnp.einsum('bhtk,bhkv->bhtv', qc, C)
            den = jnp.sum(sc, axis=-1) + inter * jnp.einsum('bhtk,bhk->bht', qc, n)
            h = num / jnp.maximum(jnp.abs(den), jnp.exp(-m_t))[..., None]
        else:
            h = None
        return (C_new, n_new, m_new), h

    state, hs = lax.scan(step, state, xs)
    if with_output:
        hs = jnp.moveaxis(hs, 0, 2).reshape(B, H, T, ML_DV)
    return state, hs


def odd_mixer(nc, nl, w_in, gate_b, head_g, w_out, need_ctx):
    pc = nc @ (w_in if need_ctx else w_in[:, :ML_QKVG])
    pl = nl @ w_in
    qc, kc, vc, gc = mlstm_inputs(pc, gate_b)
    ql, kl, vl, gl = mlstm_inputs(pl, gate_b)
    B = nl.shape[0]
    zero = (jnp.zeros((B, ML_HEADS, ML_DQK, ML_DV), jnp.float32),
            jnp.zeros((B, ML_HEADS, ML_DQK), jnp.float32),
            jnp.zeros((B, ML_HEADS), jnp.float32))
    flip = lambda a: jnp.flip(a, axis=2)
    h_c, h_l = None, None
    for d in range(2):
        ctx_seq = (qc, kc, vc, gc[2 * d], jax.nn.log_sigmoid(gc[2 * d + 1]))
        lat_seq = (ql, kl, vl, gl[2 * d], jax.nn.log_sigmoid(gl[2 * d + 1]))
        if d == 1:
            ctx_seq = tuple(flip(a) for a in ctx_seq)
            lat_seq = tuple(flip(a) for a in lat_seq)
        state, hc_d = mlstm_scan(*ctx_seq, zero, need_ctx)
        _, hl_d = mlstm_scan(*lat_seq, state, True)
        if d == 1:
            hl_d = flip(hl_d)
            hc_d = flip(hc_d) if need_ctx else None
        h_l = hl_d if h_l is None else h_l + hl_d
        if need_ctx:
            h_c = hc_d if h_c is None else h_c + hc_d

    def readout(h, p):
        Bh, _, T, _ = h.shape
        h = h.transpose(0, 2, 1, 3).astype(p.dtype)
        h = rmsnorm(h, head_g.reshape(ML_HEADS, ML_DV)).reshape(Bh, T, ML_MIX)
        return (jax.nn.sigmoid(p[..., ML_QKVG:]) * h) @ w_out

    yl = readout(h_l, pl)
    yc = readout(h_c, pc) if need_ctx else None
    return yc, yl


def peer(h, w_q, subkeys, u, v):
    B, T, D = h.shape
    tok = h.reshape(-1, PEER_BLOCK, D)

    def block(xb):
        q = (xb @ w_q).reshape(PEER_BLOCK, PEER_HEADS, 2, PEER_DHALF)
        s = jnp.einsum('thpd,hpkd->thpk', q, subkeys).astype(jnp.float32)
        s1, i1 = lax.top_k(s[:, :, 0], PEER_TOPK)
        s2, i2 = lax.top_k(s[:, :, 1], PEER_TOPK)
        cand = (s1[..., :, None] + s2[..., None, :]).reshape(PEER_BLOCK, PEER_HEADS, PEER_TOPK * PEER_TOPK)
        cidx = (i1[..., :, None] * PEER_NKEYS + i2[..., None, :]).reshape(PEER_BLOCK, PEER_HEADS, PEER_TOPK * PEER_TOPK)
        top, pos = lax.top_k(cand, PEER_TOPK)
        idx = jnp.take_along_axis(cidx, pos, axis=-1)
        g = jax.nn.softmax(top, axis=-1)
        ue, ve = u[idx], v[idx]
        act = jax.nn.gelu(jnp.einsum('td,thkd->thk', xb, ue).astype(jnp.float32), approximate=False)
        return jnp.einsum('thk,thkd->td', (g * act).astype(xb.dtype), ve)

    return lax.map(block, tok).reshape(B, T, D)


def setup_inputs(seed: int = 0) -> dict:
    key = jax.random.key(seed)
    ks = iter(jax.random.split(key, 32))
    nrm = lambda shape, s: jax.random.normal(next(ks), shape, jnp.float32) * s
    n_even = (DEPTH + 1) // 2
    n_odd = DEPTH // 2
    D = D_MODEL
    gate_b = (nrm((n_odd, 4, ML_HEADS), 0.1)
              + jnp.array([0.0, F_BIAS, 0.0, F_BIAS], jnp.float32)[:, None]).reshape(n_odd, 4 * ML_HEADS)
    return {
        'x': nrm((BATCH, SEQ, D), 1.0),
        'c': nrm((BATCH, D), 1.0),
        'ctx': nrm((BATCH, CTX_LEN, D), 1.0),
        'c_ctx': nrm((D,), 1.0),
        'norm1_g': 1.0 + nrm((DEPTH, D), 0.02),
        'norm2_g': 1.0 + nrm((DEPTH, D), 0.02),
        'w_mod': nrm((DEPTH, D, 6 * D), 0.5 * D ** -0.5),
        'b_mod': nrm((DEPTH, 6 * D), 0.02),
        'even_w_in': nrm((n_even, D, EVEN_IN), D ** -0.5),
        'mla_q_norm': 1.0 + nrm((n_even, MLA_Q_RANK), 0.02),
        'mla_kv_norm': 1.0 + nrm((n_even, MLA_KV_RANK), 0.02),
        'mla_w_uq': nrm((n_even, MLA_Q_RANK, MLA_HEADS * (MLA_NOPE + MLA_ROPE)), MLA_Q_RANK ** -0.5),
        'mla_w_ukv': nrm((n_even, MLA_KV_RANK, MLA_HEADS * (MLA_NOPE + MLA_V)), MLA_KV_RANK ** -0.5),
        'conv_w': nrm((n_even, CONV_WIDTH, CONV_DIM), CONV_WIDTH ** -0.5),
        'even_w_out': nrm((n_even, EVEN_MIX, D), EVEN_MIX ** -0.5),
        'odd_w_in': nrm((n_odd, D, ODD_IN), D ** -0.5),
        'mlstm_gate_b': gate_b,
        'mlstm_head_g': 1.0 + nrm((n_odd, ML_MIX), 0.02),
        'odd_w_out': nrm((n_odd, ML_MIX, D), ML_MIX ** -0.5),
        'peer_w_q': nrm((DEPTH, D, PEER_HEADS * PEER_DKEY), D ** -0.5),
        'peer_subkeys': nrm((DEPTH, PEER_HEADS, 2, PEER_NKEYS, PEER_DHALF), PEER_DHALF ** -0.5),
        'peer_u': nrm((DEPTH, PEER_N, D), D ** -0.5),
        'peer_v': nrm((DEPTH, PEER_N, D), PEER_HEADS ** -0.5),
        'norm_f_g': 1.0 + nrm((D,), 0.02),
    }


def reference(x, c, ctx, c_ctx, norm1_g, norm2_g, w_mod, b_mod,
              even_w_in, mla_q_norm, mla_kv_norm, mla_w_uq, mla_w_ukv, conv_w, even_w_out,
              odd_w_in, mlstm_gate_b, mlstm_head_g, odd_w_out,
              peer_w_q, peer_subkeys, peer_u, peer_v, norm_f_g):
    B, S, D = x.shape
    rows = S // GRID_W
    cos, sin = axial_rope(rows, MLA_ROPE)
    hl, hc = x, ctx
    n_ctx = ctx.shape[1]
    for i in range(DEPTH):
        last = i == DEPTH - 1
        j = i // 2
        mod_l = [m[:, None, :] for m in jnp.split(jax.nn.silu(c) @ w_mod[i] + b_mod[i], 6, axis=-1)]
        mod_c = jnp.split(jax.nn.silu(c_ctx) @ w_mod[i] + b_mod[i], 6, axis=-1)
        nl = modulate(rmsnorm(hl, norm1_g[i]), mod_l[0], mod_l[1])
        nc = modulate(rmsnorm(hc, norm1_g[i]), mod_c[0], mod_c[1])
        if i % 2 == 0:
            yc, yl = even_mixer(nc, nl, even_w_in[j], mla_q_norm[j], mla_kv_norm[j], mla_w_uq[j],
                                mla_w_ukv[j], conv_w[j], even_w_out[j], cos, sin, not last)
        else:
            yc, yl = odd_mixer(nc, nl, odd_w_in[j], mlstm_gate_b[j], mlstm_head_g[j], odd_w_out[j], not last)
        hl = hl + mod_l[2] * yl
        nl = modulate(rmsnorm(hl, norm2_g[i]), mod_l[3], mod_l[4])
        if last:
            hl = hl + mod_l[5] * peer(nl, peer_w_q[i], peer_subkeys[i], peer_u[i], peer_v[i])
        else:
            hc = hc + mod_c[2] * yc
            nc = modulate(rmsnorm(hc, norm2_g[i]), mod_c[3], mod_c[4])
            ff = peer(jnp.concatenate([nc, nl], axis=1), peer_w_q[i], peer_subkeys[i], peer_u[i], peer_v[i])
            hc = hc + mod_c[5] * ff[:, :n_ctx]
            hl = hl + mod_l[5] * ff[:, n_ctx:]
    return rmsnorm(hl, norm_f_g)
```

```python
import math
import numpy as np
from contextlib import ExitStack, contextmanager
import concourse.bass as bass
import concourse.mybir as mybir
from concourse.bass_utils import run_bass_kernel_spmd

F32 = mybir.dt.float32
BF16 = mybir.dt.bfloat16
U32 = mybir.dt.uint32
ALU = mybir.AluOpType
AF = mybir.ActivationFunctionType
AX = mybir.AxisListType

D = 1024
SEQ = 4096
NCTX = 256
NT_ALL = 34
EPS = 1e-6
MLA_SCALE = 96 ** -0.5
DBG_TILES = None
DBG_PTILES = None
SKIP = set()
SB_WORDS = 51200


class Buf:
    __slots__ = ("ap", "last_write", "reads", "name", "psum", "root")

    def __init__(self, ap, name="", psum=False, root=None):
        self.root = root.root if root is not None else self
        self.ap = ap
        self.last_write = None
        self.reads = []
        self.name = name
        self.psum = psum

    def __getitem__(self, k):
        return self.ap[k]


class Op:
    __slots__ = ("eng", "fn", "deps", "flag", "is_dma", "sem", "val", "slot")

    def __init__(self, eng, fn, is_dma):
        self.eng = eng
        self.fn = fn
        self.deps = []
        self.flag = False
        self.is_dma = is_dma
        self.sem = None
        self.val = None
        self.slot = None


ENGS = ["tensor", "vector", "scalar", "gpsimd", "sync"]
N_CSEM = 3
N_DSEM = {"sync": 44, "scalar": 4, "gpsimd": 36}


class Prog:
    def __init__(self, nc):
        self.nc = nc
        self.q = {e: [] for e in ENGS}
        self.last_real = {e: None for e in ENGS}
        self.dma_hist = {e: [] for e in N_DSEM}

    def add(self, eng, fn, reads=(), writes=(), dma=False):
        op = Op(eng, fn, dma)
        deps = []
        reads = [b.root for b in reads]
        writes = [b.root for b in writes]
        writes = list(writes) + [b for b in reads if b.psum and b not in writes]
        for b in reads:
            if b.last_write is not None:
                deps.append(b.last_write)
        for b in writes:
            if b.last_write is not None:
                deps.append(b.last_write)
            deps.extend(b.reads)
        seen = set()
        for d in deps:
            if id(d) in seen:
                continue
            seen.add(id(d))
            if d.eng == eng and not d.is_dma and not dma:
                if eng == "tensor":
                    continue
                if not any(b.last_write is d for b in reads):
                    continue
            op.deps.append(d)
            d.flag = True
        for b in reads:
            if not b.psum:
                b.reads.append(op)
        for b in writes:
            b.last_write = op
            b.reads = []
        if dma:
            h = self.dma_hist[eng]
            n = N_DSEM[eng]
            k = len(h)
            op.slot = k % n
            op.val = 16 * (k // n + 1)
            if k >= n:
                op.deps.append(h[k - n])
            h.append(op)
        elif fn is not None:
            self.last_real[eng] = op
        self.q[eng].append(op)
        return op

    def barrier(self):
        deps = []
        for e in ENGS:
            d = self.last_real[e]
            if d is not None:
                d.flag = True
                deps.append(d)
        for e, h in self.dma_hist.items():
            deps.extend(h[-N_DSEM[e]:])
        for e in ENGS:
            op = Op(e, None, False)
            op.deps = list(deps)
            self.q[e].append(op)

    def emit(self, stack):
        nc = self.nc
        csems = {e: [stack.enter_context(nc.semaphore(f"c_{e}_{i}")) for i in range(N_CSEM)]
                 for e in ["tensor", "vector", "scalar", "gpsimd"]}
        dsems = {e: [stack.enter_context(nc.semaphore(f"d_{e}_{i}")) for i in range(n)]
                 for e, n in N_DSEM.items()}
        for e in ENGS:
            nflag = 0
            for op in self.q[e]:
                if op.is_dma:
                    op.sem = dsems[e][op.slot]
                elif op.flag:
                    op.sem = csems[e][nflag % N_CSEM]
                    op.val = nflag // N_CSEM + 1
                    nflag += 1
        stats = {}

        def run(e):
            def body(eng):
                waited = {}
                nw = 0
                for op in self.q[e]:
                    for d in op.deps:
                        key = id(d.sem)
                        if waited.get(key, 0) >= d.val:
                            continue
                        waited[key] = d.val
                        eng.wait_ge(d.sem, d.val)
                        nw += 1
                    if op.fn is None:
                        continue
                    ins = op.fn(eng)
                    if op.is_dma:
                        ins.then_inc(op.sem, 16)
                    elif op.flag:
                        ins.then_inc(op.sem, 1)
                stats[e] = (len(self.q[e]), nw)
            return body

        with nc.Block() as block:
            block.sync(run("sync"))
            block.tensor(run("tensor"))
            block.vector(run("vector"))
            block.scalar(run("scalar"))
            block.gpsimd(run("gpsimd"))
        self.stats = stats


class Rot:
    def __init__(self, items):
        self.items = items
        self.i = 0

    def next(self):
        it = self.items[self.i % len(self.items)]
        self.i += 1
        return it


def _size(dt):
    return {F32: 4, BF16: 2, U32: 4}[dt]


class KB:
    def __init__(self, nc, dbg=()):
        self.nc = nc
        self.P = Prog(nc)
        self.stack = ExitStack()
        self.SB = self.stack.enter_context(nc.sbuf_tensor("SB", [128, SB_WORDS], F32))
        self.PS = self.stack.enter_context(nc.psum_tensor("PSA", [128, 8 * 512], F32))
        self.off = 0
        self.dbg = set(dbg)
        self.ndram = 0

    def T(self, shape, dt=F32, name=""):
        p = shape[0]
        n = int(np.prod(shape[1:]))
        words = (n * _size(dt) + 3) // 4
        assert self.off + words <= SB_WORDS, f"SBUF overflow allocating {name}{shape}: off={self.off} words={words}"
        ap = self.SB[0:p, self.off:self.off + words]
        self.off += words
        if dt != F32:
            ap = ap.bitcast(dt)
        ap = ap[:, 0:n]
        if len(shape) > 2:
            names = " ".join(f"d{i}" for i in range(len(shape) - 1))
            kw = {f"d{i}": shape[i + 1] for i in range(len(shape) - 2)}
            ap = ap.rearrange(f"p ({names}) -> p {names}", **kw)
        return Buf(ap, name)

    def psv(self, b0, nb, shape, dt=F32, name="", root=None):
        p = shape[0]
        n = int(np.prod(shape[1:]))
        ap = self.PS[0:p, b0 * 512:(b0 + nb) * 512]
        if dt != F32:
            ap = ap.bitcast(dt)
        ap = ap[:, 0:n]
        if len(shape) > 2:
            names = " ".join(f"d{i}" for i in range(len(shape) - 1))
            kw = {f"d{i}": shape[i + 1] for i in range(len(shape) - 2)}
            ap = ap.rearrange(f"p ({names}) -> p {names}", **kw)
        return Buf(ap, name, psum=True, root=root)

    def dram(self, name, shape, dt=F32, kind=None):
        if kind is None:
            kind = "ExternalOutput" if name in self.dbg else "Internal"
        t = self.nc.dram_tensor(name, list(shape), dt, kind=kind)
        return t.ap()

    def dram_tiles(self, name, n, shape, dt=F32):
        ap = self.dram(name, [n] + list(shape), dt)
        return [Buf(ap[i], f"{name}{i}") for i in range(n)]

    @contextmanager
    def phase(self):
        m = self.off
        yield
        self.P.barrier()
        self.off = m

    def dma(self, out, in_, reads, writes, q="sync", **kw):
        return self.P.add(q, lambda e: e.dma_start(out=out, in_=in_, **kw), reads, writes, dma=True)

    def mm(self, out, lhsT, rhs, reads, writes, start=True, stop=True):
        return self.P.add("tensor", lambda e: e.matmul(out, lhsT, rhs, start=start, stop=stop), reads, writes)

    def tr(self, out, in_, ident, reads, writes):
        return self.P.add("tensor", lambda e: e.transpose(out, in_, ident), reads, writes)

    def act(self, out, in_, func, reads, writes, **kw):
        return self.P.add("scalar", lambda e: e.activation(out=out, in_=in_, func=func, **kw), reads, writes)

    def v(self, eng, method, reads, writes, *a, **kw):
        return self.P.add(eng, lambda e: getattr(e, method)(*a, **kw), reads, writes)

    def finish(self):
        self.P.barrier()
        self.P.emit(self.stack)
        self.stack.close()


def make_consts(kb):
    c = {}
    io = kb.T([128, 128], F32, "iota")
    kb.P.add("gpsimd", lambda e: e.iota(io[:], [[1, 128]], base=0, channel_multiplier=-1,
                                        allow_small_or_imprecise_dtypes=True), [], [io])
    c["jmp"] = io
    c["ident_bf"] = kb.T([128, 128], BF16, "ident_bf")
    kb.v("vector", "tensor_single_scalar", [io], [c["ident_bf"]], c["ident_bf"][:], io[:], 0.0, ALU.is_equal)
    ij = kb.T([128, 128], F32, "iota_j")
    kb.P.add("gpsimd", lambda e: e.iota(ij[:], [[1, 128]], base=0, channel_multiplier=0,
                                        allow_small_or_imprecise_dtypes=True), [], [ij])
    c["iota_j"] = ij
    c["ident_f"] = kb.T([128, 128], F32, "ident_f")
    kb.v("vector", "tensor_single_scalar", [io], [c["ident_f"]], c["ident_f"][:], io[:], 0.0, ALU.is_equal)
    return c


def load_bcast(kb, dst, src_ap, srcbufs, q="sync"):
    p = dst.ap.shape[0]
    return kb.dma(dst[:], src_ap.partition_broadcast(p), srcbufs, [dst], q=q)


def load_w_bf16(kb, dst, w_ap, wbuf, stage_rot, K, N, c0=0, c1=None, eng_rot=None):
    c1 = N if c1 is None else c1
    kc = K // 128
    wv = w_ap.rearrange("(k p) n -> p k n", p=128)
    maxcols = stage_rot.items[0].ap.shape[1]
    i = 0
    for k in range(kc):
        for cs in range(c0, c1, maxcols):
            ce = min(c1, cs + maxcols)
            st = stage_rot.next()
            kb.dma(st[:, 0:ce - cs], wv[:, k, cs:ce], [wbuf], [st], q="sync" if i % 2 == 0 else "gpsimd")
            eng = ["gpsimd", "vector"][i % 2]
            kb.v(eng, "tensor_copy", [st], [dst], dst[:, k, cs - c0:ce - c0], st[:, 0:ce - cs])
            i += 1


def rstd_of(kb, ss, rstd, n):
    kb.act(rstd[:], ss[:], AF.Sqrt, [ss], [rstd], scale=1.0 / n, bias=kb.eps_t[:, 0:1])
    kb.v("vector", "reciprocal", [rstd], [rstd], rstd[:], rstd[:])


def build_mod_tiles(kb, mod_ap, modbuf, g_ap, gbuf, row, j_shift, j_scale, G1, SH, tmp):
    load_bcast(kb, tmp, mod_ap[row, j_scale * D:(j_scale + 1) * D], [modbuf])
    load_bcast(kb, G1, g_ap, [gbuf], q="gpsimd")
    kb.v("vector", "scalar_tensor_tensor", [tmp, G1], [G1], G1[:], tmp[:], 1.0, G1[:], ALU.add, ALU.mult)
    load_bcast(kb, SH, mod_ap[row, j_shift * D:(j_shift + 1) * D], [modbuf])


def norm_mod_T(kb, C, src_ap, srcbuf, G1, SH, xt, junk, ss, rstd, nb, psT, nT):
    kb.dma(xt[:], src_ap, [srcbuf], [xt])
    kb.act(junk[:], xt[:], AF.Square, [xt], [junk, ss], accum_out=ss[:])
    rstd_of(kb, ss, rstd, D)
    kb.v("vector", "scalar_tensor_tensor", [xt, rstd, G1], [xt], xt[:], xt[:], rstd[:, 0:1], G1[:], ALU.mult, ALU.mult)
    kb.v("gpsimd", "tensor_tensor", [xt, SH], [nb], nb[:], xt[:], SH[:], ALU.add)
    for k in range(8):
        kb.tr(psT[:, k, :], nb[:, k * 128:(k + 1) * 128], C["ident_bf"][:], [nb, C["ident_bf"]], [psT])
    kb.act(nT[:], psT[:], AF.Copy, [psT], [nT])


def phase_mod(kb, C, I, layer, MOD):
    with kb.phase():
        raw = kb.T([128, 2, 8], F32, "craw")
        kb.dma(raw[:, 0, :], I["c"].rearrange("(p k) -> p k", k=8), [I["_c"]], [raw])
        kb.dma(raw[:, 1, :], I["c_ctx"].rearrange("(p k) -> p k", k=8), [I["_c_ctx"]], [raw])
        sc = kb.T([128, 8, 2], F32, "csilu")
        kb.act(sc[:].rearrange("p k m -> p m k"), raw[:], AF.Silu, [raw], [sc])
        bm = kb.T([2, 6144], F32, "bm")
        load_bcast(kb, bm, I["b_mod"][layer], [I["_b_mod"]], q="gpsimd")
        res = kb.T([2, 6144], F32, "modres")
        wrot = Rot([kb.T([128, 8, 512], F32, f"wm{i}") for i in range(2)])
        prot = Rot([kb.psv(i, 1, [128, 512], F32, f"psm{i}") for i in range(2)])
        wv = I["w_mod"][layer].rearrange("(p k) n -> p k n", k=8)
        for ng in range(12):
            wm = wrot.next()
            ps = prot.next()
            kb.dma(wm[:], wv[:, :, ng * 512:(ng + 1) * 512], [I["_w_mod"]], [wm], q="sync" if ng % 2 == 0 else "gpsimd")
            for k in range(8):
                kb.mm(ps[0:2, :], sc[:, k, :], wm[:, k, :], [sc, wm], [ps], start=(k == 0), stop=(k == 7))
            kb.v("vector", "tensor_tensor", [ps, bm], [res], res[:, ng * 512:(ng + 1) * 512], ps[0:2, :],
                 bm[:, ng * 512:(ng + 1) * 512], ALU.add)
        kb.dma(MOD.ap, res[:], [res], [MOD])


ZCOLS = 4356


def zcol(tt):
    return 1 + tt * 128 if tt < 2 else 259 + (tt - 2) * 128


def layer_even(kb, C, I, layer, MOD, src_tile, H, need_ctx=True):
    j = 0
    P = kb.P
    PQ = kb.dram_tiles("PQ", NT_ALL, [128, 256], F32)
    ZT = kb.dram("ZT", [512, ZCOLS], F32)
    BT = kb.dram("BT", [512, ZCOLS], F32)
    ZTb = [Buf(None, f"zt{t}") for t in range(NT_ALL)]
    BTb = [Buf(None, f"bt{t}") for t in range(NT_ALL)]
    ZPAD = Buf(None, "ztpad")
    ZTv = ZT.rearrange("(c p) t -> p c t", p=128)
    BTv = BT.rearrange("(c p) t -> p c t", p=128)
    with kb.phase():
        KT = kb.T([128, 8, NT_ALL * 128], BF16, "KT_all")
        VP = kb.T([128, NT_ALL, 8, 65], BF16, "VP_all")
        kmax = kb.T([128, 8], F32, "kmax")
        kb.v("gpsimd", "memset", [], [VP], VP[:], 1.0)
        kb.v("gpsimd", "memset", [], [kmax], kmax[:], 0.0)
        rope = I["rope"]
        with kb.phase():
            stage = Rot([kb.T([128, 1024], F32, f"stg{i}") for i in range(2)])
            w_in = kb.T([128, 8, 1952], BF16, "w_in")
            load_w_bf16(kb, w_in, I["even_w_in"][j], I["_even_w_in"], stage, 1024, 1952)
            w_ukv = kb.T([128, 1, 1024], BF16, "w_ukv")
            load_w_bf16(kb, w_ukv, I["mla_w_ukv"][j], I["_mla_w_ukv"], stage, 128, 1024)
            kvg = kb.T([128, 128], F32, "kvg")
            load_bcast(kb, kvg, I["mla_kv_norm"][j], [I["_mla_kv_norm"]])
            tmpm = kb.T([128, D], F32, "tmpm")
            G1 = [kb.T([128, D], F32, f"G1_{i}") for i in range(2)]
            SH = [kb.T([128, D], F32, f"SH_{i}") for i in range(2)]
            for r in range(2):
                build_mod_tiles(kb, MOD.ap, MOD, I["norm1_g"][layer], I["_norm1_g"], r, 0, 1, G1[r], SH[r], tmpm)
            zero = kb.T([128, 4, 1], F32, "zero")
            kb.v("gpsimd", "memset", [], [zero], zero[:], 0.0)
            for col in (0, 257, 258, 4355):
                kb.dma(ZTv[:, :, col:col + 1], zero[:], [zero], [ZPAD], q="gpsimd", allow_slow_non_contiguous=True)
            xt = kb.T([128, D], F32, "xt")
            junk = kb.T([128, D], BF16, "junk")
            ss = kb.T([128, 1], F32, "ss")
            rstd = kb.T([128, 1], F32, "rstd")
            ss2 = kb.T([128, 1], F32, "ss2")
            rstd2 = kb.T([128, 1], F32, "rstd2")
            nb = kb.T([128, D], BF16, "nb")
            nT = kb.T([128, 8, 128], BF16, "nT")
            pm = Rot([kb.T([128, 416], F32, f"pm{i}") for i in range(2)])
            latn = kb.T([128, 128], BF16, "latn")
            klT = kb.T([128, 128], BF16, "klT")
            kext = Rot([kb.T([128, 8, 97], BF16, f"kext{i}") for i in range(2)])
            for kx in kext.items:
                kb.v("gpsimd", "memset", [], [kx], kx[:], 1.0)
            krot = kb.T([128, 32], F32, "krot")
            rtmp = kb.T([128, 4, 16], F32, "rtmp")
            cs = kb.T([128, 32], F32, "cs")
            ksqt = kb.T([128, 8, 96], F32, "ksqt")
            ksq = kb.T([128, 8], F32, "ksq")
            c_sb = kb.T([128, 4, 128], F32, "c_sb")
            z_sb = Rot([kb.T([128, 4, 128], F32, f"z_sb{i}") for i in range(2)])
            b_sb = Rot([kb.T([128, 4, 128], F32, f"b_sb{i}") for i in range(2)])
            psT = kb.psv(0, 1, [128, 8, 128], BF16, "psT")
            ps_mla = kb.psv(1, 1, [128, 416], F32, "ps_mla")
            ps_lat = kb.psv(2, 1, [128, 8, 128], BF16, "ps_lat")
            ps_kv = kb.psv(3, 2, [128, 8, 128], F32, "ps_kv")
            ps_conv = kb.psv(5, 3, [128, 12, 128], F32, "ps_conv")
            for tt in (DBG_TILES or range(NT_ALL)):
                isc = 1 if tt < 2 else 0
                sap, sbuf_ = src_tile(tt)
                norm_mod_T(kb, C, sap, sbuf_, G1[isc], SH[isc], xt, junk, ss, rstd, nb, psT, nT)
                for k in range(8):
                    kb.mm(ps_mla[:], nT[:, k, :], w_in[:, k, 0:416], [nT, w_in], [ps_mla], start=(k == 0), stop=(k == 7))
                pmt = pm.next()
                kb.act(pmt[:], ps_mla[:], AF.Copy, [ps_mla], [pmt])
                kb.dma(PQ[tt].ap, pmt[:, 0:256], [pmt], [PQ[tt]], q="gpsimd")
                if "kv" in SKIP:
                    continue
                kb.act(junk[:, 0:128], pmt[:, 256:384], AF.Square, [pmt], [junk, ss2], accum_out=ss2[:])
                rstd_of(kb, ss2, rstd2, 128)
                kb.v("vector", "scalar_tensor_tensor", [pmt, rstd2, kvg], [latn], latn[:], pmt[:, 256:384],
                     rstd2[:, 0:1], kvg[:], ALU.mult, ALU.mult)
                kb.tr(ps_lat[:, 0, :], latn[:], C["ident_bf"][:], [latn, C["ident_bf"]], [ps_lat])
                kb.v("vector", "tensor_copy", [ps_lat], [klT], klT[:], ps_lat[:, 0, :])
                if "kv2" in SKIP:
                    continue
                kb.mm(ps_kv[:, 0:4, :], klT[:], w_ukv[:, 0, 0:512], [klT, w_ukv], [ps_kv])
                kb.mm(ps_kv[:, 4:8, :], klT[:], w_ukv[:, 0, 512:1024], [klT, w_ukv], [ps_kv])
                if "kv3" in SKIP:
                    continue
                kx = kext.next()
                for hb in range(2):
                    hs = slice(hb * 4, hb * 4 + 4)
                    kb.act(kx[:, hs, 0:64], ps_kv[:, hs, 0:64], AF.Copy, [ps_kv], [kx], scale=MLA_SCALE)
                    kb.v("vector", "tensor_copy", [ps_kv], [VP], VP[:, tt, hs, 0:64], ps_kv[:, hs, 64:128])
                if "rope" in SKIP:
                    continue
                if tt >= 2:
                    kb.dma(cs[:], rope[(tt - 2) * 128:(tt - 1) * 128, :], [I["_rope"]], [cs], q="gpsimd")
                    x1 = pmt[:, 384:400]
                    x2 = pmt[:, 400:416]
                    kb.v("gpsimd", "tensor_tensor", [pmt, cs], [rtmp], rtmp[:, 0, :], x1, cs[:, 0:16], ALU.mult)
                    kb.v("gpsimd", "tensor_tensor", [pmt, cs], [rtmp], rtmp[:, 1, :], x2, cs[:, 16:32], ALU.mult)
                    kb.v("gpsimd", "tensor_tensor", [pmt, cs], [rtmp], rtmp[:, 2, :], x1, cs[:, 16:32], ALU.mult)
                    kb.v("gpsimd", "tensor_tensor", [pmt, cs], [rtmp], rtmp[:, 3, :], x2, cs[:, 0:16], ALU.mult)
                    kb.v("vector", "tensor_tensor", [rtmp], [krot], krot[:, 0:16], rtmp[:, 0, :], rtmp[:, 1, :], ALU.subtract)
                    kb.v("vector", "tensor_tensor", [rtmp], [krot], krot[:, 16:32], rtmp[:, 2, :], rtmp[:, 3, :], ALU.add)
                else:
                    kb.v("vector", "tensor_copy", [pmt], [krot], krot[:], pmt[:, 384:416])
                kb.act(kx[:, :, 64:96], krot[:].unsqueeze(1).to_broadcast([128, 8, 32]), AF.Copy, [krot], [kx],
                       scale=MLA_SCALE)
                if "ksq" in SKIP:
                    continue
                kb.v("vector", "tensor_tensor", [kx], [ksqt], ksqt[:], kx[:, :, 0:96], kx[:, :, 0:96], ALU.mult)
                kb.v("vector", "tensor_reduce", [ksqt], [ksq], ksq[:], ksqt[:], AX.X, ALU.add)
                kb.v("vector", "tensor_tensor", [ksq, kmax], [kmax], kmax[:], kmax[:], ksq[:], ALU.max)
                for h in range(8):
                    kb.tr(ps_lat[0:97, h, :], kx[:, h, :], C["ident_bf"][:], [kx, C["ident_bf"]], [ps_lat])
                kb.act(KT[0:97, :, tt * 128:(tt + 1) * 128], ps_lat[0:97, :, :], AF.Copy, [ps_lat], [KT])
                if "conv" in SKIP:
                    continue
                for fc in range(12):
                    for k in range(8):
                        kb.mm(ps_conv[:, fc, :], w_in[:, k, 416 + fc * 128:416 + (fc + 1) * 128], nT[:, k, :],
                              [w_in, nT], [ps_conv], start=(k == 0), stop=(k == 7))
                kb.act(c_sb[:], ps_conv[:, 4:8, :], AF.Copy, [ps_conv], [c_sb])
                zt = z_sb.next()
                bt = b_sb.next()
                kb.v("vector", "tensor_tensor", [ps_conv, c_sb], [zt], zt[:], ps_conv[:, 8:12, :], c_sb[:], ALU.mult)
                kb.act(bt[:], ps_conv[:, 0:4, :], AF.Copy, [ps_conv], [bt])
                c0 = zcol(tt)
                kb.dma(ZTv[:, :, c0:c0 + 128], zt[:], [zt], [ZTb[tt]], q="gpsimd")
                kb.dma(BTv[:, :, c0:c0 + 128], bt[:], [bt], [BTb[tt]], q="gpsimd")
        if kb.stop_after == "E1":
            return
        with kb.phase():
            stage = Rot([kb.T([128, 1024], F32, f"stg{i}") for i in range(2)])
            w_uq = kb.T([128, 2, 768], BF16, "w_uq")
            load_w_bf16(kb, w_uq, I["mla_w_uq"][j], I["_mla_w_uq"], stage, 256, 768)
            w_out = kb.T([128, 8, 1024], BF16, "w_out")
            load_w_bf16(kb, w_out, I["even_w_out"][j], I["_even_w_out"], stage, 1024, 1024)
            qg = kb.T([128, 256], F32, "qg")
            load_bcast(kb, qg, I["mla_q_norm"][j], [I["_mla_q_norm"]])
            M2 = [kb.T([128, D], F32, f"M2_{i}") for i in range(2)]
            for r in range(2):
                load_bcast(kb, M2[r], MOD.ap[r, 2 * D:3 * D], [MOD])
            cw = kb.T([128, 3, 4], F32, "cw")
            for w_ in range(3):
                for c_ in range(4):
                    kb.dma(cw[:, w_, c_:c_ + 1], I["conv_w"][j][w_, c_ * 128:(c_ + 1) * 128].unsqueeze(1),
                           [I["_conv_w"]], [cw], q="gpsimd", allow_slow_non_contiguous=True)
            psx = kb.psv(6, 1, [128, 128], F32, "psx")
            kmr = kb.T([128, 1], F32, "kmr")
            kb.v("vector", "tensor_reduce", [kmax], [kmr], kmr[:], kmax[:], AX.X, ALU.max)
            kmb = kb.T([128, 128], F32, "kmb")
            kb.v("vector", "tensor_copy", [kmr], [kmb], kmb[:], kmr[:, 0:1].to_broadcast([128, 128]))
            kb.tr(psx[:], kmb[:], C["ident_f"][:], [kmb, C["ident_f"]], [psx])
            ksm = kb.T([128, 1], F32, "ksm")
            kb.v("vector", "tensor_reduce", [psx], [ksm], ksm[:], psx[:], AX.X, ALU.max)
            pq = kb.T([128, 256], F32, "pq")
            junk = kb.T([128, 768], BF16, "junk")
            ss = kb.T([128, 1], F32, "ss")
            rstd = kb.T([128, 1], F32, "rstd")
            qn = kb.T([128, 256], BF16, "qn")
            qlT = kb.T([128, 2, 128], BF16, "qlT")
            q_sb = kb.T([128, 8, 96], F32, "q_sb")
            cs = kb.T([128, 32], F32, "cs")
            rt = kb.T([128, 4, 8, 16], F32, "rt")
            qsqt = kb.T([128, 8, 96], F32, "qsqt")
            qsq = kb.T([128, 8], F32, "qsq")
            qext = kb.T([128, 8, 97], BF16, "qext")
            QT = kb.T([128, 8, 512], BF16, "QT")
            PT = Rot([kb.T([128, 512], BF16, f"PT{i}") for i in range(3)])
            rec = kb.T([128, 4], F32, "rec")
            mix_tok = [kb.T([128, 512], BF16, f"mix{i}") for i in range(4)]
            mixT = kb.T([128, 8, 512], BF16, "mixT")
            zw = kb.T([128, 4, 514], F32, "zw")
            bw = kb.T([128, 4, 512], F32, "bw")
            yc_ = kb.T([128, 512], F32, "yconv")
            hres = stage.items[0]
            ytmp = stage.items[1]
            ps_S = Rot([kb.psv(i, 1, [128, 512], F32, f"psS{i}") for i in range(2)])
            ps_O = [kb.psv(2 + i, 1, [128, 65], F32, f"psO{i}") for i in range(4)]
            ps_q = kb.psv(2, 2, [128, 768], F32, "ps_q")
            ps_t = kb.psv(6, 1, [128, 8, 128], BF16, "ps_t")
            ps_y = kb.psv(6, 2, [128, 1024], F32, "ps_y")
            bank = {i: None for i in range(8)}
            blocks = []
            if need_ctx:
                blocks.append(([0, 1], [0, 1]))
            for b in range(8):
                blocks.append(([2 + 4 * b + i for i in range(4)], list(range(NT_ALL))))
            for tiles, ktiles in blocks:
                nq = len(tiles) * 128
                for qi, tt in enumerate(tiles):
                    kb.dma(pq[:], PQ[tt].ap, [PQ[tt]], [pq])
                    kb.act(junk[:, 0:256], pq[:], AF.Square, [pq], [junk, ss], accum_out=ss[:])
                    rstd_of(kb, ss, rstd, 256)
                    kb.v("vector", "scalar_tensor_tensor", [pq, rstd, qg], [qn], qn[:], pq[:], rstd[:, 0:1], qg[:],
                         ALU.mult, ALU.mult)
                    for k in range(2):
                        kb.tr(ps_t[:, k, :], qn[:, k * 128:(k + 1) * 128], C["ident_bf"][:], [qn, C["ident_bf"]], [ps_t])
                    kb.v("vector", "tensor_copy", [ps_t], [qlT], qlT[:], ps_t[:, 0:2, :])
                    for k in range(2):
                        kb.mm(ps_q[:, 0:512], qlT[:, k, :], w_uq[:, k, 0:512], [qlT, w_uq], [ps_q, ps_O[0]],
                              start=(k == 0), stop=(k == 1))
                    for k in range(2):
                        kb.mm(ps_q[:, 512:768], qlT[:, k, :], w_uq[:, k, 512:768], [qlT, w_uq], [ps_q, ps_O[1]],
                              start=(k == 0), stop=(k == 1))
                    q_flat = q_sb[:].rearrange("p h d -> p (h d)")
                    kb.act(q_flat[:, 0:512], ps_q[:, 0:512], AF.Copy, [ps_q, ps_O[0], ps_O[1]], [q_sb])
                    kb.act(q_flat[:, 512:768], ps_q[:, 512:768], AF.Copy, [ps_q, ps_O[0], ps_O[1]], [q_sb])
                    kb.v("vector", "tensor_tensor", [q_sb], [qsqt], qsqt[:], q_sb[:], q_sb[:], ALU.mult)
                    kb.v("vector", "tensor_reduce", [qsqt], [qsq], qsq[:], qsqt[:], AX.X, ALU.add)
                    kb.act(qsq[:], qsq[:], AF.Sqrt, [qsq, ksm], [qsq], scale=ksm[:, 0:1])
                    kb.v("vector", "tensor_scalar_mul", [qsq], [qext], qext[:, :, 96], qsq[:], -1.0)
                    kb.v("gpsimd", "tensor_copy", [q_sb], [qext], qext[:, :, 0:64], q_sb[:, :, 0:64])
                    if tt >= 2:
                        kb.dma(cs[:], rope[(tt - 2) * 128:(tt - 1) * 128, :], [I["_rope"]], [cs], q="gpsimd")
                        x1 = q_sb[:, :, 64:80]
                        x2 = q_sb[:, :, 80:96]
                        cosb = cs[:, 0:16].unsqueeze(1).to_broadcast([128, 8, 16])
                        sinb = cs[:, 16:32].unsqueeze(1).to_broadcast([128, 8, 16])
                        kb.v("gpsimd", "tensor_tensor", [q_sb, cs], [rt], rt[:, 0], x1, cosb, ALU.mult)
                        kb.v("gpsimd", "tensor_tensor", [q_sb, cs], [rt], rt[:, 1], x2, sinb, ALU.mult)
                        kb.v("gpsimd", "tensor_tensor", [q_sb, cs], [rt], rt[:, 2], x1, sinb, ALU.mult)
                        kb.v("gpsimd", "tensor_tensor", [q_sb, cs], [rt], rt[:, 3], x2, cosb, ALU.mult)
                        kb.v("vector", "tensor_tensor", [rt], [qext], qext[:, :, 64:80], rt[:, 0], rt[:, 1], ALU.subtract)
                        kb.v("vector", "tensor_tensor", [rt], [qext], qext[:, :, 80:96], rt[:, 2], rt[:, 3], ALU.add)
                    else:
                        kb.v("vector", "tensor_copy", [q_sb], [qext], qext[:, :, 64:96], q_sb[:, :, 64:96])
                    for h in range(8):
                        kb.tr(ps_t[0:97, h, :], qext[:, h, :], C["ident_bf"][:], [qext, C["ident_bf"]], [ps_t])
                    kb.act(QT[0:97, :, qi * 128:(qi + 1) * 128], ps_t[0:97, :, :], AF.Copy, [ps_t], [QT])
                nqs = len(tiles)
                for h in range(8):
                    for ki, kt in enumerate(ktiles):
                        pS = ps_S.next()
                        kb.mm(pS[:, 0:nq], KT[0:97, h, kt * 128:(kt + 1) * 128], QT[0:97, h, 0:nq], [KT, QT], [pS])
                        pt = PT.next()
                        kb.act(pt[:, 0:nq], pS[:, 0:nq], AF.Exp, [pS], [pt])
                        for qs in range(nqs):
                            kb.mm(ps_O[qs][:], pt[:, qs * 128:(qs + 1) * 128], VP[:, kt, h, :], [pt, VP], [ps_O[qs]],
                                  start=(ki == 0), stop=(ki == len(ktiles) - 1))
                    for qs in range(nqs):
                        kb.v("vector", "reciprocal", [ps_O[qs]], [rec], rec[:, qs:qs + 1], ps_O[qs][:, 64:65])
                        kb.act(mix_tok[qs][:, h * 64:(h + 1) * 64], ps_O[qs][:, 0:64], AF.Copy, [ps_O[qs], rec],
                               [mix_tok[qs]], scale=rec[:, qs:qs + 1])
                for qi in range(nqs):
                    for c_ in range(4):
                        kb.tr(ps_t[:, c_, :], mix_tok[qi][:, c_ * 128:(c_ + 1) * 128], C["ident_bf"][:],
                              [mix_tok[qi], C["ident_bf"]], [ps_t])
                    kb.v("vector", "tensor_copy", [ps_t], [mixT], mixT[:, 0:4, qi * 128:(qi + 1) * 128], ps_t[:, 0:4, :])
                c0 = zcol(tiles[0])
                kb.dma(zw[:, :, 0:nq + 2], ZTv[:, :, c0 - 1:c0 + nq + 1], ZTb + [ZPAD], [zw])
                kb.dma(bw[:, :, 0:nq], BTv[:, :, c0:c0 + nq], BTb, [bw], q="gpsimd")
                for c_ in range(4):
                    eng = "vector"
                    kb.v(eng, "tensor_scalar", [zw, cw], [yc_], yc_[:, 0:nq], zw[:, c_, 0:nq], cw[:, 0, c_:c_ + 1], None, ALU.mult)
                    kb.v(eng, "scalar_tensor_tensor", [zw, cw, yc_], [yc_], yc_[:, 0:nq], zw[:, c_, 1:nq + 1],
                         cw[:, 1, c_:c_ + 1], yc_[:, 0:nq], ALU.mult, ALU.add)
                    kb.v(eng, "scalar_tensor_tensor", [zw, cw, yc_], [yc_], yc_[:, 0:nq], zw[:, c_, 2:nq + 2],
                         cw[:, 2, c_:c_ + 1], yc_[:, 0:nq], ALU.mult, ALU.add)
                    kb.v(eng, "tensor_tensor", [yc_, bw], [mixT], mixT[:, 4 + c_, 0:nq], yc_[:, 0:nq], bw[:, c_, 0:nq], ALU.mult)
                for qi, tt in enumerate(tiles):
                    isc = 1 if tt < 2 else 0
                    sap, sbuf_ = src_tile(tt)
                    kb.dma(hres[:], sap, [sbuf_], [hres])
                    for half in range(2):
                        for c_ in range(8):
                            kb.mm(ps_y[:, half * 512:(half + 1) * 512], mixT[:, c_, qi * 128:(qi + 1) * 128],
                                  w_out[:, c_, half * 512:(half + 1) * 512], [mixT, w_out], [ps_y, ps_t],
                                  start=(c_ == 0), stop=(c_ == 7))
                    for half in range(2):
                        hs = slice(half * 512, (half + 1) * 512)
                        kb.v("vector", "tensor_tensor", [ps_y, ps_t, M2[isc]], [ytmp], ytmp[:, hs], ps_y[:, hs], M2[isc][:, hs], ALU.mult)
                    kb.v("gpsimd", "tensor_tensor", [ytmp, hres], [ytmp], ytmp[:], ytmp[:], hres[:], ALU.add)
                    kb.dma(H[tt].ap, ytmp[:], [ytmp], [H[tt]], q="gpsimd")


def layer_odd(kb, C, I, layer, MOD, src_tile, H):
    j = 0
    QKT = kb.dram_tiles("QKT", NT_ALL, [128, 8, 128], BF16)
    KHd = kb.dram_tiles("KHd", NT_ALL, [128, 8, 128], BF16)
    VPd = kb.dram_tiles("VPd", NT_ALL, [128, 4, 257], BF16)
    SCd = kb.dram_tiles("SCd", NT_ALL, [128, 32], F32)
    OGd = kb.dram_tiles("OGd", NT_ALL, [128, D], F32)
    HBd = kb.dram_tiles("HBd", NT_ALL, [128, D], F32)
    HSd = kb.dram_tiles("HSd", NT_ALL, [128, D], F32)
    lat_tiles = list(range(2, NT_ALL))
    with kb.phase():
        stage = Rot([kb.T([128, 1024], F32, f"stg{i}") for i in range(2)])
        w_in = kb.T([128, 8, 3088], BF16, "w_in_o")
        load_w_bf16(kb, w_in, I["odd_w_in"][j], I["_odd_w_in"], stage, 1024, 3088)
        tmpm = stage.items[0]
        G1 = [kb.T([128, D], F32, f"G1_{i}") for i in range(2)]
        SH = [kb.T([128, D], F32, f"SH_{i}") for i in range(2)]
        for r in range(2):
            build_mod_tiles(kb, MOD.ap, MOD, I["norm1_g"][layer], I["_norm1_g"], r, 0, 1, G1[r], SH[r], tmpm)
        gbias = kb.T([128, 16], F32, "gbias")
        load_bcast(kb, gbias, I["mlstm_gate_b"][j], [I["_mlstm_gate_b"]])
        triU = kb.T([128, 128], F32, "triU")
        triL = kb.T([128, 128], F32, "triL")
        ones = kb.T([128, 128], F32, "ones")
        kb.v("vector", "tensor_single_scalar", [C["jmp"]], [triU], triU[:], C["jmp"][:], 0.0, ALU.is_ge)
        kb.v("vector", "tensor_single_scalar", [C["jmp"]], [triL], triL[:], C["jmp"][:], 0.0, ALU.is_le)
        kb.v("gpsimd", "memset", [], [ones], ones[:], 1.0)
        xt = kb.T([128, D], F32, "xt")
        junk = kb.T([128, D], BF16, "junk")
        ss = kb.T([128, 1], F32, "ss")
        rstd = kb.T([128, 1], F32, "rstd")
        nb = kb.T([128, D], BF16, "nb")
        nT = kb.T([128, 8, 128], BF16, "nT")
        qkT = Rot([kb.T([128, 8, 128], BF16, f"qkT{i}") for i in range(2)])
        khat = Rot([kb.T([128, 8, 128], BF16, f"khat{i}") for i in range(2)])
        vp = Rot([kb.T([128, 4, 257], BF16, f"vp{i}") for i in range(2)])
        for v_ in vp.items:
            kb.v("gpsimd", "memset", [], [v_], v_[:], 1.0)
        og = Rot([kb.T([128, D], F32, f"og{i}") for i in range(2)])
        sc = Rot([kb.T([128, 32], F32, f"sc{i}") for i in range(2)])
        gb = kb.T([128, 16], F32, "gb")
        nl = kb.T([128, 8], F32, "nl")
        igc = kb.T([128, 8], F32, "igc")
        t1 = kb.T([128, 8], F32, "t1")
        t2 = kb.T([128, 8], F32, "t2")
        psT = kb.psv(0, 1, [128, 8, 128], BF16, "psT")
        ps_g = kb.psv(0, 1, [128, 64], F32, "ps_g", root=psT)
        ps_f = [kb.psv(1 + i, 1, [128, 4, 128], F32, f"ps_f{i}") for i in range(2)]
        ps_k = kb.psv(3, 1, [128, 512], F32, "ps_k")
        ps_v = [kb.psv(4 + i, 1, [128, 512], F32, f"ps_v{i}") for i in range(2)]
        ps_o = [kb.psv(6 + i, 1, [128, 512], F32, f"ps_o{i}") for i in range(2)]
        for tt in range(NT_ALL):
            isc = 1 if tt < 2 else 0
            sap, sbuf_ = src_tile(tt)
            norm_mod_T(kb, C, sap, sbuf_, G1[isc], SH[isc], xt, junk, ss, rstd, nb, psT, nT)
            for hh in range(8):
                for k in range(8):
                    kb.mm(ps_f[hh // 4][:, hh % 4, :], w_in[:, k, hh * 128:(hh + 1) * 128], nT[:, k, :], [w_in, nT],
                          [ps_f[hh // 4]], start=(k == 0), stop=(k == 7))
            qk = qkT.next()
            kb.act(qk[:, 0:4, :], ps_f[0][:], AF.Copy, [ps_f[0]], [qk], scale=128 ** -0.5)
            kb.v("vector", "tensor_copy", [ps_f[1]], [qk], qk[:, 4:8, :], ps_f[1][:])
            kb.dma(QKT[tt].ap, qk[:], [qk], [QKT[tt]], q="gpsimd")
            for k in range(8):
                kb.mm(ps_k[:], nT[:, k, :], w_in[:, k, 512:1024], [nT, w_in], [ps_k], start=(k == 0), stop=(k == 7))
            for hf in range(2):
                for k in range(8):
                    kb.mm(ps_v[hf][:], nT[:, k, :], w_in[:, k, 1024 + hf * 512:1536 + hf * 512], [nT, w_in], [ps_v[hf]],
                          start=(k == 0), stop=(k == 7))
            for k in range(8):
                kb.mm(ps_g[:, 0:16], nT[:, k, :], w_in[:, k, 2048:2064], [nT, w_in], [ps_g], start=(k == 0), stop=(k == 7))
            if tt >= 2:
                for hf in range(2):
                    for k in range(8):
                        kb.mm(ps_o[hf][:], nT[:, k, :], w_in[:, k, 2064 + hf * 512:2576 + hf * 512], [nT, w_in],
                              [ps_o[hf]], start=(k == 0), stop=(k == 7))
                o_ = og.next()
                for hf in range(2):
                    kb.act(o_[:, hf * 512:(hf + 1) * 512], ps_o[hf][:], AF.Sigmoid, [ps_o[hf]], [o_])
                kb.dma(OGd[tt].ap, o_[:], [o_], [OGd[tt]], q="gpsimd")
            kb.v("vector", "tensor_tensor", [ps_g, gbias], [gb], gb[:], ps_g[:, 0:16], gbias[:], ALU.add)
            gb4 = gb[:].rearrange("p (d two h) -> p d two h", d=2, two=2)
            nl3 = nl[:].rearrange("p (d h) -> p d h", d=2)
            kb.act(nl3, gb4[:, :, 1, :], AF.Exp, [gb], [nl], scale=-1.0)
            kb.act(nl[:], nl[:], AF.Ln, [nl], [nl], bias=kb.one_t[:, 0:1])
            kb.v("vector", "tensor_copy", [gb], [igc], igc[:].rearrange("p (d h) -> p d h", d=2), gb4[:, :, 0, :])
            kb.mm(ps_g[:, 16:20], triU[:], nl[:, 0:4], [triU, nl], [ps_g])
            kb.mm(ps_g[:, 20:24], triL[:], nl[:, 4:8], [triL, nl], [ps_g])
            kb.mm(ps_g[:, 24:32], ones[:], nl[:], [ones, nl], [ps_g])
            s_ = sc.next()
            kb.v("vector", "tensor_tensor", [igc, ps_g], [t1], t1[:], igc[:], ps_g[:, 16:24], ALU.add)
            kb.v("vector", "tensor_tensor", [t1, ps_g], [t2], t2[:], t1[:], ps_g[:, 24:32], ALU.subtract)
            kb.act(s_[:, 0:8], t1[:], AF.Exp, [t1], [s_])
            kb.act(s_[:, 8:16], ps_g[:, 16:24], AF.Exp, [ps_g], [s_], scale=-1.0)
            kb.act(s_[:, 16:24], t2[:], AF.Exp, [t2], [s_])
            kb.act(s_[:, 24:32], ps_g[:, 24:32], AF.Exp, [ps_g], [s_], scale=-1.0)
            kb.dma(SCd[tt].ap, s_[:], [s_], [SCd[tt]], q="gpsimd")
            kh = khat.next()
            for c in range(8):
                h = c % 4
                kb.act(kh[:, c, :], ps_k[:, h * 128:(h + 1) * 128], AF.Copy, [ps_k, s_], [kh], scale=s_[:, 16 + c:17 + c])
            kb.dma(KHd[tt].ap, kh[:], [kh], [KHd[tt]], q="gpsimd")
            v_ = vp.next()
            for hf in range(2):
                kb.v("vector", "tensor_copy", [ps_v[hf]], [v_], v_[:, hf * 2:hf * 2 + 2, 0:256],
                     ps_v[hf][:].rearrange("p (h d) -> p h d", h=2))
            kb.dma(VPd[tt].ap, v_[:], [v_], [VPd[tt]], q="gpsimd")
    with kb.phase():
        triU = kb.T([128, 128], F32, "triU")
        triL = kb.T([128, 128], F32, "triL")
        kb.v("vector", "tensor_single_scalar", [C["jmp"]], [triU], triU[:], C["jmp"][:], 0.0, ALU.is_ge)
        kb.v("vector", "tensor_single_scalar", [C["jmp"]], [triL], triL[:], C["jmp"][:], 0.0, ALU.is_le)
        Cst = [kb.T([128, 257], F32, f"Cst{h}") for h in range(4)]
        Cbf = [kb.T([128, 257], BF16, f"Cbf{h}") for h in range(4)]
        qk = Rot([kb.T([128, 8, 128], BF16, f"qk{i}") for i in range(3)])
        kh = Rot([kb.T([128, 4, 128], BF16, f"kh{i}") for i in range(3)])
        vp = Rot([kb.T([128, 4, 257], BF16, f"vp{i}") for i in range(3)])
        sc = Rot([kb.T([128, 32], F32, f"sc{i}") for i in range(3)])
        PTm = Rot([kb.T([128, 128], BF16, f"PTm{i}") for i in range(3)])
        t4 = kb.T([128, 4], F32, "t4")
        r4 = kb.T([128, 4], F32, "r4")
        hout = Rot([kb.T([128, D], F32, f"hout{i}") for i in range(2)])
        hb = Rot([kb.T([128, D], F32, f"hb{i}") for i in range(2)])
        ps_S = Rot([kb.psv(i, 1, [128, 128], F32, f"psS{i}") for i in range(2)])
        ps_N = [kb.psv(2 + h, 1, [128, 257], F32, f"psN{h}") for h in range(4)]
        ps_U = Rot([kb.psv(6 + i, 1, [128, 257], F32, f"psU{i}") for i in range(2)])
        for d in (1, 0):
            order = [0, 1] + lat_tiles if d == 0 else [1, 0] + lat_tiles[::-1]
            mask = triU if d == 0 else triL
            for h in range(4):
                kb.v("gpsimd", "memset", [], [Cst[h]], Cst[h][:], 0.0)
                kb.v("gpsimd", "memset", [], [Cbf[h]], Cbf[h][:], 0.0)
            for oi, tt in enumerate(order):
                q_ = qk.next(); k_ = kh.next(); v_ = vp.next(); s_ = sc.next()
                kb.dma(q_[:], QKT[tt].ap, [QKT[tt]], [q_])
                kb.dma(k_[:], KHd[tt].ap[:, d * 4:(d + 1) * 4, :], [KHd[tt]], [k_])
                kb.dma(v_[:], VPd[tt].ap, [VPd[tt]], [v_])
                kb.dma(s_[:], SCd[tt].ap, [SCd[tt]], [s_])
                if tt >= 2:
                    for h in range(4):
                        c = d * 4 + h
                        pS = ps_S.next()
                        kb.mm(pS[:], q_[:, 4 + h, :], q_[:, h, :], [q_], [pS])
                        pt = PTm.next()
                        kb.v("vector", "scalar_tensor_tensor", [pS, s_, mask], [pt], pt[:], pS[:], s_[:, c:c + 1], mask[:],
                             ALU.mult, ALU.mult)
                        kb.mm(ps_N[h][:], pt[:], v_[:, h, :], [pt, v_], [ps_N[h]], start=True, stop=False)
                        kb.mm(ps_N[h][:], q_[:, h, :], Cbf[h][:], [q_, Cbf[h]], [ps_N[h]], start=False, stop=True)
                        kb.v("vector", "tensor_tensor", [ps_N[h], s_], [t4], t4[:, h:h + 1], ps_N[h][:, 256:257],
                             s_[:, 8 + c:9 + c], ALU.mult)
                    kb.act(t4[:], t4[:], AF.Abs, [t4], [t4])
                    kb.v("vector", "tensor_scalar_max", [t4], [r4], r4[:], t4[:], 1.0)
                    kb.v("vector", "reciprocal", [r4], [r4], r4[:], r4[:])
                    kb.v("vector", "tensor_tensor", [r4, s_], [r4], r4[:], r4[:], s_[:, 8 + d * 4:12 + d * 4], ALU.mult)
                    ho = hout.next()
                    for h in range(4):
                        kb.act(ho[:, h * 256:(h + 1) * 256], ps_N[h][:, 0:256], AF.Copy, [ps_N[h], r4], [ho],
                               scale=r4[:, h:h + 1])
                    if d == 1:
                        kb.dma(HBd[tt].ap, ho[:], [ho], [HBd[tt]], q="gpsimd")
                    else:
                        hb_ = hb.next()
                        kb.dma(hb_[:], HBd[tt].ap, [HBd[tt]], [hb_])
                        kb.v("gpsimd", "tensor_tensor", [ho, hb_], [hb_], hb_[:], ho[:], hb_[:], ALU.add)
                        kb.dma(HSd[tt].ap, hb_[:], [hb_], [HSd[tt]], q="gpsimd")
                if oi == len(order) - 1:
                    continue
                for h in range(4):
                    c = d * 4 + h
                    pU = ps_U.next()
                    kb.mm(pU[:], k_[:, h, :], v_[:, h, :], [k_, v_], [pU])
                    kb.v("vector", "scalar_tensor_tensor", [Cst[h], s_, pU], [Cst[h]], Cst[h][:], Cst[h][:],
                         s_[:, 24 + c:25 + c], pU[:], ALU.mult, ALU.add)
                    kb.act(Cbf[h][:], Cst[h][:], AF.Copy, [Cst[h]], [Cbf[h]])
    with kb.phase():
        stage = Rot([kb.T([128, 1024], F32, f"stg{i}") for i in range(2)])
        w_out = kb.T([128, 8, 1024], BF16, "w_out_o")
        load_w_bf16(kb, w_out, I["odd_w_out"][j], I["_odd_w_out"], stage, 1024, 1024)
        hg = kb.T([128, D], F32, "hg")
        load_bcast(kb, hg, I["mlstm_head_g"][j], [I["_mlstm_head_g"]])
        M2 = kb.T([128, D], F32, "M2")
        load_bcast(kb, M2, MOD.ap[0, 2 * D:3 * D], [MOD])
        hs = Rot([kb.T([128, D], F32, f"hs{i}") for i in range(2)])
        og = Rot([kb.T([128, D], F32, f"og{i}") for i in range(2)])
        hres = Rot([kb.T([128, D], F32, f"hres{i}") for i in range(2)])
        sq = kb.T([128, D], F32, "sq")
        ss4 = kb.T([128, 4], F32, "ss4")
        rs4 = kb.T([128, 4], F32, "rs4")
        mb = kb.T([128, D], BF16, "mb")
        mT = kb.T([128, 8, 128], BF16, "mT")
        yo = Rot([kb.T([128, D], F32, f"yo{i}") for i in range(2)])
        psT = kb.psv(0, 1, [128, 8, 128], BF16, "psT")
        ps_y = [kb.psv(1 + i, 1, [128, 512], F32, f"ps_y{i}") for i in range(2)]
        for tt in lat_tiles:
            h_ = hs.next(); o_ = og.next(); r_ = hres.next()
            kb.dma(h_[:], HSd[tt].ap, [HSd[tt]], [h_])
            kb.dma(o_[:], OGd[tt].ap, [OGd[tt]], [o_])
            sap, sbuf_ = src_tile(tt)
            kb.dma(r_[:], sap, [sbuf_], [r_])
            kb.v("gpsimd", "tensor_tensor", [h_], [sq], sq[:], h_[:], h_[:], ALU.mult)
            kb.v("vector", "tensor_reduce", [sq], [ss4], ss4[:], sq[:].rearrange("p (h d) -> p h d", h=4), AX.X, ALU.add)
            rstd_of(kb, ss4, rs4, 256)
            h3 = h_[:].rearrange("p (h d) -> p h d", h=4)
            kb.v("vector", "tensor_tensor", [h_, rs4], [h_], h3, h3, rs4[:].unsqueeze(2).to_broadcast([128, 4, 256]), ALU.mult)
            kb.v("gpsimd", "tensor_tensor", [o_, hg], [o_], o_[:], o_[:], hg[:], ALU.mult)
            kb.v("vector", "tensor_tensor", [h_, o_], [mb], mb[:], h_[:], o_[:], ALU.mult)
            for k in range(8):
                kb.tr(psT[:, k, :], mb[:, k * 128:(k + 1) * 128], C["ident_bf"][:], [mb, C["ident_bf"]], [psT])
            kb.act(mT[:], psT[:], AF.Copy, [psT], [mT])
            for hf in range(2):
                for k in range(8):
                    kb.mm(ps_y[hf][:], mT[:, k, :], w_out[:, k, hf * 512:(hf + 1) * 512], [mT, w_out], [ps_y[hf]],
                          start=(k == 0), stop=(k == 7))
            y_ = yo.next()
            for hf in range(2):
                hsl = slice(hf * 512, (hf + 1) * 512)
                kb.v("vector", "tensor_tensor", [ps_y[hf], M2], [y_], y_[:, hsl], ps_y[hf][:], M2[:, hsl], ALU.mult)
            kb.v("gpsimd", "tensor_tensor", [y_, r_], [y_], y_[:], y_[:], r_[:], ALU.add)
            kb.dma(H[tt].ap, y_[:], [y_], [H[tt]], q="gpsimd")


def peer_prep(kb, C, I, layer, UT, VB):
    with kb.phase():
        uf = Rot([kb.T([128, D], F32, f"uf{i}") for i in range(2)])
        ub = Rot([kb.T([128, D], BF16, f"ub{i}") for i in range(2)])
        utb = Rot([kb.T([128, 8, 128], BF16, f"utb{i}") for i in range(2)])
        vf = Rot([kb.T([128, D], F32, f"vf{i}") for i in range(2)])
        vb = Rot([kb.T([128, D], BF16, f"vb{i}") for i in range(2)])
        ps = Rot([kb.psv(i, 1, [128, 8, 128], BF16, f"pst{i}") for i in range(4)])
        for i in range(128):
            a = uf.next(); b = ub.next(); t = utb.next(); p = ps.next()
            kb.dma(a[:], I["peer_u"][layer][i * 128:(i + 1) * 128, :], [I["_peer_u"]], [a])
            kb.v("gpsimd", "tensor_copy", [a], [b], b[:], a[:])
            for k in range(8):
                kb.tr(p[:, k, :], b[:, k * 128:(k + 1) * 128], C["ident_bf"][:], [b, C["ident_bf"]], [p])
            kb.act(t[:], p[:], AF.Copy, [p], [t])
            kb.dma(UT[i].ap, t[:], [t], [UT[i]], q="gpsimd")
            a2 = vf.next(); b2 = vb.next()
            kb.dma(a2[:], I["peer_v"][layer][i * 128:(i + 1) * 128, :], [I["_peer_v"]], [a2])
            kb.v("vector", "tensor_copy", [a2], [b2], b2[:], a2[:])
            kb.dma(VB[i].ap, b2[:], [b2], [VB[i]], q="gpsimd")


def peer_route(kb, C, I, layer, MOD, tiles, src_tile, NTd, RTd):
    with kb.phase():
        stage = Rot([kb.T([128, 2048], F32, f"stg{i}") for i in range(2)])
        w_q = kb.T([128, 8, 2048], BF16, "w_q")
        load_w_bf16(kb, w_q, I["peer_w_q"][layer], I["_peer_w_q"], stage, 1024, 2048)
        skT = kb.T([128, 16, 128], BF16, "skT")
        skb = kb.T([128, 128], BF16, "skb")
        pst = kb.psv(0, 1, [128, 8, 128], BF16, "pst")
        for hp in range(16):
            st = stage.next()
            kb.dma(st[:, 0:128], I["peer_subkeys"][layer][hp // 2, hp % 2], [I["_peer_subkeys"]], [st])
            kb.v("vector", "tensor_copy", [st], [skb], skb[:], st[:, 0:128])
            kb.tr(pst[:, 0, :], skb[:], C["ident_bf"][:], [skb, C["ident_bf"]], [pst])
            kb.v("vector", "tensor_copy", [pst], [skT], skT[:, hp, :], pst[:, 0, :])
        tmpm = Buf(stage.items[0].ap[:, 0:D], "tmpm", root=stage.items[0])
        G1 = [kb.T([128, D], F32, f"G1_{i}") for i in range(2)]
        SH = [kb.T([128, D], F32, f"SH_{i}") for i in range(2)]
        rows = sorted({1 if tt < 2 else 0 for tt in tiles})
        for r in rows:
            build_mod_tiles(kb, MOD.ap, MOD, I["norm2_g"][layer], I["_norm2_g"], r, 3, 4, G1[r], SH[r], tmpm)
        xt = kb.T([128, D], F32, "xt")
        junk = kb.T([128, D], BF16, "junk")
        ss = kb.T([128, 1], F32, "ss")
        rstd = kb.T([128, 1], F32, "rstd")
        nb = kb.T([128, D], BF16, "nb")
        nT = Rot([kb.T([128, 8, 128], BF16, f"nT{i}") for i in range(2)])
        qT_sb = kb.T([128, 16, 128], BF16, "qT_sb")
        s_sb = kb.T([128, 16, 128], F32, "s_sb")
        s2 = kb.T([128, 128], F32, "s2")
        stop = kb.T([128, 16, 16], F32, "stop")
        idx = kb.T([128, 16, 16], U32, "idx")
        idxf = kb.T([128, 16, 16], F32, "idxf")
        cand = kb.T([128, 8, 256], F32, "cand")
        c2 = kb.T([128, 256], F32, "c2")
        top = kb.T([128, 8, 16], F32, "top")
        pos = kb.T([128, 8, 16], U32, "pos")
        au = kb.T([128, 8, 16], U32, "au")
        bu = kb.T([128, 8, 16], U32, "bu")
        af = kb.T([128, 8, 16], F32, "af")
        bf = kb.T([128, 8, 16], F32, "bf")
        E = kb.T([128, 8, 16, 16], F32, "E")
        sel = kb.T([128, 3, 128], F32, "sel")
        gs = kb.T([128, 8], F32, "gs")
        rt_sb = Rot([kb.T([128, 3, 128], F32, f"rt_sb{i}") for i in range(2)])
        psT = kb.psv(0, 1, [128, 8, 128], BF16, "psT", root=pst)
        ps_qT = [kb.psv(i, 1, [128, 4, 128], F32, f"ps_qT{i}") for i in range(4)]
        ps_qT[0] = kb.psv(0, 1, [128, 4, 128], F32, "ps_qT0", root=pst)
        ps_s = [kb.psv(4 + i, 1, [128, 4, 128], F32, f"ps_s{i}") for i in range(4)]
        ps_r = kb.psv(4, 1, [128, 3, 128], F32, "ps_r", root=ps_s[0])
        iota16 = C["iota_j"][:, 0:16]
        for tt in tiles:
            isc = 1 if tt < 2 else 0
            sap, sbuf_ = src_tile(tt)
            nTt = nT.next()
            norm_mod_T(kb, C, sap, sbuf_, G1[isc], SH[isc], xt, junk, ss, rstd, nb, psT, nTt)
            kb.dma(NTd[tt].ap, nTt[:], [nTt], [NTd[tt]], q="gpsimd")
            for hp in range(16):
                pq_ = ps_qT[hp // 4]
                for k in range(8):
                    kb.mm(pq_[:, hp % 4, :], w_q[:, k, hp * 128:(hp + 1) * 128], nTt[:, k, :], [w_q, nTt], [pq_],
                          start=(k == 0), stop=(k == 7))
            for g in range(4):
                kb.act(qT_sb[:, g * 4:(g + 1) * 4, :], ps_qT[g][:], AF.Copy, [ps_qT[g]], [qT_sb])
            for hp in range(16):
                kb.mm(ps_s[hp // 4][:, hp % 4, :], qT_sb[:, hp, :], skT[:, hp, :], [qT_sb, skT], [ps_s[hp // 4]])
            for g in range(4):
                eng = "vector" if g % 2 == 0 else "scalar"
                if eng == "vector":
                    kb.v("vector", "tensor_copy", [ps_s[g]], [s_sb], s_sb[:, g * 4:(g + 1) * 4, :], ps_s[g][:])
                else:
                    kb.act(s_sb[:, g * 4:(g + 1) * 4, :], ps_s[g][:], AF.Copy, [ps_s[g]], [s_sb])
            for hp in range(16):
                kb.v("vector", "max", [s_sb], [stop], stop[:, hp, 0:8], s_sb[:, hp, :])
                kb.v("vector", "match_replace", [stop, s_sb], [s2], s2[:], stop[:, hp, 0:8], s_sb[:, hp, :], -1e30)
                kb.v("vector", "max", [s2], [stop], stop[:, hp, 8:16], s2[:])
                kb.v("vector", "max_index", [stop, s_sb], [idx], idx[:, hp, 0:8], stop[:, hp, 0:8], s_sb[:, hp, :])
                kb.v("vector", "max_index", [stop, s_sb], [idx], idx[:, hp, 8:16], stop[:, hp, 8:16], s_sb[:, hp, :])
            kb.v("vector", "tensor_copy", [idx], [idxf], idxf[:], idx[:])
            st4 = stop[:].rearrange("p (h two) k -> p h two k", two=2)
            if4 = idxf[:].rearrange("p (h two) k -> p h two k", two=2)
            cand4 = cand[:].rearrange("p h (a b) -> p h a b", a=16)
            kb.v("vector", "tensor_tensor", [stop], [cand], cand4,
                 st4[:, :, 0, :].unsqueeze(3).to_broadcast([128, 8, 16, 16]),
                 st4[:, :, 1, :].unsqueeze(2).to_broadcast([128, 8, 16, 16]), ALU.add)
            for h in range(8):
                kb.v("vector", "max", [cand], [top], top[:, h, 0:8], cand[:, h, :])
                kb.v("vector", "match_replace", [top, cand], [c2], c2[:], top[:, h, 0:8], cand[:, h, :], -1e30)
                kb.v("vector", "max", [c2], [top], top[:, h, 8:16], c2[:])
                kb.v("vector", "max_index", [top, cand], [pos], pos[:, h, 0:8], top[:, h, 0:8], cand[:, h, :])
                kb.v("vector", "max_index", [top, cand], [pos], pos[:, h, 8:16], top[:, h, 8:16], cand[:, h, :])
            kb.v("vector", "tensor_single_scalar", [pos], [au], au[:], pos[:], 4, ALU.logical_shift_right)
            kb.v("vector", "tensor_single_scalar", [pos], [bu], bu[:], pos[:], 15, ALU.bitwise_and)
            kb.v("vector", "tensor_copy", [au], [af], af[:], au[:])
            kb.v("vector", "tensor_copy", [bu], [bf], bf[:], bu[:])
            io4 = iota16.unsqueeze(1).unsqueeze(1).to_broadcast([128, 8, 16, 16])
            for which, sel_f in ((0, af), (1, bf)):
                kb.v("vector", "tensor_tensor", [sel_f, C["iota_j"]], [E], E[:],
                     sel_f[:].unsqueeze(3).to_broadcast([128, 8, 16, 16]), io4, ALU.is_equal)
                kb.v("vector", "tensor_tensor", [E, idxf], [E], E[:], E[:],
                     if4[:, :, which, :].unsqueeze(2).to_broadcast([128, 8, 16, 16]), ALU.mult)
                kb.v("vector", "tensor_reduce", [E], [sel], sel[:, which, :].rearrange("p (h k) -> p h k", h=8),
                     E[:], AX.X, ALU.add)
            g3 = sel[:, 2, :].rearrange("p (h k) -> p h k", h=8)
            kb.v("vector", "tensor_tensor", [top], [sel], g3, top[:], top[:, :, 0:1].to_broadcast([128, 8, 16]), ALU.subtract)
            kb.act(sel[:, 2, :], sel[:, 2, :], AF.Exp, [sel], [sel])
            kb.v("vector", "tensor_reduce", [sel], [gs], gs[:], g3, AX.X, ALU.add)
            kb.v("vector", "reciprocal", [gs], [gs], gs[:], gs[:])
            kb.v("vector", "tensor_tensor", [sel, gs], [sel], g3, g3, gs[:].unsqueeze(2).to_broadcast([128, 8, 16]), ALU.mult)
            for w_ in range(3):
                kb.tr(ps_r[:, w_, :], sel[:, w_, :], C["ident_f"][:], [sel, C["ident_f"]], [ps_r])
            rt = rt_sb.next()
            kb.act(rt[:], ps_r[:], AF.Copy, [ps_r], [rt])
            kb.dma(RTd[tt].ap, rt[:], [rt], [RTd[tt]], q="gpsimd")


def peer_apply(kb, C, I, layer, MOD, groups, src_tile, NTd, RTd, UT, VB, epilogue):
    with kb.phase():
        Gs = kb.T([128, 128, 384], BF16, "Gs")
        A = kb.T([128, 64, 128], BF16, "A")
        B = kb.T([128, 64, 128], BF16, "B")
        nTg = kb.T([128, 8, 384], BF16, "nTg")
        ntl = kb.T([128, 8, 128], BF16, "ntl")
        rt = Rot([kb.T([128, 3, 128], F32, f"rt{i}") for i in range(2)])
        uT = Rot([kb.T([128, 8, 128], BF16, f"uT{i}") for i in range(3)])
        vv = Rot([kb.T([128, D], BF16, f"vv{i}") for i in range(3)])
        ga = Rot([kb.T([128, 384], F32, f"ga{i}") for i in range(2)])
        W = Rot([kb.T([128, 384], BF16, f"W{i}") for i in range(3)])
        ep = epilogue("alloc", kb)
        acc = [[kb.psv(2 * t + h, 1, [128, 512], F32, f"acc{t}{h}") for h in range(2)] for t in range(3)]
        ps_a = [kb.psv(6 + i, 1, [128, 384], F32, f"ps_a{i}") for i in range(2)]
        ps_G = [kb.psv(6 + i, 1, [128, 4, 128], F32, f"ps_G{i}", root=ps_a[i]) for i in range(2)]
        ps_a = Rot(ps_a)
        ps_G = Rot(ps_G)
        iota_b = C["iota_j"][:].unsqueeze(1).to_broadcast([128, 64, 128])
        for tiles in groups:
            ng = len(tiles)
            ntok = ng * 128
            for gi, tt in enumerate(tiles):
                kb.dma(ntl[:], NTd[tt].ap, [NTd[tt]], [ntl])
                kb.v("gpsimd", "tensor_copy", [ntl], [nTg], nTg[:, :, gi * 128:(gi + 1) * 128], ntl[:])
                r = rt.next()
                kb.dma(r[:], RTd[tt].ap, [RTd[tt]], [r])
                for hf in range(2):
                    ts_ = slice(hf * 64, hf * 64 + 64)
                    kb.v("vector", "tensor_tensor", [r, C["iota_j"]], [A], A[:], iota_b,
                         r[:, 0, ts_].unsqueeze(2).to_broadcast([128, 64, 128]), ALU.is_equal)
                    kb.v("vector", "tensor_tensor", [A, r], [A], A[:], A[:],
                         r[:, 2, ts_].unsqueeze(2).to_broadcast([128, 64, 128]), ALU.mult)
                    kb.v("vector", "tensor_tensor", [r, C["iota_j"]], [B], B[:], iota_b,
                         r[:, 1, ts_].unsqueeze(2).to_broadcast([128, 64, 128]), ALU.is_equal)
                    for t4 in range(16):
                        pg = ps_G.next()
                        for u in range(4):
                            t = t4 * 4 + u
                            kb.mm(pg[:, u, :], B[:, t, :], A[:, t, :], [A, B], [pg])
                        tok0 = gi * 128 + hf * 64 + t4 * 4
                        dst = Gs[:, :, tok0:tok0 + 4].rearrange("j i t -> j t i")
                        if t4 % 2 == 0:
                            kb.act(dst, pg[:], AF.Copy, [pg], [Gs])
                        else:
                            kb.v("vector", "tensor_copy", [pg], [Gs], dst, pg[:])
            for i in range(128):
                u_ = uT.next(); v_ = vv.next()
                kb.dma(u_[:], UT[i].ap, [UT[i]], [u_])
                kb.dma(v_[:], VB[i].ap, [VB[i]], [v_])
                pa = ps_a.next()
                for k in range(8):
                    kb.mm(pa[:, 0:ntok], u_[:, k, :], nTg[:, k, 0:ntok], [u_, nTg], [pa], start=(k == 0), stop=(k == 7))
                g_ = ga.next()
                kb.act(g_[:, 0:ntok], pa[:, 0:ntok], AF.Gelu, [pa], [g_])
                w_ = W.next()
                eng = "vector" if i % 2 == 0 else "gpsimd"
                kb.v(eng, "tensor_tensor", [g_, Gs], [w_], w_[:, 0:ntok], g_[:, 0:ntok], Gs[:, i, 0:ntok], ALU.mult)
                for gi in range(ng):
                    for h in range(2):
                        kb.mm(acc[gi][h][:], w_[:, gi * 128:(gi + 1) * 128], v_[:, h * 512:(h + 1) * 512], [w_, v_],
                              [acc[gi][h]], start=(i == 0), stop=(i == 127))
            for gi, tt in enumerate(tiles):
                epilogue("run", kb, tt, acc[gi], ep)

def make_epilogue(I, MOD, src_tile, dst_tile, final=False):
    def ep(mode, kb, tt=None, acc=None, b=None):
        if mode == "alloc":
            b = {"M5": [kb.T([128, D], F32, f"M5_{i}") for i in range(2)],
                 "hres": kb.T([128, D], F32, "hres"), "o": kb.T([128, D], F32, "o")}
            for r in range(2):
                load_bcast(kb, b["M5"][r], MOD.ap[r, 5 * D:6 * D], [MOD])
            if final:
                b["fg"] = kb.T([128, D], F32, "fg")
                load_bcast(kb, b["fg"], I["norm_f_g"], [I["_norm_f_g"]])
                b["junk"] = kb.T([128, D], BF16, "junkf")
                b["ss"] = kb.T([128, 1], F32, "ssf")
                b["rstd"] = kb.T([128, 1], F32, "rstdf")
            return b
        isc = 1 if tt < 2 else 0
        sap, sbuf_ = src_tile(tt)
        hres, o, M5 = b["hres"], b["o"], b["M5"][isc]
        kb.dma(hres[:], sap, [sbuf_], [hres])
        for h in range(2):
            hs = slice(h * 512, (h + 1) * 512)
            kb.v("vector", "tensor_tensor", [acc[h], M5], [o], o[:, hs], acc[h][:], M5[:, hs], ALU.mult)
        kb.v("gpsimd", "tensor_tensor", [o, hres], [o], o[:], o[:], hres[:], ALU.add)
        dap, dbuf = dst_tile(tt)
        if final:
            kb.act(b["junk"][:], o[:], AF.Square, [o], [b["junk"], b["ss"]], accum_out=b["ss"][:])
            rstd_of(kb, b["ss"], b["rstd"], D)
            kb.v("vector", "scalar_tensor_tensor", [o, b["rstd"], b["fg"]], [o], o[:], o[:], b["rstd"][:, 0:1],
                 b["fg"][:], ALU.mult, ALU.mult)
        kb.dma(dap, o[:], [o], [dbuf], q="gpsimd")
    return ep


def tile_groups(tiles, n=3):
    return [tiles[i:i + n] for i in range(0, len(tiles), n)]


INPUT_SPECS = [
    ("x", [SEQ, D]), ("c", [D]), ("ctx", [NCTX, D]), ("c_ctx", [D]),
    ("norm1_g", [2, D]), ("norm2_g", [2, D]), ("w_mod", [2, D, 6 * D]), ("b_mod", [2, 6 * D]),
    ("even_w_in", [1, D, 1952]), ("mla_q_norm", [1, 256]), ("mla_kv_norm", [1, 128]),
    ("mla_w_uq", [1, 256, 768]), ("mla_w_ukv", [1, 128, 1024]), ("conv_w", [1, 3, 512]),
    ("even_w_out", [1, D, D]), ("odd_w_in", [1, D, 3088]), ("mlstm_gate_b", [1, 16]),
    ("mlstm_head_g", [1, D]), ("odd_w_out", [1, D, D]), ("peer_w_q", [2, D, 2048]),
    ("peer_subkeys", [2, 8, 2, 128, 128]), ("peer_u", [2, 16384, D]), ("peer_v", [2, 16384, D]),
    ("norm_f_g", [D]), ("rope", [SEQ, 32]),
]


def build_program(stop_after=None, dbg=()):
    nc = bass.Bass("TRN2", target_bir_lowering=False)
    kb = KB(nc, dbg)
    kb.stop_after = stop_after
    I = {}
    for name, shape in INPUT_SPECS:
        ap = nc.dram_tensor(name, shape, F32, kind="ExternalInput").ap()
        I[name] = ap
        I["_" + name] = Buf(ap, name)
    out = nc.dram_tensor("y", [SEQ, D], F32, kind="ExternalOutput").ap()
    kb.eps_t = kb.T([128, 1], F32, "eps")
    kb.v("gpsimd", "memset", [], [kb.eps_t], kb.eps_t[:], EPS)
    kb.one_t = kb.T([128, 1], F32, "one")
    kb.v("gpsimd", "memset", [], [kb.one_t], kb.one_t[:], 1.0)
    C = make_consts(kb)
    MOD = [Buf(kb.dram(f"MOD{l}", [2, 6 * D], F32), f"MOD{l}") for l in range(2)]
    H0 = kb.dram_tiles("H0", NT_ALL, [128, D], F32)

    def src0(tt):
        if tt < 2:
            return I["ctx"][tt * 128:(tt + 1) * 128, :], I["_ctx"]
        return I["x"][(tt - 2) * 128:(tt - 1) * 128, :], I["_x"]

    phase_mod(kb, C, I, 0, MOD[0])
    if stop_after == "mod0":
        kb.finish()
        return nc, kb
    layer_even(kb, C, I, 0, MOD[0], src0, H0)
    if stop_after in ("even", "E1"):
        kb.finish()
        return nc, kb
    UT = kb.dram_tiles("UT", 128, [128, 8, 128], BF16)
    VB = kb.dram_tiles("VB", 128, [128, D], BF16)
    NTd = kb.dram_tiles("NTd", NT_ALL, [128, 8, 128], BF16)
    RTd = kb.dram_tiles("RTd", NT_ALL, [128, 3, 128], F32)
    H1 = kb.dram_tiles("H1", NT_ALL, [128, D], F32)
    srcH0 = lambda tt: (H0[tt].ap, H0[tt])
    dstH1 = lambda tt: (H1[tt].ap, H1[tt])
    tiles0 = DBG_PTILES or list(range(NT_ALL))
    peer_prep(kb, C, I, 0, UT, VB)
    peer_route(kb, C, I, 0, MOD[0], tiles0, srcH0, NTd, RTd)
    if stop_after == "route0":
        kb.finish()
        return nc, kb
    peer_apply(kb, C, I, 0, MOD[0], tile_groups(tiles0), srcH0, NTd, RTd, UT, VB,
               make_epilogue(I, MOD[0], srcH0, dstH1))
    if stop_after == "peer0":
        kb.finish()
        return nc, kb
    phase_mod(kb, C, I, 1, MOD[1])
    H2 = kb.dram_tiles("H2", NT_ALL, [128, D], F32)
    srcH1 = lambda tt: (H1[tt].ap, H1[tt])
    layer_odd(kb, C, I, 1, MOD[1], srcH1, H2)
    if stop_after == "odd":
        kb.finish()
        return nc, kb
    srcH2 = lambda tt: (H2[tt].ap, H2[tt])
    outb = [Buf(out[(tt - 2) * 128:(tt - 1) * 128, :], f"y{tt}") for tt in range(NT_ALL)]
    dstY = lambda tt: (outb[tt].ap, outb[tt])
    tiles1 = list(range(2, NT_ALL))
    peer_prep(kb, C, I, 1, UT, VB)
    peer_route(kb, C, I, 1, MOD[1], tiles1, srcH2, NTd, RTd)
    peer_apply(kb, C, I, 1, MOD[1], tile_groups(tiles1), srcH2, NTd, RTd, UT, VB,
               make_epilogue(I, MOD[1], srcH2, dstY, final=True))
    kb.finish()
    return nc, kb


def rope_table():
    n_freq = 8
    inv = (10000.0 ** (-np.arange(n_freq, dtype=np.float32) / n_freq)).astype(np.float32)
    t = np.arange(SEQ)
    row = (t // 64).astype(np.float32)
    col = (t % 64).astype(np.float32)
    ang = np.concatenate([row[:, None] * inv, col[:, None] * inv], axis=-1).astype(np.float32)
    return np.concatenate([np.cos(ang), np.sin(ang)], axis=-1).astype(np.float32)


def make_in_maps(inputs, cores):
    shared = {k: np.ascontiguousarray(np.asarray(v, dtype=np.float32)) for k, v in inputs.items()
              if k not in ("x", "c", "ctx")}
    shared["rope"] = rope_table()
    maps = []
    for b in cores:
        m = dict(shared)
        m["x"] = np.ascontiguousarray(inputs["x"][b])
        m["c"] = np.ascontiguousarray(inputs["c"][b])
        m["ctx"] = np.ascontiguousarray(inputs["ctx"][b])
        maps.append(m)
    return maps


def kernel(**inputs):
    nc, kb = build_program()
    maps = make_in_maps(inputs, list(range(8)))
    res = run_bass_kernel_spmd(nc, maps, core_ids=list(range(8)))
    return np.stack([r["y"] for r in res.results], axis=0).astype(np.float32)
```

```python
import math
import numpy as np
from contextlib import ExitStack, contextmanager
import concourse.bass as bass
import concourse.mybir as mybir
from concourse.bass_utils import run_bass_kernel_spmd

F32 = mybir.dt.float32
BF16 = mybir.dt.bfloat16
U32 = mybir.dt.uint32
ALU = mybir.AluOpType
AF = mybir.ActivationFunctionType
AX = mybir.AxisListType

D = 1024
SEQ = 4096
NCTX = 256
NT_ALL = 34
EPS = 1e-6
MLA_SCALE = 96 ** -0.5
DBG_TILES = None
DBG_PTILES = None
SKIP = set()
SB_WORDS = 51200


class Buf:
    __slots__ = ("ap", "last_write", "reads", "name", "psum", "root")

    def __init__(self, ap, name="", psum=False, root=None):
        self.root = root.root if root is not None else self
        self.ap = ap
        self.last_write = None
        self.reads = []
        self.name = name
        self.psum = psum

    def __getitem__(self, k):
        return self.ap[k]


class Op:
    __slots__ = ("eng", "fn", "deps", "flag", "is_dma", "sem", "val", "slot")

    def __init__(self, eng, fn, is_dma):
        self.eng = eng
        self.fn = fn
        self.deps = []
        self.flag = False
        self.is_dma = is_dma
        self.sem = None
        self.val = None
        self.slot = None


ENGS = ["tensor", "vector", "scalar", "gpsimd", "sync"]
N_CSEM = 3
N_DSEM = {"sync": 44, "scalar": 4, "gpsimd": 36}


class Prog:
    def __init__(self, nc):
        self.nc = nc
        self.q = {e: [] for e in ENGS}
        self.last_real = {e: None for e in ENGS}
        self.dma_hist = {e: [] for e in N_DSEM}

    def add(self, eng, fn, reads=(), writes=(), dma=False):
        op = Op(eng, fn, dma)
        deps = []
        reads = [b.root for b in reads]
        writes = [b.root for b in writes]
        writes = list(writes) + [b for b in reads if b.psum and b not in writes]
        for b in reads:
            if b.last_write is not None:
                deps.append(b.last_write)
        for b in writes:
            if b.last_write is not None:
                deps.append(b.last_write)
            deps.extend(b.reads)
        seen = set()
        for d in deps:
            if id(d) in seen:
                continue
            seen.add(id(d))
            if d.eng == eng and not d.is_dma and not dma:
                if eng == "tensor":
                    continue
                if not any(b.last_write is d for b in reads):
                    continue
            op.deps.append(d)
            d.flag = True
        for b in reads:
            if not b.psum:
                b.reads.append(op)
        for b in writes:
            b.last_write = op
            b.reads = []
        if dma:
            h = self.dma_hist[eng]
            n = N_DSEM[eng]
            k = len(h)
            op.slot = k % n
            op.val = 16 * (k // n + 1)
            if k >= n:
                op.deps.append(h[k - n])
            h.append(op)
        elif fn is not None:
            self.last_real[eng] = op
        self.q[eng].append(op)
        return op

    def barrier(self):
        deps = []
        for e in ENGS:
            d = self.last_real[e]
            if d is not None:
                d.flag = True
                deps.append(d)
        for e, h in self.dma_hist.items():
            deps.extend(h[-N_DSEM[e]:])
        for e in ENGS:
            op = Op(e, None, False)
            op.deps = list(deps)
            self.q[e].append(op)

    def emit(self, stack):
        nc = self.nc
        csems = {e: [stack.enter_context(nc.semaphore(f"c_{e}_{i}")) for i in range(N_CSEM)]
                 for e in ["tensor", "vector", "scalar", "gpsimd"]}
        dsems = {e: [stack.enter_context(nc.semaphore(f"d_{e}_{i}")) for i in range(n)]
                 for e, n in N_DSEM.items()}
        for e in ENGS:
            nflag = 0
            for op in self.q[e]:
                if op.is_dma:
                    op.sem = dsems[e][op.slot]
                elif op.flag:
                    op.sem = csems[e][nflag % N_CSEM]
                    op.val = nflag // N_CSEM + 1
                    nflag += 1
        stats = {}

        def run(e):
            def body(eng):
                waited = {}
                nw = 0
                for op in self.q[e]:
                    for d in op.deps:
                        key = id(d.sem)
                        if waited.get(key, 0) >= d.val:
                            continue
                        waited[key] = d.val
                        eng.wait_ge(d.sem, d.val)
                        nw += 1
                    if op.fn is None:
                        continue
                    ins = op.fn(eng)
                    if op.is_dma:
                        ins.then_inc(op.sem, 16)
                    elif op.flag:
                        ins.then_inc(op.sem, 1)
                stats[e] = (len(self.q[e]), nw)
            return body

        with nc.Block() as block:
            block.sync(run("sync"))
            block.tensor(run("tensor"))
            block.vector(run("vector"))
            block.scalar(run("scalar"))
            block.gpsimd(run("gpsimd"))
        self.stats = stats


class Rot:
    def __init__(self, items):
        self.items = items
        self.i = 0

    def next(self):
        it = self.items[self.i % len(self.items)]
        self.i += 1
        return it


def _size(dt):
    return {F32: 4, BF16: 2, U32: 4}[dt]


class KB:
    def __init__(self, nc, dbg=()):
        self.nc = nc
        self.P = Prog(nc)
        self.stack = ExitStack()
        self.SB = self.stack.enter_context(nc.sbuf_tensor("SB", [128, SB_WORDS], F32))
        self.PS = self.stack.enter_context(nc.psum_tensor("PSA", [128, 8 * 512], F32))
        self.off = 0
        self.dbg = set(dbg)
        self.ndram = 0

    def T(self, shape, dt=F32, name=""):
        p = shape[0]
        n = int(np.prod(shape[1:]))
        words = (n * _size(dt) + 3) // 4
        assert self.off + words <= SB_WORDS, f"SBUF overflow allocating {name}{shape}: off={self.off} words={words}"
        ap = self.SB[0:p, self.off:self.off + words]
        self.off += words
        if dt != F32:
            ap = ap.bitcast(dt)
        ap = ap[:, 0:n]
        if len(shape) > 2:
            names = " ".join(f"d{i}" for i in range(len(shape) - 1))
            kw = {f"d{i}": shape[i + 1] for i in range(len(shape) - 2)}
            ap = ap.rearrange(f"p ({names}) -> p {names}", **kw)
        return Buf(ap, name)

    def psv(self, b0, nb, shape, dt=F32, name="", root=None):
        p = shape[0]
        n = int(np.prod(shape[1:]))
        ap = self.PS[0:p, b0 * 512:(b0 + nb) * 512]
        if dt != F32:
            ap = ap.bitcast(dt)
        ap = ap[:, 0:n]
        if len(shape) > 2:
            names = " ".join(f"d{i}" for i in range(len(shape) - 1))
            kw = {f"d{i}": shape[i + 1] for i in range(len(shape) - 2)}
            ap = ap.rearrange(f"p ({names}) -> p {names}", **kw)
        return Buf(ap, name, psum=True, root=root)

    def dram(self, name, shape, dt=F32, kind=None):
        if kind is None:
            kind = "ExternalOutput" if name in self.dbg else "Internal"
        t = self.nc.dram_tensor(name, list(shape), dt, kind=kind)
        return t.ap()

    def dram_tiles(self, name, n, shape, dt=F32):
        ap = self.dram(name, [n] + list(shape), dt)
        return [Buf(ap[i], f"{name}{i}") for i in range(n)]

    @contextmanager
    def phase(self):
        m = self.off
        yield
        self.P.barrier()
        self.off = m

    def dma(self, out, in_, reads, writes, q="sync", **kw):
        return self.P.add(q, lambda e: e.dma_start(out=out, in_=in_, **kw), reads, writes, dma=True)

    def mm(self, out, lhsT, rhs, reads, writes, start=True, stop=True):
        return self.P.add("tensor", lambda e: e.matmul(out, lhsT, rhs, start=start, stop=stop), reads, writes)

    def tr(self, out, in_, ident, reads, writes):
        return self.P.add("tensor", lambda e: e.transpose(out, in_, ident), reads, writes)

    def act(self, out, in_, func, reads, writes, **kw):
        return self.P.add("scalar", lambda e: e.activation(out=out, in_=in_, func=func, **kw), reads, writes)

    def v(self, eng, method, reads, writes, *a, **kw):
        return self.P.add(eng, lambda e: getattr(e, method)(*a, **kw), reads, writes)

    def finish(self):
        self.P.barrier()
        self.P.emit(self.stack)
        self.stack.close()


def make_consts(kb):
    c = {}
    io = kb.T([128, 128], F32, "iota")
    kb.P.add("gpsimd", lambda e: e.iota(io[:], [[1, 128]], base=0, channel_multiplier=-1,
                                        allow_small_or_imprecise_dtypes=True), [], [io])
    c["jmp"] = io
    c["ident_bf"] = kb.T([128, 128], BF16, "ident_bf")
    kb.v("vector", "tensor_single_scalar", [io], [c["ident_bf"]], c["ident_bf"][:], io[:], 0.0, ALU.is_equal)
    ij = kb.T([128, 128], F32, "iota_j")
    kb.P.add("gpsimd", lambda e: e.iota(ij[:], [[1, 128]], base=0, channel_multiplier=0,
                                        allow_small_or_imprecise_dtypes=True), [], [ij])
    c["iota_j"] = ij
    c["ident_f"] = kb.T([128, 128], F32, "ident_f")
    kb.v("vector", "tensor_single_scalar", [io], [c["ident_f"]], c["ident_f"][:], io[:], 0.0, ALU.is_equal)
    return c


def load_bcast(kb, dst, src_ap, srcbufs, q="sync"):
    p = dst.ap.shape[0]
    return kb.dma(dst[:], src_ap.partition_broadcast(p), srcbufs, [dst], q=q)


def load_w_bf16(kb, dst, w_ap, wbuf, stage_rot, K, N, c0=0, c1=None, eng_rot=None):
    c1 = N if c1 is None else c1
    kc = K // 128
    wv = w_ap.rearrange("(k p) n -> p k n", p=128)
    maxcols = stage_rot.items[0].ap.shape[1]
    i = 0
    for k in range(kc):
        for cs in range(c0, c1, maxcols):
            ce = min(c1, cs + maxcols)
            st = stage_rot.next()
            kb.dma(st[:, 0:ce - cs], wv[:, k, cs:ce], [wbuf], [st], q="sync" if i % 2 == 0 else "gpsimd")
            eng = ["gpsimd", "vector"][i % 2]
            kb.v(eng, "tensor_copy", [st], [dst], dst[:, k, cs - c0:ce - c0], st[:, 0:ce - cs])
            i += 1


def rstd_of(kb, ss, rstd, n):
    kb.act(rstd[:], ss[:], AF.Sqrt, [ss], [rstd], scale=1.0 / n, bias=kb.eps_t[:, 0:1])
    kb.v("vector", "reciprocal", [rstd], [rstd], rstd[:], rstd[:])


def build_mod_tiles(kb, mod_ap, modbuf, g_ap, gbuf, row, j_shift, j_scale, G1, SH, tmp):
    load_bcast(kb, tmp, mod_ap[row, j_scale * D:(j_scale + 1) * D], [modbuf])
    load_bcast(kb, G1, g_ap, [gbuf], q="gpsimd")
    kb.v("vector", "scalar_tensor_tensor", [tmp, G1], [G1], G1[:], tmp[:], 1.0, G1[:], ALU.add, ALU.mult)
    load_bcast(kb, SH, mod_ap[row, j_shift * D:(j_shift + 1) * D], [modbuf])


def norm_mod_T(kb, C, src_ap, srcbuf, G1, SH, xt, junk, ss, rstd, nb, psT, nT):
    kb.dma(xt[:], src_ap, [srcbuf], [xt])
    kb.act(junk[:], xt[:], AF.Square, [xt], [junk, ss], accum_out=ss[:])
    rstd_of(kb, ss, rstd, D)
    kb.v("vector", "scalar_tensor_tensor", [xt, rstd, G1], [xt], xt[:], xt[:], rstd[:, 0:1], G1[:], ALU.mult, ALU.mult)
    kb.v("gpsimd", "tensor_tensor", [xt, SH], [nb], nb[:], xt[:], SH[:], ALU.add)
    for k in range(8):
        kb.tr(psT[:, k, :], nb[:, k * 128:(k + 1) * 128], C["ident_bf"][:], [nb, C["ident_bf"]], [psT])
    kb.act(nT[:], psT[:], AF.Copy, [psT], [nT])


def phase_mod(kb, C, I, layer, MOD):
    with kb.phase():
        raw = kb.T([128, 2, 8], F32, "craw")
        kb.dma(raw[:, 0, :], I["c"].rearrange("(p k) -> p k", k=8), [I["_c"]], [raw])
        kb.dma(raw[:, 1, :], I["c_ctx"].rearrange("(p k) -> p k", k=8), [I["_c_ctx"]], [raw])
        sc = kb.T([128, 8, 2], F32, "csilu")
        kb.act(sc[:].rearrange("p k m -> p m k"), raw[:], AF.Silu, [raw], [sc])
        bm = kb.T([2, 6144], F32, "bm")
        load_bcast(kb, bm, I["b_mod"][layer], [I["_b_mod"]], q="gpsimd")
        res = kb.T([2, 6144], F32, "modres")
        wrot = Rot([kb.T([128, 8, 512], F32, f"wm{i}") for i in range(2)])
        prot = Rot([kb.psv(i, 1, [128, 512], F32, f"psm{i}") for i in range(2)])
        wv = I["w_mod"][layer].rearrange("(p k) n -> p k n", k=8)
        for ng in range(12):
            wm = wrot.next()
            ps = prot.next()
            kb.dma(wm[:], wv[:, :, ng * 512:(ng + 1) * 512], [I["_w_mod"]], [wm], q="sync" if ng % 2 == 0 else "gpsimd")
            for k in range(8):
                kb.mm(ps[0:2, :], sc[:, k, :], wm[:, k, :], [sc, wm], [ps], start=(k == 0), stop=(k == 7))
            kb.v("vector", "tensor_tensor", [ps, bm], [res], res[:, ng * 512:(ng + 1) * 512], ps[0:2, :],
                 bm[:, ng * 512:(ng + 1) * 512], ALU.add)
        kb.dma(MOD.ap, res[:], [res], [MOD])


ZCOLS = 4356


def zcol(tt):
    return 1 + tt * 128 if tt < 2 else 259 + (tt - 2) * 128


def layer_even(kb, C, I, layer, MOD, src_tile, H, need_ctx=True):
    j = 0
    P = kb.P
    PQ = kb.dram_tiles("PQ", NT_ALL, [128, 256], F32)
    ZT = kb.dram("ZT", [512, ZCOLS], F32)
    BT = kb.dram("BT", [512, ZCOLS], F32)
    ZTb = [Buf(None, f"zt{t}") for t in range(NT_ALL)]
    BTb = [Buf(None, f"bt{t}") for t in range(NT_ALL)]
    ZPAD = Buf(None, "ztpad")
    ZTv = ZT.rearrange("(c p) t -> p c t", p=128)
    BTv = BT.rearrange("(c p) t -> p c t", p=128)
    with kb.phase():
        KT = kb.T([128, 8, NT_ALL * 128], BF16, "KT_all")
        VP = kb.T([128, NT_ALL, 8, 65], BF16, "VP_all")
        kmax = kb.T([128, 8], F32, "kmax")
        kb.v("gpsimd", "memset", [], [VP], VP[:], 1.0)
        kb.v("gpsimd", "memset", [], [kmax], kmax[:], 0.0)
        rope = I["rope"]
        with kb.phase():
            stage = Rot([kb.T([128, 1024], F32, f"stg{i}") for i in range(2)])
            w_in = kb.T([128, 8, 1952], BF16, "w_in")
            load_w_bf16(kb, w_in, I["even_w_in"][j], I["_even_w_in"], stage, 1024, 1952)
            w_ukv = kb.T([128, 1, 1024], BF16, "w_ukv")
            load_w_bf16(kb, w_ukv, I["mla_w_ukv"][j], I["_mla_w_ukv"], stage, 128, 1024)
            kvg = kb.T([128, 128], F32, "kvg")
            load_bcast(kb, kvg, I["mla_kv_norm"][j], [I["_mla_kv_norm"]])
            tmpm = kb.T([128, D], F32, "tmpm")
            G1 = [kb.T([128, D], F32, f"G1_{i}") for i in range(2)]
            SH = [kb.T([128, D], F32, f"SH_{i}") for i in range(2)]
            for r in range(2):
                build_mod_tiles(kb, MOD.ap, MOD, I["norm1_g"][layer], I["_norm1_g"], r, 0, 1, G1[r], SH[r], tmpm)
            zero = kb.T([128, 4, 1], F32, "zero")
            kb.v("gpsimd", "memset", [], [zero], zero[:], 0.0)
            for col in (0, 257, 258, 4355):
                kb.dma(ZTv[:, :, col:col + 1], zero[:], [zero], [ZPAD], q="gpsimd", allow_slow_non_contiguous=True)
            xt = kb.T([128, D], F32, "xt")
            junk = kb.T([128, D], BF16, "junk")
            ss = kb.T([128, 1], F32, "ss")
            rstd = kb.T([128, 1], F32, "rstd")
            ss2 = kb.T([128, 1], F32, "ss2")
            rstd2 = kb.T([128, 1], F32, "rstd2")
            nb = kb.T([128, D], BF16, "nb")
            nT = kb.T([128, 8, 128], BF16, "nT")
            pm = Rot([kb.T([128, 416], F32, f"pm{i}") for i in range(2)])
            latn = kb.T([128, 128], BF16, "latn")
            klT = kb.T([128, 128], BF16, "klT")
            kext = Rot([kb.T([128, 8, 97], BF16, f"kext{i}") for i in range(2)])
            for kx in kext.items:
                kb.v("gpsimd", "memset", [], [kx], kx[:], 1.0)
            krot = kb.T([128, 32], F32, "krot")
            rtmp = kb.T([128, 4, 16], F32, "rtmp")
            cs = kb.T([128, 32], F32, "cs")
            ksqt = kb.T([128, 8, 96], F32, "ksqt")
            ksq = kb.T([128, 8], F32, "ksq")
            c_sb = kb.T([128, 4, 128], F32, "c_sb")
            z_sb = Rot([kb.T([128, 4, 128], F32, f"z_sb{i}") for i in range(2)])
            b_sb = Rot([kb.T([128, 4, 128], F32, f"b_sb{i}") for i in range(2)])
            psT = kb.psv(0, 1, [128, 8, 128], BF16, "psT")
            ps_mla = kb.psv(1, 1, [128, 416], F32, "ps_mla")
            ps_lat = kb.psv(2, 1, [128, 8, 128], BF16, "ps_lat")
            ps_kv = kb.psv(3, 2, [128, 8, 128], F32, "ps_kv")
            ps_conv = kb.psv(5, 3, [128, 12, 128], F32, "ps_conv")
            for tt in (DBG_TILES or range(NT_ALL)):
                isc = 1 if tt < 2 else 0
                sap, sbuf_ = src_tile(tt)
                norm_mod_T(kb, C, sap, sbuf_, G1[isc], SH[isc], xt, junk, ss, rstd, nb, psT, nT)
                for k in range(8):
                    kb.mm(ps_mla[:], nT[:, k, :], w_in[:, k, 0:416], [nT, w_in], [ps_mla], start=(k == 0), stop=(k == 7))
                pmt = pm.next()
                kb.act(pmt[:], ps_mla[:], AF.Copy, [ps_mla], [pmt])
                kb.dma(PQ[tt].ap, pmt[:, 0:256], [pmt], [PQ[tt]], q="gpsimd")
                if "kv" in SKIP:
                    continue
                kb.act(junk[:, 0:128], pmt[:, 256:384], AF.Square, [pmt], [junk, ss2], accum_out=ss2[:])
                rstd_of(kb, ss2, rstd2, 128)
                kb.v("vector", "scalar_tensor_tensor", [pmt, rstd2, kvg], [latn], latn[:], pmt[:, 256:384],
                     rstd2[:, 0:1], kvg[:], ALU.mult, ALU.mult)
                kb.tr(ps_lat[:, 0, :], latn[:], C["ident_bf"][:], [latn, C["ident_bf"]], [ps_lat])
                kb.v("vector", "tensor_copy", [ps_lat], [klT], klT[:], ps_lat[:, 0, :])
                if "kv2" in SKIP:
                    continue
                kb.mm(ps_kv[:, 0:4, :], klT[:], w_ukv[:, 0, 0:512], [klT, w_ukv], [ps_kv])
                kb.mm(ps_kv[:, 4:8, :], klT[:], w_ukv[:, 0, 512:1024], [klT, w_ukv], [ps_kv])
                if "kv3" in SKIP:
                    continue
                kx = kext.next()
                for hb in range(2):
                    hs = slice(hb * 4, hb * 4 + 4)
                    kb.act(kx[:, hs, 0:64], ps_kv[:, hs, 0:64], AF.Copy, [ps_kv], [kx], scale=MLA_SCALE)
                    kb.v("vector", "tensor_copy", [ps_kv], [VP], VP[:, tt, hs, 0:64], ps_kv[:, hs, 64:128])
                if "rope" in SKIP:
                    continue
                if tt >= 2:
                    kb.dma(cs[:], rope[(tt - 2) * 128:(tt - 1) * 128, :], [I["_rope"]], [cs], q="gpsimd")
                    x1 = pmt[:, 384:400]
                    x2 = pmt[:, 400:416]
                    kb.v("gpsimd", "tensor_tensor", [pmt, cs], [rtmp], rtmp[:, 0, :], x1, cs[:, 0:16], ALU.mult)
                    kb.v("gpsimd", "tensor_tensor", [pmt, cs], [rtmp], rtmp[:, 1, :], x2, cs[:, 16:32], ALU.mult)
                    kb.v("gpsimd", "tensor_tensor", [pmt, cs], [rtmp], rtmp[:, 2, :], x1, cs[:, 16:32], ALU.mult)
                    kb.v("gpsimd", "tensor_tensor", [pmt, cs], [rtmp], rtmp[:, 3, :], x2, cs[:, 0:16], ALU.mult)
                    kb.v("vector", "tensor_tensor", [rtmp], [krot], krot[:, 0:16], rtmp[:, 0, :], rtmp[:, 1, :], ALU.subtract)
                    kb.v("vector", "tensor_tensor", [rtmp], [krot], krot[:, 16:32], rtmp[:, 2, :], rtmp[:, 3, :], ALU.add)
                else:
                    kb.v("vector", "tensor_copy", [pmt], [krot], krot[:], pmt[:, 384:416])
                kb.act(kx[:, :, 64:96], krot[:].unsqueeze(1).to_broadcast([128, 8, 32]), AF.Copy, [krot], [kx],
                       scale=MLA_SCALE)
                if "ksq" in SKIP:
                    continue
                kb.v("vector", "tensor_tensor", [kx], [ksqt], ksqt[:], kx[:, :, 0:96], kx[:, :, 0:96], ALU.mult)
                kb.v("vector", "tensor_reduce", [ksqt], [ksq], ksq[:], ksqt[:], AX.X, ALU.add)
                kb.v("vector", "tensor_tensor", [ksq, kmax], [kmax], kmax[:], kmax[:], ksq[:], ALU.max)
                for h in range(8):
                    kb.tr(ps_lat[0:97, h, :], kx[:, h, :], C["ident_bf"][:], [kx, C["ident_bf"]], [ps_lat])
                kb.act(KT[0:97, :, tt * 128:(tt + 1) * 128], ps_lat[0:97, :, :], AF.Copy, [ps_lat], [KT])
                if "conv" in SKIP:
                    continue
                for fc in range(12):
                    for k in range(8):
                        kb.mm(ps_conv[:, fc, :], w_in[:, k, 416 + fc * 128:416 + (fc + 1) * 128], nT[:, k, :],
                              [w_in, nT], [ps_conv], start=(k == 0), stop=(k == 7))
                kb.act(c_sb[:], ps_conv[:, 4:8, :], AF.Copy, [ps_conv], [c_sb])
                zt = z_sb.next()
                bt = b_sb.next()
                kb.v("vector", "tensor_tensor", [ps_conv, c_sb], [zt], zt[:], ps_conv[:, 8:12, :], c_sb[:], ALU.mult)
                kb.act(bt[:], ps_conv[:, 0:4, :], AF.Copy, [ps_conv], [bt])
                c0 = zcol(tt)
                kb.dma(ZTv[:, :, c0:c0 + 128], zt[:], [zt], [ZTb[tt]], q="gpsimd")
                kb.dma(BTv[:, :, c0:c0 + 128], bt[:], [bt], [BTb[tt]], q="gpsimd")
        if kb.stop_after == "E1":
            return
        with kb.phase():
            stage = Rot([kb.T([128, 1024], F32, f"stg{i}") for i in range(2)])
            w_uq = kb.T([128, 2, 768], BF16, "w_uq")
            load_w_bf16(kb, w_uq, I["mla_w_uq"][j], I["_mla_w_uq"], stage, 256, 768)
            w_out = kb.T([128, 8, 1024], BF16, "w_out")
            load_w_bf16(kb, w_out, I["even_w_out"][j], I["_even_w_out"], stage, 1024, 1024)
            qg = kb.T([128, 256], F32, "qg")
            load_bcast(kb, qg, I["mla_q_norm"][j], [I["_mla_q_norm"]])
            M2 = [kb.T([128, D], F32, f"M2_{i}") for i in range(2)]
            for r in range(2):
                load_bcast(kb, M2[r], MOD.ap[r, 2 * D:3 * D], [MOD])
            cw = kb.T([128, 3, 4], F32, "cw")
            for w_ in range(3):
                for c_ in range(4):
                    kb.dma(cw[:, w_, c_:c_ + 1], I["conv_w"][j][w_, c_ * 128:(c_ + 1) * 128].unsqueeze(1),
                           [I["_conv_w"]], [cw], q="gpsimd", allow_slow_non_contiguous=True)
            psx = kb.psv(6, 1, [128, 128], F32, "psx")
            kmr = kb.T([128, 1], F32, "kmr")
            kb.v("vector", "tensor_reduce", [kmax], [kmr], kmr[:], kmax[:], AX.X, ALU.max)
            kmb = kb.T([128, 128], F32, "kmb")
            kb.v("vector", "tensor_copy", [kmr], [kmb], kmb[:], kmr[:, 0:1].to_broadcast([128, 128]))
            kb.tr(psx[:], kmb[:], C["ident_f"][:], [kmb, C["ident_f"]], [psx])
            ksm = kb.T([128, 1], F32, "ksm")
            kb.v("vector", "tensor_reduce", [psx], [ksm], ksm[:], psx[:], AX.X, ALU.max)
            pq = kb.T([128, 256], F32, "pq")
            junk = kb.T([128, 768], BF16, "junk")
            ss = kb.T([128, 1], F32, "ss")
            rstd = kb.T([128, 1], F32, "rstd")
            qn = kb.T([128, 256], BF16, "qn")
            qlT = kb.T([128, 2, 128], BF16, "qlT")
            q_sb = kb.T([128, 8, 96], F32, "q_sb")
            cs = kb.T([128, 32], F32, "cs")
            rt = kb.T([128, 4, 8, 16], F32, "rt")
            qsqt = kb.T([128, 8, 96], F32, "qsqt")
            qsq = kb.T([128, 8], F32, "qsq")
            qext = kb.T([128, 8, 97], BF16, "qext")
            QT = kb.T([128, 8, 512], BF16, "QT")
            PT = Rot([kb.T([128, 512], BF16, f"PT{i}") for i in range(3)])
            rec = kb.T([128, 4], F32, "rec")
            mix_tok = [kb.T([128, 512], BF16, f"mix{i}") for i in range(4)]
            mixT = kb.T([128, 8, 512], BF16, "mixT")
            zw = kb.T([128, 4, 514], F32, "zw")
            bw = kb.T([128, 4, 512], F32, "bw")
            yc_ = kb.T([128, 512], F32, "yconv")
            hres = stage.items[0]
            ytmp = stage.items[1]
            ps_S = Rot([kb.psv(i, 1, [128, 512], F32, f"psS{i}") for i in range(2)])
            ps_O = [kb.psv(2 + i, 1, [128, 65], F32, f"psO{i}") for i in range(4)]
            ps_q = kb.psv(2, 2, [128, 768], F32, "ps_q")
            ps_t = kb.psv(6, 1, [128, 8, 128], BF16, "ps_t")
            ps_y = kb.psv(6, 2, [128, 1024], F32, "ps_y")
            bank = {i: None for i in range(8)}
            blocks = []
            if need_ctx:
                blocks.append(([0, 1], [0, 1]))
            for b in range(8):
                blocks.append(([2 + 4 * b + i for i in range(4)], list(range(NT_ALL))))
            for tiles, ktiles in blocks:
                nq = len(tiles) * 128
                for qi, tt in enumerate(tiles):
                    kb.dma(pq[:], PQ[tt].ap, [PQ[tt]], [pq])
                    kb.act(junk[:, 0:256], pq[:], AF.Square, [pq], [junk, ss], accum_out=ss[:])
                    rstd_of(kb, ss, rstd, 256)
                    kb.v("vector", "scalar_tensor_tensor", [pq, rstd, qg], [qn], qn[:], pq[:], rstd[:, 0:1], qg[:],
                         ALU.mult, ALU.mult)
                    for k in range(2):
                        kb.tr(ps_t[:, k, :], qn[:, k * 128:(k + 1) * 128], C["ident_bf"][:], [qn, C["ident_bf"]], [ps_t])
                    kb.v("vector", "tensor_copy", [ps_t], [qlT], qlT[:], ps_t[:, 0:2, :])
                    for k in range(2):
                        kb.mm(ps_q[:, 0:512], qlT[:, k, :], w_uq[:, k, 0:512], [qlT, w_uq], [ps_q, ps_O[0]],
                              start=(k == 0), stop=(k == 1))
                    for k in range(2):
                        kb.mm(ps_q[:, 512:768], qlT[:, k, :], w_uq[:, k, 512:768], [qlT, w_uq], [ps_q, ps_O[1]],
                              start=(k == 0), stop=(k == 1))
                    q_flat = q_sb[:].rearrange("p h d -> p (h d)")
                    kb.act(q_flat[:, 0:512], ps_q[:, 0:512], AF.Copy, [ps_q, ps_O[0], ps_O[1]], [q_sb])
                    kb.act(q_flat[:, 512:768], ps_q[:, 512:768], AF.Copy, [ps_q, ps_O[0], ps_O[1]], [q_sb])
                    kb.v("vector", "tensor_tensor", [q_sb], [qsqt], qsqt[:], q_sb[:], q_sb[:], ALU.mult)
                    kb.v("vector", "tensor_reduce", [qsqt], [qsq], qsq[:], qsqt[:], AX.X, ALU.add)
                    kb.act(qsq[:], qsq[:], AF.Sqrt, [qsq, ksm], [qsq], scale=ksm[:, 0:1])
                    kb.v("vector", "tensor_scalar_mul", [qsq], [qext], qext[:, :, 96], qsq[:], -1.0)
                    kb.v("gpsimd", "tensor_copy", [q_sb], [qext], qext[:, :, 0:64], q_sb[:, :, 0:64])
                    if tt >= 2:
                        kb.dma(cs[:], rope[(tt - 2) * 128:(tt - 1) * 128, :], [I["_rope"]], [cs], q="gpsimd")
                        x1 = q_sb[:, :, 64:80]
                        x2 = q_sb[:, :, 80:96]
                        cosb = cs[:, 0:16].unsqueeze(1).to_broadcast([128, 8, 16])
                        sinb = cs[:, 16:32].unsqueeze(1).to_broadcast([128, 8, 16])
                        kb.v("gpsimd", "tensor_tensor", [q_sb, cs], [rt], rt[:, 0], x1, cosb, ALU.mult)
                        kb.v("gpsimd", "tensor_tensor", [q_sb, cs], [rt], rt[:, 1], x2, sinb, ALU.mult)
                        kb.v("gpsimd", "tensor_tensor", [q_sb, cs], [rt], rt[:, 2], x1, sinb, ALU.mult)
                        kb.v("gpsimd", "tensor_tensor", [q_sb, cs], [rt], rt[:, 3], x2, cosb, ALU.mult)
                        kb.v("vector", "tensor_tensor", [rt], [qext], qext[:, :, 64:80], rt[:, 0], rt[:, 1], ALU.subtract)
                        kb.v("vector", "tensor_tensor", [rt], [qext], qext[:, :, 80:96], rt[:, 2], rt[:, 3], ALU.add)
                    else:
                        kb.v("vector", "tensor_copy", [q_sb], [qext], qext[:, :, 64:96], q_sb[:, :, 64:96])
                    for h in range(8):
                        kb.tr(ps_t[0:97, h, :], qext[:, h, :], C["ident_bf"][:], [qext, C["ident_bf"]], [ps_t])
                    kb.act(QT[0:97, :, qi * 128:(qi + 1) * 128], ps_t[0:97, :, :], AF.Copy, [ps_t], [QT])
                nqs = len(tiles)
                for h in range(8):
                    def S_(ki, kt):
                        pS = ps_S.next()
                        kb.mm(pS[:, 0:nq], KT[0:97, h, kt * 128:(kt + 1) * 128], QT[0:97, h, 0:nq], [KT, QT], [pS])
                        pt = PT.next()
                        kb.act(pt[:, 0:nq], pS[:, 0:nq], AF.Exp, [pS], [pt])
                        return (ki, kt, pt)

                    def PV_(st):
                        ki, kt, pt = st
                        for qs in range(nqs):
                            kb.mm(ps_O[qs][:], pt[:, qs * 128:(qs + 1) * 128], VP[:, kt, h, :], [pt, VP], [ps_O[qs]],
                                  start=(ki == 0), stop=(ki == len(ktiles) - 1))

                    pend = None
                    for ki, kt in enumerate(ktiles):
                        cur = S_(ki, kt)
                        if pend is not None:
                            PV_(pend)
                        pend = cur
                    PV_(pend)
                    for qs in range(nqs):
                        kb.v("vector", "reciprocal", [ps_O[qs]], [rec], rec[:, qs:qs + 1], ps_O[qs][:, 64:65])
                        kb.act(mix_tok[qs][:, h * 64:(h + 1) * 64], ps_O[qs][:, 0:64], AF.Copy, [ps_O[qs], rec],
                               [mix_tok[qs]], scale=rec[:, qs:qs + 1])
                for qi in range(nqs):
                    for c_ in range(4):
                        kb.tr(ps_t[:, c_, :], mix_tok[qi][:, c_ * 128:(c_ + 1) * 128], C["ident_bf"][:],
                              [mix_tok[qi], C["ident_bf"]], [ps_t])
                    kb.v("vector", "tensor_copy", [ps_t], [mixT], mixT[:, 0:4, qi * 128:(qi + 1) * 128], ps_t[:, 0:4, :])
                c0 = zcol(tiles[0])
                kb.dma(zw[:, :, 0:nq + 2], ZTv[:, :, c0 - 1:c0 + nq + 1], ZTb + [ZPAD], [zw])
                kb.dma(bw[:, :, 0:nq], BTv[:, :, c0:c0 + nq], BTb, [bw], q="gpsimd")
                for c_ in range(4):
                    eng = "vector"
                    kb.v(eng, "tensor_scalar", [zw, cw], [yc_], yc_[:, 0:nq], zw[:, c_, 0:nq], cw[:, 0, c_:c_ + 1], None, ALU.mult)
                    kb.v(eng, "scalar_tensor_tensor", [zw, cw, yc_], [yc_], yc_[:, 0:nq], zw[:, c_, 1:nq + 1],
                         cw[:, 1, c_:c_ + 1], yc_[:, 0:nq], ALU.mult, ALU.add)
                    kb.v(eng, "scalar_tensor_tensor", [zw, cw, yc_], [yc_], yc_[:, 0:nq], zw[:, c_, 2:nq + 2],
                         cw[:, 2, c_:c_ + 1], yc_[:, 0:nq], ALU.mult, ALU.add)
                    kb.v(eng, "tensor_tensor", [yc_, bw], [mixT], mixT[:, 4 + c_, 0:nq], yc_[:, 0:nq], bw[:, c_, 0:nq], ALU.mult)
                for qi, tt in enumerate(tiles):
                    isc = 1 if tt < 2 else 0
                    sap, sbuf_ = src_tile(tt)
                    kb.dma(hres[:], sap, [sbuf_], [hres])
                    for half in range(2):
                        for c_ in range(8):
                            kb.mm(ps_y[:, half * 512:(half + 1) * 512], mixT[:, c_, qi * 128:(qi + 1) * 128],
                                  w_out[:, c_, half * 512:(half + 1) * 512], [mixT, w_out], [ps_y, ps_t],
                                  start=(c_ == 0), stop=(c_ == 7))
                    for half in range(2):
                        hs = slice(half * 512, (half + 1) * 512)
                        kb.v("vector", "tensor_tensor", [ps_y, ps_t, M2[isc]], [ytmp], ytmp[:, hs], ps_y[:, hs], M2[isc][:, hs], ALU.mult)
                    kb.v("gpsimd", "tensor_tensor", [ytmp, hres], [ytmp], ytmp[:], ytmp[:], hres[:], ALU.add)
                    kb.dma(H[tt].ap, ytmp[:], [ytmp], [H[tt]], q="gpsimd")


def layer_odd(kb, C, I, layer, MOD, src_tile, H):
    j = 0
    QKT = kb.dram_tiles("QKT", NT_ALL, [128, 8, 128], BF16)
    KHd = kb.dram_tiles("KHd", NT_ALL, [128, 8, 128], BF16)
    VPd = kb.dram_tiles("VPd", NT_ALL, [128, 4, 257], BF16)
    SCd = kb.dram_tiles("SCd", NT_ALL, [128, 32], F32)
    OGd = kb.dram_tiles("OGd", NT_ALL, [128, D], F32)
    HBd = kb.dram_tiles("HBd", NT_ALL, [128, D], F32)
    HSd = kb.dram_tiles("HSd", NT_ALL, [128, D], F32)
    lat_tiles = list(range(2, NT_ALL))
    with kb.phase():
        stage = Rot([kb.T([128, 1024], F32, f"stg{i}") for i in range(2)])
        w_in = kb.T([128, 8, 3088], BF16, "w_in_o")
        load_w_bf16(kb, w_in, I["odd_w_in"][j], I["_odd_w_in"], stage, 1024, 3088)
        tmpm = stage.items[0]
        G1 = [kb.T([128, D], F32, f"G1_{i}") for i in range(2)]
        SH = [kb.T([128, D], F32, f"SH_{i}") for i in range(2)]
        for r in range(2):
            build_mod_tiles(kb, MOD.ap, MOD, I["norm1_g"][layer], I["_norm1_g"], r, 0, 1, G1[r], SH[r], tmpm)
        gbias = kb.T([128, 16], F32, "gbias")
        load_bcast(kb, gbias, I["mlstm_gate_b"][j], [I["_mlstm_gate_b"]])
        triU = kb.T([128, 128], F32, "triU")
        triL = kb.T([128, 128], F32, "triL")
        ones = kb.T([128, 128], F32, "ones")
        kb.v("vector", "tensor_single_scalar", [C["jmp"]], [triU], triU[:], C["jmp"][:], 0.0, ALU.is_ge)
        kb.v("vector", "tensor_single_scalar", [C["jmp"]], [triL], triL[:], C["jmp"][:], 0.0, ALU.is_le)
        kb.v("gpsimd", "memset", [], [ones], ones[:], 1.0)
        xt = kb.T([128, D], F32, "xt")
        junk = kb.T([128, D], BF16, "junk")
        ss = kb.T([128, 1], F32, "ss")
        rstd = kb.T([128, 1], F32, "rstd")
        nb = kb.T([128, D], BF16, "nb")
        nT = kb.T([128, 8, 128], BF16, "nT")
        qkT = Rot([kb.T([128, 8, 128], BF16, f"qkT{i}") for i in range(2)])
        khat = Rot([kb.T([128, 8, 128], BF16, f"khat{i}") for i in range(2)])
        vp = Rot([kb.T([128, 4, 257], BF16, f"vp{i}") for i in range(2)])
        for v_ in vp.items:
            kb.v("gpsimd", "memset", [], [v_], v_[:], 1.0)
        og = Rot([kb.T([128, D], F32, f"og{i}") for i in range(2)])
        sc = Rot([kb.T([128, 32], F32, f"sc{i}") for i in range(2)])
        gb = kb.T([128, 16], F32, "gb")
        nl = kb.T([128, 8], F32, "nl")
        igc = kb.T([128, 8], F32, "igc")
        t1 = kb.T([128, 8], F32, "t1")
        t2 = kb.T([128, 8], F32, "t2")
        psT = kb.psv(0, 1, [128, 8, 128], BF16, "psT")
        ps_g = kb.psv(0, 1, [128, 64], F32, "ps_g", root=psT)
        ps_f = [kb.psv(1 + i, 1, [128, 4, 128], F32, f"ps_f{i}") for i in range(2)]
        ps_k = kb.psv(3, 1, [128, 512], F32, "ps_k")
        ps_v = [kb.psv(4 + i, 1, [128, 512], F32, f"ps_v{i}") for i in range(2)]
        ps_o = [kb.psv(6 + i, 1, [128, 512], F32, f"ps_o{i}") for i in range(2)]
        for tt in range(NT_ALL):
            isc = 1 if tt < 2 else 0
            sap, sbuf_ = src_tile(tt)
            norm_mod_T(kb, C, sap, sbuf_, G1[isc], SH[isc], xt, junk, ss, rstd, nb, psT, nT)
            for hh in range(8):
                for k in range(8):
                    kb.mm(ps_f[hh // 4][:, hh % 4, :], w_in[:, k, hh * 128:(hh + 1) * 128], nT[:, k, :], [w_in, nT],
                          [ps_f[hh // 4]], start=(k == 0), stop=(k == 7))
            qk = qkT.next()
            kb.act(qk[:, 0:4, :], ps_f[0][:], AF.Copy, [ps_f[0]], [qk], scale=128 ** -0.5)
            kb.v("vector", "tensor_copy", [ps_f[1]], [qk], qk[:, 4:8, :], ps_f[1][:])
            kb.dma(QKT[tt].ap, qk[:], [qk], [QKT[tt]], q="gpsimd")
            for k in range(8):
                kb.mm(ps_k[:], nT[:, k, :], w_in[:, k, 512:1024], [nT, w_in], [ps_k], start=(k == 0), stop=(k == 7))
            for hf in range(2):
                for k in range(8):
                    kb.mm(ps_v[hf][:], nT[:, k, :], w_in[:, k, 1024 + hf * 512:1536 + hf * 512], [nT, w_in], [ps_v[hf]],
                          start=(k == 0), stop=(k == 7))
            for k in range(8):
                kb.mm(ps_g[:, 0:16], nT[:, k, :], w_in[:, k, 2048:2064], [nT, w_in], [ps_g], start=(k == 0), stop=(k == 7))
            if tt >= 2:
                for hf in range(2):
                    for k in range(8):
                        kb.mm(ps_o[hf][:], nT[:, k, :], w_in[:, k, 2064 + hf * 512:2576 + hf * 512], [nT, w_in],
                              [ps_o[hf]], start=(k == 0), stop=(k == 7))
                o_ = og.next()
                for hf in range(2):
                    kb.act(o_[:, hf * 512:(hf + 1) * 512], ps_o[hf][:], AF.Sigmoid, [ps_o[hf]], [o_])
                kb.dma(OGd[tt].ap, o_[:], [o_], [OGd[tt]], q="gpsimd")
            kb.v("vector", "tensor_tensor", [ps_g, gbias], [gb], gb[:], ps_g[:, 0:16], gbias[:], ALU.add)
            gb4 = gb[:].rearrange("p (d two h) -> p d two h", d=2, two=2)
            nl3 = nl[:].rearrange("p (d h) -> p d h", d=2)
            kb.act(nl3, gb4[:, :, 1, :], AF.Exp, [gb], [nl], scale=-1.0)
            kb.act(nl[:], nl[:], AF.Ln, [nl], [nl], bias=kb.one_t[:, 0:1])
            kb.v("vector", "tensor_copy", [gb], [igc], igc[:].rearrange("p (d h) -> p d h", d=2), gb4[:, :, 0, :])
            kb.mm(ps_g[:, 16:20], triU[:], nl[:, 0:4], [triU, nl], [ps_g])
            kb.mm(ps_g[:, 20:24], triL[:], nl[:, 4:8], [triL, nl], [ps_g])
            kb.mm(ps_g[:, 24:32], ones[:], nl[:], [ones, nl], [ps_g])
            s_ = sc.next()
            kb.v("vector", "tensor_tensor", [igc, ps_g], [t1], t1[:], igc[:], ps_g[:, 16:24], ALU.add)
            kb.v("vector", "tensor_tensor", [t1, ps_g], [t2], t2[:], t1[:], ps_g[:, 24:32], ALU.subtract)
            kb.act(s_[:, 0:8], t1[:], AF.Exp, [t1], [s_])
            kb.act(s_[:, 8:16], ps_g[:, 16:24], AF.Exp, [ps_g], [s_], scale=-1.0)
            kb.act(s_[:, 16:24], t2[:], AF.Exp, [t2], [s_])
            kb.act(s_[:, 24:32], ps_g[:, 24:32], AF.Exp, [ps_g], [s_], scale=-1.0)
            kb.dma(SCd[tt].ap, s_[:], [s_], [SCd[tt]], q="gpsimd")
            kh = khat.next()
            for c in range(8):
                h = c % 4
                kb.act(kh[:, c, :], ps_k[:, h * 128:(h + 1) * 128], AF.Copy, [ps_k, s_], [kh], scale=s_[:, 16 + c:17 + c])
            kb.dma(KHd[tt].ap, kh[:], [kh], [KHd[tt]], q="gpsimd")
            v_ = vp.next()
            for hf in range(2):
                kb.v("vector", "tensor_copy", [ps_v[hf]], [v_], v_[:, hf * 2:hf * 2 + 2, 0:256],
                     ps_v[hf][:].rearrange("p (h d) -> p h d", h=2))
            kb.dma(VPd[tt].ap, v_[:], [v_], [VPd[tt]], q="gpsimd")
    with kb.phase():
        triU = kb.T([128, 128], F32, "triU")
        triL = kb.T([128, 128], F32, "triL")
        kb.v("vector", "tensor_single_scalar", [C["jmp"]], [triU], triU[:], C["jmp"][:], 0.0, ALU.is_ge)
        kb.v("vector", "tensor_single_scalar", [C["jmp"]], [triL], triL[:], C["jmp"][:], 0.0, ALU.is_le)
        Cst = [kb.T([128, 257], F32, f"Cst{h}") for h in range(4)]
        Cbf = [kb.T([128, 257], BF16, f"Cbf{h}") for h in range(4)]
        qk = Rot([kb.T([128, 8, 128], BF16, f"qk{i}") for i in range(3)])
        kh = Rot([kb.T([128, 4, 128], BF16, f"kh{i}") for i in range(3)])
        vp = Rot([kb.T([128, 4, 257], BF16, f"vp{i}") for i in range(3)])
        sc = Rot([kb.T([128, 32], F32, f"sc{i}") for i in range(3)])
        PTm = Rot([kb.T([128, 128], BF16, f"PTm{i}") for i in range(3)])
        t4 = kb.T([128, 4], F32, "t4")
        r4 = kb.T([128, 4], F32, "r4")
        hout = Rot([kb.T([128, D], F32, f"hout{i}") for i in range(2)])
        hb = Rot([kb.T([128, D], F32, f"hb{i}") for i in range(2)])
        ps_S = Rot([kb.psv(i, 1, [128, 128], F32, f"psS{i}") for i in range(2)])
        ps_N = [kb.psv(2 + h, 1, [128, 257], F32, f"psN{h}") for h in range(4)]
        ps_U = Rot([kb.psv(6 + i, 1, [128, 257], F32, f"psU{i}") for i in range(2)])
        for d in (1, 0):
            order = [0, 1] + lat_tiles if d == 0 else [1, 0] + lat_tiles[::-1]
            mask = triU if d == 0 else triL
            for h in range(4):
                kb.v("gpsimd", "memset", [], [Cst[h]], Cst[h][:], 0.0)
                kb.v("gpsimd", "memset", [], [Cbf[h]], Cbf[h][:], 0.0)
            for oi, tt in enumerate(order):
                q_ = qk.next(); k_ = kh.next(); v_ = vp.next(); s_ = sc.next()
                kb.dma(q_[:], QKT[tt].ap, [QKT[tt]], [q_])
                kb.dma(k_[:], KHd[tt].ap[:, d * 4:(d + 1) * 4, :], [KHd[tt]], [k_])
                kb.dma(v_[:], VPd[tt].ap, [VPd[tt]], [v_])
                kb.dma(s_[:], SCd[tt].ap, [SCd[tt]], [s_])
                if tt >= 2:
                    for h in range(4):
                        c = d * 4 + h
                        pS = ps_S.next()
                        kb.mm(pS[:], q_[:, 4 + h, :], q_[:, h, :], [q_], [pS])
                        pt = PTm.next()
                        kb.v("vector", "scalar_tensor_tensor", [pS, s_, mask], [pt], pt[:], pS[:], s_[:, c:c + 1], mask[:],
                             ALU.mult, ALU.mult)
                        kb.mm(ps_N[h][:], pt[:], v_[:, h, :], [pt, v_], [ps_N[h]], start=True, stop=False)
                        kb.mm(ps_N[h][:], q_[:, h, :], Cbf[h][:], [q_, Cbf[h]], [ps_N[h]], start=False, stop=True)
                        kb.v("vector", "tensor_tensor", [ps_N[h], s_], [t4], t4[:, h:h + 1], ps_N[h][:, 256:257],
                             s_[:, 8 + c:9 + c], ALU.mult)
                    kb.act(t4[:], t4[:], AF.Abs, [t4], [t4])
                    kb.v("vector", "tensor_scalar_max", [t4], [r4], r4[:], t4[:], 1.0)
                    kb.v("vector", "reciprocal", [r4], [r4], r4[:], r4[:])
                    kb.v("vector", "tensor_tensor", [r4, s_], [r4], r4[:], r4[:], s_[:, 8 + d * 4:12 + d * 4], ALU.mult)
                    ho = hout.next()
                    for h in range(4):
                        kb.act(ho[:, h * 256:(h + 1) * 256], ps_N[h][:, 0:256], AF.Copy, [ps_N[h], r4], [ho],
                               scale=r4[:, h:h + 1])
                    if d == 1:
                        kb.dma(HBd[tt].ap, ho[:], [ho], [HBd[tt]], q="gpsimd")
                    else:
                        hb_ = hb.next()
                        kb.dma(hb_[:], HBd[tt].ap, [HBd[tt]], [hb_])
                        kb.v("gpsimd", "tensor_tensor", [ho, hb_], [hb_], hb_[:], ho[:], hb_[:], ALU.add)
                        kb.dma(HSd[tt].ap, hb_[:], [hb_], [HSd[tt]], q="gpsimd")
                if oi == len(order) - 1:
                    continue
                for h in range(4):
                    c = d * 4 + h
                    pU = ps_U.next()
                    kb.mm(pU[:], k_[:, h, :], v_[:, h, :], [k_, v_], [pU])
                    kb.v("vector", "scalar_tensor_tensor", [Cst[h], s_, pU], [Cst[h]], Cst[h][:], Cst[h][:],
                         s_[:, 24 + c:25 + c], pU[:], ALU.mult, ALU.add)
                    kb.act(Cbf[h][:], Cst[h][:], AF.Copy, [Cst[h]], [Cbf[h]])
    with kb.phase():
        stage = Rot([kb.T([128, 1024], F32, f"stg{i}") for i in range(2)])
        w_out = kb.T([128, 8, 1024], BF16, "w_out_o")
        load_w_bf16(kb, w_out, I["odd_w_out"][j], I["_odd_w_out"], stage, 1024, 1024)
        hg = kb.T([128, D], F32, "hg")
        load_bcast(kb, hg, I["mlstm_head_g"][j], [I["_mlstm_head_g"]])
        M2 = kb.T([128, D], F32, "M2")
        load_bcast(kb, M2, MOD.ap[0, 2 * D:3 * D], [MOD])
        hs = Rot([kb.T([128, D], F32, f"hs{i}") for i in range(2)])
        og = Rot([kb.T([128, D], F32, f"og{i}") for i in range(2)])
        hres = Rot([kb.T([128, D], F32, f"hres{i}") for i in range(2)])
        sq = kb.T([128, D], F32, "sq")
        ss4 = kb.T([128, 4], F32, "ss4")
        rs4 = kb.T([128, 4], F32, "rs4")
        mb = kb.T([128, D], BF16, "mb")
        mT = kb.T([128, 8, 128], BF16, "mT")
        yo = Rot([kb.T([128, D], F32, f"yo{i}") for i in range(2)])
        psT = kb.psv(0, 1, [128, 8, 128], BF16, "psT")
        ps_y = [kb.psv(1 + i, 1, [128, 512], F32, f"ps_y{i}") for i in range(2)]
        for tt in lat_tiles:
            h_ = hs.next(); o_ = og.next(); r_ = hres.next()
            kb.dma(h_[:], HSd[tt].ap, [HSd[tt]], [h_])
            kb.dma(o_[:], OGd[tt].ap, [OGd[tt]], [o_])
            sap, sbuf_ = src_tile(tt)
            kb.dma(r_[:], sap, [sbuf_], [r_])
            kb.v("gpsimd", "tensor_tensor", [h_], [sq], sq[:], h_[:], h_[:], ALU.mult)
            kb.v("vector", "tensor_reduce", [sq], [ss4], ss4[:], sq[:].rearrange("p (h d) -> p h d", h=4), AX.X, ALU.add)
            rstd_of(kb, ss4, rs4, 256)
            h3 = h_[:].rearrange("p (h d) -> p h d", h=4)
            kb.v("vector", "tensor_tensor", [h_, rs4], [h_], h3, h3, rs4[:].unsqueeze(2).to_broadcast([128, 4, 256]), ALU.mult)
            kb.v("gpsimd", "tensor_tensor", [o_, hg], [o_], o_[:], o_[:], hg[:], ALU.mult)
            kb.v("vector", "tensor_tensor", [h_, o_], [mb], mb[:], h_[:], o_[:], ALU.mult)
            for k in range(8):
                kb.tr(psT[:, k, :], mb[:, k * 128:(k + 1) * 128], C["ident_bf"][:], [mb, C["ident_bf"]], [psT])
            kb.act(mT[:], psT[:], AF.Copy, [psT], [mT])
            for hf in range(2):
                for k in range(8):
                    kb.mm(ps_y[hf][:], mT[:, k, :], w_out[:, k, hf * 512:(hf + 1) * 512], [mT, w_out], [ps_y[hf]],
                          start=(k == 0), stop=(k == 7))
            y_ = yo.next()
            for hf in range(2):
                hsl = slice(hf * 512, (hf + 1) * 512)
                kb.v("vector", "tensor_tensor", [ps_y[hf], M2], [y_], y_[:, hsl], ps_y[hf][:], M2[:, hsl], ALU.mult)
            kb.v("gpsimd", "tensor_tensor", [y_, r_], [y_], y_[:], y_[:], r_[:], ALU.add)
            kb.dma(H[tt].ap, y_[:], [y_], [H[tt]], q="gpsimd")


def peer_prep(kb, C, I, layer, UT, VB):
    with kb.phase():
        uf = Rot([kb.T([128, D], F32, f"uf{i}") for i in range(2)])
        ub = Rot([kb.T([128, D], BF16, f"ub{i}") for i in range(2)])
        utb = Rot([kb.T([128, 8, 128], BF16, f"utb{i}") for i in range(2)])
        vf = Rot([kb.T([128, D], F32, f"vf{i}") for i in range(2)])
        vb = Rot([kb.T([128, D], BF16, f"vb{i}") for i in range(2)])
        ps = Rot([kb.psv(i, 1, [128, 8, 128], BF16, f"pst{i}") for i in range(4)])
        for i in range(128):
            a = uf.next(); b = ub.next(); t = utb.next(); p = ps.next()
            kb.dma(a[:], I["peer_u"][layer][i * 128:(i + 1) * 128, :], [I["_peer_u"]], [a])
            if i % 2 == 0:
                kb.v("gpsimd", "tensor_copy", [a], [b], b[:], a[:])
            else:
                kb.act(b[:], a[:], AF.Copy, [a], [b])
            for k in range(8):
                kb.tr(p[:, k, :], b[:, k * 128:(k + 1) * 128], C["ident_bf"][:], [b, C["ident_bf"]], [p])
            kb.v("vector", "tensor_copy", [p], [t], t[:], p[:])
            kb.dma(UT[i].ap, t[:], [t], [UT[i]], q="gpsimd")
            a2 = vf.next(); b2 = vb.next()
            kb.dma(a2[:], I["peer_v"][layer][i * 128:(i + 1) * 128, :], [I["_peer_v"]], [a2])
            kb.v("vector", "tensor_copy", [a2], [b2], b2[:], a2[:])
            kb.dma(VB[i].ap, b2[:], [b2], [VB[i]], q="gpsimd")


def peer_route(kb, C, I, layer, MOD, tiles, src_tile, NTd, RTd):
    with kb.phase():
        stage = Rot([kb.T([128, 2048], F32, f"stg{i}") for i in range(2)])
        w_q = kb.T([128, 8, 2048], BF16, "w_q")
        load_w_bf16(kb, w_q, I["peer_w_q"][layer], I["_peer_w_q"], stage, 1024, 2048)
        skT = kb.T([128, 16, 128], BF16, "skT")
        skb = kb.T([128, 128], BF16, "skb")
        pst = kb.psv(0, 1, [128, 8, 128], BF16, "pst")
        for hp in range(16):
            st = stage.next()
            kb.dma(st[:, 0:128], I["peer_subkeys"][layer][hp // 2, hp % 2], [I["_peer_subkeys"]], [st])
            kb.v("vector", "tensor_copy", [st], [skb], skb[:], st[:, 0:128])
            kb.tr(pst[:, 0, :], skb[:], C["ident_bf"][:], [skb, C["ident_bf"]], [pst])
            kb.v("vector", "tensor_copy", [pst], [skT], skT[:, hp, :], pst[:, 0, :])
        tmpm = Buf(stage.items[0].ap[:, 0:D], "tmpm", root=stage.items[0])
        G1 = [kb.T([128, D], F32, f"G1_{i}") for i in range(2)]
        SH = [kb.T([128, D], F32, f"SH_{i}") for i in range(2)]
        rows = sorted({1 if tt < 2 else 0 for tt in tiles})
        for r in rows:
            build_mod_tiles(kb, MOD.ap, MOD, I["norm2_g"][layer], I["_norm2_g"], r, 3, 4, G1[r], SH[r], tmpm)
        xt = kb.T([128, D], F32, "xt")
        junk = kb.T([128, D], BF16, "junk")
        ss = kb.T([128, 1], F32, "ss")
        rstd = kb.T([128, 1], F32, "rstd")
        nb = kb.T([128, D], BF16, "nb")
        nT = Rot([kb.T([128, 8, 128], BF16, f"nT{i}") for i in range(2)])
        qT_sb = kb.T([128, 16, 128], BF16, "qT_sb")
        s_sb = kb.T([128, 16, 128], F32, "s_sb")
        s2 = kb.T([128, 16, 128], F32, "s2")
        s2b = [Buf(None, f"s2b{i}") for i in range(16)]
        stop = kb.T([128, 16, 16], F32, "stop")
        stopa = [Buf(None, f"stopa{i}") for i in range(16)]
        stopb = [Buf(None, f"stopb{i}") for i in range(16)]
        topa = [Buf(None, f"topa{i}") for i in range(8)]
        topb = [Buf(None, f"topb{i}") for i in range(8)]
        c2b = [Buf(None, f"c2b{i}") for i in range(8)]
        idx = kb.T([128, 16, 16], U32, "idx")
        idxf = kb.T([128, 16, 16], F32, "idxf")
        cand = kb.T([128, 8, 256], F32, "cand")
        c2 = kb.T([128, 8, 256], F32, "c2")
        top = kb.T([128, 8, 16], F32, "top")
        pos = kb.T([128, 8, 16], U32, "pos")
        au = kb.T([128, 8, 16], U32, "au")
        bu = kb.T([128, 8, 16], U32, "bu")
        af = kb.T([128, 8, 16], F32, "af")
        bf = kb.T([128, 8, 16], F32, "bf")
        E = kb.T([128, 8, 16, 16], F32, "E")
        sel = kb.T([128, 3, 128], F32, "sel")
        gs = kb.T([128, 8], F32, "gs")
        rt_sb = Rot([kb.T([128, 3, 128], F32, f"rt_sb{i}") for i in range(2)])
        psT = kb.psv(0, 1, [128, 8, 128], BF16, "psT", root=pst)
        ps_qT = [kb.psv(i, 1, [128, 4, 128], F32, f"ps_qT{i}") for i in range(4)]
        ps_qT[0] = kb.psv(0, 1, [128, 4, 128], F32, "ps_qT0", root=pst)
        ps_s = [kb.psv(4 + i, 1, [128, 4, 128], F32, f"ps_s{i}") for i in range(4)]
        ps_r = kb.psv(4, 1, [128, 3, 128], F32, "ps_r", root=ps_s[0])
        iota16 = C["iota_j"][:, 0:16]
        for tt in tiles:
            isc = 1 if tt < 2 else 0
            sap, sbuf_ = src_tile(tt)
            nTt = nT.next()
            norm_mod_T(kb, C, sap, sbuf_, G1[isc], SH[isc], xt, junk, ss, rstd, nb, psT, nTt)
            kb.dma(NTd[tt].ap, nTt[:], [nTt], [NTd[tt]], q="gpsimd")
            for hp in range(16):
                pq_ = ps_qT[hp // 4]
                for k in range(8):
                    kb.mm(pq_[:, hp % 4, :], w_q[:, k, hp * 128:(hp + 1) * 128], nTt[:, k, :], [w_q, nTt], [pq_],
                          start=(k == 0), stop=(k == 7))
            for g in range(4):
                kb.act(qT_sb[:, g * 4:(g + 1) * 4, :], ps_qT[g][:], AF.Copy, [ps_qT[g]], [qT_sb])
            for hp in range(16):
                kb.mm(ps_s[hp // 4][:, hp % 4, :], qT_sb[:, hp, :], skT[:, hp, :], [qT_sb, skT], [ps_s[hp // 4]])
            for g in range(4):
                eng = "vector" if g % 2 == 0 else "scalar"
                if eng == "vector":
                    kb.v("vector", "tensor_copy", [ps_s[g]], [s_sb], s_sb[:, g * 4:(g + 1) * 4, :], ps_s[g][:])
                else:
                    kb.act(s_sb[:, g * 4:(g + 1) * 4, :], ps_s[g][:], AF.Copy, [ps_s[g]], [s_sb])
            for hp in range(16):
                kb.v("vector", "max", [s_sb], [stopa[hp]], stop[:, hp, 0:8], s_sb[:, hp, :])
            for hp in range(16):
                kb.v("vector", "match_replace", [stopa[hp], s_sb], [s2b[hp]], s2[:, hp, :], stop[:, hp, 0:8], s_sb[:, hp, :], -1e30)
            for hp in range(16):
                kb.v("vector", "max", [s2b[hp]], [stopb[hp]], stop[:, hp, 8:16], s2[:, hp, :])
            for hp in range(16):
                kb.v("vector", "max_index", [stopa[hp], s_sb], [idx], idx[:, hp, 0:8], stop[:, hp, 0:8], s_sb[:, hp, :])
            for hp in range(16):
                kb.v("vector", "max_index", [stopb[hp], s_sb], [idx], idx[:, hp, 8:16], stop[:, hp, 8:16], s_sb[:, hp, :])
            kb.v("vector", "tensor_copy", [idx], [idxf], idxf[:], idx[:])
            st4 = stop[:].rearrange("p (h two) k -> p h two k", two=2)
            if4 = idxf[:].rearrange("p (h two) k -> p h two k", two=2)
            cand4 = cand[:].rearrange("p h (a b) -> p h a b", a=16)
            kb.v("vector", "tensor_tensor", stopa + stopb, [cand], cand4,
                 st4[:, :, 0, :].unsqueeze(3).to_broadcast([128, 8, 16, 16]),
                 st4[:, :, 1, :].unsqueeze(2).to_broadcast([128, 8, 16, 16]), ALU.add)
            for h in range(8):
                kb.v("vector", "max", [cand], [topa[h]], top[:, h, 0:8], cand[:, h, :])
            for h in range(8):
                kb.v("vector", "match_replace", [topa[h], cand], [c2b[h]], c2[:, h, :], top[:, h, 0:8], cand[:, h, :], -1e30)
            for h in range(8):
                kb.v("vector", "max", [c2b[h]], [topb[h]], top[:, h, 8:16], c2[:, h, :])
            for h in range(8):
                kb.v("vector", "max_index", [topa[h], cand], [pos], pos[:, h, 0:8], top[:, h, 0:8], cand[:, h, :])
            for h in range(8):
                kb.v("vector", "max_index", [topb[h], cand], [pos], pos[:, h, 8:16], top[:, h, 8:16], cand[:, h, :])
            kb.v("vector", "tensor_single_scalar", [pos], [au], au[:], pos[:], 4, ALU.logical_shift_right)
            kb.v("vector", "tensor_single_scalar", [pos], [bu], bu[:], pos[:], 15, ALU.bitwise_and)
            kb.v("vector", "tensor_copy", [au], [af], af[:], au[:])
            kb.v("vector", "tensor_copy", [bu], [bf], bf[:], bu[:])
            io4 = iota16.unsqueeze(1).unsqueeze(1).to_broadcast([128, 8, 16, 16])
            for which, sel_f in ((0, af), (1, bf)):
                kb.v("vector", "tensor_tensor", [sel_f, C["iota_j"]], [E], E[:],
                     sel_f[:].unsqueeze(3).to_broadcast([128, 8, 16, 16]), io4, ALU.is_equal)
                kb.v("vector", "tensor_tensor", [E, idxf], [E], E[:], E[:],
                     if4[:, :, which, :].unsqueeze(2).to_broadcast([128, 8, 16, 16]), ALU.mult)
                kb.v("vector", "tensor_reduce", [E], [sel], sel[:, which, :].rearrange("p (h k) -> p h k", h=8),
                     E[:], AX.X, ALU.add)
            g3 = sel[:, 2, :].rearrange("p (h k) -> p h k", h=8)
            kb.v("vector", "tensor_tensor", topa + topb, [sel], g3, top[:], top[:, :, 0:1].to_broadcast([128, 8, 16]), ALU.subtract)
            kb.act(sel[:, 2, :], sel[:, 2, :], AF.Exp, [sel], [sel])
            kb.v("vector", "tensor_reduce", [sel], [gs], gs[:], g3, AX.X, ALU.add)
            kb.v("vector", "reciprocal", [gs], [gs], gs[:], gs[:])
            kb.v("vector", "tensor_tensor", [sel, gs], [sel], g3, g3, gs[:].unsqueeze(2).to_broadcast([128, 8, 16]), ALU.mult)
            for w_ in range(3):
                kb.tr(ps_r[:, w_, :], sel[:, w_, :], C["ident_f"][:], [sel, C["ident_f"]], [ps_r])
            rt = rt_sb.next()
            kb.act(rt[:], ps_r[:], AF.Copy, [ps_r], [rt])
            kb.dma(RTd[tt].ap, rt[:], [rt], [RTd[tt]], q="gpsimd")


def peer_apply(kb, C, I, layer, MOD, groups, src_tile, NTd, RTd, UT, VB, epilogue):
    with kb.phase():
        Gs = kb.T([128, 128, 384], BF16, "Gs")
        QT_ = 32
        A = Rot([kb.T([128, QT_, 128], BF16, f"A{i}") for i in range(2)])
        B = Rot([kb.T([128, QT_, 128], BF16, f"B{i}") for i in range(2)])
        nTg = kb.T([128, 8, 384], BF16, "nTg")
        rt = Rot([kb.T([128, 3, 128], F32, f"rt{i}") for i in range(2)])
        uT = Rot([kb.T([128, 8, 128], BF16, f"uT{i}") for i in range(4)])
        vv = Rot([kb.T([128, D], BF16, f"vv{i}") for i in range(4)])
        ga = Rot([kb.T([128, 384], F32, f"ga{i}") for i in range(2)])
        W = Rot([kb.T([128, 384], BF16, f"W{i}") for i in range(3)])
        ep = epilogue("alloc", kb)
        acc = [[kb.psv(2 * t + h, 1, [128, 512], F32, f"acc{t}{h}") for h in range(2)] for t in range(3)]
        ps_a = [kb.psv(6 + i, 1, [128, 384], F32, f"ps_a{i}") for i in range(2)]
        ps_G = [kb.psv(6 + i, 1, [128, 4, 128], F32, f"ps_G{i}", root=ps_a[i]) for i in range(2)]
        ps_a = Rot(ps_a)
        ps_G = Rot(ps_G)
        iota_b = C["iota_j"][:].unsqueeze(1).to_broadcast([128, QT_, 128])
        for tiles in groups:
            ng = len(tiles)
            ntok = ng * 128
            for gi, tt in enumerate(tiles):
                kb.dma(nTg[:, :, gi * 128:(gi + 1) * 128], NTd[tt].ap, [NTd[tt]], [nTg])
                r = rt.next()
                kb.dma(r[:], RTd[tt].ap, [RTd[tt]], [r])
                for qt in range(128 // QT_):
                    ts_ = slice(qt * QT_, (qt + 1) * QT_)
                    a_ = A.next(); b_ = B.next()
                    kb.v("vector", "tensor_tensor", [r, C["iota_j"]], [a_], a_[:], iota_b,
                         r[:, 0, ts_].unsqueeze(2).to_broadcast([128, QT_, 128]), ALU.is_equal)
                    kb.v("gpsimd", "tensor_tensor", [a_, r], [a_], a_[:], a_[:],
                         r[:, 2, ts_].unsqueeze(2).to_broadcast([128, QT_, 128]), ALU.mult)
                    kb.v("vector", "tensor_tensor", [r, C["iota_j"]], [b_], b_[:], iota_b,
                         r[:, 1, ts_].unsqueeze(2).to_broadcast([128, QT_, 128]), ALU.is_equal)
                    for t4 in range(QT_ // 4):
                        pg = ps_G.next()
                        for u in range(4):
                            t = t4 * 4 + u
                            kb.mm(pg[:, u, :], b_[:, t, :], a_[:, t, :], [a_, b_], [pg])
                        tok0 = gi * 128 + qt * QT_ + t4 * 4
                        dst = Gs[:, :, tok0:tok0 + 4].rearrange("j i t -> j t i")
                        kb.act(dst, pg[:], AF.Copy, [pg], [Gs])
            pend = None

            def U_(i):
                u_ = uT.next(); v_ = vv.next()
                kb.dma(u_[:], UT[i].ap, [UT[i]], [u_])
                kb.dma(v_[:], VB[i].ap, [VB[i]], [v_], q="sync" if i % 2 == 0 else "gpsimd")
                pa = ps_a.next()
                for k in range(8):
                    kb.mm(pa[:, 0:ntok], u_[:, k, :], nTg[:, k, 0:ntok], [u_, nTg], [pa], start=(k == 0), stop=(k == 7))
                g_ = ga.next()
                kb.act(g_[:, 0:ntok], pa[:, 0:ntok], AF.Gelu, [pa], [g_])
                w_ = W.next()
                eng = "vector" if i % 2 == 0 else "gpsimd"
                kb.v(eng, "tensor_tensor", [g_, Gs], [w_], w_[:, 0:ntok], g_[:, 0:ntok], Gs[:, i, 0:ntok], ALU.mult)
                return (i, w_, v_)

            def V_(st):
                i, w_, v_ = st
                for gi in range(ng):
                    for h in range(2):
                        kb.mm(acc[gi][h][:], w_[:, gi * 128:(gi + 1) * 128], v_[:, h * 512:(h + 1) * 512], [w_, v_],
                              [acc[gi][h]], start=(i == 0), stop=(i == 127))

            for i in range(128):
                cur = U_(i)
                if pend is not None:
                    V_(pend)
                pend = cur
            V_(pend)
            for gi, tt in enumerate(tiles):
                epilogue("run", kb, tt, acc[gi], ep)


def make_epilogue(I, MOD, src_tile, dst_tile, final=False):
    def ep(mode, kb, tt=None, acc=None, b=None):
        if mode == "alloc":
            b = {"M5": [kb.T([128, D], F32, f"M5_{i}") for i in range(2)],
                 "hres": kb.T([128, D], F32, "hres"), "o": kb.T([128, D], F32, "o")}
            for r in range(2):
                load_bcast(kb, b["M5"][r], MOD.ap[r, 5 * D:6 * D], [MOD])
            if final:
                b["fg"] = kb.T([128, D], F32, "fg")
                load_bcast(kb, b["fg"], I["norm_f_g"], [I["_norm_f_g"]])
                b["junk"] = kb.T([128, D], BF16, "junkf")
                b["ss"] = kb.T([128, 1], F32, "ssf")
                b["rstd"] = kb.T([128, 1], F32, "rstdf")
            return b
        isc = 1 if tt < 2 else 0
        sap, sbuf_ = src_tile(tt)
        hres, o, M5 = b["hres"], b["o"], b["M5"][isc]
        kb.dma(hres[:], sap, [sbuf_], [hres])
        for h in range(2):
            hs = slice(h * 512, (h + 1) * 512)
            kb.v("vector", "tensor_tensor", [acc[h], M5], [o], o[:, hs], acc[h][:], M5[:, hs], ALU.mult)
        kb.v("gpsimd", "tensor_tensor", [o, hres], [o], o[:], o[:], hres[:], ALU.add)
        dap, dbuf = dst_tile(tt)
        if final:
            kb.act(b["junk"][:], o[:], AF.Square, [o], [b["junk"], b["ss"]], accum_out=b["ss"][:])
            rstd_of(kb, b["ss"], b["rstd"], D)
            kb.v("vector", "scalar_tensor_tensor", [o, b["rstd"], b["fg"]], [o], o[:], o[:], b["rstd"][:, 0:1],
                 b["fg"][:], ALU.mult, ALU.mult)
        kb.dma(dap, o[:], [o], [dbuf], q="gpsimd")
    return ep


def tile_groups(tiles, n=3):
    return [tiles[i:i + n] for i in range(0, len(tiles), n)]


INPUT_SPECS = [
    ("x", [SEQ, D]), ("c", [D]), ("ctx", [NCTX, D]), ("c_ctx", [D]),
    ("norm1_g", [2, D]), ("norm2_g", [2, D]), ("w_mod", [2, D, 6 * D]), ("b_mod", [2, 6 * D]),
    ("even_w_in", [1, D, 1952]), ("mla_q_norm", [1, 256]), ("mla_kv_norm", [1, 128]),
    ("mla_w_uq", [1, 256, 768]), ("mla_w_ukv", [1, 128, 1024]), ("conv_w", [1, 3, 512]),
    ("even_w_out", [1, D, D]), ("odd_w_in", [1, D, 3088]), ("mlstm_gate_b", [1, 16]),
    ("mlstm_head_g", [1, D]), ("odd_w_out", [1, D, D]), ("peer_w_q", [2, D, 2048]),
    ("peer_subkeys", [2, 8, 2, 128, 128]), ("peer_u", [2, 16384, D]), ("peer_v", [2, 16384, D]),
    ("norm_f_g", [D]), ("rope", [SEQ, 32]),
]


def build_program(stop_after=None, dbg=()):
    nc = bass.Bass("TRN2", target_bir_lowering=False)
    kb = KB(nc, dbg)
    kb.stop_after = stop_after
    I = {}
    for name, shape in INPUT_SPECS:
        ap = nc.dram_tensor(name, shape, F32, kind="ExternalInput").ap()
        I[name] = ap
        I["_" + name] = Buf(ap, name)
    out = nc.dram_tensor("y", [SEQ, D], F32, kind="ExternalOutput").ap()
    kb.eps_t = kb.T([128, 1], F32, "eps")
    kb.v("gpsimd", "memset", [], [kb.eps_t], kb.eps_t[:], EPS)
    kb.one_t = kb.T([128, 1], F32, "one")
    kb.v("gpsimd", "memset", [], [kb.one_t], kb.one_t[:], 1.0)
    C = make_consts(kb)
    MOD = [Buf(kb.dram(f"MOD{l}", [2, 6 * D], F32), f"MOD{l}") for l in range(2)]
    H0 = kb.dram_tiles("H0", NT_ALL, [128, D], F32)

    def src0(tt):
        if tt < 2:
            return I["ctx"][tt * 128:(tt + 1) * 128, :], I["_ctx"]
        return I["x"][(tt - 2) * 128:(tt - 1) * 128, :], I["_x"]

    phase_mod(kb, C, I, 0, MOD[0])
    if stop_after == "mod0":
        kb.finish()
        return nc, kb
    layer_even(kb, C, I, 0, MOD[0], src0, H0)
    if stop_after in ("even", "E1"):
        kb.finish()
        return nc, kb
    UT = kb.dram_tiles("UT", 128, [128, 8, 128], BF16)
    VB = kb.dram_tiles("VB", 128, [128, D], BF16)
    NTd = kb.dram_tiles("NTd", NT_ALL, [128, 8, 128], BF16)
    RTd = kb.dram_tiles("RTd", NT_ALL, [128, 3, 128], F32)
    H1 = kb.dram_tiles("H1", NT_ALL, [128, D], F32)
    srcH0 = lambda tt: (H0[tt].ap, H0[tt])
    dstH1 = lambda tt: (H1[tt].ap, H1[tt])
    tiles0 = DBG_PTILES or list(range(NT_ALL))
    peer_prep(kb, C, I, 0, UT, VB)
    peer_route(kb, C, I, 0, MOD[0], tiles0, srcH0, NTd, RTd)
    if stop_after == "route0":
        kb.finish()
        return nc, kb
    peer_apply(kb, C, I, 0, MOD[0], tile_groups(tiles0), srcH0, NTd, RTd, UT, VB,
               make_epilogue(I, MOD[0], srcH0, dstH1))
    if stop_after == "peer0":
        kb.finish()
        return nc, kb
    phase_mod(kb, C, I, 1, MOD[1])
    H2 = kb.dram_tiles("H2", NT_ALL, [128, D], F32)
    srcH1 = lambda tt: (H1[tt].ap, H1[tt])
    layer_odd(kb, C, I, 1, MOD[1], srcH1, H2)
    if stop_after == "odd":
        kb.finish()
        return nc, kb
    srcH2 = lambda tt: (H2[tt].ap, H2[tt])
    outb = [Buf(out[(tt - 2) * 128:(tt - 1) * 128, :], f"y{tt}") for tt in range(NT_ALL)]
    dstY = lambda tt: (outb[tt].ap, outb[tt])
    tiles1 = list(range(2, NT_ALL))
    peer_prep(kb, C, I, 1, UT, VB)
    peer_route(kb, C, I, 1, MOD[1], tiles1, srcH2, NTd, RTd)
    peer_apply(kb, C, I, 1, MOD[1], tile_groups(tiles1), srcH2, NTd, RTd, UT, VB,
               make_epilogue(I, MOD[1], srcH2, dstY, final=True))
    kb.finish()
    return nc, kb


def rope_table():
    n_freq = 8
    inv = (10000.0 ** (-np.arange(n_freq, dtype=np.float32) / n_freq)).astype(np.float32)
    t = np.arange(SEQ)
    row = (t // 64).astype(np.float32)
    col = (t % 64).astype(np.float32)
    ang = np.concatenate([row[:, None] * inv, col[:, None] * inv], axis=-1).astype(np.float32)
    return np.concatenate([np.cos(ang), np.sin(ang)], axis=-1).astype(np.float32)


def make_in_maps(inputs, cores):
    shared = {k: np.ascontiguousarray(np.asarray(v, dtype=np.float32)) for k, v in inputs.items()
              if k not in ("x", "c", "ctx")}
    shared["rope"] = rope_table()
    maps = []
    for b in cores:
        m = dict(shared)
        m["x"] = np.ascontiguousarray(inputs["x"][b])
        m["c"] = np.ascontiguousarray(inputs["c"][b])
        m["ctx"] = np.ascontiguousarray(inputs["ctx"][b])
        maps.append(m)
    return maps


def kernel(**inputs):
    nc, kb = build_program()
    maps = make_in_maps(inputs, list(range(8)))
    res = run_bass_kernel_spmd(nc, maps, core_ids=list(range(8)))
    return np.stack([r["y"] for r in res.results], axis=0).astype(np.float32)
```

```python
import math
import numpy as np
from contextlib import ExitStack, contextmanager
import concourse.bass as bass
import concourse.mybir as mybir
from concourse.bass_utils import run_bass_kernel_spmd

F32 = mybir.dt.float32
BF16 = mybir.dt.bfloat16
U32 = mybir.dt.uint32
ALU = mybir.AluOpType
AF = mybir.ActivationFunctionType
AX = mybir.AxisListType

D = 1024
SEQ = 4096
NCTX = 256
NT_ALL = 34
EPS = 1e-6
MLA_SCALE = 96 ** -0.5
DBG_TILES = None
DBG_PTILES = None
SKIP = set()
SB_WORDS = 51200


class Buf:
    __slots__ = ("ap", "last_write", "reads", "name", "psum", "root")

    def __init__(self, ap, name="", psum=False, root=None):
        self.root = root.root if root is not None else self
        self.ap = ap
        self.last_write = None
        self.reads = []
        self.name = name
        self.psum = psum

    def __getitem__(self, k):
        return self.ap[k]


class Op:
    __slots__ = ("eng", "fn", "deps", "flag", "is_dma", "sem", "val", "slot")

    def __init__(self, eng, fn, is_dma):
        self.eng = eng
        self.fn = fn
        self.deps = []
        self.flag = False
        self.is_dma = is_dma
        self.sem = None
        self.val = None
        self.slot = None


ENGS = ["tensor", "vector", "scalar", "gpsimd", "sync"]
N_CSEM = 3
N_DSEM = {"sync": 44, "scalar": 4, "gpsimd": 36}


class Prog:
    def __init__(self, nc):
        self.nc = nc
        self.q = {e: [] for e in ENGS}
        self.last_real = {e: None for e in ENGS}
        self.dma_hist = {e: [] for e in N_DSEM}

    def add(self, eng, fn, reads=(), writes=(), dma=False):
        op = Op(eng, fn, dma)
        deps = []
        reads = [b.root for b in reads]
        writes = [b.root for b in writes]
        writes = list(writes) + [b for b in reads if b.psum and b not in writes]
        for b in reads:
            if b.last_write is not None:
                deps.append(b.last_write)
        for b in writes:
            if b.last_write is not None:
                deps.append(b.last_write)
            deps.extend(b.reads)
        seen = set()
        for d in deps:
            if id(d) in seen:
                continue
            seen.add(id(d))
            if d.eng == eng and not d.is_dma and not dma:
                if eng == "tensor":
                    continue
                if not any(b.last_write is d for b in reads):
                    continue
            op.deps.append(d)
            d.flag = True
        for b in reads:
            if not b.psum:
                b.reads.append(op)
        for b in writes:
            b.last_write = op
            b.reads = []
        if dma:
            h = self.dma_hist[eng]
            n = N_DSEM[eng]
            k = len(h)
            op.slot = k % n
            op.val = 16 * (k // n + 1)
            if k >= n:
                op.deps.append(h[k - n])
            h.append(op)
        elif fn is not None:
            self.last_real[eng] = op
        self.q[eng].append(op)
        return op

    def barrier(self):
        deps = []
        for e in ENGS:
            d = self.last_real[e]
            if d is not None:
                d.flag = True
                deps.append(d)
        for e, h in self.dma_hist.items():
            deps.extend(h[-N_DSEM[e]:])
        for e in ENGS:
            op = Op(e, None, False)
            op.deps = list(deps)
            self.q[e].append(op)

    def emit(self, stack):
        nc = self.nc
        csems = {e: [stack.enter_context(nc.semaphore(f"c_{e}_{i}")) for i in range(N_CSEM)]
                 for e in ["tensor", "vector", "scalar", "gpsimd"]}
        dsems = {e: [stack.enter_context(nc.semaphore(f"d_{e}_{i}")) for i in range(n)]
                 for e, n in N_DSEM.items()}
        for e in ENGS:
            nflag = 0
            for op in self.q[e]:
                if op.is_dma:
                    op.sem = dsems[e][op.slot]
                elif op.flag:
                    op.sem = csems[e][nflag % N_CSEM]
                    op.val = nflag // N_CSEM + 1
                    nflag += 1
        stats = {}

        def run(e):
            def body(eng):
                waited = {}
                nw = 0
                for op in self.q[e]:
                    for d in op.deps:
                        key = id(d.sem)
                        if waited.get(key, 0) >= d.val:
                            continue
                        waited[key] = d.val
                        eng.wait_ge(d.sem, d.val)
                        nw += 1
                    if op.fn is None:
                        continue
                    ins = op.fn(eng)
                    if op.is_dma:
                        ins.then_inc(op.sem, 16)
                    elif op.flag:
                        ins.then_inc(op.sem, 1)
                stats[e] = (len(self.q[e]), nw)
            return body

        with nc.Block() as block:
            block.sync(run("sync"))
            block.tensor(run("tensor"))
            block.vector(run("vector"))
            block.scalar(run("scalar"))
            block.gpsimd(run("gpsimd"))
        self.stats = stats


class Rot:
    def __init__(self, items):
        self.items = items
        self.i = 0

    def next(self):
        it = self.items[self.i % len(self.items)]
        self.i += 1
        return it


def _size(dt):
    return {F32: 4, BF16: 2, U32: 4}[dt]


class KB:
    def __init__(self, nc, dbg=()):
        self.nc = nc
        self.P = Prog(nc)
        self.stack = ExitStack()
        self.SB = self.stack.enter_context(nc.sbuf_tensor("SB", [128, SB_WORDS], F32))
        self.PS = self.stack.enter_context(nc.psum_tensor("PSA", [128, 8 * 512], F32))
        self.off = 0
        self.dbg = set(dbg)
        self.ndram = 0

    def T(self, shape, dt=F32, name=""):
        p = shape[0]
        n = int(np.prod(shape[1:]))
        words = (n * _size(dt) + 3) // 4
        assert self.off + words <= SB_WORDS, f"SBUF overflow allocating {name}{shape}: off={self.off} words={words}"
        ap = self.SB[0:p, self.off:self.off + words]
        self.off += words
        if dt != F32:
            ap = ap.bitcast(dt)
        ap = ap[:, 0:n]
        if len(shape) > 2:
            names = " ".join(f"d{i}" for i in range(len(shape) - 1))
            kw = {f"d{i}": shape[i + 1] for i in range(len(shape) - 2)}
            ap = ap.rearrange(f"p ({names}) -> p {names}", **kw)
        return Buf(ap, name)

    def psv(self, b0, nb, shape, dt=F32, name="", root=None):
        p = shape[0]
        n = int(np.prod(shape[1:]))
        ap = self.PS[0:p, b0 * 512:(b0 + nb) * 512]
        if dt != F32:
            ap = ap.bitcast(dt)
        ap = ap[:, 0:n]
        if len(shape) > 2:
            names = " ".join(f"d{i}" for i in range(len(shape) - 1))
            kw = {f"d{i}": shape[i + 1] for i in range(len(shape) - 2)}
            ap = ap.rearrange(f"p ({names}) -> p {names}", **kw)
        return Buf(ap, name, psum=True, root=root)

    def dram(self, name, shape, dt=F32, kind=None):
        if kind is None:
            kind = "ExternalOutput" if name in self.dbg else "Internal"
        t = self.nc.dram_tensor(name, list(shape), dt, kind=kind)
        return t.ap()

    def dram_tiles(self, name, n, shape, dt=F32):
        ap = self.dram(name, [n] + list(shape), dt)
        return [Buf(ap[i], f"{name}{i}") for i in range(n)]

    @contextmanager
    def phase(self):
        m = self.off
        yield
        self.P.barrier()
        self.off = m

    def dma(self, out, in_, reads, writes, q="sync", **kw):
        return self.P.add(q, lambda e: e.dma_start(out=out, in_=in_, **kw), reads, writes, dma=True)

    def mm(self, out, lhsT, rhs, reads, writes, start=True, stop=True):
        return self.P.add("tensor", lambda e: e.matmul(out, lhsT, rhs, start=start, stop=stop), reads, writes)

    def tr(self, out, in_, ident, reads, writes):
        return self.P.add("tensor", lambda e: e.transpose(out, in_, ident), reads, writes)

    def act(self, out, in_, func, reads, writes, **kw):
        return self.P.add("scalar", lambda e: e.activation(out=out, in_=in_, func=func, **kw), reads, writes)

    def v(self, eng, method, reads, writes, *a, **kw):
        return self.P.add(eng, lambda e: getattr(e, method)(*a, **kw), reads, writes)

    def finish(self):
        self.P.barrier()
        self.P.emit(self.stack)
        self.stack.close()


def make_consts(kb):
    c = {}
    io = kb.T([128, 128], F32, "iota")
    kb.P.add("gpsimd", lambda e: e.iota(io[:], [[1, 128]], base=0, channel_multiplier=-1,
                                        allow_small_or_imprecise_dtypes=True), [], [io])
    c["jmp"] = io
    c["ident_bf"] = kb.T([128, 128], BF16, "ident_bf")
    kb.v("vector", "tensor_single_scalar", [io], [c["ident_bf"]], c["ident_bf"][:], io[:], 0.0, ALU.is_equal)
    ij = kb.T([128, 128], F32, "iota_j")
    kb.P.add("gpsimd", lambda e: e.iota(ij[:], [[1, 128]], base=0, channel_multiplier=0,
                                        allow_small_or_imprecise_dtypes=True), [], [ij])
    c["iota_j"] = ij
    c["iota_bf"] = kb.T([128, 128], BF16, "iota_bf")
    kb.v("vector", "tensor_copy", [ij], [c["iota_bf"]], c["iota_bf"][:], ij[:])
    c["ident_f"] = kb.T([128, 128], F32, "ident_f")
    kb.v("vector", "tensor_single_scalar", [io], [c["ident_f"]], c["ident_f"][:], io[:], 0.0, ALU.is_equal)
    return c


def load_bcast(kb, dst, src_ap, srcbufs, q="sync"):
    p = dst.ap.shape[0]
    return kb.dma(dst[:], src_ap.partition_broadcast(p), srcbufs, [dst], q=q)


def load_w_bf16(kb, dst, w_ap, wbuf, stage_rot, K, N, c0=0, c1=None, eng_rot=None):
    c1 = N if c1 is None else c1
    kc = K // 128
    wv = w_ap.rearrange("(k p) n -> p k n", p=128)
    maxcols = stage_rot.items[0].ap.shape[1]
    i = 0
    for k in range(kc):
        for cs in range(c0, c1, maxcols):
            ce = min(c1, cs + maxcols)
            st = stage_rot.next()
            kb.dma(st[:, 0:ce - cs], wv[:, k, cs:ce], [wbuf], [st], q="sync" if i % 2 == 0 else "gpsimd")
            eng = ["gpsimd", "vector"][i % 2]
            kb.v(eng, "tensor_copy", [st], [dst], dst[:, k, cs - c0:ce - c0], st[:, 0:ce - cs])
            i += 1


def rstd_of(kb, ss, rstd, n):
    kb.act(rstd[:], ss[:], AF.Ln, [ss], [rstd], scale=1.0 / n, bias=kb.eps_t[:, 0:1])
    kb.act(rstd[:], rstd[:], AF.Exp, [rstd], [rstd], scale=-0.5)


def build_mod_tiles(kb, mod_ap, modbuf, g_ap, gbuf, row, j_shift, j_scale, G1, SH, tmp):
    load_bcast(kb, tmp, mod_ap[row, j_scale * D:(j_scale + 1) * D], [modbuf])
    load_bcast(kb, G1, g_ap, [gbuf], q="gpsimd")
    kb.v("vector", "scalar_tensor_tensor", [tmp, G1], [G1], G1[:], tmp[:], 1.0, G1[:], ALU.add, ALU.mult)
    load_bcast(kb, SH, mod_ap[row, j_shift * D:(j_shift + 1) * D], [modbuf])


def norm_mod_T(kb, C, src_ap, srcbuf, G1, SH, xt, junk, ss, rstd, nb, psT, nT):
    kb.dma(xt[:], src_ap, [srcbuf], [xt])
    kb.act(junk[:], xt[:], AF.Square, [xt], [junk, ss], accum_out=ss[:])
    rstd_of(kb, ss, rstd, D)
    kb.v("vector", "scalar_tensor_tensor", [xt, rstd, G1], [xt], xt[:], xt[:], rstd[:, 0:1], G1[:], ALU.mult, ALU.mult)
    kb.v("gpsimd", "tensor_tensor", [xt, SH], [nb], nb[:], xt[:], SH[:], ALU.add)
    for k in range(8):
        kb.tr(psT[:, k, :], nb[:, k * 128:(k + 1) * 128], C["ident_bf"][:], [nb, C["ident_bf"]], [psT])
    kb.act(nT[:], psT[:], AF.Copy, [psT], [nT])


def phase_mod(kb, C, I, layer, MOD):
    with kb.phase():
        raw = kb.T([128, 2, 8], F32, "craw")
        kb.dma(raw[:, 0, :], I["c"].rearrange("(p k) -> p k", k=8), [I["_c"]], [raw])
        kb.dma(raw[:, 1, :], I["c_ctx"].rearrange("(p k) -> p k", k=8), [I["_c_ctx"]], [raw])
        sc = kb.T([128, 8, 2], F32, "csilu")
        kb.act(sc[:].rearrange("p k m -> p m k"), raw[:], AF.Silu, [raw], [sc])
        bm = kb.T([2, 6144], F32, "bm")
        load_bcast(kb, bm, I["b_mod"][layer], [I["_b_mod"]], q="gpsimd")
        res = kb.T([2, 6144], F32, "modres")
        wrot = Rot([kb.T([128, 8, 512], F32, f"wm{i}") for i in range(2)])
        prot = Rot([kb.psv(i, 1, [128, 512], F32, f"psm{i}") for i in range(2)])
        wv = I["w_mod"][layer].rearrange("(p k) n -> p k n", k=8)
        for ng in range(12):
            wm = wrot.next()
            ps = prot.next()
            kb.dma(wm[:], wv[:, :, ng * 512:(ng + 1) * 512], [I["_w_mod"]], [wm], q="sync" if ng % 2 == 0 else "gpsimd")
            for k in range(8):
                kb.mm(ps[0:2, :], sc[:, k, :], wm[:, k, :], [sc, wm], [ps], start=(k == 0), stop=(k == 7))
            kb.v("vector", "tensor_tensor", [ps, bm], [res], res[:, ng * 512:(ng + 1) * 512], ps[0:2, :],
                 bm[:, ng * 512:(ng + 1) * 512], ALU.add)
        kb.dma(MOD.ap, res[:], [res], [MOD])


ZCOLS = 4356


def zcol(tt):
    return 1 + tt * 128 if tt < 2 else 259 + (tt - 2) * 128


def layer_even(kb, C, I, layer, MOD, src_tile, H, need_ctx=True):
    j = 0
    P = kb.P
    PQ = kb.dram_tiles("PQ", NT_ALL, [128, 256], F32)
    ZT = kb.dram("ZT", [512, ZCOLS], F32)
    BT = kb.dram("BT", [512, ZCOLS], F32)
    ZTb = [Buf(None, f"zt{t}") for t in range(NT_ALL)]
    BTb = [Buf(None, f"bt{t}") for t in range(NT_ALL)]
    ZPAD = Buf(None, "ztpad")
    ZTv = ZT.rearrange("(c p) t -> p c t", p=128)
    BTv = BT.rearrange("(c p) t -> p c t", p=128)
    with kb.phase():
        KT = kb.T([128, 8, NT_ALL * 128], BF16, "KT_all")
        VP = kb.T([128, NT_ALL, 8, 65], BF16, "VP_all")
        kmax = kb.T([128, 8], F32, "kmax")
        kb.v("gpsimd", "memset", [], [VP], VP[:], 1.0)
        kb.v("gpsimd", "memset", [], [kmax], kmax[:], 0.0)
        rope = I["rope"]
        with kb.phase():
            stage = Rot([kb.T([128, 1024], F32, f"stg{i}") for i in range(2)])
            w_in = kb.T([128, 8, 1952], BF16, "w_in")
            load_w_bf16(kb, w_in, I["even_w_in"][j], I["_even_w_in"], stage, 1024, 1952)
            w_ukv = kb.T([128, 1, 1024], BF16, "w_ukv")
            load_w_bf16(kb, w_ukv, I["mla_w_ukv"][j], I["_mla_w_ukv"], stage, 128, 1024)
            kvg = kb.T([128, 128], F32, "kvg")
            load_bcast(kb, kvg, I["mla_kv_norm"][j], [I["_mla_kv_norm"]])
            tmpm = kb.T([128, D], F32, "tmpm")
            G1 = [kb.T([128, D], F32, f"G1_{i}") for i in range(2)]
            SH = [kb.T([128, D], F32, f"SH_{i}") for i in range(2)]
            for r in range(2):
                build_mod_tiles(kb, MOD.ap, MOD, I["norm1_g"][layer], I["_norm1_g"], r, 0, 1, G1[r], SH[r], tmpm)
            zero = kb.T([128, 4, 1], F32, "zero")
            kb.v("gpsimd", "memset", [], [zero], zero[:], 0.0)
            for col in (0, 257, 258, 4355):
                kb.dma(ZTv[:, :, col:col + 1], zero[:], [zero], [ZPAD], q="gpsimd", allow_slow_non_contiguous=True)
            xt = kb.T([128, D], F32, "xt")
            junk = kb.T([128, D], BF16, "junk")
            ss = kb.T([128, 1], F32, "ss")
            rstd = kb.T([128, 1], F32, "rstd")
            ss2 = kb.T([128, 1], F32, "ss2")
            rstd2 = kb.T([128, 1], F32, "rstd2")
            nb = kb.T([128, D], BF16, "nb")
            nT = kb.T([128, 8, 128], BF16, "nT")
            pm = Rot([kb.T([128, 416], F32, f"pm{i}") for i in range(2)])
            latn = kb.T([128, 128], BF16, "latn")
            klT = kb.T([128, 128], BF16, "klT")
            kext = Rot([kb.T([128, 8, 97], BF16, f"kext{i}") for i in range(2)])
            for kx in kext.items:
                kb.v("gpsimd", "memset", [], [kx], kx[:], 1.0)
            krot = kb.T([128, 32], F32, "krot")
            rtmp = kb.T([128, 4, 16], F32, "rtmp")
            cs = kb.T([128, 32], F32, "cs")
            ksqt = kb.T([128, 8, 96], F32, "ksqt")
            ksq = kb.T([128, 8], F32, "ksq")
            c_sb = kb.T([128, 4, 128], F32, "c_sb")
            z_sb = Rot([kb.T([128, 4, 128], F32, f"z_sb{i}") for i in range(2)])
            b_sb = Rot([kb.T([128, 4, 128], F32, f"b_sb{i}") for i in range(2)])
            psT = kb.psv(0, 1, [128, 8, 128], BF16, "psT")
            ps_mla = kb.psv(1, 1, [128, 416], F32, "ps_mla")
            ps_lat = kb.psv(2, 1, [128, 8, 128], BF16, "ps_lat")
            ps_kv = kb.psv(3, 2, [128, 8, 128], F32, "ps_kv")
            ps_conv = kb.psv(5, 3, [128, 12, 128], F32, "ps_conv")
            for tt in (DBG_TILES or range(NT_ALL)):
                isc = 1 if tt < 2 else 0
                sap, sbuf_ = src_tile(tt)
                norm_mod_T(kb, C, sap, sbuf_, G1[isc], SH[isc], xt, junk, ss, rstd, nb, psT, nT)
                for k in range(8):
                    kb.mm(ps_mla[:], nT[:, k, :], w_in[:, k, 0:416], [nT, w_in], [ps_mla], start=(k == 0), stop=(k == 7))
                pmt = pm.next()
                kb.act(pmt[:], ps_mla[:], AF.Copy, [ps_mla], [pmt])
                kb.dma(PQ[tt].ap, pmt[:, 0:256], [pmt], [PQ[tt]], q="gpsimd")
                if "kv" in SKIP:
                    continue
                kb.act(junk[:, 0:128], pmt[:, 256:384], AF.Square, [pmt], [junk, ss2], accum_out=ss2[:])
                rstd_of(kb, ss2, rstd2, 128)
                kb.v("vector", "scalar_tensor_tensor", [pmt, rstd2, kvg], [latn], latn[:], pmt[:, 256:384],
                     rstd2[:, 0:1], kvg[:], ALU.mult, ALU.mult)
                kb.tr(ps_lat[:, 0, :], latn[:], C["ident_bf"][:], [latn, C["ident_bf"]], [ps_lat])
                kb.v("vector", "tensor_copy", [ps_lat], [klT], klT[:], ps_lat[:, 0, :])
                if "kv2" in SKIP:
                    continue
                kb.mm(ps_kv[:, 0:4, :], klT[:], w_ukv[:, 0, 0:512], [klT, w_ukv], [ps_kv])
                kb.mm(ps_kv[:, 4:8, :], klT[:], w_ukv[:, 0, 512:1024], [klT, w_ukv], [ps_kv])
                if "kv3" in SKIP:
                    continue
                kx = kext.next()
                for hb in range(2):
                    hs = slice(hb * 4, hb * 4 + 4)
                    kb.act(kx[:, hs, 0:64], ps_kv[:, hs, 0:64], AF.Copy, [ps_kv], [kx], scale=MLA_SCALE)
                    kb.v("vector", "tensor_copy", [ps_kv], [VP], VP[:, tt, hs, 0:64], ps_kv[:, hs, 64:128])
                if "rope" in SKIP:
                    continue
                if tt >= 2:
                    kb.dma(cs[:], rope[(tt - 2) * 128:(tt - 1) * 128, :], [I["_rope"]], [cs], q="gpsimd")
                    x1 = pmt[:, 384:400]
                    x2 = pmt[:, 400:416]
                    kb.v("gpsimd", "tensor_tensor", [pmt, cs], [rtmp], rtmp[:, 0, :], x1, cs[:, 0:16], ALU.mult)
                    kb.v("gpsimd", "tensor_tensor", [pmt, cs], [rtmp], rtmp[:, 1, :], x2, cs[:, 16:32], ALU.mult)
                    kb.v("gpsimd", "tensor_tensor", [pmt, cs], [rtmp], rtmp[:, 2, :], x1, cs[:, 16:32], ALU.mult)
                    kb.v("gpsimd", "tensor_tensor", [pmt, cs], [rtmp], rtmp[:, 3, :], x2, cs[:, 0:16], ALU.mult)
                    kb.v("vector", "tensor_tensor", [rtmp], [krot], krot[:, 0:16], rtmp[:, 0, :], rtmp[:, 1, :], ALU.subtract)
                    kb.v("vector", "tensor_tensor", [rtmp], [krot], krot[:, 16:32], rtmp[:, 2, :], rtmp[:, 3, :], ALU.add)
                else:
                    kb.v("vector", "tensor_copy", [pmt], [krot], krot[:], pmt[:, 384:416])
                kb.act(kx[:, :, 64:96], krot[:].unsqueeze(1).to_broadcast([128, 8, 32]), AF.Copy, [krot], [kx],
                       scale=MLA_SCALE)
                if "ksq" in SKIP:
                    continue
                kb.v("vector", "tensor_tensor", [kx], [ksqt], ksqt[:], kx[:, :, 0:96], kx[:, :, 0:96], ALU.mult)
                kb.v("vector", "tensor_reduce", [ksqt], [ksq], ksq[:], ksqt[:], AX.X, ALU.add)
                kb.v("vector", "tensor_tensor", [ksq, kmax], [kmax], kmax[:], kmax[:], ksq[:], ALU.max)
                for h in range(8):
                    kb.tr(ps_lat[0:97, h, :], kx[:, h, :], C["ident_bf"][:], [kx, C["ident_bf"]], [ps_lat])
                kb.act(KT[0:97, :, tt * 128:(tt + 1) * 128], ps_lat[0:97, :, :], AF.Copy, [ps_lat], [KT])
                if "conv" in SKIP:
                    continue
                for fc in range(12):
                    for k in range(8):
                        kb.mm(ps_conv[:, fc, :], w_in[:, k, 416 + fc * 128:416 + (fc + 1) * 128], nT[:, k, :],
                              [w_in, nT], [ps_conv], start=(k == 0), stop=(k == 7))
                kb.act(c_sb[:], ps_conv[:, 4:8, :], AF.Copy, [ps_conv], [c_sb])
                zt = z_sb.next()
                bt = b_sb.next()
                kb.v("vector", "tensor_tensor", [ps_conv, c_sb], [zt], zt[:], ps_conv[:, 8:12, :], c_sb[:], ALU.mult)
                kb.act(bt[:], ps_conv[:, 0:4, :], AF.Copy, [ps_conv], [bt])
                c0 = zcol(tt)
                kb.dma(ZTv[:, :, c0:c0 + 128], zt[:], [zt], [ZTb[tt]], q="gpsimd")
                kb.dma(BTv[:, :, c0:c0 + 128], bt[:], [bt], [BTb[tt]], q="gpsimd")
        if kb.stop_after == "E1":
            return
        with kb.phase():
            stage = Rot([kb.T([128, 1024], F32, f"stg{i}") for i in range(2)])
            w_uq = kb.T([128, 2, 768], BF16, "w_uq")
            load_w_bf16(kb, w_uq, I["mla_w_uq"][j], I["_mla_w_uq"], stage, 256, 768)
            w_out = kb.T([128, 8, 1024], BF16, "w_out")
            load_w_bf16(kb, w_out, I["even_w_out"][j], I["_even_w_out"], stage, 1024, 1024)
            qg = kb.T([128, 256], F32, "qg")
            load_bcast(kb, qg, I["mla_q_norm"][j], [I["_mla_q_norm"]])
            M2 = [kb.T([128, D], F32, f"M2_{i}") for i in range(2)]
            for r in range(2):
                load_bcast(kb, M2[r], MOD.ap[r, 2 * D:3 * D], [MOD])
            cw = kb.T([128, 3, 4], F32, "cw")
            for w_ in range(3):
                for c_ in range(4):
                    kb.dma(cw[:, w_, c_:c_ + 1], I["conv_w"][j][w_, c_ * 128:(c_ + 1) * 128].unsqueeze(1),
                           [I["_conv_w"]], [cw], q="gpsimd", allow_slow_non_contiguous=True)
            psx = kb.psv(6, 1, [128, 128], F32, "psx")
            kmr = kb.T([128, 1], F32, "kmr")
            kb.v("vector", "tensor_reduce", [kmax], [kmr], kmr[:], kmax[:], AX.X, ALU.max)
            kmb = kb.T([128, 128], F32, "kmb")
            kb.v("vector", "tensor_copy", [kmr], [kmb], kmb[:], kmr[:, 0:1].to_broadcast([128, 128]))
            kb.tr(psx[:], kmb[:], C["ident_f"][:], [kmb, C["ident_f"]], [psx])
            ksm = kb.T([128, 1], F32, "ksm")
            kb.v("vector", "tensor_reduce", [psx], [ksm], ksm[:], psx[:], AX.X, ALU.max)
            pq = kb.T([128, 256], F32, "pq")
            junk = kb.T([128, 768], BF16, "junk")
            ss = kb.T([128, 1], F32, "ss")
            rstd = kb.T([128, 1], F32, "rstd")
            qn = kb.T([128, 256], BF16, "qn")
            qlT = kb.T([128, 2, 128], BF16, "qlT")
            q_sb = kb.T([128, 8, 96], F32, "q_sb")
            cs = kb.T([128, 32], F32, "cs")
            rt = kb.T([128, 4, 8, 16], F32, "rt")
            qsqt = kb.T([128, 8, 96], F32, "qsqt")
            qsq = kb.T([128, 8], F32, "qsq")
            qext = kb.T([128, 8, 97], BF16, "qext")
            QT = kb.T([128, 8, 512], BF16, "QT")
            PT = Rot([kb.T([128, 512], BF16, f"PT{i}") for i in range(3)])
            rec = kb.T([128, 4], F32, "rec")
            mix_tok = [kb.T([128, 512], BF16, f"mix{i}") for i in range(4)]
            mixT = kb.T([128, 8, 512], BF16, "mixT")
            zw = kb.T([128, 4, 514], F32, "zw")
            bw = kb.T([128, 4, 512], F32, "bw")
            yc_ = kb.T([128, 512], F32, "yconv")
            hres = stage.items[0]
            ytmp = stage.items[1]
            ps_S = Rot([kb.psv(i, 1, [128, 512], F32, f"psS{i}") for i in range(2)])
            ps_O = [kb.psv(2 + i, 1, [128, 65], F32, f"psO{i}") for i in range(4)]
            ps_q = kb.psv(2, 2, [128, 768], F32, "ps_q")
            ps_t = kb.psv(6, 1, [128, 8, 128], BF16, "ps_t")
            ps_y = kb.psv(6, 2, [128, 1024], F32, "ps_y")
            bank = {i: None for i in range(8)}
            blocks = []
            if need_ctx:
                blocks.append(([0, 1], [0, 1]))
            for b in range(8):
                blocks.append(([2 + 4 * b + i for i in range(4)], list(range(NT_ALL))))
            for tiles, ktiles in blocks:
                nq = len(tiles) * 128
                for qi, tt in enumerate(tiles):
                    kb.dma(pq[:], PQ[tt].ap, [PQ[tt]], [pq])
                    kb.act(junk[:, 0:256], pq[:], AF.Square, [pq], [junk, ss], accum_out=ss[:])
                    rstd_of(kb, ss, rstd, 256)
                    kb.v("vector", "scalar_tensor_tensor", [pq, rstd, qg], [qn], qn[:], pq[:], rstd[:, 0:1], qg[:],
                         ALU.mult, ALU.mult)
                    for k in range(2):
                        kb.tr(ps_t[:, k, :], qn[:, k * 128:(k + 1) * 128], C["ident_bf"][:], [qn, C["ident_bf"]], [ps_t])
                    kb.v("vector", "tensor_copy", [ps_t], [qlT], qlT[:], ps_t[:, 0:2, :])
                    for k in range(2):
                        kb.mm(ps_q[:, 0:512], qlT[:, k, :], w_uq[:, k, 0:512], [qlT, w_uq], [ps_q, ps_O[0]],
                              start=(k == 0), stop=(k == 1))
                    for k in range(2):
                        kb.mm(ps_q[:, 512:768], qlT[:, k, :], w_uq[:, k, 512:768], [qlT, w_uq], [ps_q, ps_O[1]],
                              start=(k == 0), stop=(k == 1))
                    q_flat = q_sb[:].rearrange("p h d -> p (h d)")
                    kb.act(q_flat[:, 0:512], ps_q[:, 0:512], AF.Copy, [ps_q, ps_O[0], ps_O[1]], [q_sb])
                    kb.act(q_flat[:, 512:768], ps_q[:, 512:768], AF.Copy, [ps_q, ps_O[0], ps_O[1]], [q_sb])
                    kb.v("vector", "tensor_tensor", [q_sb], [qsqt], qsqt[:], q_sb[:], q_sb[:], ALU.mult)
                    kb.v("vector", "tensor_reduce", [qsqt], [qsq], qsq[:], qsqt[:], AX.X, ALU.add)
                    kb.act(qsq[:], qsq[:], AF.Sqrt, [qsq, ksm], [qsq], scale=ksm[:, 0:1])
                    kb.v("vector", "tensor_scalar_mul", [qsq], [qext], qext[:, :, 96], qsq[:], -1.0)
                    kb.v("gpsimd", "tensor_copy", [q_sb], [qext], qext[:, :, 0:64], q_sb[:, :, 0:64])
                    if tt >= 2:
                        kb.dma(cs[:], rope[(tt - 2) * 128:(tt - 1) * 128, :], [I["_rope"]], [cs], q="gpsimd")
                        x1 = q_sb[:, :, 64:80]
                        x2 = q_sb[:, :, 80:96]
                        cosb = cs[:, 0:16].unsqueeze(1).to_broadcast([128, 8, 16])
                        sinb = cs[:, 16:32].unsqueeze(1).to_broadcast([128, 8, 16])
                        kb.v("gpsimd", "tensor_tensor", [q_sb, cs], [rt], rt[:, 0], x1, cosb, ALU.mult)
                        kb.v("gpsimd", "tensor_tensor", [q_sb, cs], [rt], rt[:, 1], x2, sinb, ALU.mult)
                        kb.v("gpsimd", "tensor_tensor", [q_sb, cs], [rt], rt[:, 2], x1, sinb, ALU.mult)
                        kb.v("gpsimd", "tensor_tensor", [q_sb, cs], [rt], rt[:, 3], x2, cosb, ALU.mult)
                        kb.v("vector", "tensor_tensor", [rt], [qext], qext[:, :, 64:80], rt[:, 0], rt[:, 1], ALU.subtract)
                        kb.v("vector", "tensor_tensor", [rt], [qext], qext[:, :, 80:96], rt[:, 2], rt[:, 3], ALU.add)
                    else:
                        kb.v("vector", "tensor_copy", [q_sb], [qext], qext[:, :, 64:96], q_sb[:, :, 64:96])
                    for h in range(8):
                        kb.tr(ps_t[0:97, h, :], qext[:, h, :], C["ident_bf"][:], [qext, C["ident_bf"]], [ps_t])
                    kb.act(QT[0:97, :, qi * 128:(qi + 1) * 128], ps_t[0:97, :, :], AF.Copy, [ps_t], [QT])
                nqs = len(tiles)
                for h in range(8):
                    def S_(ki, kt):
                        pS = ps_S.next()
                        kb.mm(pS[:, 0:nq], KT[0:97, h, kt * 128:(kt + 1) * 128], QT[0:97, h, 0:nq], [KT, QT], [pS])
                        pt = PT.next()
                        kb.act(pt[:, 0:nq], pS[:, 0:nq], AF.Exp, [pS], [pt])
                        return (ki, kt, pt)

                    def PV_(st):
                        ki, kt, pt = st
                        for qs in range(nqs):
                            kb.mm(ps_O[qs][:], pt[:, qs * 128:(qs + 1) * 128], VP[:, kt, h, :], [pt, VP], [ps_O[qs]],
                                  start=(ki == 0), stop=(ki == len(ktiles) - 1))

                    pend = None
                    for ki, kt in enumerate(ktiles):
                        cur = S_(ki, kt)
                        if pend is not None:
                            PV_(pend)
                        pend = cur
                    PV_(pend)
                    for qs in range(nqs):
                        kb.v("vector", "reciprocal", [ps_O[qs]], [rec], rec[:, qs:qs + 1], ps_O[qs][:, 64:65])
                        kb.act(mix_tok[qs][:, h * 64:(h + 1) * 64], ps_O[qs][:, 0:64], AF.Copy, [ps_O[qs], rec],
                               [mix_tok[qs]], scale=rec[:, qs:qs + 1])
                for qi in range(nqs):
                    for c_ in range(4):
                        kb.tr(ps_t[:, c_, :], mix_tok[qi][:, c_ * 128:(c_ + 1) * 128], C["ident_bf"][:],
                              [mix_tok[qi], C["ident_bf"]], [ps_t])
                    kb.v("vector", "tensor_copy", [ps_t], [mixT], mixT[:, 0:4, qi * 128:(qi + 1) * 128], ps_t[:, 0:4, :])
                c0 = zcol(tiles[0])
                kb.dma(zw[:, :, 0:nq + 2], ZTv[:, :, c0 - 1:c0 + nq + 1], ZTb + [ZPAD], [zw])
                kb.dma(bw[:, :, 0:nq], BTv[:, :, c0:c0 + nq], BTb, [bw], q="gpsimd")
                for c_ in range(4):
                    eng = "vector"
                    kb.v(eng, "tensor_scalar", [zw, cw], [yc_], yc_[:, 0:nq], zw[:, c_, 0:nq], cw[:, 0, c_:c_ + 1], None, ALU.mult)
                    kb.v(eng, "scalar_tensor_tensor", [zw, cw, yc_], [yc_], yc_[:, 0:nq], zw[:, c_, 1:nq + 1],
                         cw[:, 1, c_:c_ + 1], yc_[:, 0:nq], ALU.mult, ALU.add)
                    kb.v(eng, "scalar_tensor_tensor", [zw, cw, yc_], [yc_], yc_[:, 0:nq], zw[:, c_, 2:nq + 2],
                         cw[:, 2, c_:c_ + 1], yc_[:, 0:nq], ALU.mult, ALU.add)
                    kb.v(eng, "tensor_tensor", [yc_, bw], [mixT], mixT[:, 4 + c_, 0:nq], yc_[:, 0:nq], bw[:, c_, 0:nq], ALU.mult)
                for qi, tt in enumerate(tiles):
                    isc = 1 if tt < 2 else 0
                    sap, sbuf_ = src_tile(tt)
                    kb.dma(hres[:], sap, [sbuf_], [hres])
                    for half in range(2):
                        for c_ in range(8):
                            kb.mm(ps_y[:, half * 512:(half + 1) * 512], mixT[:, c_, qi * 128:(qi + 1) * 128],
                                  w_out[:, c_, half * 512:(half + 1) * 512], [mixT, w_out], [ps_y, ps_t],
                                  start=(c_ == 0), stop=(c_ == 7))
                    for half in range(2):
                        hs = slice(half * 512, (half + 1) * 512)
                        kb.v("vector", "tensor_tensor", [ps_y, ps_t, M2[isc]], [ytmp], ytmp[:, hs], ps_y[:, hs], M2[isc][:, hs], ALU.mult)
                    kb.v("gpsimd", "tensor_tensor", [ytmp, hres], [ytmp], ytmp[:], ytmp[:], hres[:], ALU.add)
                    kb.dma(H[tt].ap, ytmp[:], [ytmp], [H[tt]], q="gpsimd")


def layer_odd(kb, C, I, layer, MOD, src_tile, H, bg=None):
    j = 0
    QKT = kb.dram_tiles("QKT", NT_ALL, [128, 8, 128], BF16)
    KHd = kb.dram_tiles("KHd", NT_ALL, [128, 8, 128], BF16)
    VPd = kb.dram_tiles("VPd", NT_ALL, [128, 4, 257], BF16)
    SCd = kb.dram_tiles("SCd", NT_ALL, [128, 32], F32)
    OGd = kb.dram_tiles("OGd", NT_ALL, [128, D], F32)
    HBd = kb.dram_tiles("HBd", NT_ALL, [128, D], F32)
    HSd = kb.dram_tiles("HSd", NT_ALL, [128, D], F32)
    lat_tiles = list(range(2, NT_ALL))
    with kb.phase():
        stage = Rot([kb.T([128, 1024], F32, f"stg{i}") for i in range(2)])
        w_in = kb.T([128, 8, 3088], BF16, "w_in_o")
        load_w_bf16(kb, w_in, I["odd_w_in"][j], I["_odd_w_in"], stage, 1024, 3088)
        tmpm = stage.items[0]
        G1 = [kb.T([128, D], F32, f"G1_{i}") for i in range(2)]
        SH = [kb.T([128, D], F32, f"SH_{i}") for i in range(2)]
        for r in range(2):
            build_mod_tiles(kb, MOD.ap, MOD, I["norm1_g"][layer], I["_norm1_g"], r, 0, 1, G1[r], SH[r], tmpm)
        gbias = kb.T([128, 16], F32, "gbias")
        load_bcast(kb, gbias, I["mlstm_gate_b"][j], [I["_mlstm_gate_b"]])
        triU = kb.T([128, 128], F32, "triU")
        triL = kb.T([128, 128], F32, "triL")
        ones = kb.T([128, 128], F32, "ones")
        kb.v("vector", "tensor_single_scalar", [C["jmp"]], [triU], triU[:], C["jmp"][:], 0.0, ALU.is_ge)
        kb.v("vector", "tensor_single_scalar", [C["jmp"]], [triL], triL[:], C["jmp"][:], 0.0, ALU.is_le)
        kb.v("gpsimd", "memset", [], [ones], ones[:], 1.0)
        xt = kb.T([128, D], F32, "xt")
        junk = kb.T([128, D], BF16, "junk")
        ss = kb.T([128, 1], F32, "ss")
        rstd = kb.T([128, 1], F32, "rstd")
        nb = kb.T([128, D], BF16, "nb")
        nT = kb.T([128, 8, 128], BF16, "nT")
        qkT = Rot([kb.T([128, 8, 128], BF16, f"qkT{i}") for i in range(2)])
        khat = Rot([kb.T([128, 8, 128], BF16, f"khat{i}") for i in range(2)])
        vp = Rot([kb.T([128, 4, 257], BF16, f"vp{i}") for i in range(2)])
        for v_ in vp.items:
            kb.v("gpsimd", "memset", [], [v_], v_[:], 1.0)
        og = Rot([kb.T([128, D], F32, f"og{i}") for i in range(2)])
        sc = Rot([kb.T([128, 32], F32, f"sc{i}") for i in range(2)])
        gb = kb.T([128, 16], F32, "gb")
        nl = kb.T([128, 8], F32, "nl")
        igc = kb.T([128, 8], F32, "igc")
        t1 = kb.T([128, 8], F32, "t1")
        t2 = kb.T([128, 8], F32, "t2")
        psT = kb.psv(0, 1, [128, 8, 128], BF16, "psT")
        ps_g = kb.psv(0, 1, [128, 64], F32, "ps_g", root=psT)
        ps_f = [kb.psv(1 + i, 1, [128, 4, 128], F32, f"ps_f{i}") for i in range(2)]
        ps_k = kb.psv(3, 1, [128, 512], F32, "ps_k")
        ps_v = [kb.psv(4 + i, 1, [128, 512], F32, f"ps_v{i}") for i in range(2)]
        ps_o = [kb.psv(6 + i, 1, [128, 512], F32, f"ps_o{i}") for i in range(2)]
        for tt in range(NT_ALL):
            isc = 1 if tt < 2 else 0
            sap, sbuf_ = src_tile(tt)
            norm_mod_T(kb, C, sap, sbuf_, G1[isc], SH[isc], xt, junk, ss, rstd, nb, psT, nT)
            for hh in range(8):
                for k in range(8):
                    kb.mm(ps_f[hh // 4][:, hh % 4, :], w_in[:, k, hh * 128:(hh + 1) * 128], nT[:, k, :], [w_in, nT],
                          [ps_f[hh // 4]], start=(k == 0), stop=(k == 7))
            qk = qkT.next()
            kb.act(qk[:, 0:4, :], ps_f[0][:], AF.Copy, [ps_f[0]], [qk], scale=128 ** -0.5)
            kb.v("vector", "tensor_copy", [ps_f[1]], [qk], qk[:, 4:8, :], ps_f[1][:])
            kb.dma(QKT[tt].ap, qk[:], [qk], [QKT[tt]], q="gpsimd")
            for k in range(8):
                kb.mm(ps_k[:], nT[:, k, :], w_in[:, k, 512:1024], [nT, w_in], [ps_k], start=(k == 0), stop=(k == 7))
            for hf in range(2):
                for k in range(8):
                    kb.mm(ps_v[hf][:], nT[:, k, :], w_in[:, k, 1024 + hf * 512:1536 + hf * 512], [nT, w_in], [ps_v[hf]],
                          start=(k == 0), stop=(k == 7))
            for k in range(8):
                kb.mm(ps_g[:, 0:16], nT[:, k, :], w_in[:, k, 2048:2064], [nT, w_in], [ps_g], start=(k == 0), stop=(k == 7))
            if tt >= 2:
                for hf in range(2):
                    for k in range(8):
                        kb.mm(ps_o[hf][:], nT[:, k, :], w_in[:, k, 2064 + hf * 512:2576 + hf * 512], [nT, w_in],
                              [ps_o[hf]], start=(k == 0), stop=(k == 7))
                o_ = og.next()
                for hf in range(2):
                    kb.act(o_[:, hf * 512:(hf + 1) * 512], ps_o[hf][:], AF.Sigmoid, [ps_o[hf]], [o_])
                kb.dma(OGd[tt].ap, o_[:], [o_], [OGd[tt]], q="gpsimd")
            kb.v("vector", "tensor_tensor", [ps_g, gbias], [gb], gb[:], ps_g[:, 0:16], gbias[:], ALU.add)
            gb4 = gb[:].rearrange("p (d two h) -> p d two h", d=2, two=2)
            nl3 = nl[:].rearrange("p (d h) -> p d h", d=2)
            kb.act(nl3, gb4[:, :, 1, :], AF.Exp, [gb], [nl], scale=-1.0)
            kb.act(nl[:], nl[:], AF.Ln, [nl], [nl], bias=kb.one_t[:, 0:1])
            kb.v("vector", "tensor_copy", [gb], [igc], igc[:].rearrange("p (d h) -> p d h", d=2), gb4[:, :, 0, :])
            kb.mm(ps_g[:, 16:20], triU[:], nl[:, 0:4], [triU, nl], [ps_g])
            kb.mm(ps_g[:, 20:24], triL[:], nl[:, 4:8], [triL, nl], [ps_g])
            kb.mm(ps_g[:, 24:32], ones[:], nl[:], [ones, nl], [ps_g])
            s_ = sc.next()
            kb.v("vector", "tensor_tensor", [igc, ps_g], [t1], t1[:], igc[:], ps_g[:, 16:24], ALU.add)
            kb.v("vector", "tensor_tensor", [t1, ps_g], [t2], t2[:], t1[:], ps_g[:, 24:32], ALU.subtract)
            kb.act(s_[:, 0:8], t1[:], AF.Exp, [t1], [s_])
            kb.act(s_[:, 8:16], ps_g[:, 16:24], AF.Exp, [ps_g], [s_], scale=-1.0)
            kb.act(s_[:, 16:24], t2[:], AF.Exp, [t2], [s_])
            kb.act(s_[:, 24:32], ps_g[:, 24:32], AF.Exp, [ps_g], [s_], scale=-1.0)
            kb.dma(SCd[tt].ap, s_[:], [s_], [SCd[tt]], q="gpsimd")
            kh = khat.next()
            for c in range(8):
                h = c % 4
                kb.act(kh[:, c, :], ps_k[:, h * 128:(h + 1) * 128], AF.Copy, [ps_k, s_], [kh], scale=s_[:, 16 + c:17 + c])
            kb.dma(KHd[tt].ap, kh[:], [kh], [KHd[tt]], q="gpsimd")
            v_ = vp.next()
            for hf in range(2):
                kb.v("vector", "tensor_copy", [ps_v[hf]], [v_], v_[:, hf * 2:hf * 2 + 2, 0:256],
                     ps_v[hf][:].rearrange("p (h d) -> p h d", h=2))
            kb.dma(VPd[tt].ap, v_[:], [v_], [VPd[tt]], q="gpsimd")
    with kb.phase():
        triU = kb.T([128, 128], F32, "triU")
        triL = kb.T([128, 128], F32, "triL")
        kb.v("vector", "tensor_single_scalar", [C["jmp"]], [triU], triU[:], C["jmp"][:], 0.0, ALU.is_ge)
        kb.v("vector", "tensor_single_scalar", [C["jmp"]], [triL], triL[:], C["jmp"][:], 0.0, ALU.is_le)
        Cst = [kb.T([128, 257], F32, f"Cst{h}") for h in range(4)]
        Cbf = [kb.T([128, 257], BF16, f"Cbf{h}") for h in range(4)]
        qk = Rot([kb.T([128, 8, 128], BF16, f"qk{i}") for i in range(3)])
        kh = Rot([kb.T([128, 4, 128], BF16, f"kh{i}") for i in range(3)])
        vp = Rot([kb.T([128, 4, 257], BF16, f"vp{i}") for i in range(3)])
        sc = Rot([kb.T([128, 32], F32, f"sc{i}") for i in range(3)])
        PTm = Rot([kb.T([128, 128], BF16, f"PTm{i}") for i in range(3)])
        t4 = kb.T([128, 4], F32, "t4")
        r4 = kb.T([128, 4], F32, "r4")
        hout = Rot([kb.T([128, D], F32, f"hout{i}") for i in range(2)])
        hb = Rot([kb.T([128, D], F32, f"hb{i}") for i in range(2)])
        ps_S = Rot([kb.psv(i, 1, [128, 128], F32, f"psS{i}") for i in range(2)])
        ps_N = [kb.psv(2 + h, 1, [128, 257], F32, f"psN{h}") for h in range(4)]
        ps_U = Rot([kb.psv(6 + i, 1, [128, 257], F32, f"psU{i}") for i in range(2)])
        bg_units = []
        if bg is not None:
            bg_units = bg([kb.psv(i, 1, [128, 8, 128], BF16, f"pstb{i}", root=ps_S.items[i]) for i in range(2)])
        for d in (1, 0):
            order = [0, 1] + lat_tiles if d == 0 else [1, 0] + lat_tiles[::-1]
            mask = triU if d == 0 else triL
            for h in range(4):
                kb.v("gpsimd", "memset", [], [Cst[h]], Cst[h][:], 0.0)
                kb.v("gpsimd", "memset", [], [Cbf[h]], Cbf[h][:], 0.0)
            for oi, tt in enumerate(order):
                q_ = qk.next(); k_ = kh.next(); v_ = vp.next(); s_ = sc.next()
                kb.dma(q_[:], QKT[tt].ap, [QKT[tt]], [q_])
                kb.dma(k_[:], KHd[tt].ap[:, d * 4:(d + 1) * 4, :], [KHd[tt]], [k_])
                kb.dma(v_[:], VPd[tt].ap, [VPd[tt]], [v_])
                kb.dma(s_[:], SCd[tt].ap, [SCd[tt]], [s_])
                if tt >= 2:
                    for h in range(4):
                        c = d * 4 + h
                        pS = ps_S.next()
                        kb.mm(pS[:], q_[:, 4 + h, :], q_[:, h, :], [q_], [pS])
                        pt = PTm.next()
                        kb.v("vector", "scalar_tensor_tensor", [pS, s_, mask], [pt], pt[:], pS[:], s_[:, c:c + 1], mask[:],
                             ALU.mult, ALU.mult)
                        kb.mm(ps_N[h][:], pt[:], v_[:, h, :], [pt, v_], [ps_N[h]], start=True, stop=False)
                        kb.mm(ps_N[h][:], q_[:, h, :], Cbf[h][:], [q_, Cbf[h]], [ps_N[h]], start=False, stop=True)
                        kb.v("vector", "tensor_tensor", [ps_N[h], s_], [t4], t4[:, h:h + 1], ps_N[h][:, 256:257],
                             s_[:, 8 + c:9 + c], ALU.mult)
                    kb.act(t4[:], t4[:], AF.Abs, [t4], [t4])
                    kb.v("vector", "tensor_scalar_max", [t4], [r4], r4[:], t4[:], 1.0)
                    kb.v("vector", "reciprocal", [r4], [r4], r4[:], r4[:])
                    kb.v("vector", "tensor_tensor", [r4, s_], [r4], r4[:], r4[:], s_[:, 8 + d * 4:12 + d * 4], ALU.mult)
                    ho = hout.next()
                    for h in range(4):
                        kb.act(ho[:, h * 256:(h + 1) * 256], ps_N[h][:, 0:256], AF.Copy, [ps_N[h], r4], [ho],
                               scale=r4[:, h:h + 1])
                    if d == 1:
                        kb.dma(HBd[tt].ap, ho[:], [ho], [HBd[tt]], q="gpsimd")
                    else:
                        hb_ = hb.next()
                        kb.dma(hb_[:], HBd[tt].ap, [HBd[tt]], [hb_])
                        kb.v("gpsimd", "tensor_tensor", [ho, hb_], [hb_], hb_[:], ho[:], hb_[:], ALU.add)
                        kb.dma(HSd[tt].ap, hb_[:], [hb_], [HSd[tt]], q="gpsimd")
                for _ in range(2):
                    if bg_units:
                        bg_units.pop(0)()
                if oi == len(order) - 1:
                    continue
                for h in range(4):
                    c = d * 4 + h
                    pU = ps_U.next()
                    kb.mm(pU[:], k_[:, h, :], v_[:, h, :], [k_, v_], [pU])
                    kb.v("vector", "scalar_tensor_tensor", [Cst[h], s_, pU], [Cst[h]], Cst[h][:], Cst[h][:],
                         s_[:, 24 + c:25 + c], pU[:], ALU.mult, ALU.add)
                    kb.act(Cbf[h][:], Cst[h][:], AF.Copy, [Cst[h]], [Cbf[h]])
        while bg_units:
            bg_units.pop(0)()
    with kb.phase():
        stage = Rot([kb.T([128, 1024], F32, f"stg{i}") for i in range(2)])
        w_out = kb.T([128, 8, 1024], BF16, "w_out_o")
        load_w_bf16(kb, w_out, I["odd_w_out"][j], I["_odd_w_out"], stage, 1024, 1024)
        hg = kb.T([128, D], F32, "hg")
        load_bcast(kb, hg, I["mlstm_head_g"][j], [I["_mlstm_head_g"]])
        M2 = kb.T([128, D], F32, "M2")
        load_bcast(kb, M2, MOD.ap[0, 2 * D:3 * D], [MOD])
        hs = Rot([kb.T([128, D], F32, f"hs{i}") for i in range(2)])
        og = Rot([kb.T([128, D], F32, f"og{i}") for i in range(2)])
        hres = Rot([kb.T([128, D], F32, f"hres{i}") for i in range(2)])
        sq = kb.T([128, D], F32, "sq")
        ss4 = kb.T([128, 4], F32, "ss4")
        rs4 = kb.T([128, 4], F32, "rs4")
        mb = kb.T([128, D], BF16, "mb")
        mT = kb.T([128, 8, 128], BF16, "mT")
        yo = Rot([kb.T([128, D], F32, f"yo{i}") for i in range(2)])
        psT = kb.psv(0, 1, [128, 8, 128], BF16, "psT")
        ps_y = [kb.psv(1 + i, 1, [128, 512], F32, f"ps_y{i}") for i in range(2)]
        for tt in lat_tiles:
            h_ = hs.next(); o_ = og.next(); r_ = hres.next()
            kb.dma(h_[:], HSd[tt].ap, [HSd[tt]], [h_])
            kb.dma(o_[:], OGd[tt].ap, [OGd[tt]], [o_])
            sap, sbuf_ = src_tile(tt)
            kb.dma(r_[:], sap, [sbuf_], [r_])
            kb.v("gpsimd", "tensor_tensor", [h_], [sq], sq[:], h_[:], h_[:], ALU.mult)
            kb.v("vector", "tensor_reduce", [sq], [ss4], ss4[:], sq[:].rearrange("p (h d) -> p h d", h=4), AX.X, ALU.add)
            rstd_of(kb, ss4, rs4, 256)
            h3 = h_[:].rearrange("p (h d) -> p h d", h=4)
            kb.v("vector", "tensor_tensor", [h_, rs4], [h_], h3, h3, rs4[:].unsqueeze(2).to_broadcast([128, 4, 256]), ALU.mult)
            kb.v("gpsimd", "tensor_tensor", [o_, hg], [o_], o_[:], o_[:], hg[:], ALU.mult)
            kb.v("vector", "tensor_tensor", [h_, o_], [mb], mb[:], h_[:], o_[:], ALU.mult)
            for k in range(8):
                kb.tr(psT[:, k, :], mb[:, k * 128:(k + 1) * 128], C["ident_bf"][:], [mb, C["ident_bf"]], [psT])
            kb.act(mT[:], psT[:], AF.Copy, [psT], [mT])
            for hf in range(2):
                for k in range(8):
                    kb.mm(ps_y[hf][:], mT[:, k, :], w_out[:, k, hf * 512:(hf + 1) * 512], [mT, w_out], [ps_y[hf]],
                          start=(k == 0), stop=(k == 7))
            y_ = yo.next()
            for hf in range(2):
                hsl = slice(hf * 512, (hf + 1) * 512)
                kb.v("vector", "tensor_tensor", [ps_y[hf], M2], [y_], y_[:, hsl], ps_y[hf][:], M2[:, hsl], ALU.mult)
            kb.v("gpsimd", "tensor_tensor", [y_, r_], [y_], y_[:], y_[:], r_[:], ALU.add)
            kb.dma(H[tt].ap, y_[:], [y_], [H[tt]], q="gpsimd")


def peer_prep_units(kb, C, I, layer, UT, VB, ps_list, light=False):
    uf = Rot([kb.T([128, D], F32, f"uf{i}") for i in range(2)])
    ub = Rot([kb.T([128, D], BF16, f"ub{i}") for i in range(2)])
    utb = Rot([kb.T([128, 8, 128], BF16, f"utb{i}") for i in range(2)])
    vf = Rot([kb.T([128, D], F32, f"vf{i}") for i in range(2)])
    vb = Rot([kb.T([128, D], BF16, f"vb{i}") for i in range(2)])
    ps = Rot(ps_list)
    units = []
    for i in range(128):
        def unit(i=i):
            a = uf.next(); b = ub.next(); t = utb.next(); p = ps.next()
            lq = "gpsimd" if light else "sync"
            kb.dma(a[:], I["peer_u"][layer][i * 128:(i + 1) * 128, :], [I["_peer_u"]], [a], q=lq)
            if i % 2 == 0:
                kb.v("gpsimd", "tensor_copy", [a], [b], b[:], a[:])
            else:
                kb.act(b[:], a[:], AF.Copy, [a], [b])
            for k in range(8):
                kb.tr(p[:, k, :], b[:, k * 128:(k + 1) * 128], C["ident_bf"][:], [b, C["ident_bf"]], [p])
            if light:
                kb.act(t[:], p[:], AF.Copy, [p], [t])
            else:
                kb.v("vector", "tensor_copy", [p], [t], t[:], p[:])
            kb.dma(UT[i].ap, t[:], [t], [UT[i]], q="gpsimd")
            a2 = vf.next(); b2 = vb.next()
            kb.dma(a2[:], I["peer_v"][layer][i * 128:(i + 1) * 128, :], [I["_peer_v"]], [a2], q=lq)
            if light:
                kb.v("gpsimd", "tensor_copy", [a2], [b2], b2[:], a2[:])
            else:
                kb.v("vector", "tensor_copy", [a2], [b2], b2[:], a2[:])
            kb.dma(VB[i].ap, b2[:], [b2], [VB[i]], q="gpsimd")
        units.append(unit)
    return units


def peer_prep(kb, C, I, layer, UT, VB):
    with kb.phase():
        ps = [kb.psv(i, 1, [128, 8, 128], BF16, f"pst{i}") for i in range(4)]
        for u in peer_prep_units(kb, C, I, layer, UT, VB, ps):
            u()


def peer_route(kb, C, I, layer, MOD, tiles, src_tile, NTd, RTd):
    with kb.phase():
        stage = Rot([kb.T([128, 2048], F32, f"stg{i}") for i in range(2)])
        w_q = kb.T([128, 8, 2048], BF16, "w_q")
        load_w_bf16(kb, w_q, I["peer_w_q"][layer], I["_peer_w_q"], stage, 1024, 2048)
        skT = kb.T([128, 16, 128], BF16, "skT")
        skb = kb.T([128, 128], BF16, "skb")
        pst = kb.psv(0, 1, [128, 8, 128], BF16, "pst")
        for hp in range(16):
            st = stage.next()
            kb.dma(st[:, 0:128], I["peer_subkeys"][layer][hp // 2, hp % 2], [I["_peer_subkeys"]], [st])
            kb.v("vector", "tensor_copy", [st], [skb], skb[:], st[:, 0:128])
            kb.tr(pst[:, 0, :], skb[:], C["ident_bf"][:], [skb, C["ident_bf"]], [pst])
            kb.v("vector", "tensor_copy", [pst], [skT], skT[:, hp, :], pst[:, 0, :])
        tmpm = Buf(stage.items[0].ap[:, 0:D], "tmpm", root=stage.items[0])
        G1 = [kb.T([128, D], F32, f"G1_{i}") for i in range(2)]
        SH = [kb.T([128, D], F32, f"SH_{i}") for i in range(2)]
        rows = sorted({1 if tt < 2 else 0 for tt in tiles})
        for r in rows:
            build_mod_tiles(kb, MOD.ap, MOD, I["norm2_g"][layer], I["_norm2_g"], r, 3, 4, G1[r], SH[r], tmpm)
        xt = kb.T([128, D], F32, "xt")
        junk = kb.T([128, D], BF16, "junk")
        ss = kb.T([128, 1], F32, "ss")
        rstd = kb.T([128, 1], F32, "rstd")
        nb = kb.T([128, D], BF16, "nb")
        nT = Rot([kb.T([128, 8, 128], BF16, f"nT{i}") for i in range(2)])
        qT_rot = Rot([kb.T([128, 16, 128], BF16, f"qT_sb{i}") for i in range(2)])
        s_rot = Rot([kb.T([128, 16, 128], F32, f"s_sb{i}") for i in range(2)])
        s2 = kb.T([128, 16, 128], F32, "s2")
        s2b = [Buf(None, f"s2b{i}") for i in range(16)]
        stop = kb.T([128, 16, 16], F32, "stop")
        stopa = [Buf(None, f"stopa{i}") for i in range(16)]
        stopb = [Buf(None, f"stopb{i}") for i in range(16)]
        topa = [Buf(None, f"topa{i}") for i in range(8)]
        topb = [Buf(None, f"topb{i}") for i in range(8)]
        c2b = [Buf(None, f"c2b{i}") for i in range(8)]
        idx = kb.T([128, 16, 16], U32, "idx")
        idxf = kb.T([128, 16, 16], F32, "idxf")
        cand = kb.T([128, 8, 256], F32, "cand")
        c2 = kb.T([128, 8, 256], F32, "c2")
        top = kb.T([128, 8, 16], F32, "top")
        pos = kb.T([128, 8, 16], U32, "pos")
        au = kb.T([128, 8, 16], U32, "au")
        bu = kb.T([128, 8, 16], U32, "bu")
        af = kb.T([128, 8, 16], F32, "af")
        bf = kb.T([128, 8, 16], F32, "bf")
        E_rot = Rot([kb.T([128, 8, 16, 16], F32, f"E{i}") for i in range(2)])
        sel = kb.T([128, 3, 128], F32, "sel")
        gs = kb.T([128, 8], F32, "gs")
        rt_sb = Rot([kb.T([128, 3, 128], F32, f"rt_sb{i}") for i in range(2)])
        psT = kb.psv(0, 1, [128, 8, 128], BF16, "psT", root=pst)
        ps_qT = [kb.psv(i, 1, [128, 4, 128], F32, f"ps_qT{i}") for i in range(4)]
        ps_qT[0] = kb.psv(0, 1, [128, 4, 128], F32, "ps_qT0", root=pst)
        ps_s = [kb.psv(4 + i, 1, [128, 4, 128], F32, f"ps_s{i}") for i in range(4)]
        ps_r = kb.psv(4, 1, [128, 3, 128], F32, "ps_r", root=ps_s[0])
        iota16 = C["iota_j"][:, 0:16]
        def front(tt):
            isc = 1 if tt < 2 else 0
            sap, sbuf_ = src_tile(tt)
            nTt = nT.next()
            norm_mod_T(kb, C, sap, sbuf_, G1[isc], SH[isc], xt, junk, ss, rstd, nb, psT, nTt)
            kb.dma(NTd[tt].ap, nTt[:], [nTt], [NTd[tt]], q="gpsimd")
            qT_sb = qT_rot.next()
            s_sb = s_rot.next()
            for hp in range(16):
                pq_ = ps_qT[hp // 4]
                for k in range(8):
                    kb.mm(pq_[:, hp % 4, :], w_q[:, k, hp * 128:(hp + 1) * 128], nTt[:, k, :], [w_q, nTt], [pq_],
                          start=(k == 0), stop=(k == 7))
            for g in range(4):
                kb.act(qT_sb[:, g * 4:(g + 1) * 4, :], ps_qT[g][:], AF.Copy, [ps_qT[g]], [qT_sb])
            for hp in range(16):
                kb.mm(ps_s[hp // 4][:, hp % 4, :], qT_sb[:, hp, :], skT[:, hp, :], [qT_sb, skT], [ps_s[hp // 4]])
            for g in range(4):
                kb.act(s_sb[:, g * 4:(g + 1) * 4, :], ps_s[g][:], AF.Copy, [ps_s[g]], [s_sb])
            return s_sb

        def chain(tt, s_sb):
            for hp in range(16):
                kb.v("vector", "max", [s_sb], [stopa[hp]], stop[:, hp, 0:8], s_sb[:, hp, :])
            for hp in range(16):
                kb.v("vector", "match_replace", [stopa[hp], s_sb], [s2b[hp]], s2[:, hp, :], stop[:, hp, 0:8], s_sb[:, hp, :], -1e30)
            for hp in range(16):
                kb.v("vector", "max", [s2b[hp]], [stopb[hp]], stop[:, hp, 8:16], s2[:, hp, :])
            for hp in range(16):
                kb.v("vector", "max_index", [stopa[hp], s_sb], [idx], idx[:, hp, 0:8], stop[:, hp, 0:8], s_sb[:, hp, :])
            for hp in range(16):
                kb.v("vector", "max_index", [stopb[hp], s_sb], [idx], idx[:, hp, 8:16], stop[:, hp, 8:16], s_sb[:, hp, :])
            kb.v("vector", "tensor_copy", [idx], [idxf], idxf[:], idx[:])
            st4 = stop[:].rearrange("p (h two) k -> p h two k", two=2)
            if4 = idxf[:].rearrange("p (h two) k -> p h two k", two=2)
            cand4 = cand[:].rearrange("p h (a b) -> p h a b", a=16)
            kb.v("vector", "tensor_tensor", stopa + stopb, [cand], cand4,
                 st4[:, :, 0, :].unsqueeze(3).to_broadcast([128, 8, 16, 16]),
                 st4[:, :, 1, :].unsqueeze(2).to_broadcast([128, 8, 16, 16]), ALU.add)
            for h in range(8):
                kb.v("vector", "max", [cand], [topa[h]], top[:, h, 0:8], cand[:, h, :])
            for h in range(8):
                kb.v("vector", "match_replace", [topa[h], cand], [c2b[h]], c2[:, h, :], top[:, h, 0:8], cand[:, h, :], -1e30)
            for h in range(8):
                kb.v("vector", "max", [c2b[h]], [topb[h]], top[:, h, 8:16], c2[:, h, :])
            for h in range(8):
                kb.v("vector", "max_index", [topa[h], cand], [pos], pos[:, h, 0:8], top[:, h, 0:8], cand[:, h, :])
            for h in range(8):
                kb.v("vector", "max_index", [topb[h], cand], [pos], pos[:, h, 8:16], top[:, h, 8:16], cand[:, h, :])
            kb.v("vector", "tensor_single_scalar", [pos], [au], au[:], pos[:], 4, ALU.logical_shift_right)
            kb.v("vector", "tensor_single_scalar", [pos], [bu], bu[:], pos[:], 15, ALU.bitwise_and)
            kb.v("vector", "tensor_copy", [au], [af], af[:], au[:])
            kb.v("vector", "tensor_copy", [bu], [bf], bf[:], bu[:])
            io4 = iota16.unsqueeze(1).unsqueeze(1).to_broadcast([128, 8, 16, 16])
            for which, sel_f in ((0, af), (1, bf)):
                E = E_rot.next()
                kb.v("vector", "tensor_tensor", [sel_f, C["iota_j"]], [E], E[:],
                     sel_f[:].unsqueeze(3).to_broadcast([128, 8, 16, 16]), io4, ALU.is_equal)
                kb.v("vector", "tensor_tensor", [E, idxf], [E], E[:], E[:],
                     if4[:, :, which, :].unsqueeze(2).to_broadcast([128, 8, 16, 16]), ALU.mult)
                kb.v("vector", "tensor_reduce", [E], [sel], sel[:, which, :].rearrange("p (h k) -> p h k", h=8),
                     E[:], AX.X, ALU.add)
            g3 = sel[:, 2, :].rearrange("p (h k) -> p h k", h=8)
            kb.v("vector", "tensor_tensor", topa + topb, [sel], g3, top[:], top[:, :, 0:1].to_broadcast([128, 8, 16]), ALU.subtract)
            kb.act(sel[:, 2, :], sel[:, 2, :], AF.Exp, [sel], [sel])
            kb.v("vector", "tensor_reduce", [sel], [gs], gs[:], g3, AX.X, ALU.add)
            kb.v("vector", "reciprocal", [gs], [gs], gs[:], gs[:])
            kb.v("vector", "tensor_tensor", [sel, gs], [sel], g3, g3, gs[:].unsqueeze(2).to_broadcast([128, 8, 16]), ALU.mult)
            for w_ in range(3):
                kb.tr(ps_r[:, w_, :], sel[:, w_, :], C["ident_f"][:], [sel, C["ident_f"]], [ps_r])
            rt = rt_sb.next()
            kb.act(rt[:], ps_r[:], AF.Copy, [ps_r], [rt])
            kb.dma(RTd[tt].ap, rt[:], [rt], [RTd[tt]], q="gpsimd")

        prev = None
        for tt in tiles:
            s_cur = front(tt)
            if prev is not None:
                chain(*prev)
            prev = (tt, s_cur)
        chain(*prev)


def peer_apply(kb, C, I, layer, MOD, groups, src_tile, NTd, RTd, UT, VB, epilogue):
    with kb.phase():
        QT_ = 32
        Gs = kb.T([128, 128, 384], BF16, "Gs")
        A = Rot([kb.T([128, QT_, 128], BF16, f"A{i}") for i in range(2)])
        B = Rot([kb.T([128, QT_, 128], BF16, f"B{i}") for i in range(2)])
        nTg_rot = Rot([kb.T([128, 8, 384], BF16, f"nTg{i}") for i in range(2)])
        rt = Rot([kb.T([128, 3, 128], F32, f"rt{i}") for i in range(6)])
        rb = Rot([kb.T([128, 2, 128], BF16, f"rb{i}") for i in range(6)])
        uT = Rot([kb.T([128, 8, 128], BF16, f"uT{i}") for i in range(4)])
        vv = Rot([kb.T([128, D], BF16, f"vv{i}") for i in range(4)])
        ga = Rot([kb.T([128, 384], F32, f"ga{i}") for i in range(2)])
        W = Rot([kb.T([128, 384], BF16, f"W{i}") for i in range(4)])
        ep = epilogue("alloc", kb)
        acc = [[kb.psv(2 * t + h, 1, [128, 512], F32, f"acc{t}{h}") for h in range(2)] for t in range(3)]
        ps_a = [kb.psv(6 + i, 1, [128, 384], F32, f"ps_a{i}") for i in range(2)]
        ps_G = [kb.psv(6 + i, 1, [128, 4, 128], F32, f"ps_G{i}", root=ps_a[i]) for i in range(2)]
        ps_rot = Rot([0, 1])
        iota_bf = C["iota_bf"]

        def load_group(tiles):
            nTg = nTg_rot.next()
            rs = []
            for gi, tt in enumerate(tiles):
                kb.dma(nTg[:, :, gi * 128:(gi + 1) * 128], NTd[tt].ap, [NTd[tt]], [nTg], q="gpsimd")
                r = rt.next(); rb_ = rb.next()
                kb.dma(r[:], RTd[tt].ap, [RTd[tt]], [r], q="gpsimd")
                kb.v("gpsimd", "tensor_copy", [r], [rb_], rb_[:], r[:, 0:2, :])
                rs.append((r, rb_))
            return {"tiles": tiles, "nTg": nTg, "rs": rs}

        def gate_units(G):
            units = []
            for gi in range(len(G["tiles"])):
                r, rb_ = G["rs"][gi]
                for qt in range(128 // QT_):
                    def build(gi=gi, qt=qt, r=r, rb_=rb_):
                        ts_ = slice(qt * QT_, (qt + 1) * QT_)
                        a_ = A.next(); b_ = B.next()
                        kb.v("vector", "tensor_tensor", [rb_, iota_bf], [a_], a_[:],
                             iota_bf[:].unsqueeze(1).to_broadcast([128, QT_, 128]),
                             rb_[:, 0, ts_].unsqueeze(2).to_broadcast([128, QT_, 128]), ALU.is_equal)
                        kb.v("vector", "tensor_tensor", [a_, r], [a_], a_[:], a_[:],
                             r[:, 2, ts_].unsqueeze(2).to_broadcast([128, QT_, 128]), ALU.mult)
                        kb.v("vector", "tensor_tensor", [rb_, iota_bf], [b_], b_[:],
                             iota_bf[:].unsqueeze(1).to_broadcast([128, QT_, 128]),
                             rb_[:, 1, ts_].unsqueeze(2).to_broadcast([128, QT_, 128]), ALU.is_equal)
                        return a_, b_

                    def mmpart(ab, gi=gi, qt=qt):
                        a_, b_ = ab
                        for t4 in range(QT_ // 4):
                            pg = ps_G[ps_rot.next()]
                            for u in range(4):
                                t = t4 * 4 + u
                                kb.mm(pg[:, u, :], b_[:, t, :], a_[:, t, :], [a_, b_], [pg])
                            tok0 = gi * 128 + qt * QT_ + t4 * 4
                            dst = Gs[:, :, tok0:tok0 + 4].rearrange("j i t -> j t i")
                            kb.act(dst, pg[:], AF.Copy, [pg], [Gs])
                    units.append((build, mmpart))
            return units

        Gstate = {0: load_group(groups[0])}
        LAG = 2
        for g in range(len(groups)):
            G = Gstate[g]
            tiles = G["tiles"]
            ng = len(tiles)
            ntok = ng * 128
            nTg = G["nTg"]
            if g + 1 < len(groups):
                Gstate[g + 1] = load_group(groups[g + 1])
            units = gate_units(G)
            built = units[0][0]()
            for n in range(len(units)):
                nxt = units[n + 1][0]() if n + 1 < len(units) else None
                units[n][1](built)
                built = nxt

            def U_(i):
                u_ = uT.next(); v_ = vv.next()
                kb.dma(u_[:], UT[i].ap, [UT[i]], [u_])
                kb.dma(v_[:], VB[i].ap, [VB[i]], [v_])
                pa = ps_a[ps_rot.next()]
                for k in range(8):
                    kb.mm(pa[:, 0:ntok], u_[:, k, :], nTg[:, k, 0:ntok], [u_, nTg], [pa], start=(k == 0), stop=(k == 7))
                g_ = ga.next()
                kb.act(g_[:, 0:ntok], pa[:, 0:ntok], AF.Gelu, [pa], [g_])
                w_ = W.next()
                eng = "vector" if i % 3 != 2 else "gpsimd"
                kb.v(eng, "tensor_tensor", [g_, Gs], [w_], w_[:, 0:ntok], g_[:, 0:ntok], Gs[:, i, 0:ntok], ALU.mult)
                return (i, w_, v_)

            def V_(st):
                i, w_, v_ = st
                for gi in range(ng):
                    for h in range(2):
                        kb.mm(acc[gi][h][:], w_[:, gi * 128:(gi + 1) * 128], v_[:, h * 512:(h + 1) * 512], [w_, v_],
                              [acc[gi][h]], start=(i == 0), stop=(i == 127))

            pend = []
            for i in range(128):
                pend.append(U_(i))
                if len(pend) > LAG:
                    V_(pend.pop(0))
            while pend:
                V_(pend.pop(0))
            for gi, tt in enumerate(tiles):
                epilogue("run", kb, tt, acc[gi], ep)


def make_epilogue(I, MOD, src_tile, dst_tile, final=False):
    def ep(mode, kb, tt=None, acc=None, b=None):
        if mode == "alloc":
            b = {"M5": [kb.T([128, D], F32, f"M5_{i}") for i in range(2)],
                 "hres": kb.T([128, D], F32, "hres"), "o": kb.T([128, D], F32, "o")}
            for r in range(2):
                load_bcast(kb, b["M5"][r], MOD.ap[r, 5 * D:6 * D], [MOD])
            if final:
                b["fg"] = kb.T([128, D], F32, "fg")
                load_bcast(kb, b["fg"], I["norm_f_g"], [I["_norm_f_g"]])
                b["junk"] = kb.T([128, D], BF16, "junkf")
                b["ss"] = kb.T([128, 1], F32, "ssf")
                b["rstd"] = kb.T([128, 1], F32, "rstdf")
            return b
        isc = 1 if tt < 2 else 0
        sap, sbuf_ = src_tile(tt)
        hres, o, M5 = b["hres"], b["o"], b["M5"][isc]
        kb.dma(hres[:], sap, [sbuf_], [hres])
        for h in range(2):
            hs = slice(h * 512, (h + 1) * 512)
            kb.v("vector", "tensor_tensor", [acc[h], M5], [o], o[:, hs], acc[h][:], M5[:, hs], ALU.mult)
        kb.v("gpsimd", "tensor_tensor", [o, hres], [o], o[:], o[:], hres[:], ALU.add)
        dap, dbuf = dst_tile(tt)
        if final:
            kb.act(b["junk"][:], o[:], AF.Square, [o], [b["junk"], b["ss"]], accum_out=b["ss"][:])
            rstd_of(kb, b["ss"], b["rstd"], D)
            kb.v("vector", "scalar_tensor_tensor", [o, b["rstd"], b["fg"]], [o], o[:], o[:], b["rstd"][:, 0:1],
                 b["fg"][:], ALU.mult, ALU.mult)
        kb.dma(dap, o[:], [o], [dbuf], q="gpsimd")
    return ep


def tile_groups(tiles, n=3):
    return [tiles[i:i + n] for i in range(0, len(tiles), n)]


INPUT_SPECS = [
    ("x", [SEQ, D]), ("c", [D]), ("ctx", [NCTX, D]), ("c_ctx", [D]),
    ("norm1_g", [2, D]), ("norm2_g", [2, D]), ("w_mod", [2, D, 6 * D]), ("b_mod", [2, 6 * D]),
    ("even_w_in", [1, D, 1952]), ("mla_q_norm", [1, 256]), ("mla_kv_norm", [1, 128]),
    ("mla_w_uq", [1, 256, 768]), ("mla_w_ukv", [1, 128, 1024]), ("conv_w", [1, 3, 512]),
    ("even_w_out", [1, D, D]), ("odd_w_in", [1, D, 3088]), ("mlstm_gate_b", [1, 16]),
    ("mlstm_head_g", [1, D]), ("odd_w_out", [1, D, D]), ("peer_w_q", [2, D, 2048]),
    ("peer_subkeys", [2, 8, 2, 128, 128]), ("peer_u", [2, 16384, D]), ("peer_v", [2, 16384, D]),
    ("norm_f_g", [D]), ("rope", [SEQ, 32]),
]


def build_program(stop_after=None, dbg=()):
    nc = bass.Bass("TRN2", target_bir_lowering=False)
    kb = KB(nc, dbg)
    kb.stop_after = stop_after
    I = {}
    for name, shape in INPUT_SPECS:
        ap = nc.dram_tensor(name, shape, F32, kind="ExternalInput").ap()
        I[name] = ap
        I["_" + name] = Buf(ap, name)
    out = nc.dram_tensor("y", [SEQ, D], F32, kind="ExternalOutput").ap()
    kb.eps_t = kb.T([128, 1], F32, "eps")
    kb.v("gpsimd", "memset", [], [kb.eps_t], kb.eps_t[:], EPS)
    kb.one_t = kb.T([128, 1], F32, "one")
    kb.v("gpsimd", "memset", [], [kb.one_t], kb.one_t[:], 1.0)
    C = make_consts(kb)
    MOD = [Buf(kb.dram(f"MOD{l}", [2, 6 * D], F32), f"MOD{l}") for l in range(2)]
    H0 = kb.dram_tiles("H0", NT_ALL, [128, D], F32)

    def src0(tt):
        if tt < 2:
            return I["ctx"][tt * 128:(tt + 1) * 128, :], I["_ctx"]
        return I["x"][(tt - 2) * 128:(tt - 1) * 128, :], I["_x"]

    phase_mod(kb, C, I, 0, MOD[0])
    if stop_after == "mod0":
        kb.finish()
        return nc, kb
    layer_even(kb, C, I, 0, MOD[0], src0, H0)
    if stop_after in ("even", "E1"):
        kb.finish()
        return nc, kb
    UT = kb.dram_tiles("UT", 128, [128, 8, 128], BF16)
    VB = kb.dram_tiles("VB", 128, [128, D], BF16)
    NTd = kb.dram_tiles("NTd", NT_ALL, [128, 8, 128], BF16)
    RTd = kb.dram_tiles("RTd", NT_ALL, [128, 3, 128], F32)
    H1 = kb.dram_tiles("H1", NT_ALL, [128, D], F32)
    srcH0 = lambda tt: (H0[tt].ap, H0[tt])
    dstH1 = lambda tt: (H1[tt].ap, H1[tt])
    tiles0 = DBG_PTILES or list(range(NT_ALL))
    peer_prep(kb, C, I, 0, UT, VB)
    peer_route(kb, C, I, 0, MOD[0], tiles0, srcH0, NTd, RTd)
    if stop_after == "route0":
        kb.finish()
        return nc, kb
    peer_apply(kb, C, I, 0, MOD[0], tile_groups(tiles0), srcH0, NTd, RTd, UT, VB,
               make_epilogue(I, MOD[0], srcH0, dstH1))
    if stop_after == "peer0":
        kb.finish()
        return nc, kb
    phase_mod(kb, C, I, 1, MOD[1])
    H2 = kb.dram_tiles("H2", NT_ALL, [128, D], F32)
    srcH1 = lambda tt: (H1[tt].ap, H1[tt])
    layer_odd(kb, C, I, 1, MOD[1], srcH1, H2)
    if stop_after == "odd":
        kb.finish()
        return nc, kb
    srcH2 = lambda tt: (H2[tt].ap, H2[tt])
    outb = [Buf(out[(tt - 2) * 128:(tt - 1) * 128, :], f"y{tt}") for tt in range(NT_ALL)]
    dstY = lambda tt: (outb[tt].ap, outb[tt])
    tiles1 = list(range(2, NT_ALL))
    peer_prep(kb, C, I, 1, UT, VB)
    peer_route(kb, C, I, 1, MOD[1], tiles1, srcH2, NTd, RTd)
    peer_apply(kb, C, I, 1, MOD[1], tile_groups(tiles1), srcH2, NTd, RTd, UT, VB,
               make_epilogue(I, MOD[1], srcH2, dstY, final=True))
    kb.finish()
    return nc, kb


def rope_table():
    n_freq = 8
    inv = (10000.0 ** (-np.arange(n_freq, dtype=np.float32) / n_freq)).astype(np.float32)
    t = np.arange(SEQ)
    row = (t // 64).astype(np.float32)
    col = (t % 64).astype(np.float32)
    ang = np.concatenate([row[:, None] * inv, col[:, None] * inv], axis=-1).astype(np.float32)
    return np.concatenate([np.cos(ang), np.sin(ang)], axis=-1).astype(np.float32)


def make_in_maps(inputs, cores):
    shared = {k: np.ascontiguousarray(np.asarray(v, dtype=np.float32)) for k, v in inputs.items()
              if k not in ("x", "c", "ctx")}
    shared["rope"] = rope_table()
    maps = []
    for b in cores:
        m = dict(shared)
        m["x"] = np.ascontiguousarray(inputs["x"][b])
        m["c"] = np.ascontiguousarray(inputs["c"][b])
        m["ctx"] = np.ascontiguousarray(inputs["ctx"][b])
        maps.append(m)
    return maps


def kernel(**inputs):
    nc, kb = build_program()
    maps = make_in_maps(inputs, list(range(8)))
    res = run_bass_kernel_spmd(nc, maps, core_ids=list(range(8)))
    return np.stack([r["y"] for r in res.results], axis=0).astype(np.float32)
```

```python
import math
import numpy as np
from contextlib import ExitStack, contextmanager
import concourse.bass as bass
import concourse.mybir as mybir
from concourse.bass_utils import run_bass_kernel_spmd

F32 = mybir.dt.float32
BF16 = mybir.dt.bfloat16
U32 = mybir.dt.uint32
ALU = mybir.AluOpType
AF = mybir.ActivationFunctionType
AX = mybir.AxisListType

D = 1024
SEQ = 4096
NCTX = 256
NT_ALL = 34
EPS = 1e-6
MLA_SCALE = 96 ** -0.5
DBG_TILES = None
DBG_PTILES = None
SKIP = set()
SB_WORDS = 51200


class Buf:
    __slots__ = ("ap", "last_write", "reads", "name", "psum", "root")

    def __init__(self, ap, name="", psum=False, root=None):
        self.root = root.root if root is not None else self
        self.ap = ap
        self.last_write = None
        self.reads = []
        self.name = name
        self.psum = psum

    def __getitem__(self, k):
        return self.ap[k]


class Op:
    __slots__ = ("eng", "fn", "deps", "flag", "is_dma", "sem", "val", "slot")

    def __init__(self, eng, fn, is_dma):
        self.eng = eng
        self.fn = fn
        self.deps = []
        self.flag = False
        self.is_dma = is_dma
        self.sem = None
        self.val = None
        self.slot = None


ENGS = ["tensor", "vector", "scalar", "gpsimd", "sync"]
N_CSEM = 3
N_DSEM = {"sync": 44, "scalar": 4, "gpsimd": 36}


class Prog:
    def __init__(self, nc):
        self.nc = nc
        self.q = {e: [] for e in ENGS}
        self.last_real = {e: None for e in ENGS}
        self.dma_hist = {e: [] for e in N_DSEM}

    def add(self, eng, fn, reads=(), writes=(), dma=False):
        op = Op(eng, fn, dma)
        deps = []
        reads = [b.root for b in reads]
        writes = [b.root for b in writes]
        writes = list(writes) + [b for b in reads if b.psum and b not in writes]
        for b in reads:
            if b.last_write is not None:
                deps.append(b.last_write)
        for b in writes:
            if b.last_write is not None:
                deps.append(b.last_write)
            deps.extend(b.reads)
        seen = set()
        for d in deps:
            if id(d) in seen:
                continue
            seen.add(id(d))
            if d.eng == eng and not d.is_dma and not dma:
                if eng == "tensor":
                    continue
                if not any(b.last_write is d for b in reads):
                    continue
            op.deps.append(d)
            d.flag = True
        for b in reads:
            if not b.psum:
                b.reads.append(op)
        for b in writes:
            b.last_write = op
            b.reads = []
        if dma:
            h = self.dma_hist[eng]
            n = N_DSEM[eng]
            k = len(h)
            op.slot = k % n
            op.val = 16 * (k // n + 1)
            if k >= n:
                op.deps.append(h[k - n])
            h.append(op)
        elif fn is not None:
            self.last_real[eng] = op
        self.q[eng].append(op)
        return op

    def barrier(self):
        deps = []
        for e in ENGS:
            d = self.last_real[e]
            if d is not None:
                d.flag = True
                deps.append(d)
        for e, h in self.dma_hist.items():
            deps.extend(h[-N_DSEM[e]:])
        for e in ENGS:
            op = Op(e, None, False)
            op.deps = list(deps)
            self.q[e].append(op)

    def emit(self, stack):
        nc = self.nc
        csems = {e: [stack.enter_context(nc.semaphore(f"c_{e}_{i}")) for i in range(N_CSEM)]
                 for e in ["tensor", "vector", "scalar", "gpsimd"]}
        dsems = {e: [stack.enter_context(nc.semaphore(f"d_{e}_{i}")) for i in range(n)]
                 for e, n in N_DSEM.items()}
        for e in ENGS:
            nflag = 0
            for op in self.q[e]:
                if op.is_dma:
                    op.sem = dsems[e][op.slot]
                elif op.flag:
                    op.sem = csems[e][nflag % N_CSEM]
                    op.val = nflag // N_CSEM + 1
                    nflag += 1
        stats = {}

        def run(e):
            def body(eng):
                waited = {}
                nw = 0
                for op in self.q[e]:
                    for d in op.deps:
                        key = id(d.sem)
                        if waited.get(key, 0) >= d.val:
                            continue
                        waited[key] = d.val
                        eng.wait_ge(d.sem, d.val)
                        nw += 1
                    if op.fn is None:
                        continue
                    ins = op.fn(eng)
                    if op.is_dma:
                        ins.then_inc(op.sem, 16)
                    elif op.flag:
                        ins.then_inc(op.sem, 1)
                stats[e] = (len(self.q[e]), nw)
            return body

        with nc.Block() as block:
            block.sync(run("sync"))
            block.tensor(run("tensor"))
            block.vector(run("vector"))
            block.scalar(run("scalar"))
            block.gpsimd(run("gpsimd"))
        self.stats = stats


class Rot:
    def __init__(self, items):
        self.items = items
        self.i = 0

    def next(self):
        it = self.items[self.i % len(self.items)]
        self.i += 1
        return it


def _size(dt):
    return {F32: 4, BF16: 2, U32: 4}[dt]


class KB:
    def __init__(self, nc, dbg=()):
        self.nc = nc
        self.P = Prog(nc)
        self.stack = ExitStack()
        self.SB = self.stack.enter_context(nc.sbuf_tensor("SB", [128, SB_WORDS], F32))
        self.PS = self.stack.enter_context(nc.psum_tensor("PSA", [128, 8 * 512], F32))
        self.off = 0
        self.dbg = set(dbg)
        self.ndram = 0

    def T(self, shape, dt=F32, name=""):
        p = shape[0]
        n = int(np.prod(shape[1:]))
        words = (n * _size(dt) + 3) // 4
        assert self.off + words <= SB_WORDS, f"SBUF overflow allocating {name}{shape}: off={self.off} words={words}"
        ap = self.SB[0:p, self.off:self.off + words]
        self.off += words
        if dt != F32:
            ap = ap.bitcast(dt)
        ap = ap[:, 0:n]
        if len(shape) > 2:
            names = " ".join(f"d{i}" for i in range(len(shape) - 1))
            kw = {f"d{i}": shape[i + 1] for i in range(len(shape) - 2)}
            ap = ap.rearrange(f"p ({names}) -> p {names}", **kw)
        return Buf(ap, name)

    def psv(self, b0, nb, shape, dt=F32, name="", root=None):
        p = shape[0]
        n = int(np.prod(shape[1:]))
        ap = self.PS[0:p, b0 * 512:(b0 + nb) * 512]
        if dt != F32:
            ap = ap.bitcast(dt)
        ap = ap[:, 0:n]
        if len(shape) > 2:
            names = " ".join(f"d{i}" for i in range(len(shape) - 1))
            kw = {f"d{i}": shape[i + 1] for i in range(len(shape) - 2)}
            ap = ap.rearrange(f"p ({names}) -> p {names}", **kw)
        return Buf(ap, name, psum=True, root=root)

    def dram(self, name, shape, dt=F32, kind=None):
        if kind is None:
            kind = "ExternalOutput" if name in self.dbg else "Internal"
        t = self.nc.dram_tensor(name, list(shape), dt, kind=kind)
        return t.ap()

    def dram_tiles(self, name, n, shape, dt=F32):
        ap = self.dram(name, [n] + list(shape), dt)
        return [Buf(ap[i], f"{name}{i}") for i in range(n)]

    @contextmanager
    def phase(self):
        m = self.off
        yield
        self.P.barrier()
        self.off = m

    def dma(self, out, in_, reads, writes, q="sync", **kw):
        return self.P.add(q, lambda e: e.dma_start(out=out, in_=in_, **kw), reads, writes, dma=True)

    def mm(self, out, lhsT, rhs, reads, writes, start=True, stop=True):
        return self.P.add("tensor", lambda e: e.matmul(out, lhsT, rhs, start=start, stop=stop), reads, writes)

    def tr(self, out, in_, ident, reads, writes):
        return self.P.add("tensor", lambda e: e.transpose(out, in_, ident), reads, writes)

    def act(self, out, in_, func, reads, writes, **kw):
        return self.P.add("scalar", lambda e: e.activation(out=out, in_=in_, func=func, **kw), reads, writes)

    def v(self, eng, method, reads, writes, *a, **kw):
        return self.P.add(eng, lambda e: getattr(e, method)(*a, **kw), reads, writes)

    def finish(self):
        self.P.barrier()
        self.P.emit(self.stack)
        self.stack.close()


def make_consts(kb):
    c = {}
    io = kb.T([128, 128], F32, "iota")
    kb.P.add("gpsimd", lambda e: e.iota(io[:], [[1, 128]], base=0, channel_multiplier=-1,
                                        allow_small_or_imprecise_dtypes=True), [], [io])
    c["jmp"] = io
    c["ident_bf"] = kb.T([128, 128], BF16, "ident_bf")
    kb.v("vector", "tensor_single_scalar", [io], [c["ident_bf"]], c["ident_bf"][:], io[:], 0.0, ALU.is_equal)
    ij = kb.T([128, 128], F32, "iota_j")
    kb.P.add("gpsimd", lambda e: e.iota(ij[:], [[1, 128]], base=0, channel_multiplier=0,
                                        allow_small_or_imprecise_dtypes=True), [], [ij])
    c["iota_j"] = ij
    c["iota_bf"] = kb.T([128, 128], BF16, "iota_bf")
    kb.v("vector", "tensor_copy", [ij], [c["iota_bf"]], c["iota_bf"][:], ij[:])
    c["ident_f"] = kb.T([128, 128], F32, "ident_f")
    kb.v("vector", "tensor_single_scalar", [io], [c["ident_f"]], c["ident_f"][:], io[:], 0.0, ALU.is_equal)
    return c


def load_bcast(kb, dst, src_ap, srcbufs, q="sync"):
    p = dst.ap.shape[0]
    return kb.dma(dst[:], src_ap.partition_broadcast(p), srcbufs, [dst], q=q)


def load_w_bf16(kb, dst, w_ap, wbuf, stage_rot, K, N, c0=0, c1=None, eng_rot=None):
    c1 = N if c1 is None else c1
    kc = K // 128
    wv = w_ap.rearrange("(k p) n -> p k n", p=128)
    maxcols = stage_rot.items[0].ap.shape[1]
    i = 0
    for k in range(kc):
        for cs in range(c0, c1, maxcols):
            ce = min(c1, cs + maxcols)
            st = stage_rot.next()
            kb.dma(st[:, 0:ce - cs], wv[:, k, cs:ce], [wbuf], [st], q="sync" if i % 2 == 0 else "gpsimd")
            eng = ["gpsimd", "vector"][i % 2]
            kb.v(eng, "tensor_copy", [st], [dst], dst[:, k, cs - c0:ce - c0], st[:, 0:ce - cs])
            i += 1


def rstd_of(kb, ss, rstd, n):
    kb.act(rstd[:], ss[:], AF.Ln, [ss], [rstd], scale=1.0 / n, bias=kb.eps_t[:, 0:1])
    kb.act(rstd[:], rstd[:], AF.Exp, [rstd], [rstd], scale=-0.5)


def build_mod_tiles(kb, mod_ap, modbuf, g_ap, gbuf, row, j_shift, j_scale, G1, SH, tmp):
    load_bcast(kb, tmp, mod_ap[row, j_scale * D:(j_scale + 1) * D], [modbuf])
    load_bcast(kb, G1, g_ap, [gbuf], q="gpsimd")
    kb.v("vector", "scalar_tensor_tensor", [tmp, G1], [G1], G1[:], tmp[:], 1.0, G1[:], ALU.add, ALU.mult)
    load_bcast(kb, SH, mod_ap[row, j_shift * D:(j_shift + 1) * D], [modbuf])


def norm_mod_T(kb, C, src_ap, srcbuf, G1, SH, xt, junk, ss, rstd, nb, psT, nT):
    kb.dma(xt[:], src_ap, [srcbuf], [xt])
    kb.act(junk[:], xt[:], AF.Square, [xt], [junk, ss], accum_out=ss[:])
    rstd_of(kb, ss, rstd, D)
    kb.v("vector", "scalar_tensor_tensor", [xt, rstd, G1], [xt], xt[:], xt[:], rstd[:, 0:1], G1[:], ALU.mult, ALU.mult)
    kb.v("gpsimd", "tensor_tensor", [xt, SH], [nb], nb[:], xt[:], SH[:], ALU.add)
    for k in range(8):
        kb.tr(psT[:, k, :], nb[:, k * 128:(k + 1) * 128], C["ident_bf"][:], [nb, C["ident_bf"]], [psT])
    kb.act(nT[:], psT[:], AF.Copy, [psT], [nT])


def phase_mod(kb, C, I, layer, MOD):
    with kb.phase():
        raw = kb.T([128, 2, 8], F32, "craw")
        kb.dma(raw[:, 0, :], I["c"].rearrange("(p k) -> p k", k=8), [I["_c"]], [raw])
        kb.dma(raw[:, 1, :], I["c_ctx"].rearrange("(p k) -> p k", k=8), [I["_c_ctx"]], [raw])
        sc = kb.T([128, 8, 2], F32, "csilu")
        kb.act(sc[:].rearrange("p k m -> p m k"), raw[:], AF.Silu, [raw], [sc])
        bm = kb.T([2, 6144], F32, "bm")
        load_bcast(kb, bm, I["b_mod"][layer], [I["_b_mod"]], q="gpsimd")
        res = kb.T([2, 6144], F32, "modres")
        wrot = Rot([kb.T([128, 8, 512], F32, f"wm{i}") for i in range(2)])
        prot = Rot([kb.psv(i, 1, [128, 512], F32, f"psm{i}") for i in range(2)])
        wv = I["w_mod"][layer].rearrange("(p k) n -> p k n", k=8)
        for ng in range(12):
            wm = wrot.next()
            ps = prot.next()
            kb.dma(wm[:], wv[:, :, ng * 512:(ng + 1) * 512], [I["_w_mod"]], [wm], q="sync" if ng % 2 == 0 else "gpsimd")
            for k in range(8):
                kb.mm(ps[0:2, :], sc[:, k, :], wm[:, k, :], [sc, wm], [ps], start=(k == 0), stop=(k == 7))
            kb.v("vector", "tensor_tensor", [ps, bm], [res], res[:, ng * 512:(ng + 1) * 512], ps[0:2, :],
                 bm[:, ng * 512:(ng + 1) * 512], ALU.add)
        kb.dma(MOD.ap, res[:], [res], [MOD])


ZCOLS = 4356


def zcol(tt):
    return 1 + tt * 128 if tt < 2 else 259 + (tt - 2) * 128


def layer_even(kb, C, I, layer, MOD, src_tile, H, need_ctx=True):
    j = 0
    P = kb.P
    PQ = kb.dram_tiles("PQ", NT_ALL, [128, 256], F32)
    ZT = kb.dram("ZT", [512, ZCOLS], F32)
    BT = kb.dram("BT", [512, ZCOLS], F32)
    ZTb = [Buf(None, f"zt{t}") for t in range(NT_ALL)]
    BTb = [Buf(None, f"bt{t}") for t in range(NT_ALL)]
    ZPAD = Buf(None, "ztpad")
    ZTv = ZT.rearrange("(c p) t -> p c t", p=128)
    BTv = BT.rearrange("(c p) t -> p c t", p=128)
    with kb.phase():
        KT = kb.T([128, 8, NT_ALL * 128], BF16, "KT_all")
        VP = kb.T([128, NT_ALL, 8, 65], BF16, "VP_all")
        kmax = kb.T([128, 8], F32, "kmax")
        kb.v("gpsimd", "memset", [], [VP], VP[:], 1.0)
        kb.v("gpsimd", "memset", [], [kmax], kmax[:], 0.0)
        rope = I["rope"]
        with kb.phase():
            stage = Rot([kb.T([128, 1024], F32, f"stg{i}") for i in range(2)])
            w_in = kb.T([128, 8, 1952], BF16, "w_in")
            load_w_bf16(kb, w_in, I["even_w_in"][j], I["_even_w_in"], stage, 1024, 1952)
            w_ukv = kb.T([128, 1, 1024], BF16, "w_ukv")
            load_w_bf16(kb, w_ukv, I["mla_w_ukv"][j], I["_mla_w_ukv"], stage, 128, 1024)
            kvg = kb.T([128, 128], F32, "kvg")
            load_bcast(kb, kvg, I["mla_kv_norm"][j], [I["_mla_kv_norm"]])
            tmpm = kb.T([128, D], F32, "tmpm")
            G1 = [kb.T([128, D], F32, f"G1_{i}") for i in range(2)]
            SH = [kb.T([128, D], F32, f"SH_{i}") for i in range(2)]
            for r in range(2):
                build_mod_tiles(kb, MOD.ap, MOD, I["norm1_g"][layer], I["_norm1_g"], r, 0, 1, G1[r], SH[r], tmpm)
            zero = kb.T([128, 4, 1], F32, "zero")
            kb.v("gpsimd", "memset", [], [zero], zero[:], 0.0)
            for col in (0, 257, 258, 4355):
                kb.dma(ZTv[:, :, col:col + 1], zero[:], [zero], [ZPAD], q="gpsimd", allow_slow_non_contiguous=True)
            xt = kb.T([128, D], F32, "xt")
            junk = kb.T([128, D], BF16, "junk")
            ss = kb.T([128, 1], F32, "ss")
            rstd = kb.T([128, 1], F32, "rstd")
            ss2 = kb.T([128, 1], F32, "ss2")
            rstd2 = kb.T([128, 1], F32, "rstd2")
            nb = kb.T([128, D], BF16, "nb")
            nT = kb.T([128, 8, 128], BF16, "nT")
            pm = Rot([kb.T([128, 416], F32, f"pm{i}") for i in range(2)])
            latn = kb.T([128, 128], BF16, "latn")
            klT = kb.T([128, 128], BF16, "klT")
            kext = Rot([kb.T([128, 8, 97], BF16, f"kext{i}") for i in range(2)])
            for kx in kext.items:
                kb.v("gpsimd", "memset", [], [kx], kx[:], 1.0)
            krot = kb.T([128, 32], F32, "krot")
            rtmp = kb.T([128, 4, 16], F32, "rtmp")
            cs = kb.T([128, 32], F32, "cs")
            ksqt = kb.T([128, 8, 96], F32, "ksqt")
            ksq = kb.T([128, 8], F32, "ksq")
            c_sb = kb.T([128, 4, 128], F32, "c_sb")
            z_sb = Rot([kb.T([128, 4, 128], F32, f"z_sb{i}") for i in range(2)])
            b_sb = Rot([kb.T([128, 4, 128], F32, f"b_sb{i}") for i in range(2)])
            psT = kb.psv(0, 1, [128, 8, 128], BF16, "psT")
            ps_mla = kb.psv(1, 1, [128, 416], F32, "ps_mla")
            ps_lat = kb.psv(2, 1, [128, 8, 128], BF16, "ps_lat")
            ps_kv = kb.psv(3, 2, [128, 8, 128], F32, "ps_kv")
            ps_conv = kb.psv(5, 3, [128, 12, 128], F32, "ps_conv")
            for tt in (DBG_TILES or range(NT_ALL)):
                isc = 1 if tt < 2 else 0
                sap, sbuf_ = src_tile(tt)
                norm_mod_T(kb, C, sap, sbuf_, G1[isc], SH[isc], xt, junk, ss, rstd, nb, psT, nT)
                for k in range(8):
                    kb.mm(ps_mla[:], nT[:, k, :], w_in[:, k, 0:416], [nT, w_in], [ps_mla], start=(k == 0), stop=(k == 7))
                pmt = pm.next()
                kb.act(pmt[:], ps_mla[:], AF.Copy, [ps_mla], [pmt])
                kb.dma(PQ[tt].ap, pmt[:, 0:256], [pmt], [PQ[tt]], q="gpsimd")
                if "kv" in SKIP:
                    continue
                kb.act(junk[:, 0:128], pmt[:, 256:384], AF.Square, [pmt], [junk, ss2], accum_out=ss2[:])
                rstd_of(kb, ss2, rstd2, 128)
                kb.v("vector", "scalar_tensor_tensor", [pmt, rstd2, kvg], [latn], latn[:], pmt[:, 256:384],
                     rstd2[:, 0:1], kvg[:], ALU.mult, ALU.mult)
                kb.tr(ps_lat[:, 0, :], latn[:], C["ident_bf"][:], [latn, C["ident_bf"]], [ps_lat])
                kb.v("vector", "tensor_copy", [ps_lat], [klT], klT[:], ps_lat[:, 0, :])
                if "kv2" in SKIP:
                    continue
                kb.mm(ps_kv[:, 0:4, :], klT[:], w_ukv[:, 0, 0:512], [klT, w_ukv], [ps_kv])
                kb.mm(ps_kv[:, 4:8, :], klT[:], w_ukv[:, 0, 512:1024], [klT, w_ukv], [ps_kv])
                if "kv3" in SKIP:
                    continue
                kx = kext.next()
                for hb in range(2):
                    hs = slice(hb * 4, hb * 4 + 4)
                    kb.act(kx[:, hs, 0:64], ps_kv[:, hs, 0:64], AF.Copy, [ps_kv], [kx], scale=MLA_SCALE)
                    kb.v("vector", "tensor_copy", [ps_kv], [VP], VP[:, tt, hs, 0:64], ps_kv[:, hs, 64:128])
                if "rope" in SKIP:
                    continue
                if tt >= 2:
                    kb.dma(cs[:], rope[(tt - 2) * 128:(tt - 1) * 128, :], [I["_rope"]], [cs], q="gpsimd")
                    x1 = pmt[:, 384:400]
                    x2 = pmt[:, 400:416]
                    kb.v("gpsimd", "tensor_tensor", [pmt, cs], [rtmp], rtmp[:, 0, :], x1, cs[:, 0:16], ALU.mult)
                    kb.v("gpsimd", "tensor_tensor", [pmt, cs], [rtmp], rtmp[:, 1, :], x2, cs[:, 16:32], ALU.mult)
                    kb.v("gpsimd", "tensor_tensor", [pmt, cs], [rtmp], rtmp[:, 2, :], x1, cs[:, 16:32], ALU.mult)
                    kb.v("gpsimd", "tensor_tensor", [pmt, cs], [rtmp], rtmp[:, 3, :], x2, cs[:, 0:16], ALU.mult)
                    kb.v("vector", "tensor_tensor", [rtmp], [krot], krot[:, 0:16], rtmp[:, 0, :], rtmp[:, 1, :], ALU.subtract)
                    kb.v("vector", "tensor_tensor", [rtmp], [krot], krot[:, 16:32], rtmp[:, 2, :], rtmp[:, 3, :], ALU.add)
                else:
                    kb.v("vector", "tensor_copy", [pmt], [krot], krot[:], pmt[:, 384:416])
                kb.act(kx[:, :, 64:96], krot[:].unsqueeze(1).to_broadcast([128, 8, 32]), AF.Copy, [krot], [kx],
                       scale=MLA_SCALE)
                if "ksq" in SKIP:
                    continue
                kb.v("vector", "tensor_tensor", [kx], [ksqt], ksqt[:], kx[:, :, 0:96], kx[:, :, 0:96], ALU.mult)
                kb.v("vector", "tensor_reduce", [ksqt], [ksq], ksq[:], ksqt[:], AX.X, ALU.add)
                kb.v("vector", "tensor_tensor", [ksq, kmax], [kmax], kmax[:], kmax[:], ksq[:], ALU.max)
                for h in range(8):
                    kb.tr(ps_lat[0:97, h, :], kx[:, h, :], C["ident_bf"][:], [kx, C["ident_bf"]], [ps_lat])
                kb.act(KT[0:97, :, tt * 128:(tt + 1) * 128], ps_lat[0:97, :, :], AF.Copy, [ps_lat], [KT])
                if "conv" in SKIP:
                    continue
                for fc in range(12):
                    for k in range(8):
                        kb.mm(ps_conv[:, fc, :], w_in[:, k, 416 + fc * 128:416 + (fc + 1) * 128], nT[:, k, :],
                              [w_in, nT], [ps_conv], start=(k == 0), stop=(k == 7))
                kb.act(c_sb[:], ps_conv[:, 4:8, :], AF.Copy, [ps_conv], [c_sb])
                zt = z_sb.next()
                bt = b_sb.next()
                kb.v("vector", "tensor_tensor", [ps_conv, c_sb], [zt], zt[:], ps_conv[:, 8:12, :], c_sb[:], ALU.mult)
                kb.act(bt[:], ps_conv[:, 0:4, :], AF.Copy, [ps_conv], [bt])
                c0 = zcol(tt)
                kb.dma(ZTv[:, :, c0:c0 + 128], zt[:], [zt], [ZTb[tt]], q="gpsimd")
                kb.dma(BTv[:, :, c0:c0 + 128], bt[:], [bt], [BTb[tt]], q="gpsimd")
        if kb.stop_after == "E1":
            return
        with kb.phase():
            stage = Rot([kb.T([128, 1024], F32, f"stg{i}") for i in range(2)])
            w_uq = kb.T([128, 2, 768], BF16, "w_uq")
            load_w_bf16(kb, w_uq, I["mla_w_uq"][j], I["_mla_w_uq"], stage, 256, 768)
            w_out = kb.T([128, 8, 1024], BF16, "w_out")
            load_w_bf16(kb, w_out, I["even_w_out"][j], I["_even_w_out"], stage, 1024, 1024)
            qg = kb.T([128, 256], F32, "qg")
            load_bcast(kb, qg, I["mla_q_norm"][j], [I["_mla_q_norm"]])
            M2 = [kb.T([128, D], F32, f"M2_{i}") for i in range(2)]
            for r in range(2):
                load_bcast(kb, M2[r], MOD.ap[r, 2 * D:3 * D], [MOD])
            cw = kb.T([128, 3, 4], F32, "cw")
            for w_ in range(3):
                for c_ in range(4):
                    kb.dma(cw[:, w_, c_:c_ + 1], I["conv_w"][j][w_, c_ * 128:(c_ + 1) * 128].unsqueeze(1),
                           [I["_conv_w"]], [cw], q="gpsimd", allow_slow_non_contiguous=True)
            psx = kb.psv(6, 1, [128, 128], F32, "psx")
            kmr = kb.T([128, 1], F32, "kmr")
            kb.v("vector", "tensor_reduce", [kmax], [kmr], kmr[:], kmax[:], AX.X, ALU.max)
            kmb = kb.T([128, 128], F32, "kmb")
            kb.v("vector", "tensor_copy", [kmr], [kmb], kmb[:], kmr[:, 0:1].to_broadcast([128, 128]))
            kb.tr(psx[:], kmb[:], C["ident_f"][:], [kmb, C["ident_f"]], [psx])
            ksm = kb.T([128, 1], F32, "ksm")
            kb.v("vector", "tensor_reduce", [psx], [ksm], ksm[:], psx[:], AX.X, ALU.max)
            pq = kb.T([128, 256], F32, "pq")
            junk = kb.T([128, 768], BF16, "junk")
            ss = kb.T([128, 1], F32, "ss")
            rstd = kb.T([128, 1], F32, "rstd")
            qn = kb.T([128, 256], BF16, "qn")
            qlT = kb.T([128, 2, 128], BF16, "qlT")
            q_sb = kb.T([128, 8, 96], F32, "q_sb")
            cs = kb.T([128, 32], F32, "cs")
            rt = kb.T([128, 4, 8, 16], F32, "rt")
            qsqt = kb.T([128, 8, 96], F32, "qsqt")
            qsq = kb.T([128, 8], F32, "qsq")
            qext = kb.T([128, 8, 97], BF16, "qext")
            QT = kb.T([128, 8, 512], BF16, "QT")
            PT = Rot([kb.T([128, 512], BF16, f"PT{i}") for i in range(3)])
            rec = kb.T([128, 4], F32, "rec")
            mix_tok = [kb.T([128, 512], BF16, f"mix{i}") for i in range(4)]
            mixT = kb.T([128, 8, 512], BF16, "mixT")
            zw = kb.T([128, 4, 514], F32, "zw")
            bw = kb.T([128, 4, 512], F32, "bw")
            yc_ = kb.T([128, 512], F32, "yconv")
            hres = stage.items[0]
            ytmp = stage.items[1]
            ps_S = Rot([kb.psv(i, 1, [128, 512], F32, f"psS{i}") for i in range(2)])
            ps_O = [kb.psv(2 + i, 1, [128, 65], F32, f"psO{i}") for i in range(4)]
            ps_q = kb.psv(2, 2, [128, 768], F32, "ps_q")
            ps_t = kb.psv(6, 1, [128, 8, 128], BF16, "ps_t")
            ps_y = kb.psv(6, 2, [128, 1024], F32, "ps_y")
            bank = {i: None for i in range(8)}
            blocks = []
            if need_ctx:
                blocks.append(([0, 1], [0, 1]))
            for b in range(8):
                blocks.append(([2 + 4 * b + i for i in range(4)], list(range(NT_ALL))))
            for tiles, ktiles in blocks:
                nq = len(tiles) * 128
                for qi, tt in enumerate(tiles):
                    kb.dma(pq[:], PQ[tt].ap, [PQ[tt]], [pq])
                    kb.act(junk[:, 0:256], pq[:], AF.Square, [pq], [junk, ss], accum_out=ss[:])
                    rstd_of(kb, ss, rstd, 256)
                    kb.v("vector", "scalar_tensor_tensor", [pq, rstd, qg], [qn], qn[:], pq[:], rstd[:, 0:1], qg[:],
                         ALU.mult, ALU.mult)
                    for k in range(2):
                        kb.tr(ps_t[:, k, :], qn[:, k * 128:(k + 1) * 128], C["ident_bf"][:], [qn, C["ident_bf"]], [ps_t])
                    kb.v("vector", "tensor_copy", [ps_t], [qlT], qlT[:], ps_t[:, 0:2, :])
                    for k in range(2):
                        kb.mm(ps_q[:, 0:512], qlT[:, k, :], w_uq[:, k, 0:512], [qlT, w_uq], [ps_q, ps_O[0]],
                              start=(k == 0), stop=(k == 1))
                    for k in range(2):
                        kb.mm(ps_q[:, 512:768], qlT[:, k, :], w_uq[:, k, 512:768], [qlT, w_uq], [ps_q, ps_O[1]],
                              start=(k == 0), stop=(k == 1))
                    q_flat = q_sb[:].rearrange("p h d -> p (h d)")
                    kb.act(q_flat[:, 0:512], ps_q[:, 0:512], AF.Copy, [ps_q, ps_O[0], ps_O[1]], [q_sb])
                    kb.act(q_flat[:, 512:768], ps_q[:, 512:768], AF.Copy, [ps_q, ps_O[0], ps_O[1]], [q_sb])
                    kb.v("vector", "tensor_tensor", [q_sb], [qsqt], qsqt[:], q_sb[:], q_sb[:], ALU.mult)
                    kb.v("vector", "tensor_reduce", [qsqt], [qsq], qsq[:], qsqt[:], AX.X, ALU.add)
                    kb.act(qsq[:], qsq[:], AF.Sqrt, [qsq, ksm], [qsq], scale=ksm[:, 0:1])
                    kb.v("vector", "tensor_scalar_mul", [qsq], [qext], qext[:, :, 96], qsq[:], -1.0)
                    kb.v("gpsimd", "tensor_copy", [q_sb], [qext], qext[:, :, 0:64], q_sb[:, :, 0:64])
                    if tt >= 2:
                        kb.dma(cs[:], rope[(tt - 2) * 128:(tt - 1) * 128, :], [I["_rope"]], [cs], q="gpsimd")
                        x1 = q_sb[:, :, 64:80]
                        x2 = q_sb[:, :, 80:96]
                        cosb = cs[:, 0:16].unsqueeze(1).to_broadcast([128, 8, 16])
                        sinb = cs[:, 16:32].unsqueeze(1).to_broadcast([128, 8, 16])
                        kb.v("gpsimd", "tensor_tensor", [q_sb, cs], [rt], rt[:, 0], x1, cosb, ALU.mult)
                        kb.v("gpsimd", "tensor_tensor", [q_sb, cs], [rt], rt[:, 1], x2, sinb, ALU.mult)
                        kb.v("gpsimd", "tensor_tensor", [q_sb, cs], [rt], rt[:, 2], x1, sinb, ALU.mult)
                        kb.v("gpsimd", "tensor_tensor", [q_sb, cs], [rt], rt[:, 3], x2, cosb, ALU.mult)
                        kb.v("vector", "tensor_tensor", [rt], [qext], qext[:, :, 64:80], rt[:, 0], rt[:, 1], ALU.subtract)
                        kb.v("vector", "tensor_tensor", [rt], [qext], qext[:, :, 80:96], rt[:, 2], rt[:, 3], ALU.add)
                    else:
                        kb.v("vector", "tensor_copy", [q_sb], [qext], qext[:, :, 64:96], q_sb[:, :, 64:96])
                    for h in range(8):
                        kb.tr(ps_t[0:97, h, :], qext[:, h, :], C["ident_bf"][:], [qext, C["ident_bf"]], [ps_t])
                    kb.act(QT[0:97, :, qi * 128:(qi + 1) * 128], ps_t[0:97, :, :], AF.Copy, [ps_t], [QT])
                nqs = len(tiles)
                for h in range(8):
                    def S_(ki, kt):
                        pS = ps_S.next()
                        kb.mm(pS[:, 0:nq], KT[0:97, h, kt * 128:(kt + 1) * 128], QT[0:97, h, 0:nq], [KT, QT], [pS])
                        pt = PT.next()
                        kb.act(pt[:, 0:nq], pS[:, 0:nq], AF.Exp, [pS], [pt])
                        return (ki, kt, pt)

                    def PV_(st):
                        ki, kt, pt = st
                        for qs in range(nqs):
                            kb.mm(ps_O[qs][:], pt[:, qs * 128:(qs + 1) * 128], VP[:, kt, h, :], [pt, VP], [ps_O[qs]],
                                  start=(ki == 0), stop=(ki == len(ktiles) - 1))

                    pend = None
                    for ki, kt in enumerate(ktiles):
                        cur = S_(ki, kt)
                        if pend is not None:
                            PV_(pend)
                        pend = cur
                    PV_(pend)
                    for qs in range(nqs):
                        kb.v("vector", "reciprocal", [ps_O[qs]], [rec], rec[:, qs:qs + 1], ps_O[qs][:, 64:65])
                        kb.act(mix_tok[qs][:, h * 64:(h + 1) * 64], ps_O[qs][:, 0:64], AF.Copy, [ps_O[qs], rec],
                               [mix_tok[qs]], scale=rec[:, qs:qs + 1])
                for qi in range(nqs):
                    for c_ in range(4):
                        kb.tr(ps_t[:, c_, :], mix_tok[qi][:, c_ * 128:(c_ + 1) * 128], C["ident_bf"][:],
                              [mix_tok[qi], C["ident_bf"]], [ps_t])
                    kb.v("vector", "tensor_copy", [ps_t], [mixT], mixT[:, 0:4, qi * 128:(qi + 1) * 128], ps_t[:, 0:4, :])
                c0 = zcol(tiles[0])
                kb.dma(zw[:, :, 0:nq + 2], ZTv[:, :, c0 - 1:c0 + nq + 1], ZTb + [ZPAD], [zw])
                kb.dma(bw[:, :, 0:nq], BTv[:, :, c0:c0 + nq], BTb, [bw], q="gpsimd")
                for c_ in range(4):
                    eng = "vector"
                    kb.v(eng, "tensor_scalar", [zw, cw], [yc_], yc_[:, 0:nq], zw[:, c_, 0:nq], cw[:, 0, c_:c_ + 1], None, ALU.mult)
                    kb.v(eng, "scalar_tensor_tensor", [zw, cw, yc_], [yc_], yc_[:, 0:nq], zw[:, c_, 1:nq + 1],
                         cw[:, 1, c_:c_ + 1], yc_[:, 0:nq], ALU.mult, ALU.add)
                    kb.v(eng, "scalar_tensor_tensor", [zw, cw, yc_], [yc_], yc_[:, 0:nq], zw[:, c_, 2:nq + 2],
                         cw[:, 2, c_:c_ + 1], yc_[:, 0:nq], ALU.mult, ALU.add)
                    kb.v(eng, "tensor_tensor", [yc_, bw], [mixT], mixT[:, 4 + c_, 0:nq], yc_[:, 0:nq], bw[:, c_, 0:nq], ALU.mult)
                for qi, tt in enumerate(tiles):
                    isc = 1 if tt < 2 else 0
                    sap, sbuf_ = src_tile(tt)
                    kb.dma(hres[:], sap, [sbuf_], [hres])
                    for half in range(2):
                        for c_ in range(8):
                            kb.mm(ps_y[:, half * 512:(half + 1) * 512], mixT[:, c_, qi * 128:(qi + 1) * 128],
                                  w_out[:, c_, half * 512:(half + 1) * 512], [mixT, w_out], [ps_y, ps_t],
                                  start=(c_ == 0), stop=(c_ == 7))
                    for half in range(2):
                        hs = slice(half * 512, (half + 1) * 512)
                        kb.v("vector", "tensor_tensor", [ps_y, ps_t, M2[isc]], [ytmp], ytmp[:, hs], ps_y[:, hs], M2[isc][:, hs], ALU.mult)
                    kb.v("gpsimd", "tensor_tensor", [ytmp, hres], [ytmp], ytmp[:], ytmp[:], hres[:], ALU.add)
                    kb.dma(H[tt].ap, ytmp[:], [ytmp], [H[tt]], q="gpsimd")


def layer_odd(kb, C, I, layer, MOD, src_tile, H, bg=None):
    j = 0
    QKT = kb.dram_tiles("QKT", NT_ALL, [128, 8, 128], BF16)
    KHd = kb.dram_tiles("KHd", NT_ALL, [128, 8, 128], BF16)
    VPd = kb.dram_tiles("VPd", NT_ALL, [128, 4, 257], BF16)
    SCd = kb.dram_tiles("SCd", NT_ALL, [128, 32], F32)
    OGd = kb.dram_tiles("OGd", NT_ALL, [128, D], F32)
    HBd = kb.dram_tiles("HBd", NT_ALL, [128, D], F32)
    HSd = kb.dram_tiles("HSd", NT_ALL, [128, D], F32)
    lat_tiles = list(range(2, NT_ALL))
    with kb.phase():
        stage = Rot([kb.T([128, 1024], F32, f"stg{i}") for i in range(2)])
        w_in = kb.T([128, 8, 3088], BF16, "w_in_o")
        load_w_bf16(kb, w_in, I["odd_w_in"][j], I["_odd_w_in"], stage, 1024, 3088)
        tmpm = stage.items[0]
        G1 = [kb.T([128, D], F32, f"G1_{i}") for i in range(2)]
        SH = [kb.T([128, D], F32, f"SH_{i}") for i in range(2)]
        for r in range(2):
            build_mod_tiles(kb, MOD.ap, MOD, I["norm1_g"][layer], I["_norm1_g"], r, 0, 1, G1[r], SH[r], tmpm)
        gbias = kb.T([128, 16], F32, "gbias")
        load_bcast(kb, gbias, I["mlstm_gate_b"][j], [I["_mlstm_gate_b"]])
        triU = kb.T([128, 128], F32, "triU")
        triL = kb.T([128, 128], F32, "triL")
        ones = kb.T([128, 128], F32, "ones")
        kb.v("vector", "tensor_single_scalar", [C["jmp"]], [triU], triU[:], C["jmp"][:], 0.0, ALU.is_ge)
        kb.v("vector", "tensor_single_scalar", [C["jmp"]], [triL], triL[:], C["jmp"][:], 0.0, ALU.is_le)
        kb.v("gpsimd", "memset", [], [ones], ones[:], 1.0)
        xt = kb.T([128, D], F32, "xt")
        junk = kb.T([128, D], BF16, "junk")
        ss = kb.T([128, 1], F32, "ss")
        rstd = kb.T([128, 1], F32, "rstd")
        nb = kb.T([128, D], BF16, "nb")
        nT = kb.T([128, 8, 128], BF16, "nT")
        qkT = Rot([kb.T([128, 8, 128], BF16, f"qkT{i}") for i in range(2)])
        khat = Rot([kb.T([128, 8, 128], BF16, f"khat{i}") for i in range(2)])
        vp = Rot([kb.T([128, 4, 257], BF16, f"vp{i}") for i in range(2)])
        for v_ in vp.items:
            kb.v("gpsimd", "memset", [], [v_], v_[:], 1.0)
        og = Rot([kb.T([128, D], F32, f"og{i}") for i in range(2)])
        sc = Rot([kb.T([128, 32], F32, f"sc{i}") for i in range(2)])
        gb = kb.T([128, 16], F32, "gb")
        nl = kb.T([128, 8], F32, "nl")
        igc = kb.T([128, 8], F32, "igc")
        t1 = kb.T([128, 8], F32, "t1")
        t2 = kb.T([128, 8], F32, "t2")
        psT = kb.psv(0, 1, [128, 8, 128], BF16, "psT")
        ps_g = kb.psv(0, 1, [128, 64], F32, "ps_g", root=psT)
        ps_f = [kb.psv(1 + i, 1, [128, 4, 128], F32, f"ps_f{i}") for i in range(2)]
        ps_k = kb.psv(3, 1, [128, 512], F32, "ps_k")
        ps_v = [kb.psv(4 + i, 1, [128, 512], F32, f"ps_v{i}") for i in range(2)]
        ps_o = [kb.psv(6 + i, 1, [128, 512], F32, f"ps_o{i}") for i in range(2)]
        for tt in range(NT_ALL):
            isc = 1 if tt < 2 else 0
            sap, sbuf_ = src_tile(tt)
            norm_mod_T(kb, C, sap, sbuf_, G1[isc], SH[isc], xt, junk, ss, rstd, nb, psT, nT)
            for hh in range(8):
                for k in range(8):
                    kb.mm(ps_f[hh // 4][:, hh % 4, :], w_in[:, k, hh * 128:(hh + 1) * 128], nT[:, k, :], [w_in, nT],
                          [ps_f[hh // 4]], start=(k == 0), stop=(k == 7))
            qk = qkT.next()
            kb.act(qk[:, 0:4, :], ps_f[0][:], AF.Copy, [ps_f[0]], [qk], scale=128 ** -0.5)
            kb.v("vector", "tensor_copy", [ps_f[1]], [qk], qk[:, 4:8, :], ps_f[1][:])
            kb.dma(QKT[tt].ap, qk[:], [qk], [QKT[tt]], q="gpsimd")
            for k in range(8):
                kb.mm(ps_k[:], nT[:, k, :], w_in[:, k, 512:1024], [nT, w_in], [ps_k], start=(k == 0), stop=(k == 7))
            for hf in range(2):
                for k in range(8):
                    kb.mm(ps_v[hf][:], nT[:, k, :], w_in[:, k, 1024 + hf * 512:1536 + hf * 512], [nT, w_in], [ps_v[hf]],
                          start=(k == 0), stop=(k == 7))
            for k in range(8):
                kb.mm(ps_g[:, 0:16], nT[:, k, :], w_in[:, k, 2048:2064], [nT, w_in], [ps_g], start=(k == 0), stop=(k == 7))
            if tt >= 2:
                for hf in range(2):
                    for k in range(8):
                        kb.mm(ps_o[hf][:], nT[:, k, :], w_in[:, k, 2064 + hf * 512:2576 + hf * 512], [nT, w_in],
                              [ps_o[hf]], start=(k == 0), stop=(k == 7))
                o_ = og.next()
                for hf in range(2):
                    kb.act(o_[:, hf * 512:(hf + 1) * 512], ps_o[hf][:], AF.Sigmoid, [ps_o[hf]], [o_])
                kb.dma(OGd[tt].ap, o_[:], [o_], [OGd[tt]], q="gpsimd")
            kb.v("vector", "tensor_tensor", [ps_g, gbias], [gb], gb[:], ps_g[:, 0:16], gbias[:], ALU.add)
            gb4 = gb[:].rearrange("p (d two h) -> p d two h", d=2, two=2)
            nl3 = nl[:].rearrange("p (d h) -> p d h", d=2)
            kb.act(nl3, gb4[:, :, 1, :], AF.Exp, [gb], [nl], scale=-1.0)
            kb.act(nl[:], nl[:], AF.Ln, [nl], [nl], bias=kb.one_t[:, 0:1])
            kb.v("vector", "tensor_copy", [gb], [igc], igc[:].rearrange("p (d h) -> p d h", d=2), gb4[:, :, 0, :])
            kb.mm(ps_g[:, 16:20], triU[:], nl[:, 0:4], [triU, nl], [ps_g])
            kb.mm(ps_g[:, 20:24], triL[:], nl[:, 4:8], [triL, nl], [ps_g])
            kb.mm(ps_g[:, 24:32], ones[:], nl[:], [ones, nl], [ps_g])
            s_ = sc.next()
            kb.v("vector", "tensor_tensor", [igc, ps_g], [t1], t1[:], igc[:], ps_g[:, 16:24], ALU.add)
            kb.v("vector", "tensor_tensor", [t1, ps_g], [t2], t2[:], t1[:], ps_g[:, 24:32], ALU.subtract)
            kb.act(s_[:, 0:8], t1[:], AF.Exp, [t1], [s_])
            kb.act(s_[:, 8:16], ps_g[:, 16:24], AF.Exp, [ps_g], [s_], scale=-1.0)
            kb.act(s_[:, 16:24], t2[:], AF.Exp, [t2], [s_])
            kb.act(s_[:, 24:32], ps_g[:, 24:32], AF.Exp, [ps_g], [s_], scale=-1.0)
            kb.dma(SCd[tt].ap, s_[:], [s_], [SCd[tt]], q="gpsimd")
            kh = khat.next()
            for c in range(8):
                h = c % 4
                kb.act(kh[:, c, :], ps_k[:, h * 128:(h + 1) * 128], AF.Copy, [ps_k, s_], [kh], scale=s_[:, 16 + c:17 + c])
            kb.dma(KHd[tt].ap, kh[:], [kh], [KHd[tt]], q="gpsimd")
            v_ = vp.next()
            for hf in range(2):
                kb.v("vector", "tensor_copy", [ps_v[hf]], [v_], v_[:, hf * 2:hf * 2 + 2, 0:256],
                     ps_v[hf][:].rearrange("p (h d) -> p h d", h=2))
            kb.dma(VPd[tt].ap, v_[:], [v_], [VPd[tt]], q="gpsimd")
    with kb.phase():
        triU = kb.T([128, 128], F32, "triU")
        triL = kb.T([128, 128], F32, "triL")
        kb.v("vector", "tensor_single_scalar", [C["jmp"]], [triU], triU[:], C["jmp"][:], 0.0, ALU.is_ge)
        kb.v("vector", "tensor_single_scalar", [C["jmp"]], [triL], triL[:], C["jmp"][:], 0.0, ALU.is_le)
        Cst = [kb.T([128, 257], F32, f"Cst{h}") for h in range(4)]
        Cbf = [kb.T([128, 257], BF16, f"Cbf{h}") for h in range(4)]
        qk = Rot([kb.T([128, 8, 128], BF16, f"qk{i}") for i in range(3)])
        kh = Rot([kb.T([128, 4, 128], BF16, f"kh{i}") for i in range(3)])
        vp = Rot([kb.T([128, 4, 257], BF16, f"vp{i}") for i in range(3)])
        sc = Rot([kb.T([128, 32], F32, f"sc{i}") for i in range(3)])
        PTm = Rot([kb.T([128, 128], BF16, f"PTm{i}") for i in range(3)])
        t4 = kb.T([128, 4], F32, "t4")
        r4 = kb.T([128, 4], F32, "r4")
        hout = Rot([kb.T([128, D], F32, f"hout{i}") for i in range(2)])
        hb = Rot([kb.T([128, D], F32, f"hb{i}") for i in range(2)])
        ps_S = Rot([kb.psv(i, 1, [128, 128], F32, f"psS{i}") for i in range(2)])
        ps_N = [kb.psv(2 + h, 1, [128, 257], F32, f"psN{h}") for h in range(4)]
        ps_U = Rot([kb.psv(6 + i, 1, [128, 257], F32, f"psU{i}") for i in range(2)])
        bg_units = []
        if bg is not None:
            bg_units = bg([kb.psv(i, 1, [128, 8, 128], BF16, f"pstb{i}", root=ps_S.items[i]) for i in range(2)])
        for d in (1, 0):
            order = [0, 1] + lat_tiles if d == 0 else [1, 0] + lat_tiles[::-1]
            mask = triU if d == 0 else triL
            for h in range(4):
                kb.v("gpsimd", "memset", [], [Cst[h]], Cst[h][:], 0.0)
                kb.v("gpsimd", "memset", [], [Cbf[h]], Cbf[h][:], 0.0)
            for oi, tt in enumerate(order):
                q_ = qk.next(); k_ = kh.next(); v_ = vp.next(); s_ = sc.next()
                kb.dma(q_[:], QKT[tt].ap, [QKT[tt]], [q_])
                kb.dma(k_[:], KHd[tt].ap[:, d * 4:(d + 1) * 4, :], [KHd[tt]], [k_])
                kb.dma(v_[:], VPd[tt].ap, [VPd[tt]], [v_])
                kb.dma(s_[:], SCd[tt].ap, [SCd[tt]], [s_])
                if tt >= 2:
                    for h in range(4):
                        c = d * 4 + h
                        pS = ps_S.next()
                        kb.mm(pS[:], q_[:, 4 + h, :], q_[:, h, :], [q_], [pS])
                        pt = PTm.next()
                        kb.v("vector", "scalar_tensor_tensor", [pS, s_, mask], [pt], pt[:], pS[:], s_[:, c:c + 1], mask[:],
                             ALU.mult, ALU.mult)
                        kb.mm(ps_N[h][:], pt[:], v_[:, h, :], [pt, v_], [ps_N[h]], start=True, stop=False)
                        kb.mm(ps_N[h][:], q_[:, h, :], Cbf[h][:], [q_, Cbf[h]], [ps_N[h]], start=False, stop=True)
                        kb.v("vector", "tensor_tensor", [ps_N[h], s_], [t4], t4[:, h:h + 1], ps_N[h][:, 256:257],
                             s_[:, 8 + c:9 + c], ALU.mult)
                    kb.act(t4[:], t4[:], AF.Abs, [t4], [t4])
                    kb.v("vector", "tensor_scalar_max", [t4], [r4], r4[:], t4[:], 1.0)
                    kb.v("vector", "reciprocal", [r4], [r4], r4[:], r4[:])
                    kb.v("vector", "tensor_tensor", [r4, s_], [r4], r4[:], r4[:], s_[:, 8 + d * 4:12 + d * 4], ALU.mult)
                    ho = hout.next()
                    for h in range(4):
                        kb.act(ho[:, h * 256:(h + 1) * 256], ps_N[h][:, 0:256], AF.Copy, [ps_N[h], r4], [ho],
                               scale=r4[:, h:h + 1])
                    if d == 1:
                        kb.dma(HBd[tt].ap, ho[:], [ho], [HBd[tt]], q="gpsimd")
                    else:
                        hb_ = hb.next()
                        kb.dma(hb_[:], HBd[tt].ap, [HBd[tt]], [hb_])
                        kb.v("gpsimd", "tensor_tensor", [ho, hb_], [hb_], hb_[:], ho[:], hb_[:], ALU.add)
                        kb.dma(HSd[tt].ap, hb_[:], [hb_], [HSd[tt]], q="gpsimd")
                for _ in range(2):
                    if bg_units:
                        bg_units.pop(0)()
                if oi == len(order) - 1:
                    continue
                for h in range(4):
                    c = d * 4 + h
                    pU = ps_U.next()
                    kb.mm(pU[:], k_[:, h, :], v_[:, h, :], [k_, v_], [pU])
                    kb.v("vector", "scalar_tensor_tensor", [Cst[h], s_, pU], [Cst[h]], Cst[h][:], Cst[h][:],
                         s_[:, 24 + c:25 + c], pU[:], ALU.mult, ALU.add)
                    kb.act(Cbf[h][:], Cst[h][:], AF.Copy, [Cst[h]], [Cbf[h]])
        while bg_units:
            bg_units.pop(0)()
    with kb.phase():
        stage = Rot([kb.T([128, 1024], F32, f"stg{i}") for i in range(2)])
        w_out = kb.T([128, 8, 1024], BF16, "w_out_o")
        load_w_bf16(kb, w_out, I["odd_w_out"][j], I["_odd_w_out"], stage, 1024, 1024)
        hg = kb.T([128, D], F32, "hg")
        load_bcast(kb, hg, I["mlstm_head_g"][j], [I["_mlstm_head_g"]])
        M2 = kb.T([128, D], F32, "M2")
        load_bcast(kb, M2, MOD.ap[0, 2 * D:3 * D], [MOD])
        hs = Rot([kb.T([128, D], F32, f"hs{i}") for i in range(2)])
        og = Rot([kb.T([128, D], F32, f"og{i}") for i in range(2)])
        hres = Rot([kb.T([128, D], F32, f"hres{i}") for i in range(2)])
        sq = kb.T([128, D], F32, "sq")
        ss4 = kb.T([128, 4], F32, "ss4")
        rs4 = kb.T([128, 4], F32, "rs4")
        mb = kb.T([128, D], BF16, "mb")
        mT = kb.T([128, 8, 128], BF16, "mT")
        yo = Rot([kb.T([128, D], F32, f"yo{i}") for i in range(2)])
        psT = kb.psv(0, 1, [128, 8, 128], BF16, "psT")
        ps_y = [kb.psv(1 + i, 1, [128, 512], F32, f"ps_y{i}") for i in range(2)]
        for tt in lat_tiles:
            h_ = hs.next(); o_ = og.next(); r_ = hres.next()
            kb.dma(h_[:], HSd[tt].ap, [HSd[tt]], [h_])
            kb.dma(o_[:], OGd[tt].ap, [OGd[tt]], [o_])
            sap, sbuf_ = src_tile(tt)
            kb.dma(r_[:], sap, [sbuf_], [r_])
            kb.v("gpsimd", "tensor_tensor", [h_], [sq], sq[:], h_[:], h_[:], ALU.mult)
            kb.v("vector", "tensor_reduce", [sq], [ss4], ss4[:], sq[:].rearrange("p (h d) -> p h d", h=4), AX.X, ALU.add)
            rstd_of(kb, ss4, rs4, 256)
            h3 = h_[:].rearrange("p (h d) -> p h d", h=4)
            kb.v("vector", "tensor_tensor", [h_, rs4], [h_], h3, h3, rs4[:].unsqueeze(2).to_broadcast([128, 4, 256]), ALU.mult)
            kb.v("gpsimd", "tensor_tensor", [o_, hg], [o_], o_[:], o_[:], hg[:], ALU.mult)
            kb.v("vector", "tensor_tensor", [h_, o_], [mb], mb[:], h_[:], o_[:], ALU.mult)
            for k in range(8):
                kb.tr(psT[:, k, :], mb[:, k * 128:(k + 1) * 128], C["ident_bf"][:], [mb, C["ident_bf"]], [psT])
            kb.act(mT[:], psT[:], AF.Copy, [psT], [mT])
            for hf in range(2):
                for k in range(8):
                    kb.mm(ps_y[hf][:], mT[:, k, :], w_out[:, k, hf * 512:(hf + 1) * 512], [mT, w_out], [ps_y[hf]],
                          start=(k == 0), stop=(k == 7))
            y_ = yo.next()
            for hf in range(2):
                hsl = slice(hf * 512, (hf + 1) * 512)
                kb.v("vector", "tensor_tensor", [ps_y[hf], M2], [y_], y_[:, hsl], ps_y[hf][:], M2[:, hsl], ALU.mult)
            kb.v("gpsimd", "tensor_tensor", [y_, r_], [y_], y_[:], y_[:], r_[:], ALU.add)
            kb.dma(H[tt].ap, y_[:], [y_], [H[tt]], q="gpsimd")


def peer_prep_units(kb, C, I, layer, UT, VB, ps_list, light=False):
    uf = Rot([kb.T([128, D], F32, f"uf{i}") for i in range(2)])
    ub = Rot([kb.T([128, D], BF16, f"ub{i}") for i in range(2)])
    utb = Rot([kb.T([128, 8, 128], BF16, f"utb{i}") for i in range(2)])
    vf = Rot([kb.T([128, D], F32, f"vf{i}") for i in range(2)])
    vb = Rot([kb.T([128, D], BF16, f"vb{i}") for i in range(2)])
    ps = Rot(ps_list)
    units = []
    for i in range(128):
        def unit(i=i):
            a = uf.next(); b = ub.next(); t = utb.next(); p = ps.next()
            lq = "gpsimd" if light else "sync"
            kb.dma(a[:], I["peer_u"][layer][i * 128:(i + 1) * 128, :], [I["_peer_u"]], [a], q=lq)
            if i % 2 == 0:
                kb.v("gpsimd", "tensor_copy", [a], [b], b[:], a[:])
            else:
                kb.act(b[:], a[:], AF.Copy, [a], [b])
            for k in range(8):
                kb.tr(p[:, k, :], b[:, k * 128:(k + 1) * 128], C["ident_bf"][:], [b, C["ident_bf"]], [p])
            if light:
                kb.act(t[:], p[:], AF.Copy, [p], [t])
            else:
                kb.v("vector", "tensor_copy", [p], [t], t[:], p[:])
            kb.dma(UT[i].ap, t[:], [t], [UT[i]], q="gpsimd")
            a2 = vf.next(); b2 = vb.next()
            kb.dma(a2[:], I["peer_v"][layer][i * 128:(i + 1) * 128, :], [I["_peer_v"]], [a2], q=lq)
            if light:
                kb.v("gpsimd", "tensor_copy", [a2], [b2], b2[:], a2[:])
            else:
                kb.v("vector", "tensor_copy", [a2], [b2], b2[:], a2[:])
            kb.dma(VB[i].ap, b2[:], [b2], [VB[i]], q="gpsimd")
        units.append(unit)
    return units


def peer_prep(kb, C, I, layer, UT, VB):
    with kb.phase():
        ps = [kb.psv(i, 1, [128, 8, 128], BF16, f"pst{i}") for i in range(4)]
        for u in peer_prep_units(kb, C, I, layer, UT, VB, ps):
            u()


def peer_route(kb, C, I, layer, MOD, tiles, src_tile, NTd, RTd):
    with kb.phase():
        stage = Rot([kb.T([128, 2048], F32, f"stg{i}") for i in range(2)])
        w_q = kb.T([128, 8, 2048], BF16, "w_q")
        load_w_bf16(kb, w_q, I["peer_w_q"][layer], I["_peer_w_q"], stage, 1024, 2048)
        skT = kb.T([128, 16, 128], BF16, "skT")
        skb = kb.T([128, 128], BF16, "skb")
        pst = kb.psv(0, 1, [128, 8, 128], BF16, "pst")
        for hp in range(16):
            st = stage.next()
            kb.dma(st[:, 0:128], I["peer_subkeys"][layer][hp // 2, hp % 2], [I["_peer_subkeys"]], [st])
            kb.v("vector", "tensor_copy", [st], [skb], skb[:], st[:, 0:128])
            kb.tr(pst[:, 0, :], skb[:], C["ident_bf"][:], [skb, C["ident_bf"]], [pst])
            kb.v("vector", "tensor_copy", [pst], [skT], skT[:, hp, :], pst[:, 0, :])
        tmpm = Buf(stage.items[0].ap[:, 0:D], "tmpm", root=stage.items[0])
        G1 = [kb.T([128, D], F32, f"G1_{i}") for i in range(2)]
        SH = [kb.T([128, D], F32, f"SH_{i}") for i in range(2)]
        rows = sorted({1 if tt < 2 else 0 for tt in tiles})
        for r in rows:
            build_mod_tiles(kb, MOD.ap, MOD, I["norm2_g"][layer], I["_norm2_g"], r, 3, 4, G1[r], SH[r], tmpm)
        xt = kb.T([128, D], F32, "xt")
        junk = kb.T([128, D], BF16, "junk")
        ss = kb.T([128, 1], F32, "ss")
        rstd = kb.T([128, 1], F32, "rstd")
        nb = kb.T([128, D], BF16, "nb")
        nT = Rot([kb.T([128, 8, 128], BF16, f"nT{i}") for i in range(2)])
        qT_rot = Rot([kb.T([128, 16, 128], BF16, f"qT_sb{i}") for i in range(2)])
        s_rot = Rot([kb.T([128, 16, 128], F32, f"s_sb{i}") for i in range(2)])
        s2 = kb.T([128, 16, 128], F32, "s2")
        s2b = [Buf(None, f"s2b{i}") for i in range(16)]
        stop = kb.T([128, 16, 16], F32, "stop")
        stopa = [Buf(None, f"stopa{i}") for i in range(16)]
        stopb = [Buf(None, f"stopb{i}") for i in range(16)]
        topa = [Buf(None, f"topa{i}") for i in range(8)]
        topb = [Buf(None, f"topb{i}") for i in range(8)]
        c2b = [Buf(None, f"c2b{i}") for i in range(8)]
        idx = kb.T([128, 16, 16], U32, "idx")
        idxf = kb.T([128, 16, 16], F32, "idxf")
        cand = kb.T([128, 8, 256], F32, "cand")
        c2 = kb.T([128, 8, 256], F32, "c2")
        top = kb.T([128, 8, 16], F32, "top")
        pos = kb.T([128, 8, 16], U32, "pos")
        au = kb.T([128, 8, 16], U32, "au")
        bu = kb.T([128, 8, 16], U32, "bu")
        af = kb.T([128, 8, 16], F32, "af")
        bf = kb.T([128, 8, 16], F32, "bf")
        E_rot = Rot([kb.T([128, 8, 16, 16], F32, f"E{i}") for i in range(2)])
        sel = kb.T([128, 3, 128], F32, "sel")
        gs = kb.T([128, 8], F32, "gs")
        rt_sb = Rot([kb.T([128, 3, 128], F32, f"rt_sb{i}") for i in range(2)])
        psT = kb.psv(0, 1, [128, 8, 128], BF16, "psT", root=pst)
        ps_qT = [kb.psv(i, 1, [128, 4, 128], F32, f"ps_qT{i}") for i in range(4)]
        ps_qT[0] = kb.psv(0, 1, [128, 4, 128], F32, "ps_qT0", root=pst)
        ps_s = [kb.psv(4 + i, 1, [128, 4, 128], F32, f"ps_s{i}") for i in range(4)]
        ps_r = kb.psv(4, 1, [128, 3, 128], F32, "ps_r", root=ps_s[0])
        iota16 = C["iota_j"][:, 0:16]
        def front(tt):
            isc = 1 if tt < 2 else 0
            sap, sbuf_ = src_tile(tt)
            nTt = nT.next()
            norm_mod_T(kb, C, sap, sbuf_, G1[isc], SH[isc], xt, junk, ss, rstd, nb, psT, nTt)
            kb.dma(NTd[tt].ap, nTt[:], [nTt], [NTd[tt]], q="gpsimd")
            qT_sb = qT_rot.next()
            s_sb = s_rot.next()
            for hp in range(16):
                pq_ = ps_qT[hp // 4]
                for k in range(8):
                    kb.mm(pq_[:, hp % 4, :], w_q[:, k, hp * 128:(hp + 1) * 128], nTt[:, k, :], [w_q, nTt], [pq_],
                          start=(k == 0), stop=(k == 7))
            for g in range(4):
                kb.act(qT_sb[:, g * 4:(g + 1) * 4, :], ps_qT[g][:], AF.Copy, [ps_qT[g]], [qT_sb])
            for hp in range(16):
                kb.mm(ps_s[hp // 4][:, hp % 4, :], qT_sb[:, hp, :], skT[:, hp, :], [qT_sb, skT], [ps_s[hp // 4]])
            for g in range(4):
                kb.act(s_sb[:, g * 4:(g + 1) * 4, :], ps_s[g][:], AF.Copy, [ps_s[g]], [s_sb])
            return s_sb

        def chain(tt, s_sb):
            for hp in range(16):
                kb.v("vector", "max", [s_sb], [stopa[hp]], stop[:, hp, 0:8], s_sb[:, hp, :])
            for hp in range(16):
                kb.v("vector", "match_replace", [stopa[hp], s_sb], [s2b[hp]], s2[:, hp, :], stop[:, hp, 0:8], s_sb[:, hp, :], -1e30)
            for hp in range(16):
                kb.v("vector", "max", [s2b[hp]], [stopb[hp]], stop[:, hp, 8:16], s2[:, hp, :])
            for hp in range(16):
                kb.v("vector", "max_index", [stopa[hp], s_sb], [idx], idx[:, hp, 0:8], stop[:, hp, 0:8], s_sb[:, hp, :])
            for hp in range(16):
                kb.v("vector", "max_index", [stopb[hp], s_sb], [idx], idx[:, hp, 8:16], stop[:, hp, 8:16], s_sb[:, hp, :])
            kb.v("vector", "tensor_copy", [idx], [idxf], idxf[:], idx[:])
            st4 = stop[:].rearrange("p (h two) k -> p h two k", two=2)
            if4 = idxf[:].rearrange("p (h two) k -> p h two k", two=2)
            cand4 = cand[:].rearrange("p h (a b) -> p h a b", a=16)
            kb.v("vector", "tensor_tensor", stopa + stopb, [cand], cand4,
                 st4[:, :, 0, :].unsqueeze(3).to_broadcast([128, 8, 16, 16]),
                 st4[:, :, 1, :].unsqueeze(2).to_broadcast([128, 8, 16, 16]), ALU.add)
            for h in range(8):
                kb.v("vector", "max", [cand], [topa[h]], top[:, h, 0:8], cand[:, h, :])
            for h in range(8):
                kb.v("vector", "match_replace", [topa[h], cand], [c2b[h]], c2[:, h, :], top[:, h, 0:8], cand[:, h, :], -1e30)
            for h in range(8):
                kb.v("vector", "max", [c2b[h]], [topb[h]], top[:, h, 8:16], c2[:, h, :])
            for h in range(8):
                kb.v("vector", "max_index", [topa[h], cand], [pos], pos[:, h, 0:8], top[:, h, 0:8], cand[:, h, :])
            for h in range(8):
                kb.v("vector", "max_index", [topb[h], cand], [pos], pos[:, h, 8:16], top[:, h, 8:16], cand[:, h, :])
            kb.v("vector", "tensor_single_scalar", [pos], [au], au[:], pos[:], 4, ALU.logical_shift_right)
            kb.v("vector", "tensor_single_scalar", [pos], [bu], bu[:], pos[:], 15, ALU.bitwise_and)
            kb.v("vector", "tensor_copy", [au], [af], af[:], au[:])
            kb.v("vector", "tensor_copy", [bu], [bf], bf[:], bu[:])
            io4 = iota16.unsqueeze(1).unsqueeze(1).to_broadcast([128, 8, 16, 16])
            for which, sel_f in ((0, af), (1, bf)):
                E = E_rot.next()
                kb.v("vector", "tensor_tensor", [sel_f, C["iota_j"]], [E], E[:],
                     sel_f[:].unsqueeze(3).to_broadcast([128, 8, 16, 16]), io4, ALU.is_equal)
                kb.v("vector", "tensor_tensor", [E, idxf], [E], E[:], E[:],
                     if4[:, :, which, :].unsqueeze(2).to_broadcast([128, 8, 16, 16]), ALU.mult)
                kb.v("vector", "tensor_reduce", [E], [sel], sel[:, which, :].rearrange("p (h k) -> p h k", h=8),
                     E[:], AX.X, ALU.add)
            g3 = sel[:, 2, :].rearrange("p (h k) -> p h k", h=8)
            kb.v("vector", "tensor_tensor", topa + topb, [sel], g3, top[:], top[:, :, 0:1].to_broadcast([128, 8, 16]), ALU.subtract)
            kb.act(sel[:, 2, :], sel[:, 2, :], AF.Exp, [sel], [sel])
            kb.v("vector", "tensor_reduce", [sel], [gs], gs[:], g3, AX.X, ALU.add)
            kb.v("vector", "reciprocal", [gs], [gs], gs[:], gs[:])
            kb.v("vector", "tensor_tensor", [sel, gs], [sel], g3, g3, gs[:].unsqueeze(2).to_broadcast([128, 8, 16]), ALU.mult)
            for w_ in range(3):
                kb.tr(ps_r[:, w_, :], sel[:, w_, :], C["ident_f"][:], [sel, C["ident_f"]], [ps_r])
            rt = rt_sb.next()
            kb.act(rt[:], ps_r[:], AF.Copy, [ps_r], [rt])
            kb.dma(RTd[tt].ap, rt[:], [rt], [RTd[tt]], q="gpsimd")

        prev = None
        for tt in tiles:
            s_cur = front(tt)
            if prev is not None:
                chain(*prev)
            prev = (tt, s_cur)
        chain(*prev)


def peer_apply(kb, C, I, layer, MOD, groups, src_tile, NTd, RTd, UT, VB, epilogue):
    with kb.phase():
        QT_ = 32
        Gs = kb.T([128, 384, 128], BF16, "Gs")
        A = Rot([kb.T([128, QT_, 128], BF16, f"A{i}") for i in range(2)])
        B = Rot([kb.T([128, QT_, 128], BF16, f"B{i}") for i in range(2)])
        nTg_rot = Rot([kb.T([128, 8, 384], BF16, f"nTg{i}") for i in range(2)])
        rt = Rot([kb.T([128, 3, 128], F32, f"rt{i}") for i in range(6)])
        rb = Rot([kb.T([128, 3, 128], BF16, f"rb{i}") for i in range(6)])
        uT = Rot([kb.T([128, 8, 128], BF16, f"uT{i}") for i in range(4)])
        vv = Rot([kb.T([128, D], BF16, f"vv{i}") for i in range(4)])
        ga = Rot([kb.T([128, 384], F32, f"ga{i}") for i in range(2)])
        W = Rot([kb.T([128, 384], BF16, f"W{i}") for i in range(4)])
        ep = epilogue("alloc", kb)
        acc = [[kb.psv(2 * t + h, 1, [128, 512], F32, f"acc{t}{h}") for h in range(2)] for t in range(3)]
        ps_a = [kb.psv(6 + i, 1, [128, 384], F32, f"ps_a{i}") for i in range(2)]
        ps_G = [kb.psv(6 + i, 1, [128, 4, 128], F32, f"ps_G{i}", root=ps_a[i]) for i in range(2)]
        ps_rot = Rot([0, 1])
        iota_bf = C["iota_bf"]

        def load_group(tiles):
            nTg = nTg_rot.next()
            rs = []
            for gi, tt in enumerate(tiles):
                kb.dma(nTg[:, :, gi * 128:(gi + 1) * 128], NTd[tt].ap, [NTd[tt]], [nTg], q="gpsimd")
                r = rt.next(); rb_ = rb.next()
                kb.dma(r[:], RTd[tt].ap, [RTd[tt]], [r], q="gpsimd")
                kb.v("gpsimd", "tensor_copy", [r], [rb_], rb_[:], r[:])
                rs.append((r, rb_))
            return {"tiles": tiles, "nTg": nTg, "rs": rs}

        def gate_units(G):
            units = []
            for gi in range(len(G["tiles"])):
                r, rb_ = G["rs"][gi]
                for qt in range(128 // QT_):
                    def build(gi=gi, qt=qt, r=r, rb_=rb_):
                        ts_ = slice(qt * QT_, (qt + 1) * QT_)
                        a_ = A.next(); b_ = B.next()
                        kb.v("vector", "tensor_tensor", [rb_, iota_bf], [a_], a_[:],
                             iota_bf[:].unsqueeze(1).to_broadcast([128, QT_, 128]),
                             rb_[:, 0, ts_].unsqueeze(2).to_broadcast([128, QT_, 128]), ALU.is_equal)
                        kb.v("vector", "tensor_tensor", [a_, rb_], [a_], a_[:], a_[:],
                             rb_[:, 2, ts_].unsqueeze(2).to_broadcast([128, QT_, 128]), ALU.mult)
                        kb.v("vector", "tensor_tensor", [rb_, iota_bf], [b_], b_[:],
                             iota_bf[:].unsqueeze(1).to_broadcast([128, QT_, 128]),
                             rb_[:, 1, ts_].unsqueeze(2).to_broadcast([128, QT_, 128]), ALU.is_equal)
                        return a_, b_

                    def mmpart(ab, gi=gi, qt=qt):
                        a_, b_ = ab
                        for t4 in range(QT_ // 4):
                            pg = ps_G[ps_rot.next()]
                            for u in range(4):
                                t = t4 * 4 + u
                                kb.mm(pg[:, u, :], b_[:, t, :], a_[:, t, :], [a_, b_], [pg])
                            tok0 = gi * 128 + qt * QT_ + t4 * 4
                            kb.act(Gs[:, tok0:tok0 + 4, :], pg[:], AF.Copy, [pg], [Gs])
                    units.append((build, mmpart))
            return units

        Gstate = {0: load_group(groups[0])}
        LAG = 2
        for g in range(len(groups)):
            G = Gstate[g]
            tiles = G["tiles"]
            ng = len(tiles)
            ntok = ng * 128
            nTg = G["nTg"]
            if g + 1 < len(groups):
                Gstate[g + 1] = load_group(groups[g + 1])
            units = G.get("units") or gate_units(G)
            built = G.get("built0") or units[0][0]()
            for n in range(len(units)):
                nxt = units[n + 1][0]() if n + 1 < len(units) else None
                units[n][1](built)
                built = nxt

            def U_(i):
                u_ = uT.next(); v_ = vv.next()
                kb.dma(u_[:], UT[i].ap, [UT[i]], [u_])
                kb.dma(v_[:], VB[i].ap, [VB[i]], [v_])
                pa = ps_a[ps_rot.next()]
                for k in range(8):
                    kb.mm(pa[:, 0:ntok], u_[:, k, :], nTg[:, k, 0:ntok], [u_, nTg], [pa], start=(k == 0), stop=(k == 7))
                g_ = ga.next()
                kb.act(g_[:, 0:ntok], pa[:, 0:ntok], AF.Gelu, [pa], [g_])
                w_ = W.next()
                eng = "vector" if i % 2 == 0 else "gpsimd"
                kb.v(eng, "tensor_tensor", [g_, Gs], [w_], w_[:, 0:ntok], g_[:, 0:ntok], Gs[:, 0:ntok, i], ALU.mult)
                return (i, w_, v_)

            def V_(st):
                i, w_, v_ = st
                for gi in range(ng):
                    for h in range(2):
                        kb.mm(acc[gi][h][:], w_[:, gi * 128:(gi + 1) * 128], v_[:, h * 512:(h + 1) * 512], [w_, v_],
                              [acc[gi][h]], start=(i == 0), stop=(i == 127))

            pend = []
            for i in range(128):
                pend.append(U_(i))
                if len(pend) > LAG:
                    V_(pend.pop(0))
                if i == 110 and g + 1 < len(groups):
                    Gn = Gstate[g + 1]
                    Gn["units"] = gate_units(Gn)
                    Gn["built0"] = Gn["units"][0][0]()
            while pend:
                V_(pend.pop(0))
            for gi, tt in enumerate(tiles):
                epilogue("run", kb, tt, acc[gi], ep)


def make_epilogue(I, MOD, src_tile, dst_tile, final=False):
    def ep(mode, kb, tt=None, acc=None, b=None):
        if mode == "alloc":
            b = {"M5": [kb.T([128, D], F32, f"M5_{i}") for i in range(2)],
                 "hres": kb.T([128, D], F32, "hres"), "o": kb.T([128, D], F32, "o")}
            for r in range(2):
                load_bcast(kb, b["M5"][r], MOD.ap[r, 5 * D:6 * D], [MOD])
            if final:
                b["fg"] = kb.T([128, D], F32, "fg")
                load_bcast(kb, b["fg"], I["norm_f_g"], [I["_norm_f_g"]])
                b["junk"] = kb.T([128, D], BF16, "junkf")
                b["ss"] = kb.T([128, 1], F32, "ssf")
                b["rstd"] = kb.T([128, 1], F32, "rstdf")
            return b
        isc = 1 if tt < 2 else 0
        sap, sbuf_ = src_tile(tt)
        hres, o, M5 = b["hres"], b["o"], b["M5"][isc]
        kb.dma(hres[:], sap, [sbuf_], [hres])
        for h in range(2):
            hs = slice(h * 512, (h + 1) * 512)
            kb.v("vector", "tensor_tensor", [acc[h], M5], [o], o[:, hs], acc[h][:], M5[:, hs], ALU.mult)
        kb.v("gpsimd", "tensor_tensor", [o, hres], [o], o[:], o[:], hres[:], ALU.add)
        dap, dbuf = dst_tile(tt)
        if final:
            kb.act(b["junk"][:], o[:], AF.Square, [o], [b["junk"], b["ss"]], accum_out=b["ss"][:])
            rstd_of(kb, b["ss"], b["rstd"], D)
            kb.v("vector", "scalar_tensor_tensor", [o, b["rstd"], b["fg"]], [o], o[:], o[:], b["rstd"][:, 0:1],
                 b["fg"][:], ALU.mult, ALU.mult)
        kb.dma(dap, o[:], [o], [dbuf], q="gpsimd")
    return ep


def tile_groups(tiles, n=3):
    return [tiles[i:i + n] for i in range(0, len(tiles), n)]


INPUT_SPECS = [
    ("x", [SEQ, D]), ("c", [D]), ("ctx", [NCTX, D]), ("c_ctx", [D]),
    ("norm1_g", [2, D]), ("norm2_g", [2, D]), ("w_mod", [2, D, 6 * D]), ("b_mod", [2, 6 * D]),
    ("even_w_in", [1, D, 1952]), ("mla_q_norm", [1, 256]), ("mla_kv_norm", [1, 128]),
    ("mla_w_uq", [1, 256, 768]), ("mla_w_ukv", [1, 128, 1024]), ("conv_w", [1, 3, 512]),
    ("even_w_out", [1, D, D]), ("odd_w_in", [1, D, 3088]), ("mlstm_gate_b", [1, 16]),
    ("mlstm_head_g", [1, D]), ("odd_w_out", [1, D, D]), ("peer_w_q", [2, D, 2048]),
    ("peer_subkeys", [2, 8, 2, 128, 128]), ("peer_u", [2, 16384, D]), ("peer_v", [2, 16384, D]),
    ("norm_f_g", [D]), ("rope", [SEQ, 32]),
]


def build_program(stop_after=None, dbg=()):
    nc = bass.Bass("TRN2", target_bir_lowering=False)
    kb = KB(nc, dbg)
    kb.stop_after = stop_after
    I = {}
    for name, shape in INPUT_SPECS:
        ap = nc.dram_tensor(name, shape, F32, kind="ExternalInput").ap()
        I[name] = ap
        I["_" + name] = Buf(ap, name)
    out = nc.dram_tensor("y", [SEQ, D], F32, kind="ExternalOutput").ap()
    kb.eps_t = kb.T([128, 1], F32, "eps")
    kb.v("gpsimd", "memset", [], [kb.eps_t], kb.eps_t[:], EPS)
    kb.one_t = kb.T([128, 1], F32, "one")
    kb.v("gpsimd", "memset", [], [kb.one_t], kb.one_t[:], 1.0)
    C = make_consts(kb)
    MOD = [Buf(kb.dram(f"MOD{l}", [2, 6 * D], F32), f"MOD{l}") for l in range(2)]
    H0 = kb.dram_tiles("H0", NT_ALL, [128, D], F32)

    def src0(tt):
        if tt < 2:
            return I["ctx"][tt * 128:(tt + 1) * 128, :], I["_ctx"]
        return I["x"][(tt - 2) * 128:(tt - 1) * 128, :], I["_x"]

    phase_mod(kb, C, I, 0, MOD[0])
    if stop_after == "mod0":
        kb.finish()
        return nc, kb
    layer_even(kb, C, I, 0, MOD[0], src0, H0)
    if stop_after in ("even", "E1"):
        kb.finish()
        return nc, kb
    UT = kb.dram_tiles("UT", 128, [128, 8, 128], BF16)
    VB = kb.dram_tiles("VB", 128, [128, D], BF16)
    NTd = kb.dram_tiles("NTd", NT_ALL, [128, 8, 128], BF16)
    RTd = kb.dram_tiles("RTd", NT_ALL, [128, 3, 128], F32)
    H1 = kb.dram_tiles("H1", NT_ALL, [128, D], F32)
    srcH0 = lambda tt: (H0[tt].ap, H0[tt])
    dstH1 = lambda tt: (H1[tt].ap, H1[tt])
    tiles0 = DBG_PTILES or list(range(NT_ALL))
    peer_prep(kb, C, I, 0, UT, VB)
    peer_route(kb, C, I, 0, MOD[0], tiles0, srcH0, NTd, RTd)
    if stop_after == "route0":
        kb.finish()
        return nc, kb
    peer_apply(kb, C, I, 0, MOD[0], tile_groups(tiles0), srcH0, NTd, RTd, UT, VB,
               make_epilogue(I, MOD[0], srcH0, dstH1))
    if stop_after == "peer0":
        kb.finish()
        return nc, kb
    phase_mod(kb, C, I, 1, MOD[1])
    H2 = kb.dram_tiles("H2", NT_ALL, [128, D], F32)
    srcH1 = lambda tt: (H1[tt].ap, H1[tt])
    layer_odd(kb, C, I, 1, MOD[1], srcH1, H2)
    if stop_after == "odd":
        kb.finish()
        return nc, kb
    srcH2 = lambda tt: (H2[tt].ap, H2[tt])
    outb = [Buf(out[(tt - 2) * 128:(tt - 1) * 128, :], f"y{tt}") for tt in range(NT_ALL)]
    dstY = lambda tt: (outb[tt].ap, outb[tt])
    tiles1 = list(range(2, NT_ALL))
    peer_prep(kb, C, I, 1, UT, VB)
    peer_route(kb, C, I, 1, MOD[1], tiles1, srcH2, NTd, RTd)
    peer_apply(kb, C, I, 1, MOD[1], tile_groups(tiles1), srcH2, NTd, RTd, UT, VB,
               make_epilogue(I, MOD[1], srcH2, dstY, final=True))
    kb.finish()
    return nc, kb


def rope_table():
    n_freq = 8
    inv = (10000.0 ** (-np.arange(n_freq, dtype=np.float32) / n_freq)).astype(np.float32)
    t = np.arange(SEQ)
    row = (t // 64).astype(np.float32)
    col = (t % 64).astype(np.float32)
    ang = np.concatenate([row[:, None] * inv, col[:, None] * inv], axis=-1).astype(np.float32)
    return np.concatenate([np.cos(ang), np.sin(ang)], axis=-1).astype(np.float32)


def make_in_maps(inputs, cores):
    shared = {k: np.ascontiguousarray(np.asarray(v, dtype=np.float32)) for k, v in inputs.items()
              if k not in ("x", "c", "ctx")}
    shared["rope"] = rope_table()
    maps = []
    for b in cores:
        m = dict(shared)
        m["x"] = np.ascontiguousarray(inputs["x"][b])
        m["c"] = np.ascontiguousarray(inputs["c"][b])
        m["ctx"] = np.ascontiguousarray(inputs["ctx"][b])
        maps.append(m)
    return maps


def kernel(**inputs):
    nc, kb = build_program()
    maps = make_in_maps(inputs, list(range(8)))
    res = run_bass_kernel_spmd(nc, maps, core_ids=list(range(8)))
    return np.stack([r["y"] for r in res.results], axis=0).astype(np.float32)
```

```python
import math
import numpy as np
from contextlib import ExitStack, contextmanager
import concourse.bass as bass
import concourse.mybir as mybir
from concourse.bass_utils import run_bass_kernel_spmd

F32 = mybir.dt.float32
BF16 = mybir.dt.bfloat16
U32 = mybir.dt.uint32
ALU = mybir.AluOpType
AF = mybir.ActivationFunctionType
AX = mybir.AxisListType

D = 1024
SEQ = 4096
NCTX = 256
NT_ALL = 34
EPS = 1e-6
MLA_SCALE = 96 ** -0.5
DBG_TILES = None
DBG_PTILES = None
SKIP = set()
SB_WORDS = 51200


class Buf:
    __slots__ = ("ap", "last_write", "reads", "name", "psum", "root")

    def __init__(self, ap, name="", psum=False, root=None):
        self.root = root.root if root is not None else self
        self.ap = ap
        self.last_write = None
        self.reads = []
        self.name = name
        self.psum = psum

    def __getitem__(self, k):
        return self.ap[k]


class Op:
    __slots__ = ("eng", "fn", "deps", "flag", "is_dma", "sem", "val", "slot")

    def __init__(self, eng, fn, is_dma):
        self.eng = eng
        self.fn = fn
        self.deps = []
        self.flag = False
        self.is_dma = is_dma
        self.sem = None
        self.val = None
        self.slot = None


ENGS = ["tensor", "vector", "scalar", "gpsimd", "sync"]
N_CSEM = 3
N_DSEM = {"sync": 44, "scalar": 4, "gpsimd": 36}


class Prog:
    def __init__(self, nc):
        self.nc = nc
        self.q = {e: [] for e in ENGS}
        self.last_real = {e: None for e in ENGS}
        self.dma_hist = {e: [] for e in N_DSEM}

    def add(self, eng, fn, reads=(), writes=(), dma=False):
        op = Op(eng, fn, dma)
        deps = []
        reads = [b.root for b in reads]
        writes = [b.root for b in writes]
        writes = list(writes) + [b for b in reads if b.psum and b not in writes]
        for b in reads:
            if b.last_write is not None:
                deps.append(b.last_write)
        for b in writes:
            if b.last_write is not None:
                deps.append(b.last_write)
            deps.extend(b.reads)
        seen = set()
        for d in deps:
            if id(d) in seen:
                continue
            seen.add(id(d))
            if d.eng == eng and not d.is_dma and not dma:
                if eng == "tensor":
                    continue
                if not any(b.last_write is d for b in reads):
                    continue
            op.deps.append(d)
            d.flag = True
        for b in reads:
            if not b.psum:
                b.reads.append(op)
        for b in writes:
            b.last_write = op
            b.reads = []
        if dma:
            h = self.dma_hist[eng]
            n = N_DSEM[eng]
            k = len(h)
            op.slot = k % n
            op.val = 16 * (k // n + 1)
            if k >= n:
                op.deps.append(h[k - n])
            h.append(op)
        elif fn is not None:
            self.last_real[eng] = op
        self.q[eng].append(op)
        return op

    def barrier(self):
        deps = []
        for e in ENGS:
            d = self.last_real[e]
            if d is not None:
                d.flag = True
                deps.append(d)
        for e, h in self.dma_hist.items():
            deps.extend(h[-N_DSEM[e]:])
        for e in ENGS:
            op = Op(e, None, False)
            op.deps = list(deps)
            self.q[e].append(op)

    def emit(self, stack):
        nc = self.nc
        csems = {e: [stack.enter_context(nc.semaphore(f"c_{e}_{i}")) for i in range(N_CSEM)]
                 for e in ["tensor", "vector", "scalar", "gpsimd"]}
        dsems = {e: [stack.enter_context(nc.semaphore(f"d_{e}_{i}")) for i in range(n)]
                 for e, n in N_DSEM.items()}
        for e in ENGS:
            nflag = 0
            for op in self.q[e]:
                if op.is_dma:
                    op.sem = dsems[e][op.slot]
                elif op.flag:
                    op.sem = csems[e][nflag % N_CSEM]
                    op.val = nflag // N_CSEM + 1
                    nflag += 1
        stats = {}

        def run(e):
            def body(eng):
                waited = {}
                nw = 0
                for op in self.q[e]:
                    for d in op.deps:
                        key = id(d.sem)
                        if waited.get(key, 0) >= d.val:
                            continue
                        waited[key] = d.val
                        eng.wait_ge(d.sem, d.val)
                        nw += 1
                    if op.fn is None:
                        continue
                    ins = op.fn(eng)
                    if op.is_dma:
                        ins.then_inc(op.sem, 16)
                    elif op.flag:
                        ins.then_inc(op.sem, 1)
                stats[e] = (len(self.q[e]), nw)
            return body

        with nc.Block() as block:
            block.sync(run("sync"))
            block.tensor(run("tensor"))
            block.vector(run("vector"))
            block.scalar(run("scalar"))
            block.gpsimd(run("gpsimd"))
        self.stats = stats


class Rot:
    def __init__(self, items):
        self.items = items
        self.i = 0

    def next(self):
        it = self.items[self.i % len(self.items)]
        self.i += 1
        return it


def _size(dt):
    return {F32: 4, BF16: 2, U32: 4}[dt]


class KB:
    def __init__(self, nc, dbg=()):
        self.nc = nc
        self.P = Prog(nc)
        self.stack = ExitStack()
        self.SB = self.stack.enter_context(nc.sbuf_tensor("SB", [128, SB_WORDS], F32))
        self.PS = self.stack.enter_context(nc.psum_tensor("PSA", [128, 8 * 512], F32))
        self.off = 0
        self.dbg = set(dbg)
        self.ndram = 0

    def T(self, shape, dt=F32, name=""):
        p = shape[0]
        n = int(np.prod(shape[1:]))
        words = (n * _size(dt) + 3) // 4
        assert self.off + words <= SB_WORDS, f"SBUF overflow allocating {name}{shape}: off={self.off} words={words}"
        ap = self.SB[0:p, self.off:self.off + words]
        self.off += words
        if dt != F32:
            ap = ap.bitcast(dt)
        ap = ap[:, 0:n]
        if len(shape) > 2:
            names = " ".join(f"d{i}" for i in range(len(shape) - 1))
            kw = {f"d{i}": shape[i + 1] for i in range(len(shape) - 2)}
            ap = ap.rearrange(f"p ({names}) -> p {names}", **kw)
        return Buf(ap, name)

    def psv(self, b0, nb, shape, dt=F32, name="", root=None):
        p = shape[0]
        n = int(np.prod(shape[1:]))
        ap = self.PS[0:p, b0 * 512:(b0 + nb) * 512]
        if dt != F32:
            ap = ap.bitcast(dt)
        ap = ap[:, 0:n]
        if len(shape) > 2:
            names = " ".join(f"d{i}" for i in range(len(shape) - 1))
            kw = {f"d{i}": shape[i + 1] for i in range(len(shape) - 2)}
            ap = ap.rearrange(f"p ({names}) -> p {names}", **kw)
        return Buf(ap, name, psum=True, root=root)

    def dram(self, name, shape, dt=F32, kind=None):
        if kind is None:
            kind = "ExternalOutput" if name in self.dbg else "Internal"
        t = self.nc.dram_tensor(name, list(shape), dt, kind=kind)
        return t.ap()

    def dram_tiles(self, name, n, shape, dt=F32):
        ap = self.dram(name, [n] + list(shape), dt)
        return [Buf(ap[i], f"{name}{i}") for i in range(n)]

    @contextmanager
    def phase(self):
        m = self.off
        yield
        self.P.barrier()
        self.off = m

    def dma(self, out, in_, reads, writes, q="sync", **kw):
        return self.P.add(q, lambda e: e.dma_start(out=out, in_=in_, **kw), reads, writes, dma=True)

    def mm(self, out, lhsT, rhs, reads, writes, start=True, stop=True):
        return self.P.add("tensor", lambda e: e.matmul(out, lhsT, rhs, start=start, stop=stop), reads, writes)

    def tr(self, out, in_, ident, reads, writes):
        return self.P.add("tensor", lambda e: e.transpose(out, in_, ident), reads, writes)

    def act(self, out, in_, func, reads, writes, **kw):
        return self.P.add("scalar", lambda e: e.activation(out=out, in_=in_, func=func, **kw), reads, writes)

    def v(self, eng, method, reads, writes, *a, **kw):
        return self.P.add(eng, lambda e: getattr(e, method)(*a, **kw), reads, writes)

    def finish(self):
        self.P.barrier()
        self.P.emit(self.stack)
        self.stack.close()


def make_consts(kb):
    c = {}
    io = kb.T([128, 128], F32, "iota")
    kb.P.add("gpsimd", lambda e: e.iota(io[:], [[1, 128]], base=0, channel_multiplier=-1,
                                        allow_small_or_imprecise_dtypes=True), [], [io])
    c["jmp"] = io
    c["ident_bf"] = kb.T([128, 128], BF16, "ident_bf")
    kb.v("vector", "tensor_single_scalar", [io], [c["ident_bf"]], c["ident_bf"][:], io[:], 0.0, ALU.is_equal)
    ij = kb.T([128, 128], F32, "iota_j")
    kb.P.add("gpsimd", lambda e: e.iota(ij[:], [[1, 128]], base=0, channel_multiplier=0,
                                        allow_small_or_imprecise_dtypes=True), [], [ij])
    c["iota_j"] = ij
    c["iota_bf"] = kb.T([128, 128], BF16, "iota_bf")
    kb.v("vector", "tensor_copy", [ij], [c["iota_bf"]], c["iota_bf"][:], ij[:])
    c["ident_f"] = kb.T([128, 128], F32, "ident_f")
    kb.v("vector", "tensor_single_scalar", [io], [c["ident_f"]], c["ident_f"][:], io[:], 0.0, ALU.is_equal)
    return c


def load_bcast(kb, dst, src_ap, srcbufs, q="sync"):
    p = dst.ap.shape[0]
    return kb.dma(dst[:], src_ap.partition_broadcast(p), srcbufs, [dst], q=q)


def load_w_bf16(kb, dst, w_ap, wbuf, stage_rot, K, N, c0=0, c1=None, eng_rot=None):
    c1 = N if c1 is None else c1
    kc = K // 128
    wv = w_ap.rearrange("(k p) n -> p k n", p=128)
    maxcols = stage_rot.items[0].ap.shape[1]
    i = 0
    for k in range(kc):
        for cs in range(c0, c1, maxcols):
            ce = min(c1, cs + maxcols)
            st = stage_rot.next()
            kb.dma(st[:, 0:ce - cs], wv[:, k, cs:ce], [wbuf], [st], q="sync" if i % 2 == 0 else "gpsimd")
            eng = ["gpsimd", "vector"][i % 2]
            kb.v(eng, "tensor_copy", [st], [dst], dst[:, k, cs - c0:ce - c0], st[:, 0:ce - cs])
            i += 1


def rstd_of(kb, ss, rstd, n):
    kb.act(rstd[:], ss[:], AF.Ln, [ss], [rstd], scale=1.0 / n, bias=kb.eps_t[:, 0:1])
    kb.act(rstd[:], rstd[:], AF.Exp, [rstd], [rstd], scale=-0.5)


def build_mod_tiles(kb, mod_ap, modbuf, g_ap, gbuf, row, j_shift, j_scale, G1, SH, tmp):
    load_bcast(kb, tmp, mod_ap[row, j_scale * D:(j_scale + 1) * D], [modbuf])
    load_bcast(kb, G1, g_ap, [gbuf], q="gpsimd")
    kb.v("vector", "scalar_tensor_tensor", [tmp, G1], [G1], G1[:], tmp[:], 1.0, G1[:], ALU.add, ALU.mult)
    load_bcast(kb, SH, mod_ap[row, j_shift * D:(j_shift + 1) * D], [modbuf])


def norm_mod_T(kb, C, src_ap, srcbuf, G1, SH, xt, junk, ss, rstd, nb, psT, nT):
    kb.dma(xt[:], src_ap, [srcbuf], [xt])
    kb.act(junk[:], xt[:], AF.Square, [xt], [junk, ss], accum_out=ss[:])
    rstd_of(kb, ss, rstd, D)
    kb.v("vector", "scalar_tensor_tensor", [xt, rstd, G1], [xt], xt[:], xt[:], rstd[:, 0:1], G1[:], ALU.mult, ALU.mult)
    kb.v("gpsimd", "tensor_tensor", [xt, SH], [nb], nb[:], xt[:], SH[:], ALU.add)
    for k in range(8):
        kb.tr(psT[:, k, :], nb[:, k * 128:(k + 1) * 128], C["ident_bf"][:], [nb, C["ident_bf"]], [psT])
    kb.act(nT[:], psT[:], AF.Copy, [psT], [nT])


def phase_mod(kb, C, I, layer, MOD):
    with kb.phase():
        raw = kb.T([128, 2, 8], F32, "craw")
        kb.dma(raw[:, 0, :], I["c"].rearrange("(p k) -> p k", k=8), [I["_c"]], [raw])
        kb.dma(raw[:, 1, :], I["c_ctx"].rearrange("(p k) -> p k", k=8), [I["_c_ctx"]], [raw])
        sc = kb.T([128, 8, 2], F32, "csilu")
        kb.act(sc[:].rearrange("p k m -> p m k"), raw[:], AF.Silu, [raw], [sc])
        bm = kb.T([2, 6144], F32, "bm")
        load_bcast(kb, bm, I["b_mod"][layer], [I["_b_mod"]], q="gpsimd")
        res = kb.T([2, 6144], F32, "modres")
        wrot = Rot([kb.T([128, 8, 512], F32, f"wm{i}") for i in range(2)])
        prot = Rot([kb.psv(i, 1, [128, 512], F32, f"psm{i}") for i in range(2)])
        wv = I["w_mod"][layer].rearrange("(p k) n -> p k n", k=8)
        for ng in range(12):
            wm = wrot.next()
            ps = prot.next()
            kb.dma(wm[:], wv[:, :, ng * 512:(ng + 1) * 512], [I["_w_mod"]], [wm], q="sync" if ng % 2 == 0 else "gpsimd")
            for k in range(8):
                kb.mm(ps[0:2, :], sc[:, k, :], wm[:, k, :], [sc, wm], [ps], start=(k == 0), stop=(k == 7))
            kb.v("vector", "tensor_tensor", [ps, bm], [res], res[:, ng * 512:(ng + 1) * 512], ps[0:2, :],
                 bm[:, ng * 512:(ng + 1) * 512], ALU.add)
        kb.dma(MOD.ap, res[:], [res], [MOD])


ZCOLS = 4356


def zcol(tt):
    return 1 + tt * 128 if tt < 2 else 259 + (tt - 2) * 128


def layer_even(kb, C, I, layer, MOD, src_tile, H, need_ctx=True):
    j = 0
    P = kb.P
    PQ = kb.dram_tiles("PQ", NT_ALL, [128, 256], F32)
    ZT = kb.dram("ZT", [512, ZCOLS], F32)
    BT = kb.dram("BT", [512, ZCOLS], F32)
    ZTb = [Buf(None, f"zt{t}") for t in range(NT_ALL)]
    BTb = [Buf(None, f"bt{t}") for t in range(NT_ALL)]
    ZPAD = Buf(None, "ztpad")
    ZTv = ZT.rearrange("(c p) t -> p c t", p=128)
    BTv = BT.rearrange("(c p) t -> p c t", p=128)
    with kb.phase():
        KT = kb.T([128, 8, NT_ALL * 128], BF16, "KT_all")
        VP = kb.T([128, NT_ALL, 8, 65], BF16, "VP_all")
        kmax = kb.T([128, 8], F32, "kmax")
        kb.v("gpsimd", "memset", [], [VP], VP[:], 1.0)
        kb.v("gpsimd", "memset", [], [kmax], kmax[:], 0.0)
        rope = I["rope"]
        with kb.phase():
            stage = Rot([kb.T([128, 1024], F32, f"stg{i}") for i in range(2)])
            w_in = kb.T([128, 8, 1952], BF16, "w_in")
            load_w_bf16(kb, w_in, I["even_w_in"][j], I["_even_w_in"], stage, 1024, 1952)
            w_ukv = kb.T([128, 1, 1024], BF16, "w_ukv")
            load_w_bf16(kb, w_ukv, I["mla_w_ukv"][j], I["_mla_w_ukv"], stage, 128, 1024)
            kvg = kb.T([128, 128], F32, "kvg")
            load_bcast(kb, kvg, I["mla_kv_norm"][j], [I["_mla_kv_norm"]])
            tmpm = kb.T([128, D], F32, "tmpm")
            G1 = [kb.T([128, D], F32, f"G1_{i}") for i in range(2)]
            SH = [kb.T([128, D], F32, f"SH_{i}") for i in range(2)]
            for r in range(2):
                build_mod_tiles(kb, MOD.ap, MOD, I["norm1_g"][layer], I["_norm1_g"], r, 0, 1, G1[r], SH[r], tmpm)
            zero = kb.T([128, 4, 1], F32, "zero")
            kb.v("gpsimd", "memset", [], [zero], zero[:], 0.0)
            for col in (0, 257, 258, 4355):
                kb.dma(ZTv[:, :, col:col + 1], zero[:], [zero], [ZPAD], q="gpsimd", allow_slow_non_contiguous=True)
            xt = kb.T([128, D], F32, "xt")
            junk = kb.T([128, D], BF16, "junk")
            ss = kb.T([128, 1], F32, "ss")
            rstd = kb.T([128, 1], F32, "rstd")
            ss2 = kb.T([128, 1], F32, "ss2")
            rstd2 = kb.T([128, 1], F32, "rstd2")
            nb = kb.T([128, D], BF16, "nb")
            nT = kb.T([128, 8, 128], BF16, "nT")
            pm = Rot([kb.T([128, 416], F32, f"pm{i}") for i in range(2)])
            latn = kb.T([128, 128], BF16, "latn")
            klT = kb.T([128, 128], BF16, "klT")
            kext = Rot([kb.T([128, 8, 97], BF16, f"kext{i}") for i in range(2)])
            for kx in kext.items:
                kb.v("gpsimd", "memset", [], [kx], kx[:], 1.0)
            krot = kb.T([128, 32], F32, "krot")
            rtmp = kb.T([128, 4, 16], F32, "rtmp")
            cs = kb.T([128, 32], F32, "cs")
            ksqt = kb.T([128, 8, 96], F32, "ksqt")
            ksq = kb.T([128, 8], F32, "ksq")
            c_sb = kb.T([128, 4, 128], F32, "c_sb")
            z_sb = Rot([kb.T([128, 4, 128], F32, f"z_sb{i}") for i in range(2)])
            b_sb = Rot([kb.T([128, 4, 128], F32, f"b_sb{i}") for i in range(2)])
            psT = kb.psv(0, 1, [128, 8, 128], BF16, "psT")
            ps_mla = kb.psv(1, 1, [128, 416], F32, "ps_mla")
            ps_lat = kb.psv(2, 1, [128, 8, 128], BF16, "ps_lat")
            ps_kv = kb.psv(3, 2, [128, 8, 128], F32, "ps_kv")
            ps_conv = kb.psv(5, 3, [128, 12, 128], F32, "ps_conv")
            for tt in (DBG_TILES or range(NT_ALL)):
                isc = 1 if tt < 2 else 0
                sap, sbuf_ = src_tile(tt)
                norm_mod_T(kb, C, sap, sbuf_, G1[isc], SH[isc], xt, junk, ss, rstd, nb, psT, nT)
                for k in range(8):
                    kb.mm(ps_mla[:], nT[:, k, :], w_in[:, k, 0:416], [nT, w_in], [ps_mla], start=(k == 0), stop=(k == 7))
                pmt = pm.next()
                kb.act(pmt[:], ps_mla[:], AF.Copy, [ps_mla], [pmt])
                kb.dma(PQ[tt].ap, pmt[:, 0:256], [pmt], [PQ[tt]], q="gpsimd")
                if "kv" in SKIP:
                    continue
                kb.act(junk[:, 0:128], pmt[:, 256:384], AF.Square, [pmt], [junk, ss2], accum_out=ss2[:])
                rstd_of(kb, ss2, rstd2, 128)
                kb.v("vector", "scalar_tensor_tensor", [pmt, rstd2, kvg], [latn], latn[:], pmt[:, 256:384],
                     rstd2[:, 0:1], kvg[:], ALU.mult, ALU.mult)
                kb.tr(ps_lat[:, 0, :], latn[:], C["ident_bf"][:], [latn, C["ident_bf"]], [ps_lat])
                kb.v("vector", "tensor_copy", [ps_lat], [klT], klT[:], ps_lat[:, 0, :])
                if "kv2" in SKIP:
                    continue
                kb.mm(ps_kv[:, 0:4, :], klT[:], w_ukv[:, 0, 0:512], [klT, w_ukv], [ps_kv])
                kb.mm(ps_kv[:, 4:8, :], klT[:], w_ukv[:, 0, 512:1024], [klT, w_ukv], [ps_kv])
                if "kv3" in SKIP:
                    continue
                kx = kext.next()
                for hb in range(2):
                    hs = slice(hb * 4, hb * 4 + 4)
                    kb.act(kx[:, hs, 0:64], ps_kv[:, hs, 0:64], AF.Copy, [ps_kv], [kx], scale=MLA_SCALE)
                    kb.v("vector", "tensor_copy", [ps_kv], [VP], VP[:, tt, hs, 0:64], ps_kv[:, hs, 64:128])
                if "rope" in SKIP:
                    continue
                if tt >= 2:
                    kb.dma(cs[:], rope[(tt - 2) * 128:(tt - 1) * 128, :], [I["_rope"]], [cs], q="gpsimd")
                    x1 = pmt[:, 384:400]
                    x2 = pmt[:, 400:416]
                    kb.v("gpsimd", "tensor_tensor", [pmt, cs], [rtmp], rtmp[:, 0, :], x1, cs[:, 0:16], ALU.mult)
                    kb.v("gpsimd", "tensor_tensor", [pmt, cs], [rtmp], rtmp[:, 1, :], x2, cs[:, 16:32], ALU.mult)
                    kb.v("gpsimd", "tensor_tensor", [pmt, cs], [rtmp], rtmp[:, 2, :], x1, cs[:, 16:32], ALU.mult)
                    kb.v("gpsimd", "tensor_tensor", [pmt, cs], [rtmp], rtmp[:, 3, :], x2, cs[:, 0:16], ALU.mult)
                    kb.v("vector", "tensor_tensor", [rtmp], [krot], krot[:, 0:16], rtmp[:, 0, :], rtmp[:, 1, :], ALU.subtract)
                    kb.v("vector", "tensor_tensor", [rtmp], [krot], krot[:, 16:32], rtmp[:, 2, :], rtmp[:, 3, :], ALU.add)
                else:
                    kb.v("vector", "tensor_copy", [pmt], [krot], krot[:], pmt[:, 384:416])
                kb.act(kx[:, :, 64:96], krot[:].unsqueeze(1).to_broadcast([128, 8, 32]), AF.Copy, [krot], [kx],
                       scale=MLA_SCALE)
                if "ksq" in SKIP:
                    continue
                kb.v("vector", "tensor_tensor", [kx], [ksqt], ksqt[:], kx[:, :, 0:96], kx[:, :, 0:96], ALU.mult)
                kb.v("vector", "tensor_reduce", [ksqt], [ksq], ksq[:], ksqt[:], AX.X, ALU.add)
                kb.v("vector", "tensor_tensor", [ksq, kmax], [kmax], kmax[:], kmax[:], ksq[:], ALU.max)
                for h in range(8):
                    kb.tr(ps_lat[0:97, h, :], kx[:, h, :], C["ident_bf"][:], [kx, C["ident_bf"]], [ps_lat])
                kb.act(KT[0:97, :, tt * 128:(tt + 1) * 128], ps_lat[0:97, :, :], AF.Copy, [ps_lat], [KT])
                if "conv" in SKIP:
                    continue
                for fc in range(12):
                    for k in range(8):
                        kb.mm(ps_conv[:, fc, :], w_in[:, k, 416 + fc * 128:416 + (fc + 1) * 128], nT[:, k, :],
                              [w_in, nT], [ps_conv], start=(k == 0), stop=(k == 7))
                kb.act(c_sb[:], ps_conv[:, 4:8, :], AF.Copy, [ps_conv], [c_sb])
                zt = z_sb.next()
                bt = b_sb.next()
                kb.v("vector", "tensor_tensor", [ps_conv, c_sb], [zt], zt[:], ps_conv[:, 8:12, :], c_sb[:], ALU.mult)
                kb.act(bt[:], ps_conv[:, 0:4, :], AF.Copy, [ps_conv], [bt])
                c0 = zcol(tt)
                kb.dma(ZTv[:, :, c0:c0 + 128], zt[:], [zt], [ZTb[tt]], q="gpsimd")
                kb.dma(BTv[:, :, c0:c0 + 128], bt[:], [bt], [BTb[tt]], q="gpsimd")
        if kb.stop_after == "E1":
            return
        with kb.phase():
            stage = Rot([kb.T([128, 1024], F32, f"stg{i}") for i in range(2)])
            w_uq = kb.T([128, 2, 768], BF16, "w_uq")
            load_w_bf16(kb, w_uq, I["mla_w_uq"][j], I["_mla_w_uq"], stage, 256, 768)
            w_out = kb.T([128, 8, 1024], BF16, "w_out")
            load_w_bf16(kb, w_out, I["even_w_out"][j], I["_even_w_out"], stage, 1024, 1024)
            qg = kb.T([128, 256], F32, "qg")
            load_bcast(kb, qg, I["mla_q_norm"][j], [I["_mla_q_norm"]])
            M2 = [kb.T([128, D], F32, f"M2_{i}") for i in range(2)]
            for r in range(2):
                load_bcast(kb, M2[r], MOD.ap[r, 2 * D:3 * D], [MOD])
            cw = kb.T([128, 3, 4], F32, "cw")
            for w_ in range(3):
                for c_ in range(4):
                    kb.dma(cw[:, w_, c_:c_ + 1], I["conv_w"][j][w_, c_ * 128:(c_ + 1) * 128].unsqueeze(1),
                           [I["_conv_w"]], [cw], q="gpsimd", allow_slow_non_contiguous=True)
            psx = kb.psv(6, 1, [128, 128], F32, "psx")
            kmr = kb.T([128, 1], F32, "kmr")
            kb.v("vector", "tensor_reduce", [kmax], [kmr], kmr[:], kmax[:], AX.X, ALU.max)
            kmb = kb.T([128, 128], F32, "kmb")
            kb.v("vector", "tensor_copy", [kmr], [kmb], kmb[:], kmr[:, 0:1].to_broadcast([128, 128]))
            kb.tr(psx[:], kmb[:], C["ident_f"][:], [kmb, C["ident_f"]], [psx])
            ksm = kb.T([128, 1], F32, "ksm")
            kb.v("vector", "tensor_reduce", [psx], [ksm], ksm[:], psx[:], AX.X, ALU.max)
            pq = kb.T([128, 256], F32, "pq")
            junk = kb.T([128, 768], BF16, "junk")
            ss = kb.T([128, 1], F32, "ss")
            rstd = kb.T([128, 1], F32, "rstd")
            qn = kb.T([128, 256], BF16, "qn")
            qlT = kb.T([128, 2, 128], BF16, "qlT")
            q_sb = kb.T([128, 8, 96], F32, "q_sb")
            cs = kb.T([128, 32], F32, "cs")
            rt = kb.T([128, 4, 8, 16], F32, "rt")
            qsqt = kb.T([128, 8, 96], F32, "qsqt")
            qsq = kb.T([128, 8], F32, "qsq")
            qext = kb.T([128, 8, 97], BF16, "qext")
            QT = kb.T([128, 8, 512], BF16, "QT")
            PT = Rot([kb.T([128, 512], BF16, f"PT{i}") for i in range(3)])
            rec = kb.T([128, 4], F32, "rec")
            mix_tok = [kb.T([128, 512], BF16, f"mix{i}") for i in range(4)]
            mixT = kb.T([128, 8, 512], BF16, "mixT")
            zw = kb.T([128, 4, 514], F32, "zw")
            bw = kb.T([128, 4, 512], F32, "bw")
            yc_ = kb.T([128, 512], F32, "yconv")
            hres = stage.items[0]
            ytmp = stage.items[1]
            ps_S = Rot([kb.psv(i, 1, [128, 512], F32, f"psS{i}") for i in range(2)])
            ps_O = [kb.psv(2 + i, 1, [128, 65], F32, f"psO{i}") for i in range(4)]
            ps_q = kb.psv(2, 2, [128, 768], F32, "ps_q")
            ps_t = kb.psv(6, 1, [128, 8, 128], BF16, "ps_t")
            ps_y = kb.psv(6, 2, [128, 1024], F32, "ps_y")
            bank = {i: None for i in range(8)}
            blocks = []
            if need_ctx:
                blocks.append(([0, 1], [0, 1]))
            for b in range(8):
                blocks.append(([2 + 4 * b + i for i in range(4)], list(range(NT_ALL))))
            for tiles, ktiles in blocks:
                nq = len(tiles) * 128
                for qi, tt in enumerate(tiles):
                    kb.dma(pq[:], PQ[tt].ap, [PQ[tt]], [pq])
                    kb.act(junk[:, 0:256], pq[:], AF.Square, [pq], [junk, ss], accum_out=ss[:])
                    rstd_of(kb, ss, rstd, 256)
                    kb.v("vector", "scalar_tensor_tensor", [pq, rstd, qg], [qn], qn[:], pq[:], rstd[:, 0:1], qg[:],
                         ALU.mult, ALU.mult)
                    for k in range(2):
                        kb.tr(ps_t[:, k, :], qn[:, k * 128:(k + 1) * 128], C["ident_bf"][:], [qn, C["ident_bf"]], [ps_t])
                    kb.v("vector", "tensor_copy", [ps_t], [qlT], qlT[:], ps_t[:, 0:2, :])
                    for k in range(2):
                        kb.mm(ps_q[:, 0:512], qlT[:, k, :], w_uq[:, k, 0:512], [qlT, w_uq], [ps_q, ps_O[0]],
                              start=(k == 0), stop=(k == 1))
                    for k in range(2):
                        kb.mm(ps_q[:, 512:768], qlT[:, k, :], w_uq[:, k, 512:768], [qlT, w_uq], [ps_q, ps_O[1]],
                              start=(k == 0), stop=(k == 1))
                    q_flat = q_sb[:].rearrange("p h d -> p (h d)")
                    kb.act(q_flat[:, 0:512], ps_q[:, 0:512], AF.Copy, [ps_q, ps_O[0], ps_O[1]], [q_sb])
                    kb.act(q_flat[:, 512:768], ps_q[:, 512:768], AF.Copy, [ps_q, ps_O[0], ps_O[1]], [q_sb])
                    kb.v("vector", "tensor_tensor", [q_sb], [qsqt], qsqt[:], q_sb[:], q_sb[:], ALU.mult)
                    kb.v("vector", "tensor_reduce", [qsqt], [qsq], qsq[:], qsqt[:], AX.X, ALU.add)
                    kb.act(qsq[:], qsq[:], AF.Sqrt, [qsq, ksm], [qsq], scale=ksm[:, 0:1])
                    kb.v("vector", "tensor_scalar_mul", [qsq], [qext], qext[:, :, 96], qsq[:], -1.0)
                    kb.v("gpsimd", "tensor_copy", [q_sb], [qext], qext[:, :, 0:64], q_sb[:, :, 0:64])
                    if tt >= 2:
                        kb.dma(cs[:], rope[(tt - 2) * 128:(tt - 1) * 128, :], [I["_rope"]], [cs], q="gpsimd")
                        x1 = q_sb[:, :, 64:80]
                        x2 = q_sb[:, :, 80:96]
                        cosb = cs[:, 0:16].unsqueeze(1).to_broadcast([128, 8, 16])
                        sinb = cs[:, 16:32].unsqueeze(1).to_broadcast([128, 8, 16])
                        kb.v("gpsimd", "tensor_tensor", [q_sb, cs], [rt], rt[:, 0], x1, cosb, ALU.mult)
                        kb.v("gpsimd", "tensor_tensor", [q_sb, cs], [rt], rt[:, 1], x2, sinb, ALU.mult)
                        kb.v("gpsimd", "tensor_tensor", [q_sb, cs], [rt], rt[:, 2], x1, sinb, ALU.mult)
                        kb.v("gpsimd", "tensor_tensor", [q_sb, cs], [rt], rt[:, 3], x2, cosb, ALU.mult)
                        kb.v("vector", "tensor_tensor", [rt], [qext], qext[:, :, 64:80], rt[:, 0], rt[:, 1], ALU.subtract)
                        kb.v("vector", "tensor_tensor", [rt], [qext], qext[:, :, 80:96], rt[:, 2], rt[:, 3], ALU.add)
                    else:
                        kb.v("vector", "tensor_copy", [q_sb], [qext], qext[:, :, 64:96], q_sb[:, :, 64:96])
                    for h in range(8):
                        kb.tr(ps_t[0:97, h, :], qext[:, h, :], C["ident_bf"][:], [qext, C["ident_bf"]], [ps_t])
                    kb.act(QT[0:97, :, qi * 128:(qi + 1) * 128], ps_t[0:97, :, :], AF.Copy, [ps_t], [QT])
                nqs = len(tiles)
                for h in range(8):
                    def S_(ki, kt):
                        pS = ps_S.next()
                        kb.mm(pS[:, 0:nq], KT[0:97, h, kt * 128:(kt + 1) * 128], QT[0:97, h, 0:nq], [KT, QT], [pS])
                        pt = PT.next()
                        kb.act(pt[:, 0:nq], pS[:, 0:nq], AF.Exp, [pS], [pt])
                        return (ki, kt, pt)

                    def PV_(st):
                        ki, kt, pt = st
                        for qs in range(nqs):
                            kb.mm(ps_O[qs][:], pt[:, qs * 128:(qs + 1) * 128], VP[:, kt, h, :], [pt, VP], [ps_O[qs]],
                                  start=(ki == 0), stop=(ki == len(ktiles) - 1))

                    pend = None
                    for ki, kt in enumerate(ktiles):
                        cur = S_(ki, kt)
                        if pend is not None:
                            PV_(pend)
                        pend = cur
                    PV_(pend)
                    for qs in range(nqs):
                        kb.v("vector", "reciprocal", [ps_O[qs]], [rec], rec[:, qs:qs + 1], ps_O[qs][:, 64:65])
                        kb.act(mix_tok[qs][:, h * 64:(h + 1) * 64], ps_O[qs][:, 0:64], AF.Copy, [ps_O[qs], rec],
                               [mix_tok[qs]], scale=rec[:, qs:qs + 1])
                for qi in range(nqs):
                    for c_ in range(4):
                        kb.tr(ps_t[:, c_, :], mix_tok[qi][:, c_ * 128:(c_ + 1) * 128], C["ident_bf"][:],
                              [mix_tok[qi], C["ident_bf"]], [ps_t])
                    kb.v("vector", "tensor_copy", [ps_t], [mixT], mixT[:, 0:4, qi * 128:(qi + 1) * 128], ps_t[:, 0:4, :])
                c0 = zcol(tiles[0])
                kb.dma(zw[:, :, 0:nq + 2], ZTv[:, :, c0 - 1:c0 + nq + 1], ZTb + [ZPAD], [zw])
                kb.dma(bw[:, :, 0:nq], BTv[:, :, c0:c0 + nq], BTb, [bw], q="gpsimd")
                for c_ in range(4):
                    eng = "vector"
                    kb.v(eng, "tensor_scalar", [zw, cw], [yc_], yc_[:, 0:nq], zw[:, c_, 0:nq], cw[:, 0, c_:c_ + 1], None, ALU.mult)
                    kb.v(eng, "scalar_tensor_tensor", [zw, cw, yc_], [yc_], yc_[:, 0:nq], zw[:, c_, 1:nq + 1],
                         cw[:, 1, c_:c_ + 1], yc_[:, 0:nq], ALU.mult, ALU.add)
                    kb.v(eng, "scalar_tensor_tensor", [zw, cw, yc_], [yc_], yc_[:, 0:nq], zw[:, c_, 2:nq + 2],
                         cw[:, 2, c_:c_ + 1], yc_[:, 0:nq], ALU.mult, ALU.add)
                    kb.v(eng, "tensor_tensor", [yc_, bw], [mixT], mixT[:, 4 + c_, 0:nq], yc_[:, 0:nq], bw[:, c_, 0:nq], ALU.mult)
                for qi, tt in enumerate(tiles):
                    isc = 1 if tt < 2 else 0
                    sap, sbuf_ = src_tile(tt)
                    kb.dma(hres[:], sap, [sbuf_], [hres])
                    for half in range(2):
                        for c_ in range(8):
                            kb.mm(ps_y[:, half * 512:(half + 1) * 512], mixT[:, c_, qi * 128:(qi + 1) * 128],
                                  w_out[:, c_, half * 512:(half + 1) * 512], [mixT, w_out], [ps_y, ps_t],
                                  start=(c_ == 0), stop=(c_ == 7))
                    for half in range(2):
                        hs = slice(half * 512, (half + 1) * 512)
                        kb.v("vector", "tensor_tensor", [ps_y, ps_t, M2[isc]], [ytmp], ytmp[:, hs], ps_y[:, hs], M2[isc][:, hs], ALU.mult)
                    kb.v("gpsimd", "tensor_tensor", [ytmp, hres], [ytmp], ytmp[:], ytmp[:], hres[:], ALU.add)
                    kb.dma(H[tt].ap, ytmp[:], [ytmp], [H[tt]], q="gpsimd")


def layer_odd(kb, C, I, layer, MOD, src_tile, H, bg=None):
    j = 0
    QKT = kb.dram_tiles("QKT", NT_ALL, [128, 8, 128], BF16)
    KHd = kb.dram_tiles("KHd", NT_ALL, [128, 8, 128], BF16)
    VPd = kb.dram_tiles("VPd", NT_ALL, [128, 4, 257], BF16)
    SCd = kb.dram_tiles("SCd", NT_ALL, [128, 32], F32)
    OGd = kb.dram_tiles("OGd", NT_ALL, [128, D], F32)
    HBd = kb.dram_tiles("HBd", NT_ALL, [128, D], F32)
    HSd = kb.dram_tiles("HSd", NT_ALL, [128, D], F32)
    lat_tiles = list(range(2, NT_ALL))
    with kb.phase():
        stage = Rot([kb.T([128, 1024], F32, f"stg{i}") for i in range(2)])
        w_in = kb.T([128, 8, 3088], BF16, "w_in_o")
        load_w_bf16(kb, w_in, I["odd_w_in"][j], I["_odd_w_in"], stage, 1024, 3088)
        tmpm = stage.items[0]
        G1 = [kb.T([128, D], F32, f"G1_{i}") for i in range(2)]
        SH = [kb.T([128, D], F32, f"SH_{i}") for i in range(2)]
        for r in range(2):
            build_mod_tiles(kb, MOD.ap, MOD, I["norm1_g"][layer], I["_norm1_g"], r, 0, 1, G1[r], SH[r], tmpm)
        gbias = kb.T([128, 16], F32, "gbias")
        load_bcast(kb, gbias, I["mlstm_gate_b"][j], [I["_mlstm_gate_b"]])
        triU = kb.T([128, 128], F32, "triU")
        triL = kb.T([128, 128], F32, "triL")
        ones = kb.T([128, 128], F32, "ones")
        kb.v("vector", "tensor_single_scalar", [C["jmp"]], [triU], triU[:], C["jmp"][:], 0.0, ALU.is_ge)
        kb.v("vector", "tensor_single_scalar", [C["jmp"]], [triL], triL[:], C["jmp"][:], 0.0, ALU.is_le)
        kb.v("gpsimd", "memset", [], [ones], ones[:], 1.0)
        xt = kb.T([128, D], F32, "xt")
        junk = kb.T([128, D], BF16, "junk")
        ss = kb.T([128, 1], F32, "ss")
        rstd = kb.T([128, 1], F32, "rstd")
        nb = kb.T([128, D], BF16, "nb")
        nT = kb.T([128, 8, 128], BF16, "nT")
        qkT = Rot([kb.T([128, 8, 128], BF16, f"qkT{i}") for i in range(2)])
        khat = Rot([kb.T([128, 8, 128], BF16, f"khat{i}") for i in range(2)])
        vp = Rot([kb.T([128, 4, 257], BF16, f"vp{i}") for i in range(2)])
        for v_ in vp.items:
            kb.v("gpsimd", "memset", [], [v_], v_[:], 1.0)
        og = Rot([kb.T([128, D], F32, f"og{i}") for i in range(2)])
        sc = Rot([kb.T([128, 32], F32, f"sc{i}") for i in range(2)])
        gb = kb.T([128, 16], F32, "gb")
        nl = kb.T([128, 8], F32, "nl")
        igc = kb.T([128, 8], F32, "igc")
        t1 = kb.T([128, 8], F32, "t1")
        t2 = kb.T([128, 8], F32, "t2")
        psT = kb.psv(0, 1, [128, 8, 128], BF16, "psT")
        ps_g = kb.psv(0, 1, [128, 64], F32, "ps_g", root=psT)
        ps_f = [kb.psv(1 + i, 1, [128, 4, 128], F32, f"ps_f{i}") for i in range(2)]
        ps_k = kb.psv(3, 1, [128, 512], F32, "ps_k")
        ps_v = [kb.psv(4 + i, 1, [128, 512], F32, f"ps_v{i}") for i in range(2)]
        ps_o = [kb.psv(6 + i, 1, [128, 512], F32, f"ps_o{i}") for i in range(2)]
        bg_units = []
        if bg is not None:
            bg_units = bg([kb.psv(0, 1, [128, 8, 128], BF16, "pstb", root=psT)])
        for tt in range(NT_ALL):
            for _ in range(4):
                if bg_units:
                    bg_units.pop(0)()
            isc = 1 if tt < 2 else 0
            sap, sbuf_ = src_tile(tt)
            norm_mod_T(kb, C, sap, sbuf_, G1[isc], SH[isc], xt, junk, ss, rstd, nb, psT, nT)
            for hh in range(8):
                for k in range(8):
                    kb.mm(ps_f[hh // 4][:, hh % 4, :], w_in[:, k, hh * 128:(hh + 1) * 128], nT[:, k, :], [w_in, nT],
                          [ps_f[hh // 4]], start=(k == 0), stop=(k == 7))
            qk = qkT.next()
            kb.act(qk[:, 0:4, :], ps_f[0][:], AF.Copy, [ps_f[0]], [qk], scale=128 ** -0.5)
            kb.v("vector", "tensor_copy", [ps_f[1]], [qk], qk[:, 4:8, :], ps_f[1][:])
            kb.dma(QKT[tt].ap, qk[:], [qk], [QKT[tt]], q="gpsimd")
            for k in range(8):
                kb.mm(ps_k[:], nT[:, k, :], w_in[:, k, 512:1024], [nT, w_in], [ps_k], start=(k == 0), stop=(k == 7))
            for hf in range(2):
                for k in range(8):
                    kb.mm(ps_v[hf][:], nT[:, k, :], w_in[:, k, 1024 + hf * 512:1536 + hf * 512], [nT, w_in], [ps_v[hf]],
                          start=(k == 0), stop=(k == 7))
            for k in range(8):
                kb.mm(ps_g[:, 0:16], nT[:, k, :], w_in[:, k, 2048:2064], [nT, w_in], [ps_g], start=(k == 0), stop=(k == 7))
            if tt >= 2:
                for hf in range(2):
                    for k in range(8):
                        kb.mm(ps_o[hf][:], nT[:, k, :], w_in[:, k, 2064 + hf * 512:2576 + hf * 512], [nT, w_in],
                              [ps_o[hf]], start=(k == 0), stop=(k == 7))
                o_ = og.next()
                for hf in range(2):
                    kb.act(o_[:, hf * 512:(hf + 1) * 512], ps_o[hf][:], AF.Sigmoid, [ps_o[hf]], [o_])
                kb.dma(OGd[tt].ap, o_[:], [o_], [OGd[tt]], q="gpsimd")
            kb.v("vector", "tensor_tensor", [ps_g, gbias], [gb], gb[:], ps_g[:, 0:16], gbias[:], ALU.add)
            gb4 = gb[:].rearrange("p (d two h) -> p d two h", d=2, two=2)
            nl3 = nl[:].rearrange("p (d h) -> p d h", d=2)
            kb.act(nl3, gb4[:, :, 1, :], AF.Exp, [gb], [nl], scale=-1.0)
            kb.act(nl[:], nl[:], AF.Ln, [nl], [nl], bias=kb.one_t[:, 0:1])
            kb.v("vector", "tensor_copy", [gb], [igc], igc[:].rearrange("p (d h) -> p d h", d=2), gb4[:, :, 0, :])
            kb.mm(ps_g[:, 16:20], triU[:], nl[:, 0:4], [triU, nl], [ps_g])
            kb.mm(ps_g[:, 20:24], triL[:], nl[:, 4:8], [triL, nl], [ps_g])
            kb.mm(ps_g[:, 24:32], ones[:], nl[:], [ones, nl], [ps_g])
            s_ = sc.next()
            kb.v("vector", "tensor_tensor", [igc, ps_g], [t1], t1[:], igc[:], ps_g[:, 16:24], ALU.add)
            kb.v("vector", "tensor_tensor", [t1, ps_g], [t2], t2[:], t1[:], ps_g[:, 24:32], ALU.subtract)
            kb.act(s_[:, 0:8], t1[:], AF.Exp, [t1], [s_])
            kb.act(s_[:, 8:16], ps_g[:, 16:24], AF.Exp, [ps_g], [s_], scale=-1.0)
            kb.act(s_[:, 16:24], t2[:], AF.Exp, [t2], [s_])
            kb.act(s_[:, 24:32], ps_g[:, 24:32], AF.Exp, [ps_g], [s_], scale=-1.0)
            kb.dma(SCd[tt].ap, s_[:], [s_], [SCd[tt]], q="gpsimd")
            kh = khat.next()
            for c in range(8):
                h = c % 4
                kb.act(kh[:, c, :], ps_k[:, h * 128:(h + 1) * 128], AF.Copy, [ps_k, s_], [kh], scale=s_[:, 16 + c:17 + c])
            kb.dma(KHd[tt].ap, kh[:], [kh], [KHd[tt]], q="gpsimd")
            v_ = vp.next()
            for hf in range(2):
                kb.v("vector", "tensor_copy", [ps_v[hf]], [v_], v_[:, hf * 2:hf * 2 + 2, 0:256],
                     ps_v[hf][:].rearrange("p (h d) -> p h d", h=2))
            kb.dma(VPd[tt].ap, v_[:], [v_], [VPd[tt]], q="gpsimd")
        while bg_units:
            bg_units.pop(0)()
    with kb.phase():
        bg = None
        triU = kb.T([128, 128], F32, "triU")
        triL = kb.T([128, 128], F32, "triL")
        kb.v("vector", "tensor_single_scalar", [C["jmp"]], [triU], triU[:], C["jmp"][:], 0.0, ALU.is_ge)
        kb.v("vector", "tensor_single_scalar", [C["jmp"]], [triL], triL[:], C["jmp"][:], 0.0, ALU.is_le)
        Cst = [kb.T([128, 257], F32, f"Cst{h}") for h in range(4)]
        Cbf = [kb.T([128, 257], BF16, f"Cbf{h}") for h in range(4)]
        qk = Rot([kb.T([128, 8, 128], BF16, f"qk{i}") for i in range(3)])
        kh = Rot([kb.T([128, 4, 128], BF16, f"kh{i}") for i in range(3)])
        vp = Rot([kb.T([128, 4, 257], BF16, f"vp{i}") for i in range(3)])
        sc = Rot([kb.T([128, 32], F32, f"sc{i}") for i in range(3)])
        PTm = Rot([kb.T([128, 128], BF16, f"PTm{i}") for i in range(3)])
        t4 = kb.T([128, 4], F32, "t4")
        r4 = kb.T([128, 4], F32, "r4")
        hout = Rot([kb.T([128, D], F32, f"hout{i}") for i in range(2)])
        hb = Rot([kb.T([128, D], F32, f"hb{i}") for i in range(2)])
        ps_S = Rot([kb.psv(i, 1, [128, 128], F32, f"psS{i}") for i in range(2)])
        ps_N = [kb.psv(2 + h, 1, [128, 257], F32, f"psN{h}") for h in range(4)]
        ps_U = Rot([kb.psv(6 + i, 1, [128, 257], F32, f"psU{i}") for i in range(2)])
        bg_units = []
        if bg is not None:
            bg_units = bg([kb.psv(i, 1, [128, 8, 128], BF16, f"pstb{i}", root=ps_S.items[i]) for i in range(2)])
        for d in (1, 0):
            order = [0, 1] + lat_tiles if d == 0 else [1, 0] + lat_tiles[::-1]
            mask = triU if d == 0 else triL
            for h in range(4):
                kb.v("gpsimd", "memset", [], [Cst[h]], Cst[h][:], 0.0)
                kb.v("gpsimd", "memset", [], [Cbf[h]], Cbf[h][:], 0.0)
            for oi, tt in enumerate(order):
                q_ = qk.next(); k_ = kh.next(); v_ = vp.next(); s_ = sc.next()
                kb.dma(q_[:], QKT[tt].ap, [QKT[tt]], [q_])
                kb.dma(k_[:], KHd[tt].ap[:, d * 4:(d + 1) * 4, :], [KHd[tt]], [k_])
                kb.dma(v_[:], VPd[tt].ap, [VPd[tt]], [v_])
                kb.dma(s_[:], SCd[tt].ap, [SCd[tt]], [s_])
                if tt >= 2:
                    for h in range(4):
                        c = d * 4 + h
                        pS = ps_S.next()
                        kb.mm(pS[:], q_[:, 4 + h, :], q_[:, h, :], [q_], [pS])
                        pt = PTm.next()
                        kb.v("vector", "scalar_tensor_tensor", [pS, s_, mask], [pt], pt[:], pS[:], s_[:, c:c + 1], mask[:],
                             ALU.mult, ALU.mult)
                        kb.mm(ps_N[h][:], pt[:], v_[:, h, :], [pt, v_], [ps_N[h]], start=True, stop=False)
                        kb.mm(ps_N[h][:], q_[:, h, :], Cbf[h][:], [q_, Cbf[h]], [ps_N[h]], start=False, stop=True)
                        kb.v("vector", "tensor_tensor", [ps_N[h], s_], [t4], t4[:, h:h + 1], ps_N[h][:, 256:257],
                             s_[:, 8 + c:9 + c], ALU.mult)
                    kb.act(t4[:], t4[:], AF.Abs, [t4], [t4])
                    kb.v("vector", "tensor_scalar_max", [t4], [r4], r4[:], t4[:], 1.0)
                    kb.v("vector", "reciprocal", [r4], [r4], r4[:], r4[:])
                    kb.v("vector", "tensor_tensor", [r4, s_], [r4], r4[:], r4[:], s_[:, 8 + d * 4:12 + d * 4], ALU.mult)
                    ho = hout.next()
                    for h in range(4):
                        kb.act(ho[:, h * 256:(h + 1) * 256], ps_N[h][:, 0:256], AF.Copy, [ps_N[h], r4], [ho],
                               scale=r4[:, h:h + 1])
                    if d == 1:
                        kb.dma(HBd[tt].ap, ho[:], [ho], [HBd[tt]], q="gpsimd")
                    else:
                        hb_ = hb.next()
                        kb.dma(hb_[:], HBd[tt].ap, [HBd[tt]], [hb_])
                        kb.v("gpsimd", "tensor_tensor", [ho, hb_], [hb_], hb_[:], ho[:], hb_[:], ALU.add)
                        kb.dma(HSd[tt].ap, hb_[:], [hb_], [HSd[tt]], q="gpsimd")
                for _ in range(2):
                    if bg_units:
                        bg_units.pop(0)()
                if oi == len(order) - 1:
                    continue
                for h in range(4):
                    c = d * 4 + h
                    pU = ps_U.next()
                    kb.mm(pU[:], k_[:, h, :], v_[:, h, :], [k_, v_], [pU])
                    kb.v("vector", "scalar_tensor_tensor", [Cst[h], s_, pU], [Cst[h]], Cst[h][:], Cst[h][:],
                         s_[:, 24 + c:25 + c], pU[:], ALU.mult, ALU.add)
                    kb.act(Cbf[h][:], Cst[h][:], AF.Copy, [Cst[h]], [Cbf[h]])
        while bg_units:
            bg_units.pop(0)()
    with kb.phase():
        stage = Rot([kb.T([128, 1024], F32, f"stg{i}") for i in range(2)])
        w_out = kb.T([128, 8, 1024], BF16, "w_out_o")
        load_w_bf16(kb, w_out, I["odd_w_out"][j], I["_odd_w_out"], stage, 1024, 1024)
        hg = kb.T([128, D], F32, "hg")
        load_bcast(kb, hg, I["mlstm_head_g"][j], [I["_mlstm_head_g"]])
        M2 = kb.T([128, D], F32, "M2")
        load_bcast(kb, M2, MOD.ap[0, 2 * D:3 * D], [MOD])
        hs = Rot([kb.T([128, D], F32, f"hs{i}") for i in range(2)])
        og = Rot([kb.T([128, D], F32, f"og{i}") for i in range(2)])
        hres = Rot([kb.T([128, D], F32, f"hres{i}") for i in range(2)])
        sq = kb.T([128, D], F32, "sq")
        ss4 = kb.T([128, 4], F32, "ss4")
        rs4 = kb.T([128, 4], F32, "rs4")
        mb = kb.T([128, D], BF16, "mb")
        mT = kb.T([128, 8, 128], BF16, "mT")
        yo = Rot([kb.T([128, D], F32, f"yo{i}") for i in range(2)])
        psT = kb.psv(0, 1, [128, 8, 128], BF16, "psT")
        ps_y = [kb.psv(1 + i, 1, [128, 512], F32, f"ps_y{i}") for i in range(2)]
        for tt in lat_tiles:
            h_ = hs.next(); o_ = og.next(); r_ = hres.next()
            kb.dma(h_[:], HSd[tt].ap, [HSd[tt]], [h_])
            kb.dma(o_[:], OGd[tt].ap, [OGd[tt]], [o_])
            sap, sbuf_ = src_tile(tt)
            kb.dma(r_[:], sap, [sbuf_], [r_])
            kb.v("gpsimd", "tensor_tensor", [h_], [sq], sq[:], h_[:], h_[:], ALU.mult)
            kb.v("vector", "tensor_reduce", [sq], [ss4], ss4[:], sq[:].rearrange("p (h d) -> p h d", h=4), AX.X, ALU.add)
            rstd_of(kb, ss4, rs4, 256)
            h3 = h_[:].rearrange("p (h d) -> p h d", h=4)
            kb.v("vector", "tensor_tensor", [h_, rs4], [h_], h3, h3, rs4[:].unsqueeze(2).to_broadcast([128, 4, 256]), ALU.mult)
            kb.v("gpsimd", "tensor_tensor", [o_, hg], [o_], o_[:], o_[:], hg[:], ALU.mult)
            kb.v("vector", "tensor_tensor", [h_, o_], [mb], mb[:], h_[:], o_[:], ALU.mult)
            for k in range(8):
                kb.tr(psT[:, k, :], mb[:, k * 128:(k + 1) * 128], C["ident_bf"][:], [mb, C["ident_bf"]], [psT])
            kb.act(mT[:], psT[:], AF.Copy, [psT], [mT])
            for hf in range(2):
                for k in range(8):
                    kb.mm(ps_y[hf][:], mT[:, k, :], w_out[:, k, hf * 512:(hf + 1) * 512], [mT, w_out], [ps_y[hf]],
                          start=(k == 0), stop=(k == 7))
            y_ = yo.next()
            for hf in range(2):
                hsl = slice(hf * 512, (hf + 1) * 512)
                kb.v("vector", "tensor_tensor", [ps_y[hf], M2], [y_], y_[:, hsl], ps_y[hf][:], M2[:, hsl], ALU.mult)
            kb.v("gpsimd", "tensor_tensor", [y_, r_], [y_], y_[:], y_[:], r_[:], ALU.add)
            kb.dma(H[tt].ap, y_[:], [y_], [H[tt]], q="gpsimd")


def peer_prep_units(kb, C, I, layer, UT, VB, ps_list, light=False):
    uf = Rot([kb.T([128, D], F32, f"uf{i}") for i in range(2)])
    ub = Rot([kb.T([128, D], BF16, f"ub{i}") for i in range(2)])
    utb = Rot([kb.T([128, 8, 128], BF16, f"utb{i}") for i in range(2)])
    vf = Rot([kb.T([128, D], F32, f"vf{i}") for i in range(2)])
    vb = Rot([kb.T([128, D], BF16, f"vb{i}") for i in range(2)])
    ps = Rot(ps_list)
    units = []
    for i in range(128):
        def unit(i=i):
            a = uf.next(); b = ub.next(); t = utb.next(); p = ps.next()
            lq = "gpsimd" if light else "sync"
            kb.dma(a[:], I["peer_u"][layer][i * 128:(i + 1) * 128, :], [I["_peer_u"]], [a], q=lq)
            if i % 2 == 0:
                kb.v("gpsimd", "tensor_copy", [a], [b], b[:], a[:])
            else:
                kb.act(b[:], a[:], AF.Copy, [a], [b])
            for k in range(8):
                kb.tr(p[:, k, :], b[:, k * 128:(k + 1) * 128], C["ident_bf"][:], [b, C["ident_bf"]], [p])
            if light:
                kb.act(t[:], p[:], AF.Copy, [p], [t])
            else:
                kb.v("vector", "tensor_copy", [p], [t], t[:], p[:])
            kb.dma(UT[i].ap, t[:], [t], [UT[i]], q="gpsimd")
            a2 = vf.next(); b2 = vb.next()
            kb.dma(a2[:], I["peer_v"][layer][i * 128:(i + 1) * 128, :], [I["_peer_v"]], [a2], q=lq)
            if light:
                kb.v("gpsimd", "tensor_copy", [a2], [b2], b2[:], a2[:])
            else:
                kb.v("vector", "tensor_copy", [a2], [b2], b2[:], a2[:])
            kb.dma(VB[i].ap, b2[:], [b2], [VB[i]], q="gpsimd")
        units.append(unit)
    return units


def peer_prep(kb, C, I, layer, UT, VB):
    with kb.phase():
        ps = [kb.psv(i, 1, [128, 8, 128], BF16, f"pst{i}") for i in range(4)]
        for u in peer_prep_units(kb, C, I, layer, UT, VB, ps):
            u()


def peer_route(kb, C, I, layer, MOD, tiles, src_tile, NTd, RTd):
    with kb.phase():
        stage = Rot([kb.T([128, 2048], F32, f"stg{i}") for i in range(2)])
        w_q = kb.T([128, 8, 2048], BF16, "w_q")
        load_w_bf16(kb, w_q, I["peer_w_q"][layer], I["_peer_w_q"], stage, 1024, 2048)
        skT = kb.T([128, 16, 128], BF16, "skT")
        skb = kb.T([128, 128], BF16, "skb")
        pst = kb.psv(0, 1, [128, 8, 128], BF16, "pst")
        for hp in range(16):
            st = stage.next()
            kb.dma(st[:, 0:128], I["peer_subkeys"][layer][hp // 2, hp % 2], [I["_peer_subkeys"]], [st])
            kb.v("vector", "tensor_copy", [st], [skb], skb[:], st[:, 0:128])
            kb.tr(pst[:, 0, :], skb[:], C["ident_bf"][:], [skb, C["ident_bf"]], [pst])
            kb.v("vector", "tensor_copy", [pst], [skT], skT[:, hp, :], pst[:, 0, :])
        tmpm = Buf(stage.items[0].ap[:, 0:D], "tmpm", root=stage.items[0])
        G1 = [kb.T([128, D], F32, f"G1_{i}") for i in range(2)]
        SH = [kb.T([128, D], F32, f"SH_{i}") for i in range(2)]
        rows = sorted({1 if tt < 2 else 0 for tt in tiles})
        for r in rows:
            build_mod_tiles(kb, MOD.ap, MOD, I["norm2_g"][layer], I["_norm2_g"], r, 3, 4, G1[r], SH[r], tmpm)
        xt = kb.T([128, D], F32, "xt")
        junk = kb.T([128, D], BF16, "junk")
        ss = kb.T([128, 1], F32, "ss")
        rstd = kb.T([128, 1], F32, "rstd")
        nb = kb.T([128, D], BF16, "nb")
        nT = Rot([kb.T([128, 8, 128], BF16, f"nT{i}") for i in range(2)])
        qT_rot = Rot([kb.T([128, 16, 128], BF16, f"qT_sb{i}") for i in range(2)])
        s_rot = Rot([kb.T([128, 16, 128], F32, f"s_sb{i}") for i in range(2)])
        s2 = kb.T([128, 16, 128], F32, "s2")
        s2b = [Buf(None, f"s2b{i}") for i in range(16)]
        stop = kb.T([128, 16, 16], F32, "stop")
        stopa = [Buf(None, f"stopa{i}") for i in range(16)]
        stopb = [Buf(None, f"stopb{i}") for i in range(16)]
        topa = [Buf(None, f"topa{i}") for i in range(8)]
        topb = [Buf(None, f"topb{i}") for i in range(8)]
        c2b = [Buf(None, f"c2b{i}") for i in range(8)]
        idx = kb.T([128, 16, 16], U32, "idx")
        idxf = kb.T([128, 16, 16], F32, "idxf")
        cand = kb.T([128, 8, 256], F32, "cand")
        c2 = kb.T([128, 8, 256], F32, "c2")
        top = kb.T([128, 8, 16], F32, "top")
        pos = kb.T([128, 8, 16], U32, "pos")
        au = kb.T([128, 8, 16], U32, "au")
        bu = kb.T([128, 8, 16], U32, "bu")
        af = kb.T([128, 8, 16], F32, "af")
        bf = kb.T([128, 8, 16], F32, "bf")
        E_rot = Rot([kb.T([128, 8, 16, 16], F32, f"E{i}") for i in range(2)])
        sel = kb.T([128, 3, 128], F32, "sel")
        gs = kb.T([128, 8], F32, "gs")
        rt_sb = Rot([kb.T([128, 3, 128], F32, f"rt_sb{i}") for i in range(2)])
        psT = kb.psv(0, 1, [128, 8, 128], BF16, "psT", root=pst)
        ps_qT = [kb.psv(i, 1, [128, 4, 128], F32, f"ps_qT{i}") for i in range(4)]
        ps_qT[0] = kb.psv(0, 1, [128, 4, 128], F32, "ps_qT0", root=pst)
        ps_s = [kb.psv(4 + i, 1, [128, 4, 128], F32, f"ps_s{i}") for i in range(4)]
        ps_r = kb.psv(4, 1, [128, 3, 128], F32, "ps_r", root=ps_s[0])
        iota16 = C["iota_j"][:, 0:16]
        def front(tt):
            isc = 1 if tt < 2 else 0
            sap, sbuf_ = src_tile(tt)
            nTt = nT.next()
            norm_mod_T(kb, C, sap, sbuf_, G1[isc], SH[isc], xt, junk, ss, rstd, nb, psT, nTt)
            kb.dma(NTd[tt].ap, nTt[:], [nTt], [NTd[tt]], q="gpsimd")
            qT_sb = qT_rot.next()
            s_sb = s_rot.next()
            for hp in range(16):
                pq_ = ps_qT[hp // 4]
                for k in range(8):
                    kb.mm(pq_[:, hp % 4, :], w_q[:, k, hp * 128:(hp + 1) * 128], nTt[:, k, :], [w_q, nTt], [pq_],
                          start=(k == 0), stop=(k == 7))
            for g in range(4):
                kb.act(qT_sb[:, g * 4:(g + 1) * 4, :], ps_qT[g][:], AF.Copy, [ps_qT[g]], [qT_sb])
            for hp in range(16):
                kb.mm(ps_s[hp // 4][:, hp % 4, :], qT_sb[:, hp, :], skT[:, hp, :], [qT_sb, skT], [ps_s[hp // 4]])
            for g in range(4):
                kb.act(s_sb[:, g * 4:(g + 1) * 4, :], ps_s[g][:], AF.Copy, [ps_s[g]], [s_sb])
            return s_sb

        def chain(tt, s_sb):
            for hp in range(16):
                kb.v("vector", "max", [s_sb], [stopa[hp]], stop[:, hp, 0:8], s_sb[:, hp, :])
            for hp in range(16):
                kb.v("vector", "match_replace", [stopa[hp], s_sb], [s2b[hp]], s2[:, hp, :], stop[:, hp, 0:8], s_sb[:, hp, :], -1e30)
            for hp in range(16):
                kb.v("vector", "max", [s2b[hp]], [stopb[hp]], stop[:, hp, 8:16], s2[:, hp, :])
            for hp in range(16):
                kb.v("vector", "max_index", [stopa[hp], s_sb], [idx], idx[:, hp, 0:8], stop[:, hp, 0:8], s_sb[:, hp, :])
            for hp in range(16):
                kb.v("vector", "max_index", [stopb[hp], s_sb], [idx], idx[:, hp, 8:16], stop[:, hp, 8:16], s_sb[:, hp, :])
            kb.v("vector", "tensor_copy", [idx], [idxf], idxf[:], idx[:])
            st4 = stop[:].rearrange("p (h two) k -> p h two k", two=2)
            if4 = idxf[:].rearrange("p (h two) k -> p h two k", two=2)
            cand4 = cand[:].rearrange("p h (a b) -> p h a b", a=16)
            kb.v("vector", "tensor_tensor", stopa + stopb, [cand], cand4,
                 st4[:, :, 0, :].unsqueeze(3).to_broadcast([128, 8, 16, 16]),
                 st4[:, :, 1, :].unsqueeze(2).to_broadcast([128, 8, 16, 16]), ALU.add)
            for h in range(8):
                kb.v("vector", "max", [cand], [topa[h]], top[:, h, 0:8], cand[:, h, :])
            for h in range(8):
                kb.v("vector", "match_replace", [topa[h], cand], [c2b[h]], c2[:, h, :], top[:, h, 0:8], cand[:, h, :], -1e30)
            for h in range(8):
                kb.v("vector", "max", [c2b[h]], [topb[h]], top[:, h, 8:16], c2[:, h, :])
            for h in range(8):
                kb.v("vector", "max_index", [topa[h], cand], [pos], pos[:, h, 0:8], top[:, h, 0:8], cand[:, h, :])
            for h in range(8):
                kb.v("vector", "max_index", [topb[h], cand], [pos], pos[:, h, 8:16], top[:, h, 8:16], cand[:, h, :])
            kb.v("vector", "tensor_single_scalar", [pos], [au], au[:], pos[:], 4, ALU.logical_shift_right)
            kb.v("vector", "tensor_single_scalar", [pos], [bu], bu[:], pos[:], 15, ALU.bitwise_and)
            kb.v("vector", "tensor_copy", [au], [af], af[:], au[:])
            kb.v("vector", "tensor_copy", [bu], [bf], bf[:], bu[:])
            io4 = iota16.unsqueeze(1).unsqueeze(1).to_broadcast([128, 8, 16, 16])
            for which, sel_f in ((0, af), (1, bf)):
                E = E_rot.next()
                kb.v("vector", "tensor_tensor", [sel_f, C["iota_j"]], [E], E[:],
                     sel_f[:].unsqueeze(3).to_broadcast([128, 8, 16, 16]), io4, ALU.is_equal)
                kb.v("vector", "tensor_tensor", [E, idxf], [E], E[:], E[:],
                     if4[:, :, which, :].unsqueeze(2).to_broadcast([128, 8, 16, 16]), ALU.mult)
                kb.v("vector", "tensor_reduce", [E], [sel], sel[:, which, :].rearrange("p (h k) -> p h k", h=8),
                     E[:], AX.X, ALU.add)
            g3 = sel[:, 2, :].rearrange("p (h k) -> p h k", h=8)
            kb.v("vector", "tensor_tensor", topa + topb, [sel], g3, top[:], top[:, :, 0:1].to_broadcast([128, 8, 16]), ALU.subtract)
            kb.act(sel[:, 2, :], sel[:, 2, :], AF.Exp, [sel], [sel])
            kb.v("vector", "tensor_reduce", [sel], [gs], gs[:], g3, AX.X, ALU.add)
            kb.v("vector", "reciprocal", [gs], [gs], gs[:], gs[:])
            kb.v("vector", "tensor_tensor", [sel, gs], [sel], g3, g3, gs[:].unsqueeze(2).to_broadcast([128, 8, 16]), ALU.mult)
            for w_ in range(3):
                kb.tr(ps_r[:, w_, :], sel[:, w_, :], C["ident_f"][:], [sel, C["ident_f"]], [ps_r])
            rt = rt_sb.next()
            kb.act(rt[:], ps_r[:], AF.Copy, [ps_r], [rt])
            kb.dma(RTd[tt].ap, rt[:], [rt], [RTd[tt]], q="gpsimd")

        prev = None
        for tt in tiles:
            s_cur = front(tt)
            if prev is not None:
                chain(*prev)
            prev = (tt, s_cur)
        chain(*prev)


def peer_apply(kb, C, I, layer, MOD, groups, src_tile, NTd, RTd, UT, VB, epilogue):
    with kb.phase():
        QT_ = 32
        Gs_rot = Rot([kb.T([128, 384, 64], BF16, f"Gs{i}") for i in range(2)])
        A = Rot([kb.T([128, QT_, 64], BF16, f"A{i}") for i in range(2)])
        B = Rot([kb.T([128, QT_, 128], BF16, f"B{i}") for i in range(2)])
        nTg_rot = Rot([kb.T([128, 8, 384], BF16, f"nTg{i}") for i in range(2)])
        rt = Rot([kb.T([128, 3, 128], F32, f"rt{i}") for i in range(6)])
        rb = Rot([kb.T([128, 3, 128], BF16, f"rb{i}") for i in range(6)])
        uT = Rot([kb.T([128, 8, 128], BF16, f"uT{i}") for i in range(4)])
        vv = Rot([kb.T([128, D], BF16, f"vv{i}") for i in range(4)])
        ga = Rot([kb.T([128, 384], F32, f"ga{i}") for i in range(2)])
        W = Rot([kb.T([128, 384], BF16, f"W{i}") for i in range(4)])
        ep = epilogue("alloc", kb)
        acc = [[kb.psv(2 * t + h, 1, [128, 512], F32, f"acc{t}{h}") for h in range(2)] for t in range(3)]
        ps_a = [kb.psv(6 + i, 1, [128, 384], F32, f"ps_a{i}") for i in range(2)]
        ps_G = [kb.psv(6 + i, 1, [128, 8, 64], F32, f"ps_G{i}", root=ps_a[i]) for i in range(2)]
        ps_rot = Rot([0, 1])
        iota_bf = C["iota_bf"]

        def load_group(tiles):
            nTg = nTg_rot.next()
            rs = []
            for gi, tt in enumerate(tiles):
                kb.dma(nTg[:, :, gi * 128:(gi + 1) * 128], NTd[tt].ap, [NTd[tt]], [nTg], q="gpsimd")
                r = rt.next(); rb_ = rb.next()
                kb.dma(r[:], RTd[tt].ap, [RTd[tt]], [r], q="gpsimd")
                kb.v("gpsimd", "tensor_copy", [r], [rb_], rb_[:], r[:])
                rs.append((r, rb_))
            return {"tiles": tiles, "nTg": nTg, "rs": rs}

        def gate_units(G, hf):
            Gs = Gs_rot.next()
            units = []
            for gi in range(len(G["tiles"])):
                r, rb_ = G["rs"][gi]
                for qt in range(128 // QT_):
                    def build(gi=gi, qt=qt, rb_=rb_):
                        ts_ = slice(qt * QT_, (qt + 1) * QT_)
                        a_ = A.next(); b_ = B.next()
                        kb.v("vector", "tensor_tensor", [rb_, iota_bf], [a_], a_[:],
                             iota_bf[:, hf * 64:(hf + 1) * 64].unsqueeze(1).to_broadcast([128, QT_, 64]),
                             rb_[:, 0, ts_].unsqueeze(2).to_broadcast([128, QT_, 64]), ALU.is_equal)
                        kb.v("vector", "tensor_tensor", [a_, rb_], [a_], a_[:], a_[:],
                             rb_[:, 2, ts_].unsqueeze(2).to_broadcast([128, QT_, 64]), ALU.mult)
                        kb.v("vector", "tensor_tensor", [rb_, iota_bf], [b_], b_[:],
                             iota_bf[:].unsqueeze(1).to_broadcast([128, QT_, 128]),
                             rb_[:, 1, ts_].unsqueeze(2).to_broadcast([128, QT_, 128]), ALU.is_equal)
                        return a_, b_

                    def mmpart(ab, gi=gi, qt=qt):
                        a_, b_ = ab
                        for t8 in range(QT_ // 8):
                            pg = ps_G[ps_rot.next()]
                            for u in range(8):
                                t = t8 * 8 + u
                                kb.mm(pg[:, u, :], b_[:, t, :], a_[:, t, :], [a_, b_], [pg])
                            tok0 = gi * 128 + qt * QT_ + t8 * 8
                            kb.act(Gs[:, tok0:tok0 + 8, :], pg[:], AF.Copy, [pg], [Gs])
                    units.append((build, mmpart))
            return Gs, units

        class Stagger:
            def __init__(self, units):
                self.units = list(units)
                self.built = None
                self.n = 0

            def step(self):
                if self.n > len(self.units):
                    return False
                nb_ = self.units[self.n][0]() if self.n < len(self.units) else None
                if self.built is not None:
                    self.units[self.n - 1][1](self.built)
                self.built = nb_
                self.n += 1
                return self.n <= len(self.units)

            def drain(self):
                while self.step():
                    pass

        sched = [(g, hf) for g in range(len(groups)) for hf in range(2)]
        Gstate = {0: load_group(groups[0])}
        Gs_cur, units = gate_units(Gstate[0], 0)
        Stagger(units).drain()
        pend = []
        LAG = 2
        for si, (g, hf) in enumerate(sched):
            G = Gstate[g]
            tiles = G["tiles"]
            ng = len(tiles)
            ntok = ng * 128
            nTg = G["nTg"]
            nxt_units, Gs_next = [], None
            if si + 1 < len(sched):
                g2, hf2 = sched[si + 1]
                if g2 not in Gstate:
                    Gstate[g2] = load_group(groups[g2])
                Gs_next, nxt_units = gate_units(Gstate[g2], hf2)
            stg = Stagger(nxt_units)
            every = max(1, 60 // (len(nxt_units) + 1))

            def U_(i, Gs=Gs_cur):
                u_ = uT.next(); v_ = vv.next()
                kb.dma(u_[:], UT[i].ap, [UT[i]], [u_])
                kb.dma(v_[:], VB[i].ap, [VB[i]], [v_])
                pa = ps_a[ps_rot.next()]
                for k in range(8):
                    kb.mm(pa[:, 0:ntok], u_[:, k, :], nTg[:, k, 0:ntok], [u_, nTg], [pa], start=(k == 0), stop=(k == 7))
                g_ = ga.next()
                kb.act(g_[:, 0:ntok], pa[:, 0:ntok], AF.Gelu, [pa], [g_])
                w_ = W.next()
                eng = "vector" if i % 2 == 0 else "gpsimd"
                kb.v(eng, "tensor_tensor", [g_, Gs], [w_], w_[:, 0:ntok], g_[:, 0:ntok], Gs[:, 0:ntok, i % 64], ALU.mult)
                return (i, w_, v_, ng)

            def V_(st):
                i, w_, v_, ng_ = st
                for gi in range(ng_):
                    for h in range(2):
                        kb.mm(acc[gi][h][:], w_[:, gi * 128:(gi + 1) * 128], v_[:, h * 512:(h + 1) * 512], [w_, v_],
                              [acc[gi][h]], start=(i == 0), stop=(i == 127))

            for ii in range(64):
                pend.append(U_(hf * 64 + ii))
                if len(pend) > LAG:
                    V_(pend.pop(0))
                if ii % every == every - 1:
                    stg.step()
            if hf == 1:
                while pend:
                    V_(pend.pop(0))
                for gi, tt in enumerate(tiles):
                    epilogue("run", kb, tt, acc[gi], ep)
            stg.drain()
            Gs_cur = Gs_next


def make_epilogue(I, MOD, src_tile, dst_tile, final=False):
    def ep(mode, kb, tt=None, acc=None, b=None):
        if mode == "alloc":
            b = {"M5": [kb.T([128, D], F32, f"M5_{i}") for i in range(2)],
                 "hres": kb.T([128, D], F32, "hres"), "o": kb.T([128, D], F32, "o")}
            for r in range(2):
                load_bcast(kb, b["M5"][r], MOD.ap[r, 5 * D:6 * D], [MOD])
            if final:
                b["fg"] = kb.T([128, D], F32, "fg")
                load_bcast(kb, b["fg"], I["norm_f_g"], [I["_norm_f_g"]])
                b["junk"] = kb.T([128, D], BF16, "junkf")
                b["ss"] = kb.T([128, 1], F32, "ssf")
                b["rstd"] = kb.T([128, 1], F32, "rstdf")
            return b
        isc = 1 if tt < 2 else 0
        sap, sbuf_ = src_tile(tt)
        hres, o, M5 = b["hres"], b["o"], b["M5"][isc]
        kb.dma(hres[:], sap, [sbuf_], [hres])
        for h in range(2):
            hs = slice(h * 512, (h + 1) * 512)
            kb.v("vector", "tensor_tensor", [acc[h], M5], [o], o[:, hs], acc[h][:], M5[:, hs], ALU.mult)
        kb.v("gpsimd", "tensor_tensor", [o, hres], [o], o[:], o[:], hres[:], ALU.add)
        dap, dbuf = dst_tile(tt)
        if final:
            kb.act(b["junk"][:], o[:], AF.Square, [o], [b["junk"], b["ss"]], accum_out=b["ss"][:])
            rstd_of(kb, b["ss"], b["rstd"], D)
            kb.v("vector", "scalar_tensor_tensor", [o, b["rstd"], b["fg"]], [o], o[:], o[:], b["rstd"][:, 0:1],
                 b["fg"][:], ALU.mult, ALU.mult)
        kb.dma(dap, o[:], [o], [dbuf], q="gpsimd")
    return ep


def tile_groups(tiles, n=3):
    return [tiles[i:i + n] for i in range(0, len(tiles), n)]


INPUT_SPECS = [
    ("x", [SEQ, D]), ("c", [D]), ("ctx", [NCTX, D]), ("c_ctx", [D]),
    ("norm1_g", [2, D]), ("norm2_g", [2, D]), ("w_mod", [2, D, 6 * D]), ("b_mod", [2, 6 * D]),
    ("even_w_in", [1, D, 1952]), ("mla_q_norm", [1, 256]), ("mla_kv_norm", [1, 128]),
    ("mla_w_uq", [1, 256, 768]), ("mla_w_ukv", [1, 128, 1024]), ("conv_w", [1, 3, 512]),
    ("even_w_out", [1, D, D]), ("odd_w_in", [1, D, 3088]), ("mlstm_gate_b", [1, 16]),
    ("mlstm_head_g", [1, D]), ("odd_w_out", [1, D, D]), ("peer_w_q", [2, D, 2048]),
    ("peer_subkeys", [2, 8, 2, 128, 128]), ("peer_u", [2, 16384, D]), ("peer_v", [2, 16384, D]),
    ("norm_f_g", [D]), ("rope", [SEQ, 32]),
]


def build_program(stop_after=None, dbg=()):
    nc = bass.Bass("TRN2", target_bir_lowering=False)
    kb = KB(nc, dbg)
    kb.stop_after = stop_after
    I = {}
    for name, shape in INPUT_SPECS:
        ap = nc.dram_tensor(name, shape, F32, kind="ExternalInput").ap()
        I[name] = ap
        I["_" + name] = Buf(ap, name)
    out = nc.dram_tensor("y", [SEQ, D], F32, kind="ExternalOutput").ap()
    kb.eps_t = kb.T([128, 1], F32, "eps")
    kb.v("gpsimd", "memset", [], [kb.eps_t], kb.eps_t[:], EPS)
    kb.one_t = kb.T([128, 1], F32, "one")
    kb.v("gpsimd", "memset", [], [kb.one_t], kb.one_t[:], 1.0)
    C = make_consts(kb)
    MOD = [Buf(kb.dram(f"MOD{l}", [2, 6 * D], F32), f"MOD{l}") for l in range(2)]
    H0 = kb.dram_tiles("H0", NT_ALL, [128, D], F32)

    def src0(tt):
        if tt < 2:
            return I["ctx"][tt * 128:(tt + 1) * 128, :], I["_ctx"]
        return I["x"][(tt - 2) * 128:(tt - 1) * 128, :], I["_x"]

    phase_mod(kb, C, I, 0, MOD[0])
    if stop_after == "mod0":
        kb.finish()
        return nc, kb
    layer_even(kb, C, I, 0, MOD[0], src0, H0)
    if stop_after in ("even", "E1"):
        kb.finish()
        return nc, kb
    UT = kb.dram_tiles("UT", 128, [128, 8, 128], BF16)
    VB = kb.dram_tiles("VB", 128, [128, D], BF16)
    NTd = kb.dram_tiles("NTd", NT_ALL, [128, 8, 128], BF16)
    RTd = kb.dram_tiles("RTd", NT_ALL, [128, 3, 128], F32)
    H1 = kb.dram_tiles("H1", NT_ALL, [128, D], F32)
    srcH0 = lambda tt: (H0[tt].ap, H0[tt])
    dstH1 = lambda tt: (H1[tt].ap, H1[tt])
    tiles0 = DBG_PTILES or list(range(NT_ALL))
    peer_prep(kb, C, I, 0, UT, VB)
    peer_route(kb, C, I, 0, MOD[0], tiles0, srcH0, NTd, RTd)
    if stop_after == "route0":
        kb.finish()
        return nc, kb
    peer_apply(kb, C, I, 0, MOD[0], tile_groups(tiles0), srcH0, NTd, RTd, UT, VB,
               make_epilogue(I, MOD[0], srcH0, dstH1))
    if stop_after == "peer0":
        kb.finish()
        return nc, kb
    phase_mod(kb, C, I, 1, MOD[1])
    H2 = kb.dram_tiles("H2", NT_ALL, [128, D], F32)
    srcH1 = lambda tt: (H1[tt].ap, H1[tt])
    layer_odd(kb, C, I, 1, MOD[1], srcH1, H2,
              bg=lambda ps: peer_prep_units(kb, C, I, 1, UT, VB, ps))
    if stop_after == "odd":
        kb.finish()
        return nc, kb
    srcH2 = lambda tt: (H2[tt].ap, H2[tt])
    outb = [Buf(out[(tt - 2) * 128:(tt - 1) * 128, :], f"y{tt}") for tt in range(NT_ALL)]
    dstY = lambda tt: (outb[tt].ap, outb[tt])
    tiles1 = list(range(2, NT_ALL))
    peer_route(kb, C, I, 1, MOD[1], tiles1, srcH2, NTd, RTd)
    peer_apply(kb, C, I, 1, MOD[1], tile_groups(tiles1), srcH2, NTd, RTd, UT, VB,
               make_epilogue(I, MOD[1], srcH2, dstY, final=True))
    kb.finish()
    return nc, kb


def rope_table():
    n_freq = 8
    inv = (10000.0 ** (-np.arange(n_freq, dtype=np.float32) / n_freq)).astype(np.float32)
    t = np.arange(SEQ)
    row = (t // 64).astype(np.float32)
    col = (t % 64).astype(np.float32)
    ang = np.concatenate([row[:, None] * inv, col[:, None] * inv], axis=-1).astype(np.float32)
    return np.concatenate([np.cos(ang), np.sin(ang)], axis=-1).astype(np.float32)


def make_in_maps(inputs, cores):
    shared = {k: np.ascontiguousarray(np.asarray(v, dtype=np.float32)) for k, v in inputs.items()
              if k not in ("x", "c", "ctx")}
    shared["rope"] = rope_table()
    maps = []
    for b in cores:
        m = dict(shared)
        m["x"] = np.ascontiguousarray(inputs["x"][b])
        m["c"] = np.ascontiguousarray(inputs["c"][b])
        m["ctx"] = np.ascontiguousarray(inputs["ctx"][b])
        maps.append(m)
    return maps


def kernel(**inputs):
    nc, kb = build_program()
    maps = make_in_maps(inputs, list(range(8)))
    res = run_bass_kernel_spmd(nc, maps, core_ids=list(range(8)))
    return np.stack([r["y"] for r in res.results], axis=0).astype(np.float32)
```

```python
import math
import numpy as np
from contextlib import ExitStack, contextmanager
import concourse.bass as bass
import concourse.mybir as mybir
from concourse.bass_utils import run_bass_kernel_spmd

F32 = mybir.dt.float32
BF16 = mybir.dt.bfloat16
U32 = mybir.dt.uint32
ALU = mybir.AluOpType
AF = mybir.ActivationFunctionType
AX = mybir.AxisListType

D = 1024
SEQ = 4096
NCTX = 256
NT_ALL = 34
EPS = 1e-6
MLA_SCALE = 96 ** -0.5
DBG_TILES = None
DBG_PTILES = None
SKIP = set()
SB_WORDS = 51200


class Buf:
    __slots__ = ("ap", "last_write", "reads", "name", "psum", "root")

    def __init__(self, ap, name="", psum=False, root=None):
        self.root = root.root if root is not None else self
        self.ap = ap
        self.last_write = None
        self.reads = []
        self.name = name
        self.psum = psum

    def __getitem__(self, k):
        return self.ap[k]


class Op:
    __slots__ = ("eng", "fn", "deps", "flag", "is_dma", "sem", "val", "slot")

    def __init__(self, eng, fn, is_dma):
        self.eng = eng
        self.fn = fn
        self.deps = []
        self.flag = False
        self.is_dma = is_dma
        self.sem = None
        self.val = None
        self.slot = None


ENGS = ["tensor", "vector", "scalar", "gpsimd", "sync"]
N_CSEM = 3
N_DSEM = {"sync": 44, "scalar": 4, "gpsimd": 36}


class Prog:
    def __init__(self, nc):
        self.nc = nc
        self.q = {e: [] for e in ENGS}
        self.last_real = {e: None for e in ENGS}
        self.dma_hist = {e: [] for e in N_DSEM}

    def add(self, eng, fn, reads=(), writes=(), dma=False):
        op = Op(eng, fn, dma)
        deps = []
        reads = [b.root for b in reads]
        writes = [b.root for b in writes]
        writes = list(writes) + [b for b in reads if b.psum and b not in writes]
        for b in reads:
            if b.last_write is not None:
                deps.append(b.last_write)
        for b in writes:
            if b.last_write is not None:
                deps.append(b.last_write)
            deps.extend(b.reads)
        seen = set()
        for d in deps:
            if id(d) in seen:
                continue
            seen.add(id(d))
            if d.eng == eng and not d.is_dma and not dma:
                if eng == "tensor":
                    continue
                if not any(b.last_write is d for b in reads):
                    continue
            op.deps.append(d)
            d.flag = True
        for b in reads:
            if not b.psum:
                b.reads.append(op)
        for b in writes:
            b.last_write = op
            b.reads = []
        if dma:
            h = self.dma_hist[eng]
            n = N_DSEM[eng]
            k = len(h)
            op.slot = k % n
            op.val = 16 * (k // n + 1)
            if k >= n:
                op.deps.append(h[k - n])
            h.append(op)
        elif fn is not None:
            self.last_real[eng] = op
        self.q[eng].append(op)
        return op

    def barrier(self):
        deps = []
        for e in ENGS:
            d = self.last_real[e]
            if d is not None:
                d.flag = True
                deps.append(d)
        for e, h in self.dma_hist.items():
            deps.extend(h[-N_DSEM[e]:])
        for e in ENGS:
            op = Op(e, None, False)
            op.deps = list(deps)
            self.q[e].append(op)

    def emit(self, stack):
        nc = self.nc
        csems = {e: [stack.enter_context(nc.semaphore(f"c_{e}_{i}")) for i in range(N_CSEM)]
                 for e in ["tensor", "vector", "scalar", "gpsimd"]}
        dsems = {e: [stack.enter_context(nc.semaphore(f"d_{e}_{i}")) for i in range(n)]
                 for e, n in N_DSEM.items()}
        for e in ENGS:
            nflag = 0
            for op in self.q[e]:
                if op.is_dma:
                    op.sem = dsems[e][op.slot]
                elif op.flag:
                    op.sem = csems[e][nflag % N_CSEM]
                    op.val = nflag // N_CSEM + 1
                    nflag += 1
        stats = {}

        def run(e):
            def body(eng):
                waited = {}
                nw = 0
                for op in self.q[e]:
                    for d in op.deps:
                        key = id(d.sem)
                        if waited.get(key, 0) >= d.val:
                            continue
                        waited[key] = d.val
                        eng.wait_ge(d.sem, d.val)
                        nw += 1
                    if op.fn is None:
                        continue
                    ins = op.fn(eng)
                    if op.is_dma:
                        ins.then_inc(op.sem, 16)
                    elif op.flag:
                        ins.then_inc(op.sem, 1)
                stats[e] = (len(self.q[e]), nw)
            return body

        with nc.Block() as block:
            block.sync(run("sync"))
            block.tensor(run("tensor"))
            block.vector(run("vector"))
            block.scalar(run("scalar"))
            block.gpsimd(run("gpsimd"))
        self.stats = stats


class Rot:
    def __init__(self, items):
        self.items = items
        self.i = 0

    def next(self):
        it = self.items[self.i % len(self.items)]
        self.i += 1
        return it


def _size(dt):
    return {F32: 4, BF16: 2, U32: 4}[dt]


class KB:
    def __init__(self, nc, dbg=()):
        self.nc = nc
        self.P = Prog(nc)
        self.stack = ExitStack()
        self.SB = self.stack.enter_context(nc.sbuf_tensor("SB", [128, SB_WORDS], F32))
        self.PS = self.stack.enter_context(nc.psum_tensor("PSA", [128, 8 * 512], F32))
        self.off = 0
        self.dbg = set(dbg)
        self.ndram = 0

    def T(self, shape, dt=F32, name=""):
        p = shape[0]
        n = int(np.prod(shape[1:]))
        words = (n * _size(dt) + 3) // 4
        assert self.off + words <= SB_WORDS, f"SBUF overflow allocating {name}{shape}: off={self.off} words={words}"
        ap = self.SB[0:p, self.off:self.off + words]
        self.off += words
        if dt != F32:
            ap = ap.bitcast(dt)
        ap = ap[:, 0:n]
        if len(shape) > 2:
            names = " ".join(f"d{i}" for i in range(len(shape) - 1))
            kw = {f"d{i}": shape[i + 1] for i in range(len(shape) - 2)}
            ap = ap.rearrange(f"p ({names}) -> p {names}", **kw)
        return Buf(ap, name)

    def psv(self, b0, nb, shape, dt=F32, name="", root=None):
        p = shape[0]
        n = int(np.prod(shape[1:]))
        ap = self.PS[0:p, b0 * 512:(b0 + nb) * 512]
        if dt != F32:
            ap = ap.bitcast(dt)
        ap = ap[:, 0:n]
        if len(shape) > 2:
            names = " ".join(f"d{i}" for i in range(len(shape) - 1))
            kw = {f"d{i}": shape[i + 1] for i in range(len(shape) - 2)}
            ap = ap.rearrange(f"p ({names}) -> p {names}", **kw)
        return Buf(ap, name, psum=True, root=root)

    def dram(self, name, shape, dt=F32, kind=None):
        if kind is None:
            kind = "ExternalOutput" if name in self.dbg else "Internal"
        t = self.nc.dram_tensor(name, list(shape), dt, kind=kind)
        return t.ap()

    def dram_tiles(self, name, n, shape, dt=F32):
        ap = self.dram(name, [n] + list(shape), dt)
        return [Buf(ap[i], f"{name}{i}") for i in range(n)]

    @contextmanager
    def phase(self):
        m = self.off
        yield
        self.P.barrier()
        self.off = m

    def dma(self, out, in_, reads, writes, q="sync", **kw):
        return self.P.add(q, lambda e: e.dma_start(out=out, in_=in_, **kw), reads, writes, dma=True)

    def mm(self, out, lhsT, rhs, reads, writes, start=True, stop=True):
        return self.P.add("tensor", lambda e: e.matmul(out, lhsT, rhs, start=start, stop=stop), reads, writes)

    def tr(self, out, in_, ident, reads, writes):
        return self.P.add("tensor", lambda e: e.transpose(out, in_, ident), reads, writes)

    def act(self, out, in_, func, reads, writes, **kw):
        return self.P.add("scalar", lambda e: e.activation(out=out, in_=in_, func=func, **kw), reads, writes)

    def v(self, eng, method, reads, writes, *a, **kw):
        return self.P.add(eng, lambda e: getattr(e, method)(*a, **kw), reads, writes)

    def finish(self):
        self.P.barrier()
        self.P.emit(self.stack)
        self.stack.close()


def make_consts(kb):
    c = {}
    io = kb.T([128, 128], F32, "iota")
    kb.P.add("gpsimd", lambda e: e.iota(io[:], [[1, 128]], base=0, channel_multiplier=-1,
                                        allow_small_or_imprecise_dtypes=True), [], [io])
    c["jmp"] = io
    c["ident_bf"] = kb.T([128, 128], BF16, "ident_bf")
    kb.v("vector", "tensor_single_scalar", [io], [c["ident_bf"]], c["ident_bf"][:], io[:], 0.0, ALU.is_equal)
    ij = kb.T([128, 128], F32, "iota_j")
    kb.P.add("gpsimd", lambda e: e.iota(ij[:], [[1, 128]], base=0, channel_multiplier=0,
                                        allow_small_or_imprecise_dtypes=True), [], [ij])
    c["iota_j"] = ij
    c["iota_bf"] = kb.T([128, 128], BF16, "iota_bf")
    kb.v("vector", "tensor_copy", [ij], [c["iota_bf"]], c["iota_bf"][:], ij[:])
    c["ident_f"] = kb.T([128, 128], F32, "ident_f")
    kb.v("vector", "tensor_single_scalar", [io], [c["ident_f"]], c["ident_f"][:], io[:], 0.0, ALU.is_equal)
    return c


def load_bcast(kb, dst, src_ap, srcbufs, q="sync"):
    p = dst.ap.shape[0]
    return kb.dma(dst[:], src_ap.partition_broadcast(p), srcbufs, [dst], q=q)


def load_w_bf16(kb, dst, w_ap, wbuf, stage_rot, K, N, c0=0, c1=None, eng_rot=None):
    c1 = N if c1 is None else c1
    kc = K // 128
    wv = w_ap.rearrange("(k p) n -> p k n", p=128)
    maxcols = stage_rot.items[0].ap.shape[1]
    i = 0
    for k in range(kc):
        for cs in range(c0, c1, maxcols):
            ce = min(c1, cs + maxcols)
            st = stage_rot.next()
            kb.dma(st[:, 0:ce - cs], wv[:, k, cs:ce], [wbuf], [st], q="sync" if i % 2 == 0 else "gpsimd")
            eng = ["gpsimd", "vector"][i % 2]
            kb.v(eng, "tensor_copy", [st], [dst], dst[:, k, cs - c0:ce - c0], st[:, 0:ce - cs])
            i += 1


def rstd_of(kb, ss, rstd, n):
    kb.act(rstd[:], ss[:], AF.Ln, [ss], [rstd], scale=1.0 / n, bias=kb.eps_t[:, 0:1])
    kb.act(rstd[:], rstd[:], AF.Exp, [rstd], [rstd], scale=-0.5)


def build_mod_tiles(kb, mod_ap, modbuf, g_ap, gbuf, row, j_shift, j_scale, G1, SH, tmp):
    load_bcast(kb, tmp, mod_ap[row, j_scale * D:(j_scale + 1) * D], [modbuf])
    load_bcast(kb, G1, g_ap, [gbuf], q="gpsimd")
    kb.v("vector", "scalar_tensor_tensor", [tmp, G1], [G1], G1[:], tmp[:], 1.0, G1[:], ALU.add, ALU.mult)
    load_bcast(kb, SH, mod_ap[row, j_shift * D:(j_shift + 1) * D], [modbuf])


def norm_mod_T(kb, C, src_ap, srcbuf, G1, SH, xt, junk, ss, rstd, nb, psT, nT):
    kb.dma(xt[:], src_ap, [srcbuf], [xt])
    kb.act(junk[:], xt[:], AF.Square, [xt], [junk, ss], accum_out=ss[:])
    rstd_of(kb, ss, rstd, D)
    kb.v("vector", "scalar_tensor_tensor", [xt, rstd, G1], [xt], xt[:], xt[:], rstd[:, 0:1], G1[:], ALU.mult, ALU.mult)
    kb.v("gpsimd", "tensor_tensor", [xt, SH], [nb], nb[:], xt[:], SH[:], ALU.add)
    for k in range(8):
        kb.tr(psT[:, k, :], nb[:, k * 128:(k + 1) * 128], C["ident_bf"][:], [nb, C["ident_bf"]], [psT])
    kb.act(nT[:], psT[:], AF.Copy, [psT], [nT])


def phase_mod(kb, C, I, layer, MOD):
    with kb.phase():
        raw = kb.T([128, 2, 8], F32, "craw")
        kb.dma(raw[:, 0, :], I["c"].rearrange("(p k) -> p k", k=8), [I["_c"]], [raw])
        kb.dma(raw[:, 1, :], I["c_ctx"].rearrange("(p k) -> p k", k=8), [I["_c_ctx"]], [raw])
        sc = kb.T([128, 8, 2], F32, "csilu")
        kb.act(sc[:].rearrange("p k m -> p m k"), raw[:], AF.Silu, [raw], [sc])
        bm = kb.T([2, 6144], F32, "bm")
        load_bcast(kb, bm, I["b_mod"][layer], [I["_b_mod"]], q="gpsimd")
        res = kb.T([2, 6144], F32, "modres")
        wrot = Rot([kb.T([128, 8, 512], F32, f"wm{i}") for i in range(2)])
        prot = Rot([kb.psv(i, 1, [128, 512], F32, f"psm{i}") for i in range(2)])
        wv = I["w_mod"][layer].rearrange("(p k) n -> p k n", k=8)
        for ng in range(12):
            wm = wrot.next()
            ps = prot.next()
            kb.dma(wm[:], wv[:, :, ng * 512:(ng + 1) * 512], [I["_w_mod"]], [wm], q="sync" if ng % 2 == 0 else "gpsimd")
            for k in range(8):
                kb.mm(ps[0:2, :], sc[:, k, :], wm[:, k, :], [sc, wm], [ps], start=(k == 0), stop=(k == 7))
            kb.v("vector", "tensor_tensor", [ps, bm], [res], res[:, ng * 512:(ng + 1) * 512], ps[0:2, :],
                 bm[:, ng * 512:(ng + 1) * 512], ALU.add)
        kb.dma(MOD.ap, res[:], [res], [MOD])


ZCOLS = 4356


def zcol(tt):
    return 1 + tt * 128 if tt < 2 else 259 + (tt - 2) * 128


def layer_even(kb, C, I, layer, MOD, src_tile, H, need_ctx=True):
    j = 0
    P = kb.P
    PQ = kb.dram_tiles("PQ", NT_ALL, [128, 256], F32)
    ZT = kb.dram("ZT", [512, ZCOLS], F32)
    BT = kb.dram("BT", [512, ZCOLS], F32)
    ZTb = [Buf(None, f"zt{t}") for t in range(NT_ALL)]
    BTb = [Buf(None, f"bt{t}") for t in range(NT_ALL)]
    ZPAD = Buf(None, "ztpad")
    ZTv = ZT.rearrange("(c p) t -> p c t", p=128)
    BTv = BT.rearrange("(c p) t -> p c t", p=128)
    with kb.phase():
        KT = kb.T([128, 8, NT_ALL * 128], BF16, "KT_all")
        VP = kb.T([128, NT_ALL, 8, 65], BF16, "VP_all")
        kmax = kb.T([128, 8], F32, "kmax")
        kb.v("gpsimd", "memset", [], [VP], VP[:], 1.0)
        kb.v("gpsimd", "memset", [], [kmax], kmax[:], 0.0)
        rope = I["rope"]
        with kb.phase():
            stage = Rot([kb.T([128, 1024], F32, f"stg{i}") for i in range(2)])
            w_in = kb.T([128, 8, 1952], BF16, "w_in")
            load_w_bf16(kb, w_in, I["even_w_in"][j], I["_even_w_in"], stage, 1024, 1952)
            w_ukv = kb.T([128, 1, 1024], BF16, "w_ukv")
            load_w_bf16(kb, w_ukv, I["mla_w_ukv"][j], I["_mla_w_ukv"], stage, 128, 1024)
            kvg = kb.T([128, 128], F32, "kvg")
            load_bcast(kb, kvg, I["mla_kv_norm"][j], [I["_mla_kv_norm"]])
            tmpm = kb.T([128, D], F32, "tmpm")
            G1 = [kb.T([128, D], F32, f"G1_{i}") for i in range(2)]
            SH = [kb.T([128, D], F32, f"SH_{i}") for i in range(2)]
            for r in range(2):
                build_mod_tiles(kb, MOD.ap, MOD, I["norm1_g"][layer], I["_norm1_g"], r, 0, 1, G1[r], SH[r], tmpm)
            zero = kb.T([128, 4, 1], F32, "zero")
            kb.v("gpsimd", "memset", [], [zero], zero[:], 0.0)
            for col in (0, 257, 258, 4355):
                kb.dma(ZTv[:, :, col:col + 1], zero[:], [zero], [ZPAD], q="gpsimd", allow_slow_non_contiguous=True)
            xt = kb.T([128, D], F32, "xt")
            junk = kb.T([128, D], BF16, "junk")
            ss = kb.T([128, 1], F32, "ss")
            rstd = kb.T([128, 1], F32, "rstd")
            ss2 = kb.T([128, 1], F32, "ss2")
            rstd2 = kb.T([128, 1], F32, "rstd2")
            nb = kb.T([128, D], BF16, "nb")
            nT = kb.T([128, 8, 128], BF16, "nT")
            pm = Rot([kb.T([128, 416], F32, f"pm{i}") for i in range(2)])
            latn = kb.T([128, 128], BF16, "latn")
            klT = kb.T([128, 128], BF16, "klT")
            kext = Rot([kb.T([128, 8, 97], BF16, f"kext{i}") for i in range(2)])
            for kx in kext.items:
                kb.v("gpsimd", "memset", [], [kx], kx[:], 1.0)
            krot = kb.T([128, 32], F32, "krot")
            rtmp = kb.T([128, 4, 16], F32, "rtmp")
            cs = kb.T([128, 32], F32, "cs")
            ksqt = kb.T([128, 8, 96], F32, "ksqt")
            ksq = kb.T([128, 8], F32, "ksq")
            c_sb = kb.T([128, 4, 128], F32, "c_sb")
            z_sb = Rot([kb.T([128, 4, 128], F32, f"z_sb{i}") for i in range(2)])
            b_sb = Rot([kb.T([128, 4, 128], F32, f"b_sb{i}") for i in range(2)])
            psT = kb.psv(0, 1, [128, 8, 128], BF16, "psT")
            ps_mla = kb.psv(1, 1, [128, 416], F32, "ps_mla")
            ps_lat = kb.psv(2, 1, [128, 8, 128], BF16, "ps_lat")
            ps_kv = kb.psv(3, 2, [128, 8, 128], F32, "ps_kv")
            ps_conv = kb.psv(5, 3, [128, 12, 128], F32, "ps_conv")
            for tt in (DBG_TILES or range(NT_ALL)):
                isc = 1 if tt < 2 else 0
                sap, sbuf_ = src_tile(tt)
                norm_mod_T(kb, C, sap, sbuf_, G1[isc], SH[isc], xt, junk, ss, rstd, nb, psT, nT)
                for k in range(8):
                    kb.mm(ps_mla[:], nT[:, k, :], w_in[:, k, 0:416], [nT, w_in], [ps_mla], start=(k == 0), stop=(k == 7))
                pmt = pm.next()
                kb.act(pmt[:], ps_mla[:], AF.Copy, [ps_mla], [pmt])
                kb.dma(PQ[tt].ap, pmt[:, 0:256], [pmt], [PQ[tt]], q="gpsimd")
                if "kv" in SKIP:
                    continue
                kb.act(junk[:, 0:128], pmt[:, 256:384], AF.Square, [pmt], [junk, ss2], accum_out=ss2[:])
                rstd_of(kb, ss2, rstd2, 128)
                kb.v("vector", "scalar_tensor_tensor", [pmt, rstd2, kvg], [latn], latn[:], pmt[:, 256:384],
                     rstd2[:, 0:1], kvg[:], ALU.mult, ALU.mult)
                kb.tr(ps_lat[:, 0, :], latn[:], C["ident_bf"][:], [latn, C["ident_bf"]], [ps_lat])
                kb.v("vector", "tensor_copy", [ps_lat], [klT], klT[:], ps_lat[:, 0, :])
                if "kv2" in SKIP:
                    continue
                kb.mm(ps_kv[:, 0:4, :], klT[:], w_ukv[:, 0, 0:512], [klT, w_ukv], [ps_kv])
                kb.mm(ps_kv[:, 4:8, :], klT[:], w_ukv[:, 0, 512:1024], [klT, w_ukv], [ps_kv])
                if "kv3" in SKIP:
                    continue
                kx = kext.next()
                for hb in range(2):
                    hs = slice(hb * 4, hb * 4 + 4)
                    kb.act(kx[:, hs, 0:64], ps_kv[:, hs, 0:64], AF.Copy, [ps_kv], [kx], scale=MLA_SCALE)
                    kb.v("vector", "tensor_copy", [ps_kv], [VP], VP[:, tt, hs, 0:64], ps_kv[:, hs, 64:128])
                if "rope" in SKIP:
                    continue
                if tt >= 2:
                    kb.dma(cs[:], rope[(tt - 2) * 128:(tt - 1) * 128, :], [I["_rope"]], [cs], q="gpsimd")
                    x1 = pmt[:, 384:400]
                    x2 = pmt[:, 400:416]
                    kb.v("gpsimd", "tensor_tensor", [pmt, cs], [rtmp], rtmp[:, 0, :], x1, cs[:, 0:16], ALU.mult)
                    kb.v("gpsimd", "tensor_tensor", [pmt, cs], [rtmp], rtmp[:, 1, :], x2, cs[:, 16:32], ALU.mult)
                    kb.v("gpsimd", "tensor_tensor", [pmt, cs], [rtmp], rtmp[:, 2, :], x1, cs[:, 16:32], ALU.mult)
                    kb.v("gpsimd", "tensor_tensor", [pmt, cs], [rtmp], rtmp[:, 3, :], x2, cs[:, 0:16], ALU.mult)
                    kb.v("vector", "tensor_tensor", [rtmp], [krot], krot[:, 0:16], rtmp[:, 0, :], rtmp[:, 1, :], ALU.subtract)
                    kb.v("vector", "tensor_tensor", [rtmp], [krot], krot[:, 16:32], rtmp[:, 2, :], rtmp[:, 3, :], ALU.add)
                else:
                    kb.v("vector", "tensor_copy", [pmt], [krot], krot[:], pmt[:, 384:416])
                kb.act(kx[:, :, 64:96], krot[:].unsqueeze(1).to_broadcast([128, 8, 32]), AF.Copy, [krot], [kx],
                       scale=MLA_SCALE)
                if "ksq" in SKIP:
                    continue
                kb.v("vector", "tensor_tensor", [kx], [ksqt], ksqt[:], kx[:, :, 0:96], kx[:, :, 0:96], ALU.mult)
                kb.v("vector", "tensor_reduce", [ksqt], [ksq], ksq[:], ksqt[:], AX.X, ALU.add)
                kb.v("vector", "tensor_tensor", [ksq, kmax], [kmax], kmax[:], kmax[:], ksq[:], ALU.max)
                for h in range(8):
                    kb.tr(ps_lat[0:97, h, :], kx[:, h, :], C["ident_bf"][:], [kx, C["ident_bf"]], [ps_lat])
                kb.act(KT[0:97, :, tt * 128:(tt + 1) * 128], ps_lat[0:97, :, :], AF.Copy, [ps_lat], [KT])
                if "conv" in SKIP:
                    continue
                for fc in range(12):
                    for k in range(8):
                        kb.mm(ps_conv[:, fc, :], w_in[:, k, 416 + fc * 128:416 + (fc + 1) * 128], nT[:, k, :],
                              [w_in, nT], [ps_conv], start=(k == 0), stop=(k == 7))
                kb.act(c_sb[:], ps_conv[:, 4:8, :], AF.Copy, [ps_conv], [c_sb])
                zt = z_sb.next()
                bt = b_sb.next()
                kb.v("vector", "tensor_tensor", [ps_conv, c_sb], [zt], zt[:], ps_conv[:, 8:12, :], c_sb[:], ALU.mult)
                kb.act(bt[:], ps_conv[:, 0:4, :], AF.Copy, [ps_conv], [bt])
                c0 = zcol(tt)
                kb.dma(ZTv[:, :, c0:c0 + 128], zt[:], [zt], [ZTb[tt]], q="gpsimd")
                kb.dma(BTv[:, :, c0:c0 + 128], bt[:], [bt], [BTb[tt]], q="gpsimd")
        if kb.stop_after == "E1":
            return
        with kb.phase():
            stage = Rot([kb.T([128, 1024], F32, f"stg{i}") for i in range(2)])
            w_uq = kb.T([128, 2, 768], BF16, "w_uq")
            load_w_bf16(kb, w_uq, I["mla_w_uq"][j], I["_mla_w_uq"], stage, 256, 768)
            w_out = kb.T([128, 8, 1024], BF16, "w_out")
            load_w_bf16(kb, w_out, I["even_w_out"][j], I["_even_w_out"], stage, 1024, 1024)
            qg = kb.T([128, 256], F32, "qg")
            load_bcast(kb, qg, I["mla_q_norm"][j], [I["_mla_q_norm"]])
            M2 = [kb.T([128, D], F32, f"M2_{i}") for i in range(2)]
            for r in range(2):
                load_bcast(kb, M2[r], MOD.ap[r, 2 * D:3 * D], [MOD])
            cw = kb.T([128, 3, 4], F32, "cw")
            for w_ in range(3):
                for c_ in range(4):
                    kb.dma(cw[:, w_, c_:c_ + 1], I["conv_w"][j][w_, c_ * 128:(c_ + 1) * 128].unsqueeze(1),
                           [I["_conv_w"]], [cw], q="gpsimd", allow_slow_non_contiguous=True)
            psx = kb.psv(6, 1, [128, 128], F32, "psx")
            kmr = kb.T([128, 1], F32, "kmr")
            kb.v("vector", "tensor_reduce", [kmax], [kmr], kmr[:], kmax[:], AX.X, ALU.max)
            kmb = kb.T([128, 128], F32, "kmb")
            kb.v("vector", "tensor_copy", [kmr], [kmb], kmb[:], kmr[:, 0:1].to_broadcast([128, 128]))
            kb.tr(psx[:], kmb[:], C["ident_f"][:], [kmb, C["ident_f"]], [psx])
            ksm = kb.T([128, 1], F32, "ksm")
            kb.v("vector", "tensor_reduce", [psx], [ksm], ksm[:], psx[:], AX.X, ALU.max)
            pq = kb.T([128, 256], F32, "pq")
            junk = kb.T([128, 768], BF16, "junk")
            ss = kb.T([128, 1], F32, "ss")
            rstd = kb.T([128, 1], F32, "rstd")
            qn = kb.T([128, 256], BF16, "qn")
            qlT = kb.T([128, 2, 128], BF16, "qlT")
            q_sb = kb.T([128, 8, 96], F32, "q_sb")
            cs = kb.T([128, 32], F32, "cs")
            rt = kb.T([128, 4, 8, 16], F32, "rt")
            qsqt = kb.T([128, 8, 96], F32, "qsqt")
            qsq = kb.T([128, 8], F32, "qsq")
            qext = kb.T([128, 8, 97], BF16, "qext")
            QT = kb.T([128, 8, 512], BF16, "QT")
            PT = Rot([kb.T([128, 512], BF16, f"PT{i}") for i in range(3)])
            rec = kb.T([128, 4], F32, "rec")
            mix_tok = [kb.T([128, 512], BF16, f"mix{i}") for i in range(4)]
            mixT = kb.T([128, 8, 512], BF16, "mixT")
            zw = kb.T([128, 4, 514], F32, "zw")
            bw = kb.T([128, 4, 512], F32, "bw")
            yc_ = kb.T([128, 512], F32, "yconv")
            hres = stage.items[0]
            ytmp = stage.items[1]
            ps_S = Rot([kb.psv(i, 1, [128, 512], F32, f"psS{i}") for i in range(2)])
            ps_O = [kb.psv(2 + i, 1, [128, 65], F32, f"psO{i}") for i in range(4)]
            ps_q = kb.psv(2, 2, [128, 768], F32, "ps_q")
            ps_t = kb.psv(6, 1, [128, 8, 128], BF16, "ps_t")
            ps_y = kb.psv(6, 2, [128, 1024], F32, "ps_y")
            bank = {i: None for i in range(8)}
            blocks = []
            if need_ctx:
                blocks.append(([0, 1], [0, 1]))
            for b in range(8):
                blocks.append(([2 + 4 * b + i for i in range(4)], list(range(NT_ALL))))
            for tiles, ktiles in blocks:
                nq = len(tiles) * 128
                for qi, tt in enumerate(tiles):
                    kb.dma(pq[:], PQ[tt].ap, [PQ[tt]], [pq])
                    kb.act(junk[:, 0:256], pq[:], AF.Square, [pq], [junk, ss], accum_out=ss[:])
                    rstd_of(kb, ss, rstd, 256)
                    kb.v("vector", "scalar_tensor_tensor", [pq, rstd, qg], [qn], qn[:], pq[:], rstd[:, 0:1], qg[:],
                         ALU.mult, ALU.mult)
                    for k in range(2):
                        kb.tr(ps_t[:, k, :], qn[:, k * 128:(k + 1) * 128], C["ident_bf"][:], [qn, C["ident_bf"]], [ps_t])
                    kb.v("vector", "tensor_copy", [ps_t], [qlT], qlT[:], ps_t[:, 0:2, :])
                    for k in range(2):
                        kb.mm(ps_q[:, 0:512], qlT[:, k, :], w_uq[:, k, 0:512], [qlT, w_uq], [ps_q, ps_O[0]],
                              start=(k == 0), stop=(k == 1))
                    for k in range(2):
                        kb.mm(ps_q[:, 512:768], qlT[:, k, :], w_uq[:, k, 512:768], [qlT, w_uq], [ps_q, ps_O[1]],
                              start=(k == 0), stop=(k == 1))
                    q_flat = q_sb[:].rearrange("p h d -> p (h d)")
                    kb.act(q_flat[:, 0:512], ps_q[:, 0:512], AF.Copy, [ps_q, ps_O[0], ps_O[1]], [q_sb])
                    kb.act(q_flat[:, 512:768], ps_q[:, 512:768], AF.Copy, [ps_q, ps_O[0], ps_O[1]], [q_sb])
                    kb.v("vector", "tensor_tensor", [q_sb], [qsqt], qsqt[:], q_sb[:], q_sb[:], ALU.mult)
                    kb.v("vector", "tensor_reduce", [qsqt], [qsq], qsq[:], qsqt[:], AX.X, ALU.add)
                    kb.act(qsq[:], qsq[:], AF.Sqrt, [qsq, ksm], [qsq], scale=ksm[:, 0:1])
                    kb.v("vector", "tensor_scalar_mul", [qsq], [qext], qext[:, :, 96], qsq[:], -1.0)
                    kb.v("gpsimd", "tensor_copy", [q_sb], [qext], qext[:, :, 0:64], q_sb[:, :, 0:64])
                    if tt >= 2:
                        kb.dma(cs[:], rope[(tt - 2) * 128:(tt - 1) * 128, :], [I["_rope"]], [cs], q="gpsimd")
                        x1 = q_sb[:, :, 64:80]
                        x2 = q_sb[:, :, 80:96]
                        cosb = cs[:, 0:16].unsqueeze(1).to_broadcast([128, 8, 16])
                        sinb = cs[:, 16:32].unsqueeze(1).to_broadcast([128, 8, 16])
                        kb.v("gpsimd", "tensor_tensor", [q_sb, cs], [rt], rt[:, 0], x1, cosb, ALU.mult)
                        kb.v("gpsimd", "tensor_tensor", [q_sb, cs], [rt], rt[:, 1], x2, sinb, ALU.mult)
                        kb.v("gpsimd", "tensor_tensor", [q_sb, cs], [rt], rt[:, 2], x1, sinb, ALU.mult)
                        kb.v("gpsimd", "tensor_tensor", [q_sb, cs], [rt], rt[:, 3], x2, cosb, ALU.mult)
                        kb.v("vector", "tensor_tensor", [rt], [qext], qext[:, :, 64:80], rt[:, 0], rt[:, 1], ALU.subtract)
                        kb.v("vector", "tensor_tensor", [rt], [qext], qext[:, :, 80:96], rt[:, 2], rt[:, 3], ALU.add)
                    else:
                        kb.v("vector", "tensor_copy", [q_sb], [qext], qext[:, :, 64:96], q_sb[:, :, 64:96])
                    for h in range(8):
                        kb.tr(ps_t[0:97, h, :], qext[:, h, :], C["ident_bf"][:], [qext, C["ident_bf"]], [ps_t])
                    kb.act(QT[0:97, :, qi * 128:(qi + 1) * 128], ps_t[0:97, :, :], AF.Copy, [ps_t], [QT])
                nqs = len(tiles)
                for h in range(8):
                    def S_(ki, kt):
                        pS = ps_S.next()
                        kb.mm(pS[:, 0:nq], KT[0:97, h, kt * 128:(kt + 1) * 128], QT[0:97, h, 0:nq], [KT, QT], [pS])
                        pt = PT.next()
                        kb.act(pt[:, 0:nq], pS[:, 0:nq], AF.Exp, [pS], [pt])
                        return (ki, kt, pt)

                    def PV_(st):
                        ki, kt, pt = st
                        for qs in range(nqs):
                            kb.mm(ps_O[qs][:], pt[:, qs * 128:(qs + 1) * 128], VP[:, kt, h, :], [pt, VP], [ps_O[qs]],
                                  start=(ki == 0), stop=(ki == len(ktiles) - 1))

                    pend = None
                    for ki, kt in enumerate(ktiles):
                        cur = S_(ki, kt)
                        if pend is not None:
                            PV_(pend)
                        pend = cur
                    PV_(pend)
                    for qs in range(nqs):
                        kb.v("vector", "reciprocal", [ps_O[qs]], [rec], rec[:, qs:qs + 1], ps_O[qs][:, 64:65])
                        kb.act(mix_tok[qs][:, h * 64:(h + 1) * 64], ps_O[qs][:, 0:64], AF.Copy, [ps_O[qs], rec],
                               [mix_tok[qs]], scale=rec[:, qs:qs + 1])
                for qi in range(nqs):
                    for c_ in range(4):
                        kb.tr(ps_t[:, c_, :], mix_tok[qi][:, c_ * 128:(c_ + 1) * 128], C["ident_bf"][:],
                              [mix_tok[qi], C["ident_bf"]], [ps_t])
                    kb.v("vector", "tensor_copy", [ps_t], [mixT], mixT[:, 0:4, qi * 128:(qi + 1) * 128], ps_t[:, 0:4, :])
                c0 = zcol(tiles[0])
                kb.dma(zw[:, :, 0:nq + 2], ZTv[:, :, c0 - 1:c0 + nq + 1], ZTb + [ZPAD], [zw])
                kb.dma(bw[:, :, 0:nq], BTv[:, :, c0:c0 + nq], BTb, [bw], q="gpsimd")
                for c_ in range(4):
                    eng = "vector"
                    kb.v(eng, "tensor_scalar", [zw, cw], [yc_], yc_[:, 0:nq], zw[:, c_, 0:nq], cw[:, 0, c_:c_ + 1], None, ALU.mult)
                    kb.v(eng, "scalar_tensor_tensor", [zw, cw, yc_], [yc_], yc_[:, 0:nq], zw[:, c_, 1:nq + 1],
                         cw[:, 1, c_:c_ + 1], yc_[:, 0:nq], ALU.mult, ALU.add)
                    kb.v(eng, "scalar_tensor_tensor", [zw, cw, yc_], [yc_], yc_[:, 0:nq], zw[:, c_, 2:nq + 2],
                         cw[:, 2, c_:c_ + 1], yc_[:, 0:nq], ALU.mult, ALU.add)
                    kb.v(eng, "tensor_tensor", [yc_, bw], [mixT], mixT[:, 4 + c_, 0:nq], yc_[:, 0:nq], bw[:, c_, 0:nq], ALU.mult)
                for qi, tt in enumerate(tiles):
                    isc = 1 if tt < 2 else 0
                    sap, sbuf_ = src_tile(tt)
                    kb.dma(hres[:], sap, [sbuf_], [hres])
                    for half in range(2):
                        for c_ in range(8):
                            kb.mm(ps_y[:, half * 512:(half + 1) * 512], mixT[:, c_, qi * 128:(qi + 1) * 128],
                                  w_out[:, c_, half * 512:(half + 1) * 512], [mixT, w_out], [ps_y, ps_t],
                                  start=(c_ == 0), stop=(c_ == 7))
                    for half in range(2):
                        hs = slice(half * 512, (half + 1) * 512)
                        kb.v("vector", "tensor_tensor", [ps_y, ps_t, M2[isc]], [ytmp], ytmp[:, hs], ps_y[:, hs], M2[isc][:, hs], ALU.mult)
                    kb.v("gpsimd", "tensor_tensor", [ytmp, hres], [ytmp], ytmp[:], ytmp[:], hres[:], ALU.add)
                    kb.dma(H[tt].ap, ytmp[:], [ytmp], [H[tt]], q="gpsimd")


def layer_odd(kb, C, I, layer, MOD, src_tile, H, bg=None):
    j = 0
    QKT = kb.dram_tiles("QKT", NT_ALL, [128, 8, 128], BF16)
    KHd = kb.dram_tiles("KHd", NT_ALL, [128, 8, 128], BF16)
    VPd = kb.dram_tiles("VPd", NT_ALL, [128, 4, 257], BF16)
    SCd = kb.dram_tiles("SCd", NT_ALL, [128, 32], F32)
    OGd = kb.dram_tiles("OGd", NT_ALL, [128, D], F32)
    HBd = kb.dram_tiles("HBd", NT_ALL, [128, D], F32)
    HSd = kb.dram_tiles("HSd", NT_ALL, [128, D], F32)
    lat_tiles = list(range(2, NT_ALL))
    with kb.phase():
        stage = Rot([kb.T([128, 1024], F32, f"stg{i}") for i in range(2)])
        w_in = kb.T([128, 8, 3088], BF16, "w_in_o")
        load_w_bf16(kb, w_in, I["odd_w_in"][j], I["_odd_w_in"], stage, 1024, 3088)
        tmpm = stage.items[0]
        G1 = [kb.T([128, D], F32, f"G1_{i}") for i in range(2)]
        SH = [kb.T([128, D], F32, f"SH_{i}") for i in range(2)]
        for r in range(2):
            build_mod_tiles(kb, MOD.ap, MOD, I["norm1_g"][layer], I["_norm1_g"], r, 0, 1, G1[r], SH[r], tmpm)
        gbias = kb.T([128, 16], F32, "gbias")
        load_bcast(kb, gbias, I["mlstm_gate_b"][j], [I["_mlstm_gate_b"]])
        triU = kb.T([128, 128], F32, "triU")
        triL = kb.T([128, 128], F32, "triL")
        ones = kb.T([128, 128], F32, "ones")
        kb.v("vector", "tensor_single_scalar", [C["jmp"]], [triU], triU[:], C["jmp"][:], 0.0, ALU.is_ge)
        kb.v("vector", "tensor_single_scalar", [C["jmp"]], [triL], triL[:], C["jmp"][:], 0.0, ALU.is_le)
        kb.v("gpsimd", "memset", [], [ones], ones[:], 1.0)
        xt = kb.T([128, D], F32, "xt")
        junk = kb.T([128, D], BF16, "junk")
        ss = kb.T([128, 1], F32, "ss")
        rstd = kb.T([128, 1], F32, "rstd")
        nb = kb.T([128, D], BF16, "nb")
        nT = kb.T([128, 8, 128], BF16, "nT")
        qkT = Rot([kb.T([128, 8, 128], BF16, f"qkT{i}") for i in range(2)])
        khat = Rot([kb.T([128, 8, 128], BF16, f"khat{i}") for i in range(2)])
        vp = Rot([kb.T([128, 4, 257], BF16, f"vp{i}") for i in range(2)])
        for v_ in vp.items:
            kb.v("gpsimd", "memset", [], [v_], v_[:], 1.0)
        og = Rot([kb.T([128, D], F32, f"og{i}") for i in range(2)])
        sc = Rot([kb.T([128, 32], F32, f"sc{i}") for i in range(2)])
        gb = kb.T([128, 16], F32, "gb")
        nl = kb.T([128, 8], F32, "nl")
        igc = kb.T([128, 8], F32, "igc")
        t1 = kb.T([128, 8], F32, "t1")
        t2 = kb.T([128, 8], F32, "t2")
        psT = kb.psv(0, 1, [128, 8, 128], BF16, "psT")
        ps_g = kb.psv(0, 1, [128, 64], F32, "ps_g", root=psT)
        ps_f = [kb.psv(1 + i, 1, [128, 4, 128], F32, f"ps_f{i}") for i in range(2)]
        ps_k = kb.psv(3, 1, [128, 512], F32, "ps_k")
        ps_v = [kb.psv(4 + i, 1, [128, 512], F32, f"ps_v{i}") for i in range(2)]
        ps_o = [kb.psv(6 + i, 1, [128, 512], F32, f"ps_o{i}") for i in range(2)]
        bg_units = []
        if bg is not None:
            bg_units = bg([kb.psv(0, 1, [128, 8, 128], BF16, "pstb", root=psT)])
        for tt in range(NT_ALL):
            for _ in range(4):
                if bg_units:
                    bg_units.pop(0)()
            isc = 1 if tt < 2 else 0
            sap, sbuf_ = src_tile(tt)
            norm_mod_T(kb, C, sap, sbuf_, G1[isc], SH[isc], xt, junk, ss, rstd, nb, psT, nT)
            for hh in range(8):
                for k in range(8):
                    kb.mm(ps_f[hh // 4][:, hh % 4, :], w_in[:, k, hh * 128:(hh + 1) * 128], nT[:, k, :], [w_in, nT],
                          [ps_f[hh // 4]], start=(k == 0), stop=(k == 7))
            qk = qkT.next()
            kb.act(qk[:, 0:4, :], ps_f[0][:], AF.Copy, [ps_f[0]], [qk], scale=128 ** -0.5)
            kb.v("vector", "tensor_copy", [ps_f[1]], [qk], qk[:, 4:8, :], ps_f[1][:])
            kb.dma(QKT[tt].ap, qk[:], [qk], [QKT[tt]], q="gpsimd")
            for k in range(8):
                kb.mm(ps_k[:], nT[:, k, :], w_in[:, k, 512:1024], [nT, w_in], [ps_k], start=(k == 0), stop=(k == 7))
            for hf in range(2):
                for k in range(8):
                    kb.mm(ps_v[hf][:], nT[:, k, :], w_in[:, k, 1024 + hf * 512:1536 + hf * 512], [nT, w_in], [ps_v[hf]],
                          start=(k == 0), stop=(k == 7))
            for k in range(8):
                kb.mm(ps_g[:, 0:16], nT[:, k, :], w_in[:, k, 2048:2064], [nT, w_in], [ps_g], start=(k == 0), stop=(k == 7))
            if tt >= 2:
                for hf in range(2):
                    for k in range(8):
                        kb.mm(ps_o[hf][:], nT[:, k, :], w_in[:, k, 2064 + hf * 512:2576 + hf * 512], [nT, w_in],
                              [ps_o[hf]], start=(k == 0), stop=(k == 7))
                o_ = og.next()
                for hf in range(2):
                    kb.act(o_[:, hf * 512:(hf + 1) * 512], ps_o[hf][:], AF.Sigmoid, [ps_o[hf]], [o_])
                kb.dma(OGd[tt].ap, o_[:], [o_], [OGd[tt]], q="gpsimd")
            kb.v("vector", "tensor_tensor", [ps_g, gbias], [gb], gb[:], ps_g[:, 0:16], gbias[:], ALU.add)
            gb4 = gb[:].rearrange("p (d two h) -> p d two h", d=2, two=2)
            nl3 = nl[:].rearrange("p (d h) -> p d h", d=2)
            kb.act(nl3, gb4[:, :, 1, :], AF.Exp, [gb], [nl], scale=-1.0)
            kb.act(nl[:], nl[:], AF.Ln, [nl], [nl], bias=kb.one_t[:, 0:1])
            kb.v("vector", "tensor_copy", [gb], [igc], igc[:].rearrange("p (d h) -> p d h", d=2), gb4[:, :, 0, :])
            kb.mm(ps_g[:, 16:20], triU[:], nl[:, 0:4], [triU, nl], [ps_g])
            kb.mm(ps_g[:, 20:24], triL[:], nl[:, 4:8], [triL, nl], [ps_g])
            kb.mm(ps_g[:, 24:32], ones[:], nl[:], [ones, nl], [ps_g])
            s_ = sc.next()
            kb.v("vector", "tensor_tensor", [igc, ps_g], [t1], t1[:], igc[:], ps_g[:, 16:24], ALU.add)
            kb.v("vector", "tensor_tensor", [t1, ps_g], [t2], t2[:], t1[:], ps_g[:, 24:32], ALU.subtract)
            kb.act(s_[:, 0:8], t1[:], AF.Exp, [t1], [s_])
            kb.act(s_[:, 8:16], ps_g[:, 16:24], AF.Exp, [ps_g], [s_], scale=-1.0)
            kb.act(s_[:, 16:24], t2[:], AF.Exp, [t2], [s_])
            kb.act(s_[:, 24:32], ps_g[:, 24:32], AF.Exp, [ps_g], [s_], scale=-1.0)
            kb.dma(SCd[tt].ap, s_[:], [s_], [SCd[tt]], q="gpsimd")
            kh = khat.next()
            for c in range(8):
                h = c % 4
                kb.act(kh[:, c, :], ps_k[:, h * 128:(h + 1) * 128], AF.Copy, [ps_k, s_], [kh], scale=s_[:, 16 + c:17 + c])
            kb.dma(KHd[tt].ap, kh[:], [kh], [KHd[tt]], q="gpsimd")
            v_ = vp.next()
            for hf in range(2):
                kb.v("vector", "tensor_copy", [ps_v[hf]], [v_], v_[:, hf * 2:hf * 2 + 2, 0:256],
                     ps_v[hf][:].rearrange("p (h d) -> p h d", h=2))
            kb.dma(VPd[tt].ap, v_[:], [v_], [VPd[tt]], q="gpsimd")
        while bg_units:
            bg_units.pop(0)()
    with kb.phase():
        bg = None
        triU = kb.T([128, 128], F32, "triU")
        triL = kb.T([128, 128], F32, "triL")
        kb.v("vector", "tensor_single_scalar", [C["jmp"]], [triU], triU[:], C["jmp"][:], 0.0, ALU.is_ge)
        kb.v("vector", "tensor_single_scalar", [C["jmp"]], [triL], triL[:], C["jmp"][:], 0.0, ALU.is_le)
        Cst = [kb.T([128, 257], F32, f"Cst{h}") for h in range(4)]
        Cbf = [kb.T([128, 257], BF16, f"Cbf{h}") for h in range(4)]
        qk = Rot([kb.T([128, 8, 128], BF16, f"qk{i}") for i in range(3)])
        kh = Rot([kb.T([128, 4, 128], BF16, f"kh{i}") for i in range(3)])
        vp = Rot([kb.T([128, 4, 257], BF16, f"vp{i}") for i in range(3)])
        sc = Rot([kb.T([128, 32], F32, f"sc{i}") for i in range(3)])
        PTm = Rot([kb.T([128, 128], BF16, f"PTm{i}") for i in range(3)])
        t4 = kb.T([128, 4], F32, "t4")
        r4 = kb.T([128, 4], F32, "r4")
        hout = Rot([kb.T([128, D], F32, f"hout{i}") for i in range(2)])
        hb = Rot([kb.T([128, D], F32, f"hb{i}") for i in range(2)])
        ps_S = Rot([kb.psv(i, 1, [128, 128], F32, f"psS{i}") for i in range(2)])
        ps_N = [kb.psv(2 + h, 1, [128, 257], F32, f"psN{h}") for h in range(4)]
        ps_U = Rot([kb.psv(6 + i, 1, [128, 257], F32, f"psU{i}") for i in range(2)])
        bg_units = []
        if bg is not None:
            bg_units = bg([kb.psv(i, 1, [128, 8, 128], BF16, f"pstb{i}", root=ps_S.items[i]) for i in range(2)])
        for d in (1, 0):
            order = [0, 1] + lat_tiles if d == 0 else [1, 0] + lat_tiles[::-1]
            mask = triU if d == 0 else triL
            for h in range(4):
                kb.v("gpsimd", "memset", [], [Cst[h]], Cst[h][:], 0.0)
                kb.v("gpsimd", "memset", [], [Cbf[h]], Cbf[h][:], 0.0)
            for oi, tt in enumerate(order):
                q_ = qk.next(); k_ = kh.next(); v_ = vp.next(); s_ = sc.next()
                kb.dma(q_[:], QKT[tt].ap, [QKT[tt]], [q_])
                kb.dma(k_[:], KHd[tt].ap[:, d * 4:(d + 1) * 4, :], [KHd[tt]], [k_])
                kb.dma(v_[:], VPd[tt].ap, [VPd[tt]], [v_])
                kb.dma(s_[:], SCd[tt].ap, [SCd[tt]], [s_])
                if tt >= 2:
                    for h in range(4):
                        c = d * 4 + h
                        pS = ps_S.next()
                        kb.mm(pS[:], q_[:, 4 + h, :], q_[:, h, :], [q_], [pS])
                        pt = PTm.next()
                        kb.v("vector", "scalar_tensor_tensor", [pS, s_, mask], [pt], pt[:], pS[:], s_[:, c:c + 1], mask[:],
                             ALU.mult, ALU.mult)
                        kb.mm(ps_N[h][:], pt[:], v_[:, h, :], [pt, v_], [ps_N[h]], start=True, stop=False)
                        kb.mm(ps_N[h][:], q_[:, h, :], Cbf[h][:], [q_, Cbf[h]], [ps_N[h]], start=False, stop=True)
                        kb.v("vector", "tensor_tensor", [ps_N[h], s_], [t4], t4[:, h:h + 1], ps_N[h][:, 256:257],
                             s_[:, 8 + c:9 + c], ALU.mult)
                    kb.act(t4[:], t4[:], AF.Abs, [t4], [t4])
                    kb.v("vector", "tensor_scalar_max", [t4], [r4], r4[:], t4[:], 1.0)
                    kb.v("vector", "reciprocal", [r4], [r4], r4[:], r4[:])
                    kb.v("vector", "tensor_tensor", [r4, s_], [r4], r4[:], r4[:], s_[:, 8 + d * 4:12 + d * 4], ALU.mult)
                    ho = hout.next()
                    for h in range(4):
                        kb.act(ho[:, h * 256:(h + 1) * 256], ps_N[h][:, 0:256], AF.Copy, [ps_N[h], r4], [ho],
                               scale=r4[:, h:h + 1])
                    if d == 1:
                        kb.dma(HBd[tt].ap, ho[:], [ho], [HBd[tt]], q="gpsimd")
                    else:
                        hb_ = hb.next()
                        kb.dma(hb_[:], HBd[tt].ap, [HBd[tt]], [hb_])
                        kb.v("gpsimd", "tensor_tensor", [ho, hb_], [hb_], hb_[:], ho[:], hb_[:], ALU.add)
                        kb.dma(HSd[tt].ap, hb_[:], [hb_], [HSd[tt]], q="gpsimd")
                for _ in range(2):
                    if bg_units:
                        bg_units.pop(0)()
                if oi == len(order) - 1:
                    continue
                for h in range(4):
                    c = d * 4 + h
                    pU = ps_U.next()
                    kb.mm(pU[:], k_[:, h, :], v_[:, h, :], [k_, v_], [pU])
                    kb.v("vector", "scalar_tensor_tensor", [Cst[h], s_, pU], [Cst[h]], Cst[h][:], Cst[h][:],
                         s_[:, 24 + c:25 + c], pU[:], ALU.mult, ALU.add)
                    kb.act(Cbf[h][:], Cst[h][:], AF.Copy, [Cst[h]], [Cbf[h]])
        while bg_units:
            bg_units.pop(0)()
    with kb.phase():
        stage = Rot([kb.T([128, 1024], F32, f"stg{i}") for i in range(2)])
        w_out = kb.T([128, 8, 1024], BF16, "w_out_o")
        load_w_bf16(kb, w_out, I["odd_w_out"][j], I["_odd_w_out"], stage, 1024, 1024)
        hg = kb.T([128, D], F32, "hg")
        load_bcast(kb, hg, I["mlstm_head_g"][j], [I["_mlstm_head_g"]])
        M2 = kb.T([128, D], F32, "M2")
        load_bcast(kb, M2, MOD.ap[0, 2 * D:3 * D], [MOD])
        hs = Rot([kb.T([128, D], F32, f"hs{i}") for i in range(2)])
        og = Rot([kb.T([128, D], F32, f"og{i}") for i in range(2)])
        hres = Rot([kb.T([128, D], F32, f"hres{i}") for i in range(2)])
        sq = kb.T([128, D], F32, "sq")
        ss4 = kb.T([128, 4], F32, "ss4")
        rs4 = kb.T([128, 4], F32, "rs4")
        mb = kb.T([128, D], BF16, "mb")
        mT = kb.T([128, 8, 128], BF16, "mT")
        yo = Rot([kb.T([128, D], F32, f"yo{i}") for i in range(2)])
        psT = kb.psv(0, 1, [128, 8, 128], BF16, "psT")
        ps_y = [kb.psv(1 + i, 1, [128, 512], F32, f"ps_y{i}") for i in range(2)]
        for tt in lat_tiles:
            h_ = hs.next(); o_ = og.next(); r_ = hres.next()
            kb.dma(h_[:], HSd[tt].ap, [HSd[tt]], [h_])
            kb.dma(o_[:], OGd[tt].ap, [OGd[tt]], [o_])
            sap, sbuf_ = src_tile(tt)
            kb.dma(r_[:], sap, [sbuf_], [r_])
            kb.v("gpsimd", "tensor_tensor", [h_], [sq], sq[:], h_[:], h_[:], ALU.mult)
            kb.v("vector", "tensor_reduce", [sq], [ss4], ss4[:], sq[:].rearrange("p (h d) -> p h d", h=4), AX.X, ALU.add)
            rstd_of(kb, ss4, rs4, 256)
            h3 = h_[:].rearrange("p (h d) -> p h d", h=4)
            kb.v("vector", "tensor_tensor", [h_, rs4], [h_], h3, h3, rs4[:].unsqueeze(2).to_broadcast([128, 4, 256]), ALU.mult)
            kb.v("gpsimd", "tensor_tensor", [o_, hg], [o_], o_[:], o_[:], hg[:], ALU.mult)
            kb.v("vector", "tensor_tensor", [h_, o_], [mb], mb[:], h_[:], o_[:], ALU.mult)
            for k in range(8):
                kb.tr(psT[:, k, :], mb[:, k * 128:(k + 1) * 128], C["ident_bf"][:], [mb, C["ident_bf"]], [psT])
            kb.act(mT[:], psT[:], AF.Copy, [psT], [mT])
            for hf in range(2):
                for k in range(8):
                    kb.mm(ps_y[hf][:], mT[:, k, :], w_out[:, k, hf * 512:(hf + 1) * 512], [mT, w_out], [ps_y[hf]],
                          start=(k == 0), stop=(k == 7))
            y_ = yo.next()
            for hf in range(2):
                hsl = slice(hf * 512, (hf + 1) * 512)
                kb.v("vector", "tensor_tensor", [ps_y[hf], M2], [y_], y_[:, hsl], ps_y[hf][:], M2[:, hsl], ALU.mult)
            kb.v("gpsimd", "tensor_tensor", [y_, r_], [y_], y_[:], y_[:], r_[:], ALU.add)
            kb.dma(H[tt].ap, y_[:], [y_], [H[tt]], q="gpsimd")


def peer_prep_units(kb, C, I, layer, UT, VB, ps_list, light=False):
    uf = Rot([kb.T([128, D], F32, f"uf{i}") for i in range(2)])
    ub = Rot([kb.T([128, D], BF16, f"ub{i}") for i in range(2)])
    utb = Rot([kb.T([128, 8, 128], BF16, f"utb{i}") for i in range(2)])
    vf = Rot([kb.T([128, D], F32, f"vf{i}") for i in range(2)])
    vb = Rot([kb.T([128, D], BF16, f"vb{i}") for i in range(2)])
    ps = Rot(ps_list)
    units = []
    for i in range(128):
        def unit(i=i):
            a = uf.next(); b = ub.next(); t = utb.next(); p = ps.next()
            lq = "gpsimd" if light else "sync"
            kb.dma(a[:], I["peer_u"][layer][i * 128:(i + 1) * 128, :], [I["_peer_u"]], [a], q=lq)
            if i % 2 == 0:
                kb.v("gpsimd", "tensor_copy", [a], [b], b[:], a[:])
            else:
                kb.act(b[:], a[:], AF.Copy, [a], [b])
            for k in range(8):
                kb.tr(p[:, k, :], b[:, k * 128:(k + 1) * 128], C["ident_bf"][:], [b, C["ident_bf"]], [p])
            if light:
                kb.act(t[:], p[:], AF.Copy, [p], [t])
            else:
                kb.v("vector", "tensor_copy", [p], [t], t[:], p[:])
            kb.dma(UT[i].ap, t[:], [t], [UT[i]], q="gpsimd")
            a2 = vf.next(); b2 = vb.next()
            kb.dma(a2[:], I["peer_v"][layer][i * 128:(i + 1) * 128, :], [I["_peer_v"]], [a2], q=lq)
            if light:
                kb.v("gpsimd", "tensor_copy", [a2], [b2], b2[:], a2[:])
            else:
                kb.v("vector", "tensor_copy", [a2], [b2], b2[:], a2[:])
            kb.dma(VB[i].ap, b2[:], [b2], [VB[i]], q="gpsimd")
        units.append(unit)
    return units


def peer_prep(kb, C, I, layer, UT, VB):
    with kb.phase():
        ps = [kb.psv(i, 1, [128, 8, 128], BF16, f"pst{i}") for i in range(4)]
        for u in peer_prep_units(kb, C, I, layer, UT, VB, ps):
            u()


def peer_route(kb, C, I, layer, MOD, tiles, src_tile, NTd, RTd):
    with kb.phase():
        stage = Rot([kb.T([128, 2048], F32, f"stg{i}") for i in range(2)])
        w_q = kb.T([128, 8, 2048], BF16, "w_q")
        load_w_bf16(kb, w_q, I["peer_w_q"][layer], I["_peer_w_q"], stage, 1024, 2048)
        skT = kb.T([128, 16, 128], BF16, "skT")
        skb = kb.T([128, 128], BF16, "skb")
        pst = kb.psv(0, 1, [128, 8, 128], BF16, "pst")
        for hp in range(16):
            st = stage.next()
            kb.dma(st[:, 0:128], I["peer_subkeys"][layer][hp // 2, hp % 2], [I["_peer_subkeys"]], [st])
            kb.v("vector", "tensor_copy", [st], [skb], skb[:], st[:, 0:128])
            kb.tr(pst[:, 0, :], skb[:], C["ident_bf"][:], [skb, C["ident_bf"]], [pst])
            kb.v("vector", "tensor_copy", [pst], [skT], skT[:, hp, :], pst[:, 0, :])
        tmpm = Buf(stage.items[0].ap[:, 0:D], "tmpm", root=stage.items[0])
        G1 = [kb.T([128, D], F32, f"G1_{i}") for i in range(2)]
        SH = [kb.T([128, D], F32, f"SH_{i}") for i in range(2)]
        rows = sorted({1 if tt < 2 else 0 for tt in tiles})
        for r in rows:
            build_mod_tiles(kb, MOD.ap, MOD, I["norm2_g"][layer], I["_norm2_g"], r, 3, 4, G1[r], SH[r], tmpm)
        xt = kb.T([128, D], F32, "xt")
        junk = kb.T([128, D], BF16, "junk")
        ss = kb.T([128, 1], F32, "ss")
        rstd = kb.T([128, 1], F32, "rstd")
        nb = kb.T([128, D], BF16, "nb")
        nT = Rot([kb.T([128, 8, 128], BF16, f"nT{i}") for i in range(2)])
        qT_rot = Rot([kb.T([128, 16, 128], BF16, f"qT_sb{i}") for i in range(2)])
        s_rot = Rot([kb.T([128, 16, 128], F32, f"s_sb{i}") for i in range(2)])
        s2 = kb.T([128, 16, 128], F32, "s2")
        s2b = [Buf(None, f"s2b{i}") for i in range(16)]
        stop = kb.T([128, 16, 16], F32, "stop")
        stopa = [Buf(None, f"stopa{i}") for i in range(16)]
        stopb = [Buf(None, f"stopb{i}") for i in range(16)]
        topa = [Buf(None, f"topa{i}") for i in range(8)]
        topb = [Buf(None, f"topb{i}") for i in range(8)]
        c2b = [Buf(None, f"c2b{i}") for i in range(8)]
        idx = kb.T([128, 16, 16], U32, "idx")
        idxf = kb.T([128, 16, 16], F32, "idxf")
        cand = kb.T([128, 8, 256], F32, "cand")
        c2 = kb.T([128, 8, 256], F32, "c2")
        top = kb.T([128, 8, 16], F32, "top")
        pos = kb.T([128, 8, 16], U32, "pos")
        au = kb.T([128, 8, 16], U32, "au")
        bu = kb.T([128, 8, 16], U32, "bu")
        af = kb.T([128, 8, 16], F32, "af")
        bf = kb.T([128, 8, 16], F32, "bf")
        E_rot = Rot([kb.T([128, 8, 16, 16], F32, f"E{i}") for i in range(2)])
        sel = kb.T([128, 3, 128], F32, "sel")
        gs = kb.T([128, 8], F32, "gs")
        rt_sb = Rot([kb.T([128, 3, 128], F32, f"rt_sb{i}") for i in range(2)])
        psT = kb.psv(0, 1, [128, 8, 128], BF16, "psT", root=pst)
        ps_qT = [kb.psv(i, 1, [128, 4, 128], F32, f"ps_qT{i}") for i in range(4)]
        ps_qT[0] = kb.psv(0, 1, [128, 4, 128], F32, "ps_qT0", root=pst)
        ps_s = [kb.psv(4 + i, 1, [128, 4, 128], F32, f"ps_s{i}") for i in range(4)]
        ps_r = kb.psv(4, 1, [128, 3, 128], F32, "ps_r", root=ps_s[0])
        iota16 = C["iota_j"][:, 0:16]
        def front(tt):
            isc = 1 if tt < 2 else 0
            sap, sbuf_ = src_tile(tt)
            nTt = nT.next()
            norm_mod_T(kb, C, sap, sbuf_, G1[isc], SH[isc], xt, junk, ss, rstd, nb, psT, nTt)
            kb.dma(NTd[tt].ap, nTt[:], [nTt], [NTd[tt]], q="gpsimd")
            qT_sb = qT_rot.next()
            s_sb = s_rot.next()
            for hp in range(16):
                pq_ = ps_qT[hp // 4]
                for k in range(8):
                    kb.mm(pq_[:, hp % 4, :], w_q[:, k, hp * 128:(hp + 1) * 128], nTt[:, k, :], [w_q, nTt], [pq_],
                          start=(k == 0), stop=(k == 7))
            for g in range(4):
                kb.act(qT_sb[:, g * 4:(g + 1) * 4, :], ps_qT[g][:], AF.Copy, [ps_qT[g]], [qT_sb])
            for hp in range(16):
                kb.mm(ps_s[hp // 4][:, hp % 4, :], qT_sb[:, hp, :], skT[:, hp, :], [qT_sb, skT], [ps_s[hp // 4]])
            for g in range(4):
                kb.act(s_sb[:, g * 4:(g + 1) * 4, :], ps_s[g][:], AF.Copy, [ps_s[g]], [s_sb])
            return s_sb

        def chain(tt, s_sb):
            for hp in range(16):
                kb.v("vector", "max", [s_sb], [stopa[hp]], stop[:, hp, 0:8], s_sb[:, hp, :])
            for hp in range(16):
                kb.v("vector", "match_replace", [stopa[hp], s_sb], [s2b[hp]], s2[:, hp, :], stop[:, hp, 0:8], s_sb[:, hp, :], -1e30)
            for hp in range(16):
                kb.v("vector", "max", [s2b[hp]], [stopb[hp]], stop[:, hp, 8:16], s2[:, hp, :])
            for hp in range(16):
                kb.v("vector", "max_index", [stopa[hp], s_sb], [idx], idx[:, hp, 0:8], stop[:, hp, 0:8], s_sb[:, hp, :])
            for hp in range(16):
                kb.v("vector", "max_index", [stopb[hp], s_sb], [idx], idx[:, hp, 8:16], stop[:, hp, 8:16], s_sb[:, hp, :])
            kb.v("vector", "tensor_copy", [idx], [idxf], idxf[:], idx[:])
            st4 = stop[:].rearrange("p (h two) k -> p h two k", two=2)
            if4 = idxf[:].rearrange("p (h two) k -> p h two k", two=2)
            cand4 = cand[:].rearrange("p h (a b) -> p h a b", a=16)
            kb.v("vector", "tensor_tensor", stopa + stopb, [cand], cand4,
                 st4[:, :, 0, :].unsqueeze(3).to_broadcast([128, 8, 16, 16]),
                 st4[:, :, 1, :].unsqueeze(2).to_broadcast([128, 8, 16, 16]), ALU.add)
            for h in range(8):
                kb.v("vector", "max", [cand], [topa[h]], top[:, h, 0:8], cand[:, h, :])
            for h in range(8):
                kb.v("vector", "match_replace", [topa[h], cand], [c2b[h]], c2[:, h, :], top[:, h, 0:8], cand[:, h, :], -1e30)
            for h in range(8):
                kb.v("vector", "max", [c2b[h]], [topb[h]], top[:, h, 8:16], c2[:, h, :])
            for h in range(8):
                kb.v("vector", "max_index", [topa[h], cand], [pos], pos[:, h, 0:8], top[:, h, 0:8], cand[:, h, :])
            for h in range(8):
                kb.v("vector", "max_index", [topb[h], cand], [pos], pos[:, h, 8:16], top[:, h, 8:16], cand[:, h, :])
            kb.v("vector", "tensor_single_scalar", [pos], [au], au[:], pos[:], 4, ALU.logical_shift_right)
            kb.v("vector", "tensor_single_scalar", [pos], [bu], bu[:], pos[:], 15, ALU.bitwise_and)
            kb.v("vector", "tensor_copy", [au], [af], af[:], au[:])
            kb.v("vector", "tensor_copy", [bu], [bf], bf[:], bu[:])
            io4 = iota16.unsqueeze(1).unsqueeze(1).to_broadcast([128, 8, 16, 16])
            for which, sel_f in ((0, af), (1, bf)):
                E = E_rot.next()
                kb.v("vector", "tensor_tensor", [sel_f, C["iota_j"]], [E], E[:],
                     sel_f[:].unsqueeze(3).to_broadcast([128, 8, 16, 16]), io4, ALU.is_equal)
                kb.v("vector", "tensor_tensor", [E, idxf], [E], E[:], E[:],
                     if4[:, :, which, :].unsqueeze(2).to_broadcast([128, 8, 16, 16]), ALU.mult)
                kb.v("vector", "tensor_reduce", [E], [sel], sel[:, which, :].rearrange("p (h k) -> p h k", h=8),
                     E[:], AX.X, ALU.add)
            g3 = sel[:, 2, :].rearrange("p (h k) -> p h k", h=8)
            kb.v("vector", "tensor_tensor", topa + topb, [sel], g3, top[:], top[:, :, 0:1].to_broadcast([128, 8, 16]), ALU.subtract)
            kb.act(sel[:, 2, :], sel[:, 2, :], AF.Exp, [sel], [sel])
            kb.v("vector", "tensor_reduce", [sel], [gs], gs[:], g3, AX.X, ALU.add)
            kb.v("vector", "reciprocal", [gs], [gs], gs[:], gs[:])
            kb.v("vector", "tensor_tensor", [sel, gs], [sel], g3, g3, gs[:].unsqueeze(2).to_broadcast([128, 8, 16]), ALU.mult)
            for w_ in range(3):
                kb.tr(ps_r[:, w_, :], sel[:, w_, :], C["ident_f"][:], [sel, C["ident_f"]], [ps_r])
            rt = rt_sb.next()
            kb.act(rt[:], ps_r[:], AF.Copy, [ps_r], [rt])
            kb.dma(RTd[tt].ap, rt[:], [rt], [RTd[tt]], q="gpsimd")

        prev = None
        for tt in tiles:
            s_cur = front(tt)
            if prev is not None:
                chain(*prev)
            prev = (tt, s_cur)
        chain(*prev)


def peer_apply(kb, C, I, layer, MOD, groups, src_tile, NTd, RTd, UT, VB, epilogue):
    with kb.phase():
        QT_ = 16
        Gs_rot = Rot([kb.T([128, 384, 64], BF16, f"Gs{i}") for i in range(2)])
        A = Rot([kb.T([128, QT_, 64], BF16, f"A{i}") for i in range(2)])
        B = Rot([kb.T([128, QT_, 128], BF16, f"B{i}") for i in range(2)])
        nTg_rot = Rot([kb.T([128, 8, 384], BF16, f"nTg{i}") for i in range(2)])
        rt = Rot([kb.T([128, 3, 128], F32, f"rt{i}") for i in range(6)])
        rb = Rot([kb.T([128, 3, 128], BF16, f"rb{i}") for i in range(6)])
        uT = Rot([kb.T([128, 8, 128], BF16, f"uT{i}") for i in range(4)])
        vv = Rot([kb.T([128, D], BF16, f"vv{i}") for i in range(4)])
        ga = Rot([kb.T([128, 384], F32, f"ga{i}") for i in range(2)])
        W = Rot([kb.T([128, 384], BF16, f"W{i}") for i in range(4)])
        ep = epilogue("alloc", kb)
        acc = [[kb.psv(2 * t + h, 1, [128, 512], F32, f"acc{t}{h}") for h in range(2)] for t in range(3)]
        ps_a = [kb.psv(6 + i, 1, [128, 384], F32, f"ps_a{i}") for i in range(2)]
        ps_G = [kb.psv(6 + i, 1, [128, 8, 64], F32, f"ps_G{i}", root=ps_a[i]) for i in range(2)]
        ps_rot = Rot([0, 1])
        iota_bf = C["iota_bf"]

        def load_group(tiles):
            nTg = nTg_rot.next()
            rs = []
            for gi, tt in enumerate(tiles):
                kb.dma(nTg[:, :, gi * 128:(gi + 1) * 128], NTd[tt].ap, [NTd[tt]], [nTg], q="gpsimd")
                r = rt.next(); rb_ = rb.next()
                kb.dma(r[:], RTd[tt].ap, [RTd[tt]], [r], q="gpsimd")
                kb.v("gpsimd", "tensor_copy", [r], [rb_], rb_[:], r[:])
                rs.append((r, rb_))
            return {"tiles": tiles, "nTg": nTg, "rs": rs}

        def gate_units(G, hf):
            Gs = Gs_rot.next()
            units = []
            for gi in range(len(G["tiles"])):
                r, rb_ = G["rs"][gi]
                for qt in range(128 // QT_):
                    def build(gi=gi, qt=qt, rb_=rb_):
                        ts_ = slice(qt * QT_, (qt + 1) * QT_)
                        a_ = A.next(); b_ = B.next()
                        kb.v("vector", "tensor_tensor", [rb_, iota_bf], [a_], a_[:],
                             iota_bf[:, hf * 64:(hf + 1) * 64].unsqueeze(1).to_broadcast([128, QT_, 64]),
                             rb_[:, 0, ts_].unsqueeze(2).to_broadcast([128, QT_, 64]), ALU.is_equal)
                        kb.v("vector", "tensor_tensor", [a_, rb_], [a_], a_[:], a_[:],
                             rb_[:, 2, ts_].unsqueeze(2).to_broadcast([128, QT_, 64]), ALU.mult)
                        kb.v("vector", "tensor_tensor", [rb_, iota_bf], [b_], b_[:],
                             iota_bf[:].unsqueeze(1).to_broadcast([128, QT_, 128]),
                             rb_[:, 1, ts_].unsqueeze(2).to_broadcast([128, QT_, 128]), ALU.is_equal)
                        return a_, b_

                    def mmpart(ab, gi=gi, qt=qt):
                        a_, b_ = ab
                        for t8 in range(QT_ // 8):
                            pg = ps_G[ps_rot.next()]
                            for u in range(8):
                                t = t8 * 8 + u
                                kb.mm(pg[:, u, :], b_[:, t, :], a_[:, t, :], [a_, b_], [pg])
                            tok0 = gi * 128 + qt * QT_ + t8 * 8
                            kb.act(Gs[:, tok0:tok0 + 8, :], pg[:], AF.Copy, [pg], [Gs])
                    units.append((build, mmpart))
            return Gs, units

        class Stagger:
            def __init__(self, units):
                self.units = list(units)
                self.built = None
                self.n = 0

            def step(self):
                if self.n > len(self.units):
                    return False
                nb_ = self.units[self.n][0]() if self.n < len(self.units) else None
                if self.built is not None:
                    self.units[self.n - 1][1](self.built)
                self.built = nb_
                self.n += 1
                return self.n <= len(self.units)

            def drain(self):
                while self.step():
                    pass

        sched = [(g, hf) for g in range(len(groups)) for hf in range(2)]
        Gstate = {0: load_group(groups[0])}
        Gs_cur, units = gate_units(Gstate[0], 0)
        Stagger(units).drain()
        pend = []
        LAG = 2
        for si, (g, hf) in enumerate(sched):
            G = Gstate[g]
            tiles = G["tiles"]
            ng = len(tiles)
            ntok = ng * 128
            nTg = G["nTg"]
            nxt_units, Gs_next = [], None
            if si + 1 < len(sched):
                g2, hf2 = sched[si + 1]
                if g2 not in Gstate:
                    Gstate[g2] = load_group(groups[g2])
                Gs_next, nxt_units = gate_units(Gstate[g2], hf2)
            stg = Stagger(nxt_units)
            every = max(1, 60 // (len(nxt_units) + 1))

            def U_(i, Gs=Gs_cur):
                u_ = uT.next(); v_ = vv.next()
                kb.dma(u_[:], UT[i].ap, [UT[i]], [u_])
                kb.dma(v_[:], VB[i].ap, [VB[i]], [v_])
                pa = ps_a[ps_rot.next()]
                for k in range(8):
                    kb.mm(pa[:, 0:ntok], u_[:, k, :], nTg[:, k, 0:ntok], [u_, nTg], [pa], start=(k == 0), stop=(k == 7))
                g_ = ga.next()
                kb.act(g_[:, 0:ntok], pa[:, 0:ntok], AF.Gelu, [pa], [g_])
                w_ = W.next()
                eng = "vector" if i % 2 == 0 else "gpsimd"
                kb.v(eng, "tensor_tensor", [g_, Gs], [w_], w_[:, 0:ntok], g_[:, 0:ntok], Gs[:, 0:ntok, i % 64], ALU.mult)
                return (i, w_, v_, ng)

            def V_(st):
                i, w_, v_, ng_ = st
                for gi in range(ng_):
                    for h in range(2):
                        kb.mm(acc[gi][h][:], w_[:, gi * 128:(gi + 1) * 128], v_[:, h * 512:(h + 1) * 512], [w_, v_],
                              [acc[gi][h]], start=(i == 0), stop=(i == 127))

            for ii in range(64):
                pend.append(U_(hf * 64 + ii))
                if len(pend) > LAG:
                    V_(pend.pop(0))
                if ii % every == every - 1:
                    stg.step()
            if hf == 1:
                while pend:
                    V_(pend.pop(0))
                for gi, tt in enumerate(tiles):
                    epilogue("run", kb, tt, acc[gi], ep)
            stg.drain()
            Gs_cur = Gs_next


def make_epilogue(I, MOD, src_tile, dst_tile, final=False):
    def ep(mode, kb, tt=None, acc=None, b=None):
        if mode == "alloc":
            b = {"M5": [kb.T([128, D], F32, f"M5_{i}") for i in range(2)],
                 "hres": kb.T([128, D], F32, "hres"), "o": kb.T([128, D], F32, "o")}
            for r in range(2):
                load_bcast(kb, b["M5"][r], MOD.ap[r, 5 * D:6 * D], [MOD])
            if final:
                b["fg"] = kb.T([128, D], F32, "fg")
                load_bcast(kb, b["fg"], I["norm_f_g"], [I["_norm_f_g"]])
                b["junk"] = kb.T([128, D], BF16, "junkf")
                b["ss"] = kb.T([128, 1], F32, "ssf")
                b["rstd"] = kb.T([128, 1], F32, "rstdf")
            return b
        isc = 1 if tt < 2 else 0
        sap, sbuf_ = src_tile(tt)
        hres, o, M5 = b["hres"], b["o"], b["M5"][isc]
        kb.dma(hres[:], sap, [sbuf_], [hres])
        for h in range(2):
            hs = slice(h * 512, (h + 1) * 512)
            kb.v("vector", "tensor_tensor", [acc[h], M5], [o], o[:, hs], acc[h][:], M5[:, hs], ALU.mult)
        kb.v("gpsimd", "tensor_tensor", [o, hres], [o], o[:], o[:], hres[:], ALU.add)
        dap, dbuf = dst_tile(tt)
        if final:
            kb.act(b["junk"][:], o[:], AF.Square, [o], [b["junk"], b["ss"]], accum_out=b["ss"][:])
            rstd_of(kb, b["ss"], b["rstd"], D)
            kb.v("vector", "scalar_tensor_tensor", [o, b["rstd"], b["fg"]], [o], o[:], o[:], b["rstd"][:, 0:1],
                 b["fg"][:], ALU.mult, ALU.mult)
        kb.dma(dap, o[:], [o], [dbuf], q="gpsimd")
    return ep


def tile_groups(tiles, n=3):
    return [tiles[i:i + n] for i in range(0, len(tiles), n)]


INPUT_SPECS = [
    ("x", [SEQ, D]), ("c", [D]), ("ctx", [NCTX, D]), ("c_ctx", [D]),
    ("norm1_g", [2, D]), ("norm2_g", [2, D]), ("w_mod", [2, D, 6 * D]), ("b_mod", [2, 6 * D]),
    ("even_w_in", [1, D, 1952]), ("mla_q_norm", [1, 256]), ("mla_kv_norm", [1, 128]),
    ("mla_w_uq", [1, 256, 768]), ("mla_w_ukv", [1, 128, 1024]), ("conv_w", [1, 3, 512]),
    ("even_w_out", [1, D, D]), ("odd_w_in", [1, D, 3088]), ("mlstm_gate_b", [1, 16]),
    ("mlstm_head_g", [1, D]), ("odd_w_out", [1, D, D]), ("peer_w_q", [2, D, 2048]),
    ("peer_subkeys", [2, 8, 2, 128, 128]), ("peer_u", [2, 16384, D]), ("peer_v", [2, 16384, D]),
    ("norm_f_g", [D]), ("rope", [SEQ, 32]),
]


def build_program(stop_after=None, dbg=()):
    nc = bass.Bass("TRN2", target_bir_lowering=False)
    kb = KB(nc, dbg)
    kb.stop_after = stop_after
    I = {}
    for name, shape in INPUT_SPECS:
        ap = nc.dram_tensor(name, shape, F32, kind="ExternalInput").ap()
        I[name] = ap
        I["_" + name] = Buf(ap, name)
    out = nc.dram_tensor("y", [SEQ, D], F32, kind="ExternalOutput").ap()
    kb.eps_t = kb.T([128, 1], F32, "eps")
    kb.v("gpsimd", "memset", [], [kb.eps_t], kb.eps_t[:], EPS)
    kb.one_t = kb.T([128, 1], F32, "one")
    kb.v("gpsimd", "memset", [], [kb.one_t], kb.one_t[:], 1.0)
    C = make_consts(kb)
    MOD = [Buf(kb.dram(f"MOD{l}", [2, 6 * D], F32), f"MOD{l}") for l in range(2)]
    H0 = kb.dram_tiles("H0", NT_ALL, [128, D], F32)

    def src0(tt):
        if tt < 2:
            return I["ctx"][tt * 128:(tt + 1) * 128, :], I["_ctx"]
        return I["x"][(tt - 2) * 128:(tt - 1) * 128, :], I["_x"]

    phase_mod(kb, C, I, 0, MOD[0])
    if stop_after == "mod0":
        kb.finish()
        return nc, kb
    layer_even(kb, C, I, 0, MOD[0], src0, H0)
    if stop_after in ("even", "E1"):
        kb.finish()
        return nc, kb
    UT = kb.dram_tiles("UT", 128, [128, 8, 128], BF16)
    VB = kb.dram_tiles("VB", 128, [128, D], BF16)
    NTd = kb.dram_tiles("NTd", NT_ALL, [128, 8, 128], BF16)
    RTd = kb.dram_tiles("RTd", NT_ALL, [128, 3, 128], F32)
    H1 = kb.dram_tiles("H1", NT_ALL, [128, D], F32)
    srcH0 = lambda tt: (H0[tt].ap, H0[tt])
    dstH1 = lambda tt: (H1[tt].ap, H1[tt])
    tiles0 = DBG_PTILES or list(range(NT_ALL))
    peer_prep(kb, C, I, 0, UT, VB)
    peer_route(kb, C, I, 0, MOD[0], tiles0, srcH0, NTd, RTd)
    if stop_after == "route0":
        kb.finish()
        return nc, kb
    peer_apply(kb, C, I, 0, MOD[0], tile_groups(tiles0), srcH0, NTd, RTd, UT, VB,
               make_epilogue(I, MOD[0], srcH0, dstH1))
    if stop_after == "peer0":
        kb.finish()
        return nc, kb
    phase_mod(kb, C, I, 1, MOD[1])
    H2 = kb.dram_tiles("H2", NT_ALL, [128, D], F32)
    srcH1 = lambda tt: (H1[tt].ap, H1[tt])
    layer_odd(kb, C, I, 1, MOD[1], srcH1, H2,
              bg=lambda ps: peer_prep_units(kb, C, I, 1, UT, VB, ps))
    if stop_after == "odd":
        kb.finish()
        return nc, kb
    srcH2 = lambda tt: (H2[tt].ap, H2[tt])
    outb = [Buf(out[(tt - 2) * 128:(tt - 1) * 128, :], f"y{tt}") for tt in range(NT_ALL)]
    dstY = lambda tt: (outb[tt].ap, outb[tt])
    tiles1 = list(range(2, NT_ALL))
    peer_route(kb, C, I, 1, MOD[1], tiles1, srcH2, NTd, RTd)
    peer_apply(kb, C, I, 1, MOD[1], tile_groups(tiles1), srcH2, NTd, RTd, UT, VB,
               make_epilogue(I, MOD[1], srcH2, dstY, final=True))
    kb.finish()
    return nc, kb


def rope_table():
    n_freq = 8
    inv = (10000.0 ** (-np.arange(n_freq, dtype=np.float32) / n_freq)).astype(np.float32)
    t = np.arange(SEQ)
    row = (t // 64).astype(np.float32)
    col = (t % 64).astype(np.float32)
    ang = np.concatenate([row[:, None] * inv, col[:, None] * inv], axis=-1).astype(np.float32)
    return np.concatenate([np.cos(ang), np.sin(ang)], axis=-1).astype(np.float32)


def make_in_maps(inputs, cores):
    shared = {k: np.ascontiguousarray(np.asarray(v, dtype=np.float32)) for k, v in inputs.items()
              if k not in ("x", "c", "ctx")}
    shared["rope"] = rope_table()
    maps = []
    for b in cores:
        m = dict(shared)
        m["x"] = np.ascontiguousarray(inputs["x"][b])
        m["c"] = np.ascontiguousarray(inputs["c"][b])
        m["ctx"] = np.ascontiguousarray(inputs["ctx"][b])
        maps.append(m)
    return maps


def kernel(**inputs):
    nc, kb = build_program()
    maps = make_in_maps(inputs, list(range(8)))
    res = run_bass_kernel_spmd(nc, maps, core_ids=list(range(8)))
    return np.stack([r["y"] for r in res.results], axis=0).astype(np.float32)
```

```python
import math
import numpy as np
from contextlib import ExitStack, contextmanager
import concourse.bass as bass
import concourse.mybir as mybir
from concourse.bass_utils import run_bass_kernel_spmd

F32 = mybir.dt.float32
BF16 = mybir.dt.bfloat16
U32 = mybir.dt.uint32
ALU = mybir.AluOpType
AF = mybir.ActivationFunctionType
AX = mybir.AxisListType

D = 1024
SEQ = 4096
NCTX = 256
NT_ALL = 34
EPS = 1e-6
MLA_SCALE = 96 ** -0.5
DBG_TILES = None
DBG_PTILES = None
SKIP = set()
SB_WORDS = 51200


class Buf:
    __slots__ = ("ap", "last_write", "reads", "name", "psum", "root")

    def __init__(self, ap, name="", psum=False, root=None):
        self.root = root.root if root is not None else self
        self.ap = ap
        self.last_write = None
        self.reads = []
        self.name = name
        self.psum = psum

    def __getitem__(self, k):
        return self.ap[k]


class Op:
    __slots__ = ("eng", "fn", "deps", "flag", "is_dma", "sem", "val", "slot")

    def __init__(self, eng, fn, is_dma):
        self.eng = eng
        self.fn = fn
        self.deps = []
        self.flag = False
        self.is_dma = is_dma
        self.sem = None
        self.val = None
        self.slot = None


ENGS = ["tensor", "vector", "scalar", "gpsimd", "sync"]
N_CSEM = 3
N_DSEM = {"sync": 44, "scalar": 4, "gpsimd": 36}


class Prog:
    def __init__(self, nc):
        self.nc = nc
        self.q = {e: [] for e in ENGS}
        self.last_real = {e: None for e in ENGS}
        self.dma_hist = {e: [] for e in N_DSEM}

    def add(self, eng, fn, reads=(), writes=(), dma=False):
        op = Op(eng, fn, dma)
        deps = []
        reads = [b.root for b in reads]
        writes = [b.root for b in writes]
        writes = list(writes) + [b for b in reads if b.psum and b not in writes]
        for b in reads:
            if b.last_write is not None:
                deps.append(b.last_write)
        for b in writes:
            if b.last_write is not None:
                deps.append(b.last_write)
            deps.extend(b.reads)
        seen = set()
        for d in deps:
            if id(d) in seen:
                continue
            seen.add(id(d))
            if d.eng == eng and not d.is_dma and not dma:
                if eng == "tensor":
                    continue
                if not any(b.last_write is d for b in reads):
                    continue
            op.deps.append(d)
            d.flag = True
        for b in reads:
            if not b.psum:
                b.reads.append(op)
        for b in writes:
            b.last_write = op
            b.reads = []
        if dma:
            h = self.dma_hist[eng]
            n = N_DSEM[eng]
            k = len(h)
            op.slot = k % n
            op.val = 16 * (k // n + 1)
            if k >= n:
                op.deps.append(h[k - n])
            h.append(op)
        elif fn is not None:
            self.last_real[eng] = op
        self.q[eng].append(op)
        return op

    def barrier(self):
        deps = []
        for e in ENGS:
            d = self.last_real[e]
            if d is not None:
                d.flag = True
                deps.append(d)
        for e, h in self.dma_hist.items():
            deps.extend(h[-N_DSEM[e]:])
        for e in ENGS:
            op = Op(e, None, False)
            op.deps = list(deps)
            self.q[e].append(op)

    def emit(self, stack):
        nc = self.nc
        csems = {e: [stack.enter_context(nc.semaphore(f"c_{e}_{i}")) for i in range(N_CSEM)]
                 for e in ["tensor", "vector", "scalar", "gpsimd"]}
        dsems = {e: [stack.enter_context(nc.semaphore(f"d_{e}_{i}")) for i in range(n)]
                 for e, n in N_DSEM.items()}
        for e in ENGS:
            nflag = 0
            for op in self.q[e]:
                if op.is_dma:
                    op.sem = dsems[e][op.slot]
                elif op.flag:
                    op.sem = csems[e][nflag % N_CSEM]
                    op.val = nflag // N_CSEM + 1
                    nflag += 1
        stats = {}

        def run(e):
            def body(eng):
                waited = {}
                nw = 0
                for op in self.q[e]:
                    for d in op.deps:
                        key = id(d.sem)
                        if waited.get(key, 0) >= d.val:
                            continue
                        waited[key] = d.val
                        eng.wait_ge(d.sem, d.val)
                        nw += 1
                    if op.fn is None:
                        continue
                    ins = op.fn(eng)
                    if op.is_dma:
                        ins.then_inc(op.sem, 16)
                    elif op.flag:
                        ins.then_inc(op.sem, 1)
                stats[e] = (len(self.q[e]), nw)
            return body

        with nc.Block() as block:
            block.sync(run("sync"))
            block.tensor(run("tensor"))
            block.vector(run("vector"))
            block.scalar(run("scalar"))
            block.gpsimd(run("gpsimd"))
        self.stats = stats


class Rot:
    def __init__(self, items):
        self.items = items
        self.i = 0

    def next(self):
        it = self.items[self.i % len(self.items)]
        self.i += 1
        return it


def _size(dt):
    return {F32: 4, BF16: 2, U32: 4}[dt]


class KB:
    def __init__(self, nc, dbg=()):
        self.nc = nc
        self.P = Prog(nc)
        self.stack = ExitStack()
        self.SB = self.stack.enter_context(nc.sbuf_tensor("SB", [128, SB_WORDS], F32))
        self.PS = self.stack.enter_context(nc.psum_tensor("PSA", [128, 8 * 512], F32))
        self.off = 0
        self.dbg = set(dbg)
        self.ndram = 0

    def T(self, shape, dt=F32, name=""):
        p = shape[0]
        n = int(np.prod(shape[1:]))
        words = (n * _size(dt) + 3) // 4
        assert self.off + words <= SB_WORDS, f"SBUF overflow allocating {name}{shape}: off={self.off} words={words}"
        ap = self.SB[0:p, self.off:self.off + words]
        self.off += words
        if dt != F32:
            ap = ap.bitcast(dt)
        ap = ap[:, 0:n]
        if len(shape) > 2:
            names = " ".join(f"d{i}" for i in range(len(shape) - 1))
            kw = {f"d{i}": shape[i + 1] for i in range(len(shape) - 2)}
            ap = ap.rearrange(f"p ({names}) -> p {names}", **kw)
        return Buf(ap, name)

    def psv(self, b0, nb, shape, dt=F32, name="", root=None):
        p = shape[0]
        n = int(np.prod(shape[1:]))
        ap = self.PS[0:p, b0 * 512:(b0 + nb) * 512]
        if dt != F32:
            ap = ap.bitcast(dt)
        ap = ap[:, 0:n]
        if len(shape) > 2:
            names = " ".join(f"d{i}" for i in range(len(shape) - 1))
            kw = {f"d{i}": shape[i + 1] for i in range(len(shape) - 2)}
            ap = ap.rearrange(f"p ({names}) -> p {names}", **kw)
        return Buf(ap, name, psum=True, root=root)

    def dram(self, name, shape, dt=F32, kind=None):
        if kind is None:
            kind = "ExternalOutput" if name in self.dbg else "Internal"
        t = self.nc.dram_tensor(name, list(shape), dt, kind=kind)
        return t.ap()

    def dram_tiles(self, name, n, shape, dt=F32):
        ap = self.dram(name, [n] + list(shape), dt)
        return [Buf(ap[i], f"{name}{i}") for i in range(n)]

    @contextmanager
    def phase(self):
        m = self.off
        yield
        self.P.barrier()
        self.off = m

    def dma(self, out, in_, reads, writes, q="sync", **kw):
        return self.P.add(q, lambda e: e.dma_start(out=out, in_=in_, **kw), reads, writes, dma=True)

    def mm(self, out, lhsT, rhs, reads, writes, start=True, stop=True):
        return self.P.add("tensor", lambda e: e.matmul(out, lhsT, rhs, start=start, stop=stop), reads, writes)

    def tr(self, out, in_, ident, reads, writes):
        return self.P.add("tensor", lambda e: e.transpose(out, in_, ident), reads, writes)

    def act(self, out, in_, func, reads, writes, **kw):
        return self.P.add("scalar", lambda e: e.activation(out=out, in_=in_, func=func, **kw), reads, writes)

    def v(self, eng, method, reads, writes, *a, **kw):
        return self.P.add(eng, lambda e: getattr(e, method)(*a, **kw), reads, writes)

    def finish(self):
        self.P.barrier()
        self.P.emit(self.stack)
        self.stack.close()


def make_consts(kb):
    c = {}
    io = kb.T([128, 128], F32, "iota")
    kb.P.add("gpsimd", lambda e: e.iota(io[:], [[1, 128]], base=0, channel_multiplier=-1,
                                        allow_small_or_imprecise_dtypes=True), [], [io])
    c["jmp"] = io
    c["ident_bf"] = kb.T([128, 128], BF16, "ident_bf")
    kb.v("vector", "tensor_single_scalar", [io], [c["ident_bf"]], c["ident_bf"][:], io[:], 0.0, ALU.is_equal)
    ij = kb.T([128, 128], F32, "iota_j")
    kb.P.add("gpsimd", lambda e: e.iota(ij[:], [[1, 128]], base=0, channel_multiplier=0,
                                        allow_small_or_imprecise_dtypes=True), [], [ij])
    c["iota_j"] = ij
    c["iota_bf"] = kb.T([128, 128], BF16, "iota_bf")
    kb.v("vector", "tensor_copy", [ij], [c["iota_bf"]], c["iota_bf"][:], ij[:])
    c["ident_f"] = kb.T([128, 128], F32, "ident_f")
    kb.v("vector", "tensor_single_scalar", [io], [c["ident_f"]], c["ident_f"][:], io[:], 0.0, ALU.is_equal)
    return c


def load_bcast(kb, dst, src_ap, srcbufs, q="sync"):
    p = dst.ap.shape[0]
    return kb.dma(dst[:], src_ap.partition_broadcast(p), srcbufs, [dst], q=q)


def load_w_bf16(kb, dst, w_ap, wbuf, stage_rot, K, N, c0=0, c1=None, eng_rot=None):
    c1 = N if c1 is None else c1
    kc = K // 128
    wv = w_ap.rearrange("(k p) n -> p k n", p=128)
    maxcols = stage_rot.items[0].ap.shape[1]
    i = 0
    for k in range(kc):
        for cs in range(c0, c1, maxcols):
            ce = min(c1, cs + maxcols)
            st = stage_rot.next()
            kb.dma(st[:, 0:ce - cs], wv[:, k, cs:ce], [wbuf], [st], q="sync" if i % 2 == 0 else "gpsimd")
            eng = ["gpsimd", "vector"][i % 2]
            kb.v(eng, "tensor_copy", [st], [dst], dst[:, k, cs - c0:ce - c0], st[:, 0:ce - cs])
            i += 1


def rstd_of(kb, ss, rstd, n):
    kb.act(rstd[:], ss[:], AF.Ln, [ss], [rstd], scale=1.0 / n, bias=kb.eps_t[:, 0:1])
    kb.act(rstd[:], rstd[:], AF.Exp, [rstd], [rstd], scale=-0.5)


def build_mod_tiles(kb, mod_ap, modbuf, g_ap, gbuf, row, j_shift, j_scale, G1, SH, tmp):
    load_bcast(kb, tmp, mod_ap[row, j_scale * D:(j_scale + 1) * D], [modbuf])
    load_bcast(kb, G1, g_ap, [gbuf], q="gpsimd")
    kb.v("vector", "scalar_tensor_tensor", [tmp, G1], [G1], G1[:], tmp[:], 1.0, G1[:], ALU.add, ALU.mult)
    load_bcast(kb, SH, mod_ap[row, j_shift * D:(j_shift + 1) * D], [modbuf])


def norm_mod_T(kb, C, src_ap, srcbuf, G1, SH, xt, junk, ss, rstd, nb, psT, nT):
    kb.dma(xt[:], src_ap, [srcbuf], [xt])
    kb.act(junk[:], xt[:], AF.Square, [xt], [junk, ss], accum_out=ss[:])
    rstd_of(kb, ss, rstd, D)
    kb.v("vector", "scalar_tensor_tensor", [xt, rstd, G1], [xt], xt[:], xt[:], rstd[:, 0:1], G1[:], ALU.mult, ALU.mult)
    kb.v("gpsimd", "tensor_tensor", [xt, SH], [nb], nb[:], xt[:], SH[:], ALU.add)
    for k in range(8):
        kb.tr(psT[:, k, :], nb[:, k * 128:(k + 1) * 128], C["ident_bf"][:], [nb, C["ident_bf"]], [psT])
    kb.act(nT[:], psT[:], AF.Copy, [psT], [nT])


def phase_mod(kb, C, I, layer, MOD):
    with kb.phase():
        raw = kb.T([128, 2, 8], F32, "craw")
        kb.dma(raw[:, 0, :], I["c"].rearrange("(p k) -> p k", k=8), [I["_c"]], [raw])
        kb.dma(raw[:, 1, :], I["c_ctx"].rearrange("(p k) -> p k", k=8), [I["_c_ctx"]], [raw])
        sc = kb.T([128, 8, 2], F32, "csilu")
        kb.act(sc[:].rearrange("p k m -> p m k"), raw[:], AF.Silu, [raw], [sc])
        bm = kb.T([2, 6144], F32, "bm")
        load_bcast(kb, bm, I["b_mod"][layer], [I["_b_mod"]], q="gpsimd")
        res = kb.T([2, 6144], F32, "modres")
        wrot = Rot([kb.T([128, 8, 512], F32, f"wm{i}") for i in range(2)])
        prot = Rot([kb.psv(i, 1, [128, 512], F32, f"psm{i}") for i in range(2)])
        wv = I["w_mod"][layer].rearrange("(p k) n -> p k n", k=8)
        for ng in range(12):
            wm = wrot.next()
            ps = prot.next()
            kb.dma(wm[:], wv[:, :, ng * 512:(ng + 1) * 512], [I["_w_mod"]], [wm], q="sync" if ng % 2 == 0 else "gpsimd")
            for k in range(8):
                kb.mm(ps[0:2, :], sc[:, k, :], wm[:, k, :], [sc, wm], [ps], start=(k == 0), stop=(k == 7))
            kb.v("vector", "tensor_tensor", [ps, bm], [res], res[:, ng * 512:(ng + 1) * 512], ps[0:2, :],
                 bm[:, ng * 512:(ng + 1) * 512], ALU.add)
        kb.dma(MOD.ap, res[:], [res], [MOD])


ZCOLS = 4356


def zcol(tt):
    return 1 + tt * 128 if tt < 2 else 259 + (tt - 2) * 128


def layer_even(kb, C, I, layer, MOD, src_tile, H, need_ctx=True):
    j = 0
    P = kb.P
    PQ = kb.dram_tiles("PQ", NT_ALL, [128, 256], F32)
    ZT = kb.dram("ZT", [512, ZCOLS], F32)
    BT = kb.dram("BT", [512, ZCOLS], F32)
    ZTb = [Buf(None, f"zt{t}") for t in range(NT_ALL)]
    BTb = [Buf(None, f"bt{t}") for t in range(NT_ALL)]
    ZPAD = Buf(None, "ztpad")
    ZTv = ZT.rearrange("(c p) t -> p c t", p=128)
    BTv = BT.rearrange("(c p) t -> p c t", p=128)
    with kb.phase():
        KT = kb.T([128, 8, NT_ALL * 128], BF16, "KT_all")
        VP = kb.T([128, NT_ALL, 8, 65], BF16, "VP_all")
        kmax = kb.T([128, 8], F32, "kmax")
        kb.v("gpsimd", "memset", [], [VP], VP[:], 1.0)
        kb.v("gpsimd", "memset", [], [kmax], kmax[:], 0.0)
        rope = I["rope"]
        with kb.phase():
            stage = Rot([kb.T([128, 1024], F32, f"stg{i}") for i in range(2)])
            w_in = kb.T([128, 8, 1952], BF16, "w_in")
            load_w_bf16(kb, w_in, I["even_w_in"][j], I["_even_w_in"], stage, 1024, 1952)
            w_ukv = kb.T([128, 1, 1024], BF16, "w_ukv")
            load_w_bf16(kb, w_ukv, I["mla_w_ukv"][j], I["_mla_w_ukv"], stage, 128, 1024)
            kvg = kb.T([128, 128], F32, "kvg")
            load_bcast(kb, kvg, I["mla_kv_norm"][j], [I["_mla_kv_norm"]])
            tmpm = kb.T([128, D], F32, "tmpm")
            G1 = [kb.T([128, D], F32, f"G1_{i}") for i in range(2)]
            SH = [kb.T([128, D], F32, f"SH_{i}") for i in range(2)]
            for r in range(2):
                build_mod_tiles(kb, MOD.ap, MOD, I["norm1_g"][layer], I["_norm1_g"], r, 0, 1, G1[r], SH[r], tmpm)
            zero = kb.T([128, 4, 1], F32, "zero")
            kb.v("gpsimd", "memset", [], [zero], zero[:], 0.0)
            for col in (0, 257, 258, 4355):
                kb.dma(ZTv[:, :, col:col + 1], zero[:], [zero], [ZPAD], q="gpsimd", allow_slow_non_contiguous=True)
            xt = kb.T([128, D], F32, "xt")
            junk = kb.T([128, D], BF16, "junk")
            ss = kb.T([128, 1], F32, "ss")
            rstd = kb.T([128, 1], F32, "rstd")
            ss2 = kb.T([128, 1], F32, "ss2")
            rstd2 = kb.T([128, 1], F32, "rstd2")
            nb = kb.T([128, D], BF16, "nb")
            nT = kb.T([128, 8, 128], BF16, "nT")
            pm = Rot([kb.T([128, 416], F32, f"pm{i}") for i in range(2)])
            latn = kb.T([128, 128], BF16, "latn")
            klT = kb.T([128, 128], BF16, "klT")
            kext = Rot([kb.T([128, 8, 97], BF16, f"kext{i}") for i in range(2)])
            for kx in kext.items:
                kb.v("gpsimd", "memset", [], [kx], kx[:], 1.0)
            krot = kb.T([128, 32], F32, "krot")
            rtmp = kb.T([128, 4, 16], F32, "rtmp")
            cs = kb.T([128, 32], F32, "cs")
            ksqt = kb.T([128, 8, 96], F32, "ksqt")
            ksq = kb.T([128, 8], F32, "ksq")
            c_sb = kb.T([128, 4, 128], F32, "c_sb")
            z_sb = Rot([kb.T([128, 4, 128], F32, f"z_sb{i}") for i in range(2)])
            b_sb = Rot([kb.T([128, 4, 128], F32, f"b_sb{i}") for i in range(2)])
            psT = kb.psv(0, 1, [128, 8, 128], BF16, "psT")
            ps_mla = kb.psv(1, 1, [128, 416], F32, "ps_mla")
            ps_lat = kb.psv(2, 1, [128, 8, 128], BF16, "ps_lat")
            ps_kv = kb.psv(3, 2, [128, 8, 128], F32, "ps_kv")
            ps_conv = kb.psv(5, 3, [128, 12, 128], F32, "ps_conv")
            for tt in (DBG_TILES or range(NT_ALL)):
                isc = 1 if tt < 2 else 0
                sap, sbuf_ = src_tile(tt)
                norm_mod_T(kb, C, sap, sbuf_, G1[isc], SH[isc], xt, junk, ss, rstd, nb, psT, nT)
                for k in range(8):
                    kb.mm(ps_mla[:], nT[:, k, :], w_in[:, k, 0:416], [nT, w_in], [ps_mla], start=(k == 0), stop=(k == 7))
                pmt = pm.next()
                kb.act(pmt[:], ps_mla[:], AF.Copy, [ps_mla], [pmt])
                kb.dma(PQ[tt].ap, pmt[:, 0:256], [pmt], [PQ[tt]], q="gpsimd")
                if "kv" in SKIP:
                    continue
                kb.act(junk[:, 0:128], pmt[:, 256:384], AF.Square, [pmt], [junk, ss2], accum_out=ss2[:])
                rstd_of(kb, ss2, rstd2, 128)
                kb.v("vector", "scalar_tensor_tensor", [pmt, rstd2, kvg], [latn], latn[:], pmt[:, 256:384],
                     rstd2[:, 0:1], kvg[:], ALU.mult, ALU.mult)
                kb.tr(ps_lat[:, 0, :], latn[:], C["ident_bf"][:], [latn, C["ident_bf"]], [ps_lat])
                kb.v("vector", "tensor_copy", [ps_lat], [klT], klT[:], ps_lat[:, 0, :])
                if "kv2" in SKIP:
                    continue
                kb.mm(ps_kv[:, 0:4, :], klT[:], w_ukv[:, 0, 0:512], [klT, w_ukv], [ps_kv])
                kb.mm(ps_kv[:, 4:8, :], klT[:], w_ukv[:, 0, 512:1024], [klT, w_ukv], [ps_kv])
                if "kv3" in SKIP:
                    continue
                kx = kext.next()
                for hb in range(2):
                    hs = slice(hb * 4, hb * 4 + 4)
                    kb.act(kx[:, hs, 0:64], ps_kv[:, hs, 0:64], AF.Copy, [ps_kv], [kx], scale=MLA_SCALE)
                    kb.v("vector", "tensor_copy", [ps_kv], [VP], VP[:, tt, hs, 0:64], ps_kv[:, hs, 64:128])
                if "rope" in SKIP:
                    continue
                if tt >= 2:
                    kb.dma(cs[:], rope[(tt - 2) * 128:(tt - 1) * 128, :], [I["_rope"]], [cs], q="gpsimd")
                    x1 = pmt[:, 384:400]
                    x2 = pmt[:, 400:416]
                    kb.v("gpsimd", "tensor_tensor", [pmt, cs], [rtmp], rtmp[:, 0, :], x1, cs[:, 0:16], ALU.mult)
                    kb.v("gpsimd", "tensor_tensor", [pmt, cs], [rtmp], rtmp[:, 1, :], x2, cs[:, 16:32], ALU.mult)
                    kb.v("gpsimd", "tensor_tensor", [pmt, cs], [rtmp], rtmp[:, 2, :], x1, cs[:, 16:32], ALU.mult)
                    kb.v("gpsimd", "tensor_tensor", [pmt, cs], [rtmp], rtmp[:, 3, :], x2, cs[:, 0:16], ALU.mult)
                    kb.v("vector", "tensor_tensor", [rtmp], [krot], krot[:, 0:16], rtmp[:, 0, :], rtmp[:, 1, :], ALU.subtract)
                    kb.v("vector", "tensor_tensor", [rtmp], [krot], krot[:, 16:32], rtmp[:, 2, :], rtmp[:, 3, :], ALU.add)
                else:
                    kb.v("vector", "tensor_copy", [pmt], [krot], krot[:], pmt[:, 384:416])
                kb.act(kx[:, :, 64:96], krot[:].unsqueeze(1).to_broadcast([128, 8, 32]), AF.Copy, [krot], [kx],
                       scale=MLA_SCALE)
                if "ksq" in SKIP:
                    continue
                kb.v("vector", "tensor_tensor", [kx], [ksqt], ksqt[:], kx[:, :, 0:96], kx[:, :, 0:96], ALU.mult)
                kb.v("vector", "tensor_reduce", [ksqt], [ksq], ksq[:], ksqt[:], AX.X, ALU.add)
                kb.v("vector", "tensor_tensor", [ksq, kmax], [kmax], kmax[:], kmax[:], ksq[:], ALU.max)
                for h in range(8):
                    kb.tr(ps_lat[0:97, h, :], kx[:, h, :], C["ident_bf"][:], [kx, C["ident_bf"]], [ps_lat])
                kb.act(KT[0:97, :, tt * 128:(tt + 1) * 128], ps_lat[0:97, :, :], AF.Copy, [ps_lat], [KT])
                if "conv" in SKIP:
                    continue
                for fc in range(12):
                    for k in range(8):
                        kb.mm(ps_conv[:, fc, :], w_in[:, k, 416 + fc * 128:416 + (fc + 1) * 128], nT[:, k, :],
                              [w_in, nT], [ps_conv], start=(k == 0), stop=(k == 7))
                kb.act(c_sb[:], ps_conv[:, 4:8, :], AF.Copy, [ps_conv], [c_sb])
                zt = z_sb.next()
                bt = b_sb.next()
                kb.v("vector", "tensor_tensor", [ps_conv, c_sb], [zt], zt[:], ps_conv[:, 8:12, :], c_sb[:], ALU.mult)
                kb.act(bt[:], ps_conv[:, 0:4, :], AF.Copy, [ps_conv], [bt])
                c0 = zcol(tt)
                kb.dma(ZTv[:, :, c0:c0 + 128], zt[:], [zt], [ZTb[tt]], q="gpsimd")
                kb.dma(BTv[:, :, c0:c0 + 128], bt[:], [bt], [BTb[tt]], q="gpsimd")
        if kb.stop_after == "E1":
            return
        with kb.phase():
            stage = Rot([kb.T([128, 1024], F32, f"stg{i}") for i in range(2)])
            w_uq = kb.T([128, 2, 768], BF16, "w_uq")
            load_w_bf16(kb, w_uq, I["mla_w_uq"][j], I["_mla_w_uq"], stage, 256, 768)
            w_out = kb.T([128, 8, 1024], BF16, "w_out")
            load_w_bf16(kb, w_out, I["even_w_out"][j], I["_even_w_out"], stage, 1024, 1024)
            qg = kb.T([128, 256], F32, "qg")
            load_bcast(kb, qg, I["mla_q_norm"][j], [I["_mla_q_norm"]])
            M2 = [kb.T([128, D], F32, f"M2_{i}") for i in range(2)]
            for r in range(2):
                load_bcast(kb, M2[r], MOD.ap[r, 2 * D:3 * D], [MOD])
            cw = kb.T([128, 3, 4], F32, "cw")
            for w_ in range(3):
                for c_ in range(4):
                    kb.dma(cw[:, w_, c_:c_ + 1], I["conv_w"][j][w_, c_ * 128:(c_ + 1) * 128].unsqueeze(1),
                           [I["_conv_w"]], [cw], q="gpsimd", allow_slow_non_contiguous=True)
            psx = kb.psv(6, 1, [128, 128], F32, "psx")
            kmr = kb.T([128, 1], F32, "kmr")
            kb.v("vector", "tensor_reduce", [kmax], [kmr], kmr[:], kmax[:], AX.X, ALU.max)
            kmb = kb.T([128, 128], F32, "kmb")
            kb.v("vector", "tensor_copy", [kmr], [kmb], kmb[:], kmr[:, 0:1].to_broadcast([128, 128]))
            kb.tr(psx[:], kmb[:], C["ident_f"][:], [kmb, C["ident_f"]], [psx])
            ksm = kb.T([128, 1], F32, "ksm")
            kb.v("vector", "tensor_reduce", [psx], [ksm], ksm[:], psx[:], AX.X, ALU.max)
            pq = kb.T([128, 256], F32, "pq")
            junk = kb.T([128, 256], BF16, "junk")
            ss = kb.T([128, 1], F32, "ss")
            rstd = kb.T([128, 1], F32, "rstd")
            qn = kb.T([128, 256], BF16, "qn")
            qlT = kb.T([128, 2, 128], BF16, "qlT")
            q_sb = kb.T([128, 8, 96], F32, "q_sb")
            cs = kb.T([128, 32], F32, "cs")
            rt = kb.T([128, 4, 8, 16], F32, "rt")
            qsqt = kb.T([128, 8, 96], F32, "qsqt")
            qsq = kb.T([128, 8], F32, "qsq")
            qext = kb.T([128, 8, 97], BF16, "qext")
            QT = kb.T([128, 8, 512], BF16, "QT")
            PT = Rot([kb.T([128, 512], BF16, f"PT{i}") for i in range(4)])
            rec = kb.T([128, 4], F32, "rec")
            mix_tok = [kb.T([128, 512], BF16, f"mix{i}") for i in range(4)]
            mixT = kb.T([128, 8, 512], BF16, "mixT")
            zw = kb.T([128, 4, 514], F32, "zw")
            bw = kb.T([128, 4, 512], F32, "bw")
            yc_ = kb.T([128, 512], F32, "yconv")
            hres = stage.items[0]
            ytmp = stage.items[1]
            ps_S = Rot([kb.psv(i, 1, [128, 512], F32, f"psS{i}") for i in range(2)])
            ps_O = [kb.psv(2 + i, 1, [128, 65], F32, f"psO{i}") for i in range(4)]
            ps_q = kb.psv(2, 2, [128, 768], F32, "ps_q")
            ps_t = kb.psv(6, 1, [128, 8, 128], BF16, "ps_t")
            ps_y = kb.psv(6, 2, [128, 1024], F32, "ps_y")
            bank = {i: None for i in range(8)}
            blocks = []
            if need_ctx:
                blocks.append(([0, 1], [0, 1]))
            for b in range(8):
                blocks.append(([2 + 4 * b + i for i in range(4)], list(range(NT_ALL))))
            for tiles, ktiles in blocks:
                nq = len(tiles) * 128
                for qi, tt in enumerate(tiles):
                    kb.dma(pq[:], PQ[tt].ap, [PQ[tt]], [pq])
                    kb.act(junk[:, 0:256], pq[:], AF.Square, [pq], [junk, ss], accum_out=ss[:])
                    rstd_of(kb, ss, rstd, 256)
                    kb.v("vector", "scalar_tensor_tensor", [pq, rstd, qg], [qn], qn[:], pq[:], rstd[:, 0:1], qg[:],
                         ALU.mult, ALU.mult)
                    for k in range(2):
                        kb.tr(ps_t[:, k, :], qn[:, k * 128:(k + 1) * 128], C["ident_bf"][:], [qn, C["ident_bf"]], [ps_t])
                    kb.v("vector", "tensor_copy", [ps_t], [qlT], qlT[:], ps_t[:, 0:2, :])
                    for k in range(2):
                        kb.mm(ps_q[:, 0:512], qlT[:, k, :], w_uq[:, k, 0:512], [qlT, w_uq], [ps_q, ps_O[0]],
                              start=(k == 0), stop=(k == 1))
                    for k in range(2):
                        kb.mm(ps_q[:, 512:768], qlT[:, k, :], w_uq[:, k, 512:768], [qlT, w_uq], [ps_q, ps_O[1]],
                              start=(k == 0), stop=(k == 1))
                    q_flat = q_sb[:].rearrange("p h d -> p (h d)")
                    kb.act(q_flat[:, 0:512], ps_q[:, 0:512], AF.Copy, [ps_q, ps_O[0], ps_O[1]], [q_sb])
                    kb.act(q_flat[:, 512:768], ps_q[:, 512:768], AF.Copy, [ps_q, ps_O[0], ps_O[1]], [q_sb])
                    kb.v("vector", "tensor_tensor", [q_sb], [qsqt], qsqt[:], q_sb[:], q_sb[:], ALU.mult)
                    kb.v("vector", "tensor_reduce", [qsqt], [qsq], qsq[:], qsqt[:], AX.X, ALU.add)
                    kb.act(qsq[:], qsq[:], AF.Sqrt, [qsq, ksm], [qsq], scale=ksm[:, 0:1])
                    kb.v("vector", "tensor_scalar_mul", [qsq], [qext], qext[:, :, 96], qsq[:], -1.0)
                    kb.v("gpsimd", "tensor_copy", [q_sb], [qext], qext[:, :, 0:64], q_sb[:, :, 0:64])
                    if tt >= 2:
                        kb.dma(cs[:], rope[(tt - 2) * 128:(tt - 1) * 128, :], [I["_rope"]], [cs], q="gpsimd")
                        x1 = q_sb[:, :, 64:80]
                        x2 = q_sb[:, :, 80:96]
                        cosb = cs[:, 0:16].unsqueeze(1).to_broadcast([128, 8, 16])
                        sinb = cs[:, 16:32].unsqueeze(1).to_broadcast([128, 8, 16])
                        kb.v("gpsimd", "tensor_tensor", [q_sb, cs], [rt], rt[:, 0], x1, cosb, ALU.mult)
                        kb.v("gpsimd", "tensor_tensor", [q_sb, cs], [rt], rt[:, 1], x2, sinb, ALU.mult)
                        kb.v("gpsimd", "tensor_tensor", [q_sb, cs], [rt], rt[:, 2], x1, sinb, ALU.mult)
                        kb.v("gpsimd", "tensor_tensor", [q_sb, cs], [rt], rt[:, 3], x2, cosb, ALU.mult)
                        kb.v("vector", "tensor_tensor", [rt], [qext], qext[:, :, 64:80], rt[:, 0], rt[:, 1], ALU.subtract)
                        kb.v("vector", "tensor_tensor", [rt], [qext], qext[:, :, 80:96], rt[:, 2], rt[:, 3], ALU.add)
                    else:
                        kb.v("vector", "tensor_copy", [q_sb], [qext], qext[:, :, 64:96], q_sb[:, :, 64:96])
                    for h in range(8):
                        kb.tr(ps_t[0:97, h, :], qext[:, h, :], C["ident_bf"][:], [qext, C["ident_bf"]], [ps_t])
                    kb.act(QT[0:97, :, qi * 128:(qi + 1) * 128], ps_t[0:97, :, :], AF.Copy, [ps_t], [QT])
                nqs = len(tiles)
                for h in range(8):
                    def S_(ki, kt):
                        pS = ps_S.next()
                        kb.mm(pS[:, 0:nq], KT[0:97, h, kt * 128:(kt + 1) * 128], QT[0:97, h, 0:nq], [KT, QT], [pS])
                        pt = PT.next()
                        kb.act(pt[:, 0:nq], pS[:, 0:nq], AF.Exp, [pS], [pt])
                        return (ki, kt, pt)

                    def PV_(st):
                        ki, kt, pt = st
                        for qs in range(nqs):
                            kb.mm(ps_O[qs][:], pt[:, qs * 128:(qs + 1) * 128], VP[:, kt, h, :], [pt, VP], [ps_O[qs]],
                                  start=(ki == 0), stop=(ki == len(ktiles) - 1))

                    pend = []
                    for ki, kt in enumerate(ktiles):
                        pend.append(S_(ki, kt))
                        if len(pend) > 2:
                            PV_(pend.pop(0))
                    while pend:
                        PV_(pend.pop(0))
                    for qs in range(nqs):
                        kb.v("vector", "reciprocal", [ps_O[qs]], [rec], rec[:, qs:qs + 1], ps_O[qs][:, 64:65])
                        kb.act(mix_tok[qs][:, h * 64:(h + 1) * 64], ps_O[qs][:, 0:64], AF.Copy, [ps_O[qs], rec],
                               [mix_tok[qs]], scale=rec[:, qs:qs + 1])
                for qi in range(nqs):
                    for c_ in range(4):
                        kb.tr(ps_t[:, c_, :], mix_tok[qi][:, c_ * 128:(c_ + 1) * 128], C["ident_bf"][:],
                              [mix_tok[qi], C["ident_bf"]], [ps_t])
                    kb.v("vector", "tensor_copy", [ps_t], [mixT], mixT[:, 0:4, qi * 128:(qi + 1) * 128], ps_t[:, 0:4, :])
                c0 = zcol(tiles[0])
                kb.dma(zw[:, :, 0:nq + 2], ZTv[:, :, c0 - 1:c0 + nq + 1], ZTb + [ZPAD], [zw])
                kb.dma(bw[:, :, 0:nq], BTv[:, :, c0:c0 + nq], BTb, [bw], q="gpsimd")
                for c_ in range(4):
                    eng = "vector"
                    kb.v(eng, "tensor_scalar", [zw, cw], [yc_], yc_[:, 0:nq], zw[:, c_, 0:nq], cw[:, 0, c_:c_ + 1], None, ALU.mult)
                    kb.v(eng, "scalar_tensor_tensor", [zw, cw, yc_], [yc_], yc_[:, 0:nq], zw[:, c_, 1:nq + 1],
                         cw[:, 1, c_:c_ + 1], yc_[:, 0:nq], ALU.mult, ALU.add)
                    kb.v(eng, "scalar_tensor_tensor", [zw, cw, yc_], [yc_], yc_[:, 0:nq], zw[:, c_, 2:nq + 2],
                         cw[:, 2, c_:c_ + 1], yc_[:, 0:nq], ALU.mult, ALU.add)
                    kb.v(eng, "tensor_tensor", [yc_, bw], [mixT], mixT[:, 4 + c_, 0:nq], yc_[:, 0:nq], bw[:, c_, 0:nq], ALU.mult)
                for qi, tt in enumerate(tiles):
                    isc = 1 if tt < 2 else 0
                    sap, sbuf_ = src_tile(tt)
                    kb.dma(hres[:], sap, [sbuf_], [hres])
                    for half in range(2):
                        for c_ in range(8):
                            kb.mm(ps_y[:, half * 512:(half + 1) * 512], mixT[:, c_, qi * 128:(qi + 1) * 128],
                                  w_out[:, c_, half * 512:(half + 1) * 512], [mixT, w_out], [ps_y, ps_t],
                                  start=(c_ == 0), stop=(c_ == 7))
                    for half in range(2):
                        hs = slice(half * 512, (half + 1) * 512)
                        kb.v("vector", "tensor_tensor", [ps_y, ps_t, M2[isc]], [ytmp], ytmp[:, hs], ps_y[:, hs], M2[isc][:, hs], ALU.mult)
                    kb.v("gpsimd", "tensor_tensor", [ytmp, hres], [ytmp], ytmp[:], ytmp[:], hres[:], ALU.add)
                    kb.dma(H[tt].ap, ytmp[:], [ytmp], [H[tt]], q="gpsimd")


def layer_odd(kb, C, I, layer, MOD, src_tile, H, bg=None):
    j = 0
    QKT = kb.dram_tiles("QKT", NT_ALL, [128, 8, 128], BF16)
    KHd = kb.dram_tiles("KHd", NT_ALL, [128, 8, 128], BF16)
    VPd = kb.dram_tiles("VPd", NT_ALL, [128, 4, 257], BF16)
    SCd = kb.dram_tiles("SCd", NT_ALL, [128, 32], F32)
    OGd = kb.dram_tiles("OGd", NT_ALL, [128, D], F32)
    HBd = kb.dram_tiles("HBd", NT_ALL, [128, D], F32)
    HSd = kb.dram_tiles("HSd", NT_ALL, [128, D], F32)
    lat_tiles = list(range(2, NT_ALL))
    with kb.phase():
        stage = Rot([kb.T([128, 1024], F32, f"stg{i}") for i in range(2)])
        w_in = kb.T([128, 8, 3088], BF16, "w_in_o")
        load_w_bf16(kb, w_in, I["odd_w_in"][j], I["_odd_w_in"], stage, 1024, 3088)
        tmpm = stage.items[0]
        G1 = [kb.T([128, D], F32, f"G1_{i}") for i in range(2)]
        SH = [kb.T([128, D], F32, f"SH_{i}") for i in range(2)]
        for r in range(2):
            build_mod_tiles(kb, MOD.ap, MOD, I["norm1_g"][layer], I["_norm1_g"], r, 0, 1, G1[r], SH[r], tmpm)
        gbias = kb.T([128, 16], F32, "gbias")
        load_bcast(kb, gbias, I["mlstm_gate_b"][j], [I["_mlstm_gate_b"]])
        triU = kb.T([128, 128], F32, "triU")
        triL = kb.T([128, 128], F32, "triL")
        ones = kb.T([128, 128], F32, "ones")
        kb.v("vector", "tensor_single_scalar", [C["jmp"]], [triU], triU[:], C["jmp"][:], 0.0, ALU.is_ge)
        kb.v("vector", "tensor_single_scalar", [C["jmp"]], [triL], triL[:], C["jmp"][:], 0.0, ALU.is_le)
        kb.v("gpsimd", "memset", [], [ones], ones[:], 1.0)
        xt = kb.T([128, D], F32, "xt")
        junk = kb.T([128, D], BF16, "junk")
        ss = kb.T([128, 1], F32, "ss")
        rstd = kb.T([128, 1], F32, "rstd")
        nb = kb.T([128, D], BF16, "nb")
        nT = kb.T([128, 8, 128], BF16, "nT")
        qkT = Rot([kb.T([128, 8, 128], BF16, f"qkT{i}") for i in range(2)])
        khat = Rot([kb.T([128, 8, 128], BF16, f"khat{i}") for i in range(2)])
        vp = Rot([kb.T([128, 4, 257], BF16, f"vp{i}") for i in range(2)])
        for v_ in vp.items:
            kb.v("gpsimd", "memset", [], [v_], v_[:], 1.0)
        og = Rot([kb.T([128, D], F32, f"og{i}") for i in range(2)])
        sc = Rot([kb.T([128, 32], F32, f"sc{i}") for i in range(2)])
        gb = kb.T([128, 16], F32, "gb")
        nl = kb.T([128, 8], F32, "nl")
        igc = kb.T([128, 8], F32, "igc")
        t1 = kb.T([128, 8], F32, "t1")
        t2 = kb.T([128, 8], F32, "t2")
        psT = kb.psv(0, 1, [128, 8, 128], BF16, "psT")
        ps_g = kb.psv(0, 1, [128, 64], F32, "ps_g", root=psT)
        ps_f = [kb.psv(1 + i, 1, [128, 4, 128], F32, f"ps_f{i}") for i in range(2)]
        ps_k = kb.psv(3, 1, [128, 512], F32, "ps_k")
        ps_v = [kb.psv(4 + i, 1, [128, 512], F32, f"ps_v{i}") for i in range(2)]
        ps_o = [kb.psv(6 + i, 1, [128, 512], F32, f"ps_o{i}") for i in range(2)]
        bg_units = []
        if bg is not None:
            bg_units = bg([kb.psv(0, 1, [128, 8, 128], BF16, "pstb", root=psT)])
        for tt in range(NT_ALL):
            for _ in range(4):
                if bg_units:
                    bg_units.pop(0)()
            isc = 1 if tt < 2 else 0
            sap, sbuf_ = src_tile(tt)
            norm_mod_T(kb, C, sap, sbuf_, G1[isc], SH[isc], xt, junk, ss, rstd, nb, psT, nT)
            for hh in range(8):
                for k in range(8):
                    kb.mm(ps_f[hh // 4][:, hh % 4, :], w_in[:, k, hh * 128:(hh + 1) * 128], nT[:, k, :], [w_in, nT],
                          [ps_f[hh // 4]], start=(k == 0), stop=(k == 7))
            qk = qkT.next()
            kb.act(qk[:, 0:4, :], ps_f[0][:], AF.Copy, [ps_f[0]], [qk], scale=128 ** -0.5)
            kb.v("vector", "tensor_copy", [ps_f[1]], [qk], qk[:, 4:8, :], ps_f[1][:])
            kb.dma(QKT[tt].ap, qk[:], [qk], [QKT[tt]], q="gpsimd")
            for k in range(8):
                kb.mm(ps_k[:], nT[:, k, :], w_in[:, k, 512:1024], [nT, w_in], [ps_k], start=(k == 0), stop=(k == 7))
            for hf in range(2):
                for k in range(8):
                    kb.mm(ps_v[hf][:], nT[:, k, :], w_in[:, k, 1024 + hf * 512:1536 + hf * 512], [nT, w_in], [ps_v[hf]],
                          start=(k == 0), stop=(k == 7))
            for k in range(8):
                kb.mm(ps_g[:, 0:16], nT[:, k, :], w_in[:, k, 2048:2064], [nT, w_in], [ps_g], start=(k == 0), stop=(k == 7))
            if tt >= 2:
                for hf in range(2):
                    for k in range(8):
                        kb.mm(ps_o[hf][:], nT[:, k, :], w_in[:, k, 2064 + hf * 512:2576 + hf * 512], [nT, w_in],
                              [ps_o[hf]], start=(k == 0), stop=(k == 7))
                o_ = og.next()
                for hf in range(2):
                    kb.act(o_[:, hf * 512:(hf + 1) * 512], ps_o[hf][:], AF.Sigmoid, [ps_o[hf]], [o_])
                kb.dma(OGd[tt].ap, o_[:], [o_], [OGd[tt]], q="gpsimd")
            kb.v("vector", "tensor_tensor", [ps_g, gbias], [gb], gb[:], ps_g[:, 0:16], gbias[:], ALU.add)
            gb4 = gb[:].rearrange("p (d two h) -> p d two h", d=2, two=2)
            nl3 = nl[:].rearrange("p (d h) -> p d h", d=2)
            kb.act(nl3, gb4[:, :, 1, :], AF.Exp, [gb], [nl], scale=-1.0)
            kb.act(nl[:], nl[:], AF.Ln, [nl], [nl], bias=kb.one_t[:, 0:1])
            kb.v("vector", "tensor_copy", [gb], [igc], igc[:].rearrange("p (d h) -> p d h", d=2), gb4[:, :, 0, :])
            kb.mm(ps_g[:, 16:20], triU[:], nl[:, 0:4], [triU, nl], [ps_g])
            kb.mm(ps_g[:, 20:24], triL[:], nl[:, 4:8], [triL, nl], [ps_g])
            kb.mm(ps_g[:, 24:32], ones[:], nl[:], [ones, nl], [ps_g])
            s_ = sc.next()
            kb.v("vector", "tensor_tensor", [igc, ps_g], [t1], t1[:], igc[:], ps_g[:, 16:24], ALU.add)
            kb.v("vector", "tensor_tensor", [t1, ps_g], [t2], t2[:], t1[:], ps_g[:, 24:32], ALU.subtract)
            kb.act(s_[:, 0:8], t1[:], AF.Exp, [t1], [s_])
            kb.act(s_[:, 8:16], ps_g[:, 16:24], AF.Exp, [ps_g], [s_], scale=-1.0)
            kb.act(s_[:, 16:24], t2[:], AF.Exp, [t2], [s_])
            kb.act(s_[:, 24:32], ps_g[:, 24:32], AF.Exp, [ps_g], [s_], scale=-1.0)
            kb.dma(SCd[tt].ap, s_[:], [s_], [SCd[tt]], q="gpsimd")
            kh = khat.next()
            for c in range(8):
                h = c % 4
                kb.act(kh[:, c, :], ps_k[:, h * 128:(h + 1) * 128], AF.Copy, [ps_k, s_], [kh], scale=s_[:, 16 + c:17 + c])
            kb.dma(KHd[tt].ap, kh[:], [kh], [KHd[tt]], q="gpsimd")
            v_ = vp.next()
            for hf in range(2):
                kb.v("vector", "tensor_copy", [ps_v[hf]], [v_], v_[:, hf * 2:hf * 2 + 2, 0:256],
                     ps_v[hf][:].rearrange("p (h d) -> p h d", h=2))
            kb.dma(VPd[tt].ap, v_[:], [v_], [VPd[tt]], q="gpsimd")
        while bg_units:
            bg_units.pop(0)()
    with kb.phase():
        bg = None
        triU = kb.T([128, 128], F32, "triU")
        triL = kb.T([128, 128], F32, "triL")
        kb.v("vector", "tensor_single_scalar", [C["jmp"]], [triU], triU[:], C["jmp"][:], 0.0, ALU.is_ge)
        kb.v("vector", "tensor_single_scalar", [C["jmp"]], [triL], triL[:], C["jmp"][:], 0.0, ALU.is_le)
        Cst = [kb.T([128, 257], F32, f"Cst{h}") for h in range(4)]
        Cbf = [kb.T([128, 257], BF16, f"Cbf{h}") for h in range(4)]
        qk = Rot([kb.T([128, 8, 128], BF16, f"qk{i}") for i in range(3)])
        kh = Rot([kb.T([128, 4, 128], BF16, f"kh{i}") for i in range(3)])
        vp = Rot([kb.T([128, 4, 257], BF16, f"vp{i}") for i in range(3)])
        sc = Rot([kb.T([128, 32], F32, f"sc{i}") for i in range(3)])
        PTm = Rot([kb.T([128, 128], BF16, f"PTm{i}") for i in range(3)])
        t4 = kb.T([128, 4], F32, "t4")
        r4 = kb.T([128, 4], F32, "r4")
        hout = Rot([kb.T([128, D], F32, f"hout{i}") for i in range(2)])
        hb = Rot([kb.T([128, D], F32, f"hb{i}") for i in range(2)])
        ps_S = Rot([kb.psv(i, 1, [128, 128], F32, f"psS{i}") for i in range(2)])
        ps_N = [kb.psv(2 + h, 1, [128, 257], F32, f"psN{h}") for h in range(4)]
        ps_U = Rot([kb.psv(6 + i, 1, [128, 257], F32, f"psU{i}") for i in range(2)])
        bg_units = []
        if bg is not None:
            bg_units = bg([kb.psv(i, 1, [128, 8, 128], BF16, f"pstb{i}", root=ps_S.items[i]) for i in range(2)])
        for d in (1, 0):
            order = [0, 1] + lat_tiles if d == 0 else [1, 0] + lat_tiles[::-1]
            mask = triU if d == 0 else triL
            for h in range(4):
                kb.v("gpsimd", "memset", [], [Cst[h]], Cst[h][:], 0.0)
                kb.v("gpsimd", "memset", [], [Cbf[h]], Cbf[h][:], 0.0)
            for oi, tt in enumerate(order):
                q_ = qk.next(); k_ = kh.next(); v_ = vp.next(); s_ = sc.next()
                kb.dma(q_[:], QKT[tt].ap, [QKT[tt]], [q_])
                kb.dma(k_[:], KHd[tt].ap[:, d * 4:(d + 1) * 4, :], [KHd[tt]], [k_])
                kb.dma(v_[:], VPd[tt].ap, [VPd[tt]], [v_])
                kb.dma(s_[:], SCd[tt].ap, [SCd[tt]], [s_])
                if tt >= 2:
                    for h in range(4):
                        c = d * 4 + h
                        pS = ps_S.next()
                        kb.mm(pS[:], q_[:, 4 + h, :], q_[:, h, :], [q_], [pS])
                        pt = PTm.next()
                        kb.v("vector", "scalar_tensor_tensor", [pS, s_, mask], [pt], pt[:], pS[:], s_[:, c:c + 1], mask[:],
                             ALU.mult, ALU.mult)
                        kb.mm(ps_N[h][:], pt[:], v_[:, h, :], [pt, v_], [ps_N[h]], start=True, stop=False)
                        kb.mm(ps_N[h][:], q_[:, h, :], Cbf[h][:], [q_, Cbf[h]], [ps_N[h]], start=False, stop=True)
                        kb.v("vector", "tensor_tensor", [ps_N[h], s_], [t4], t4[:, h:h + 1], ps_N[h][:, 256:257],
                             s_[:, 8 + c:9 + c], ALU.mult)
                    kb.act(t4[:], t4[:], AF.Abs, [t4], [t4])
                    kb.v("vector", "tensor_scalar_max", [t4], [r4], r4[:], t4[:], 1.0)
                    kb.v("vector", "reciprocal", [r4], [r4], r4[:], r4[:])
                    kb.v("vector", "tensor_tensor", [r4, s_], [r4], r4[:], r4[:], s_[:, 8 + d * 4:12 + d * 4], ALU.mult)
                    ho = hout.next()
                    for h in range(4):
                        kb.act(ho[:, h * 256:(h + 1) * 256], ps_N[h][:, 0:256], AF.Copy, [ps_N[h], r4], [ho],
                               scale=r4[:, h:h + 1])
                    if d == 1:
                        kb.dma(HBd[tt].ap, ho[:], [ho], [HBd[tt]], q="gpsimd")
                    else:
                        hb_ = hb.next()
                        kb.dma(hb_[:], HBd[tt].ap, [HBd[tt]], [hb_])
                        kb.v("gpsimd", "tensor_tensor", [ho, hb_], [hb_], hb_[:], ho[:], hb_[:], ALU.add)
                        kb.dma(HSd[tt].ap, hb_[:], [hb_], [HSd[tt]], q="gpsimd")
                for _ in range(2):
                    if bg_units:
                        bg_units.pop(0)()
                if oi == len(order) - 1:
                    continue
                for h in range(4):
                    c = d * 4 + h
                    pU = ps_U.next()
                    kb.mm(pU[:], k_[:, h, :], v_[:, h, :], [k_, v_], [pU])
                    kb.v("vector", "scalar_tensor_tensor", [Cst[h], s_, pU], [Cst[h]], Cst[h][:], Cst[h][:],
                         s_[:, 24 + c:25 + c], pU[:], ALU.mult, ALU.add)
                    kb.act(Cbf[h][:], Cst[h][:], AF.Copy, [Cst[h]], [Cbf[h]])
        while bg_units:
            bg_units.pop(0)()
    with kb.phase():
        stage = Rot([kb.T([128, 1024], F32, f"stg{i}") for i in range(2)])
        w_out = kb.T([128, 8, 1024], BF16, "w_out_o")
        load_w_bf16(kb, w_out, I["odd_w_out"][j], I["_odd_w_out"], stage, 1024, 1024)
        hg = kb.T([128, D], F32, "hg")
        load_bcast(kb, hg, I["mlstm_head_g"][j], [I["_mlstm_head_g"]])
        M2 = kb.T([128, D], F32, "M2")
        load_bcast(kb, M2, MOD.ap[0, 2 * D:3 * D], [MOD])
        hs = Rot([kb.T([128, D], F32, f"hs{i}") for i in range(2)])
        og = Rot([kb.T([128, D], F32, f"og{i}") for i in range(2)])
        hres = Rot([kb.T([128, D], F32, f"hres{i}") for i in range(2)])
        sq = kb.T([128, D], F32, "sq")
        ss4 = kb.T([128, 4], F32, "ss4")
        rs4 = kb.T([128, 4], F32, "rs4")
        mb = kb.T([128, D], BF16, "mb")
        mT = kb.T([128, 8, 128], BF16, "mT")
        yo = Rot([kb.T([128, D], F32, f"yo{i}") for i in range(2)])
        psT = kb.psv(0, 1, [128, 8, 128], BF16, "psT")
        ps_y = [kb.psv(1 + i, 1, [128, 512], F32, f"ps_y{i}") for i in range(2)]
        for tt in lat_tiles:
            h_ = hs.next(); o_ = og.next(); r_ = hres.next()
            kb.dma(h_[:], HSd[tt].ap, [HSd[tt]], [h_])
            kb.dma(o_[:], OGd[tt].ap, [OGd[tt]], [o_])
            sap, sbuf_ = src_tile(tt)
            kb.dma(r_[:], sap, [sbuf_], [r_])
            kb.v("gpsimd", "tensor_tensor", [h_], [sq], sq[:], h_[:], h_[:], ALU.mult)
            kb.v("vector", "tensor_reduce", [sq], [ss4], ss4[:], sq[:].rearrange("p (h d) -> p h d", h=4), AX.X, ALU.add)
            rstd_of(kb, ss4, rs4, 256)
            h3 = h_[:].rearrange("p (h d) -> p h d", h=4)
            kb.v("vector", "tensor_tensor", [h_, rs4], [h_], h3, h3, rs4[:].unsqueeze(2).to_broadcast([128, 4, 256]), ALU.mult)
            kb.v("gpsimd", "tensor_tensor", [o_, hg], [o_], o_[:], o_[:], hg[:], ALU.mult)
            kb.v("vector", "tensor_tensor", [h_, o_], [mb], mb[:], h_[:], o_[:], ALU.mult)
            for k in range(8):
                kb.tr(psT[:, k, :], mb[:, k * 128:(k + 1) * 128], C["ident_bf"][:], [mb, C["ident_bf"]], [psT])
            kb.act(mT[:], psT[:], AF.Copy, [psT], [mT])
            for hf in range(2):
                for k in range(8):
                    kb.mm(ps_y[hf][:], mT[:, k, :], w_out[:, k, hf * 512:(hf + 1) * 512], [mT, w_out], [ps_y[hf]],
                          start=(k == 0), stop=(k == 7))
            y_ = yo.next()
            for hf in range(2):
                hsl = slice(hf * 512, (hf + 1) * 512)
                kb.v("vector", "tensor_tensor", [ps_y[hf], M2], [y_], y_[:, hsl], ps_y[hf][:], M2[:, hsl], ALU.mult)
            kb.v("gpsimd", "tensor_tensor", [y_, r_], [y_], y_[:], y_[:], r_[:], ALU.add)
            kb.dma(H[tt].ap, y_[:], [y_], [H[tt]], q="gpsimd")


def peer_prep_units(kb, C, I, layer, UT, VB, ps_list, light=False):
    uf = Rot([kb.T([128, D], F32, f"uf{i}") for i in range(2)])
    ub = Rot([kb.T([128, D], BF16, f"ub{i}") for i in range(2)])
    utb = Rot([kb.T([128, 8, 128], BF16, f"utb{i}") for i in range(2)])
    vf = Rot([kb.T([128, D], F32, f"vf{i}") for i in range(2)])
    vb = Rot([kb.T([128, D], BF16, f"vb{i}") for i in range(2)])
    ps = Rot(ps_list)
    units = []
    for i in range(128):
        def unit(i=i):
            a = uf.next(); b = ub.next(); t = utb.next(); p = ps.next()
            lq = "gpsimd" if light else "sync"
            kb.dma(a[:], I["peer_u"][layer][i * 128:(i + 1) * 128, :], [I["_peer_u"]], [a], q=lq)
            if i % 2 == 0:
                kb.v("gpsimd", "tensor_copy", [a], [b], b[:], a[:])
            else:
                kb.act(b[:], a[:], AF.Copy, [a], [b])
            for k in range(8):
                kb.tr(p[:, k, :], b[:, k * 128:(k + 1) * 128], C["ident_bf"][:], [b, C["ident_bf"]], [p])
            if light:
                kb.act(t[:], p[:], AF.Copy, [p], [t])
            else:
                kb.v("vector", "tensor_copy", [p], [t], t[:], p[:])
            kb.dma(UT[i].ap, t[:], [t], [UT[i]], q="gpsimd")
            a2 = vf.next(); b2 = vb.next()
            kb.dma(a2[:], I["peer_v"][layer][i * 128:(i + 1) * 128, :], [I["_peer_v"]], [a2], q=lq)
            if light:
                kb.v("gpsimd", "tensor_copy", [a2], [b2], b2[:], a2[:])
            else:
                kb.v("vector", "tensor_copy", [a2], [b2], b2[:], a2[:])
            kb.dma(VB[i].ap, b2[:], [b2], [VB[i]], q="gpsimd")
        units.append(unit)
    return units


def peer_prep(kb, C, I, layer, UT, VB):
    with kb.phase():
        ps = [kb.psv(i, 1, [128, 8, 128], BF16, f"pst{i}") for i in range(4)]
        for u in peer_prep_units(kb, C, I, layer, UT, VB, ps):
            u()


def peer_route(kb, C, I, layer, MOD, tiles, src_tile, NTd, RTd):
    with kb.phase():
        stage = Rot([kb.T([128, 2048], F32, f"stg{i}") for i in range(2)])
        w_q = kb.T([128, 8, 2048], BF16, "w_q")
        load_w_bf16(kb, w_q, I["peer_w_q"][layer], I["_peer_w_q"], stage, 1024, 2048)
        skT = kb.T([128, 16, 128], BF16, "skT")
        skb = kb.T([128, 128], BF16, "skb")
        pst = kb.psv(0, 1, [128, 8, 128], BF16, "pst")
        for hp in range(16):
            st = stage.next()
            kb.dma(st[:, 0:128], I["peer_subkeys"][layer][hp // 2, hp % 2], [I["_peer_subkeys"]], [st])
            kb.v("vector", "tensor_copy", [st], [skb], skb[:], st[:, 0:128])
            kb.tr(pst[:, 0, :], skb[:], C["ident_bf"][:], [skb, C["ident_bf"]], [pst])
            kb.v("vector", "tensor_copy", [pst], [skT], skT[:, hp, :], pst[:, 0, :])
        tmpm = Buf(stage.items[0].ap[:, 0:D], "tmpm", root=stage.items[0])
        G1 = [kb.T([128, D], F32, f"G1_{i}") for i in range(2)]
        SH = [kb.T([128, D], F32, f"SH_{i}") for i in range(2)]
        rows = sorted({1 if tt < 2 else 0 for tt in tiles})
        for r in rows:
            build_mod_tiles(kb, MOD.ap, MOD, I["norm2_g"][layer], I["_norm2_g"], r, 3, 4, G1[r], SH[r], tmpm)
        xt = kb.T([128, D], F32, "xt")
        junk = kb.T([128, D], BF16, "junk")
        ss = kb.T([128, 1], F32, "ss")
        rstd = kb.T([128, 1], F32, "rstd")
        nb = kb.T([128, D], BF16, "nb")
        nT = Rot([kb.T([128, 8, 128], BF16, f"nT{i}") for i in range(2)])
        qT_rot = Rot([kb.T([128, 16, 128], BF16, f"qT_sb{i}") for i in range(2)])
        s_rot = Rot([kb.T([128, 16, 128], F32, f"s_sb{i}") for i in range(2)])
        s2 = kb.T([128, 16, 128], F32, "s2")
        s2b = [Buf(None, f"s2b{i}") for i in range(16)]
        stop = kb.T([128, 16, 16], F32, "stop")
        stopa = [Buf(None, f"stopa{i}") for i in range(16)]
        stopb = [Buf(None, f"stopb{i}") for i in range(16)]
        topa = [Buf(None, f"topa{i}") for i in range(8)]
        topb = [Buf(None, f"topb{i}") for i in range(8)]
        c2b = [Buf(None, f"c2b{i}") for i in range(8)]
        idx = kb.T([128, 16, 16], U32, "idx")
        idxf = kb.T([128, 16, 16], F32, "idxf")
        cand = kb.T([128, 8, 256], F32, "cand")
        c2 = kb.T([128, 8, 256], F32, "c2")
        top = kb.T([128, 8, 16], F32, "top")
        pos = kb.T([128, 8, 16], U32, "pos")
        au = kb.T([128, 8, 16], U32, "au")
        bu = kb.T([128, 8, 16], U32, "bu")
        af = kb.T([128, 8, 16], F32, "af")
        bf = kb.T([128, 8, 16], F32, "bf")
        E_rot = Rot([kb.T([128, 8, 16, 16], F32, f"E{i}") for i in range(2)])
        sel = kb.T([128, 3, 128], F32, "sel")
        gs = kb.T([128, 8], F32, "gs")
        rt_sb = Rot([kb.T([128, 3, 128], F32, f"rt_sb{i}") for i in range(2)])
        psT = kb.psv(0, 1, [128, 8, 128], BF16, "psT", root=pst)
        ps_qT = [kb.psv(i, 1, [128, 4, 128], F32, f"ps_qT{i}") for i in range(4)]
        ps_qT[0] = kb.psv(0, 1, [128, 4, 128], F32, "ps_qT0", root=pst)
        ps_s = [kb.psv(4 + i, 1, [128, 4, 128], F32, f"ps_s{i}") for i in range(4)]
        ps_r = kb.psv(4, 1, [128, 3, 128], F32, "ps_r", root=ps_s[0])
        iota16 = C["iota_j"][:, 0:16]
        def front(tt):
            isc = 1 if tt < 2 else 0
            sap, sbuf_ = src_tile(tt)
            nTt = nT.next()
            norm_mod_T(kb, C, sap, sbuf_, G1[isc], SH[isc], xt, junk, ss, rstd, nb, psT, nTt)
            kb.dma(NTd[tt].ap, nTt[:], [nTt], [NTd[tt]], q="gpsimd")
            qT_sb = qT_rot.next()
            s_sb = s_rot.next()
            for hp in range(16):
                pq_ = ps_qT[hp // 4]
                for k in range(8):
                    kb.mm(pq_[:, hp % 4, :], w_q[:, k, hp * 128:(hp + 1) * 128], nTt[:, k, :], [w_q, nTt], [pq_],
                          start=(k == 0), stop=(k == 7))
            for g in range(4):
                kb.act(qT_sb[:, g * 4:(g + 1) * 4, :], ps_qT[g][:], AF.Copy, [ps_qT[g]], [qT_sb])
            for hp in range(16):
                kb.mm(ps_s[hp // 4][:, hp % 4, :], qT_sb[:, hp, :], skT[:, hp, :], [qT_sb, skT], [ps_s[hp // 4]])
            for g in range(4):
                kb.act(s_sb[:, g * 4:(g + 1) * 4, :], ps_s[g][:], AF.Copy, [ps_s[g]], [s_sb])
            return s_sb

        def chain(tt, s_sb):
            for hp in range(16):
                kb.v("vector", "max", [s_sb], [stopa[hp]], stop[:, hp, 0:8], s_sb[:, hp, :])
            for hp in range(16):
                kb.v("vector", "match_replace", [stopa[hp], s_sb], [s2b[hp]], s2[:, hp, :], stop[:, hp, 0:8], s_sb[:, hp, :], -1e30)
            for hp in range(16):
                kb.v("vector", "max", [s2b[hp]], [stopb[hp]], stop[:, hp, 8:16], s2[:, hp, :])
            for hp in range(16):
                kb.v("vector", "max_index", [stopa[hp], s_sb], [idx], idx[:, hp, 0:8], stop[:, hp, 0:8], s_sb[:, hp, :])
            for hp in range(16):
                kb.v("vector", "max_index", [stopb[hp], s_sb], [idx], idx[:, hp, 8:16], stop[:, hp, 8:16], s_sb[:, hp, :])
            kb.v("vector", "tensor_copy", [idx], [idxf], idxf[:], idx[:])
            st4 = stop[:].rearrange("p (h two) k -> p h two k", two=2)
            if4 = idxf[:].rearrange("p (h two) k -> p h two k", two=2)
            cand4 = cand[:].rearrange("p h (a b) -> p h a b", a=16)
            kb.v("vector", "tensor_tensor", stopa + stopb, [cand], cand4,
                 st4[:, :, 0, :].unsqueeze(3).to_broadcast([128, 8, 16, 16]),
                 st4[:, :, 1, :].unsqueeze(2).to_broadcast([128, 8, 16, 16]), ALU.add)
            for h in range(8):
                kb.v("vector", "max", [cand], [topa[h]], top[:, h, 0:8], cand[:, h, :])
            for h in range(8):
                kb.v("vector", "match_replace", [topa[h], cand], [c2b[h]], c2[:, h, :], top[:, h, 0:8], cand[:, h, :], -1e30)
            for h in range(8):
                kb.v("vector", "max", [c2b[h]], [topb[h]], top[:, h, 8:16], c2[:, h, :])
            for h in range(8):
                kb.v("vector", "max_index", [topa[h], cand], [pos], pos[:, h, 0:8], top[:, h, 0:8], cand[:, h, :])
            for h in range(8):
                kb.v("vector", "max_index", [topb[h], cand], [pos], pos[:, h, 8:16], top[:, h, 8:16], cand[:, h, :])
            kb.v("vector", "tensor_single_scalar", [pos], [au], au[:], pos[:], 4, ALU.logical_shift_right)
            kb.v("vector", "tensor_single_scalar", [pos], [bu], bu[:], pos[:], 15, ALU.bitwise_and)
            kb.v("vector", "tensor_copy", [au], [af], af[:], au[:])
            kb.v("vector", "tensor_copy", [bu], [bf], bf[:], bu[:])
            io4 = iota16.unsqueeze(1).unsqueeze(1).to_broadcast([128, 8, 16, 16])
            for which, sel_f in ((0, af), (1, bf)):
                E = E_rot.next()
                kb.v("vector", "tensor_tensor", [sel_f, C["iota_j"]], [E], E[:],
                     sel_f[:].unsqueeze(3).to_broadcast([128, 8, 16, 16]), io4, ALU.is_equal)
                kb.v("vector", "tensor_tensor", [E, idxf], [E], E[:], E[:],
                     if4[:, :, which, :].unsqueeze(2).to_broadcast([128, 8, 16, 16]), ALU.mult)
                kb.v("vector", "tensor_reduce", [E], [sel], sel[:, which, :].rearrange("p (h k) -> p h k", h=8),
                     E[:], AX.X, ALU.add)
            g3 = sel[:, 2, :].rearrange("p (h k) -> p h k", h=8)
            kb.v("vector", "tensor_tensor", topa + topb, [sel], g3, top[:], top[:, :, 0:1].to_broadcast([128, 8, 16]), ALU.subtract)
            kb.act(sel[:, 2, :], sel[:, 2, :], AF.Exp, [sel], [sel])
            kb.v("vector", "tensor_reduce", [sel], [gs], gs[:], g3, AX.X, ALU.add)
            kb.v("vector", "reciprocal", [gs], [gs], gs[:], gs[:])
            kb.v("vector", "tensor_tensor", [sel, gs], [sel], g3, g3, gs[:].unsqueeze(2).to_broadcast([128, 8, 16]), ALU.mult)
            for w_ in range(3):
                kb.tr(ps_r[:, w_, :], sel[:, w_, :], C["ident_f"][:], [sel, C["ident_f"]], [ps_r])
            rt = rt_sb.next()
            kb.act(rt[:], ps_r[:], AF.Copy, [ps_r], [rt])
            kb.dma(RTd[tt].ap, rt[:], [rt], [RTd[tt]], q="gpsimd")

        prev = None
        for tt in tiles:
            s_cur = front(tt)
            if prev is not None:
                chain(*prev)
            prev = (tt, s_cur)
        chain(*prev)


def peer_apply(kb, C, I, layer, MOD, groups, src_tile, NTd, RTd, UT, VB, epilogue):
    with kb.phase():
        QT_ = 32
        Gs_rot = Rot([kb.T([128, 384, 64], BF16, f"Gs{i}") for i in range(2)])
        A = Rot([kb.T([128, QT_, 64], BF16, f"A{i}") for i in range(2)])
        B = Rot([kb.T([128, QT_, 128], BF16, f"B{i}") for i in range(2)])
        nTg_rot = Rot([kb.T([128, 8, 384], BF16, f"nTg{i}") for i in range(2)])
        rt = Rot([kb.T([128, 3, 128], F32, f"rt{i}") for i in range(6)])
        rb = Rot([kb.T([128, 3, 128], BF16, f"rb{i}") for i in range(6)])
        uT = Rot([kb.T([128, 8, 128], BF16, f"uT{i}") for i in range(4)])
        vv = Rot([kb.T([128, D], BF16, f"vv{i}") for i in range(4)])
        ga = Rot([kb.T([128, 384], F32, f"ga{i}") for i in range(2)])
        W = Rot([kb.T([128, 384], BF16, f"W{i}") for i in range(4)])
        ep = epilogue("alloc", kb)
        acc = [[kb.psv(2 * t + h, 1, [128, 512], F32, f"acc{t}{h}") for h in range(2)] for t in range(3)]
        ps_a = [kb.psv(6 + i, 1, [128, 384], F32, f"ps_a{i}") for i in range(2)]
        ps_G = [kb.psv(6 + i, 1, [128, 8, 64], F32, f"ps_G{i}", root=ps_a[i]) for i in range(2)]
        ps_rot = Rot([0, 1])
        iota_bf = C["iota_bf"]

        def load_group(tiles):
            nTg = nTg_rot.next()
            rs = []
            for gi, tt in enumerate(tiles):
                kb.dma(nTg[:, :, gi * 128:(gi + 1) * 128], NTd[tt].ap, [NTd[tt]], [nTg], q="gpsimd")
                r = rt.next(); rb_ = rb.next()
                kb.dma(r[:], RTd[tt].ap, [RTd[tt]], [r], q="gpsimd")
                kb.v("gpsimd", "tensor_copy", [r], [rb_], rb_[:], r[:])
                rs.append((r, rb_))
            return {"tiles": tiles, "nTg": nTg, "rs": rs}

        def gate_units(G, hf):
            Gs = Gs_rot.next()
            units = []
            for gi in range(len(G["tiles"])):
                r, rb_ = G["rs"][gi]
                for qt in range(128 // QT_):
                    def build(gi=gi, qt=qt, rb_=rb_):
                        ts_ = slice(qt * QT_, (qt + 1) * QT_)
                        a_ = A.next(); b_ = B.next()
                        kb.v("vector", "tensor_tensor", [rb_, iota_bf], [a_], a_[:],
                             iota_bf[:, hf * 64:(hf + 1) * 64].unsqueeze(1).to_broadcast([128, QT_, 64]),
                             rb_[:, 0, ts_].unsqueeze(2).to_broadcast([128, QT_, 64]), ALU.is_equal)
                        kb.v("vector", "tensor_tensor", [a_, rb_], [a_], a_[:], a_[:],
                             rb_[:, 2, ts_].unsqueeze(2).to_broadcast([128, QT_, 64]), ALU.mult)
                        kb.v("vector", "tensor_tensor", [rb_, iota_bf], [b_], b_[:],
                             iota_bf[:].unsqueeze(1).to_broadcast([128, QT_, 128]),
                             rb_[:, 1, ts_].unsqueeze(2).to_broadcast([128, QT_, 128]), ALU.is_equal)
                        return a_, b_

                    def mmpart(ab, gi=gi, qt=qt):
                        a_, b_ = ab
                        for t8 in range(QT_ // 8):
                            pg = ps_G[ps_rot.next()]
                            for u in range(8):
                                t = t8 * 8 + u
                                kb.mm(pg[:, u, :], b_[:, t, :], a_[:, t, :], [a_, b_], [pg])
                            tok0 = gi * 128 + qt * QT_ + t8 * 8
                            kb.act(Gs[:, tok0:tok0 + 8, :], pg[:], AF.Copy, [pg], [Gs])
                    units.append((build, mmpart))
            return Gs, units

        class Stagger:
            def __init__(self, units):
                self.units = list(units)
                self.built = None
                self.n = 0

            def step(self):
                if self.n > len(self.units):
                    return False
                nb_ = self.units[self.n][0]() if self.n < len(self.units) else None
                if self.built is not None:
                    self.units[self.n - 1][1](self.built)
                self.built = nb_
                self.n += 1
                return self.n <= len(self.units)

            def drain(self):
                while self.step():
                    pass

        sched = [(g, hf) for g in range(len(groups)) for hf in range(2)]
        Gstate = {0: load_group(groups[0])}
        Gs_cur, units = gate_units(Gstate[0], 0)
        Stagger(units).drain()
        pend = []
        LAG = 2
        for si, (g, hf) in enumerate(sched):
            G = Gstate[g]
            tiles = G["tiles"]
            ng = len(tiles)
            ntok = ng * 128
            nTg = G["nTg"]
            nxt_units, Gs_next = [], None
            if si + 1 < len(sched):
                g2, hf2 = sched[si + 1]
                if g2 not in Gstate:
                    Gstate[g2] = load_group(groups[g2])
                Gs_next, nxt_units = gate_units(Gstate[g2], hf2)
            stg = Stagger(nxt_units)
            every = max(1, 60 // (len(nxt_units) + 1))

            def U_(i, Gs=Gs_cur):
                u_ = uT.next(); v_ = vv.next()
                kb.dma(u_[:], UT[i].ap, [UT[i]], [u_])
                kb.dma(v_[:], VB[i].ap, [VB[i]], [v_])
                pa = ps_a[ps_rot.next()]
                for k in range(8):
                    kb.mm(pa[:, 0:ntok], u_[:, k, :], nTg[:, k, 0:ntok], [u_, nTg], [pa], start=(k == 0), stop=(k == 7))
                g_ = ga.next()
                kb.act(g_[:, 0:ntok], pa[:, 0:ntok], AF.Gelu, [pa], [g_])
                w_ = W.next()
                eng = "vector" if i % 2 == 0 else "gpsimd"
                kb.v(eng, "tensor_tensor", [g_, Gs], [w_], w_[:, 0:ntok], g_[:, 0:ntok], Gs[:, 0:ntok, i % 64], ALU.mult)
                return (i, w_, v_, ng)

            def V_(st):
                i, w_, v_, ng_ = st
                for gi in range(ng_):
                    for h in range(2):
                        kb.mm(acc[gi][h][:], w_[:, gi * 128:(gi + 1) * 128], v_[:, h * 512:(h + 1) * 512], [w_, v_],
                              [acc[gi][h]], start=(i == 0), stop=(i == 127))

            for ii in range(64):
                pend.append(U_(hf * 64 + ii))
                if len(pend) > LAG:
                    V_(pend.pop(0))
                if ii % every == every - 1:
                    stg.step()
            if hf == 1:
                while pend:
                    V_(pend.pop(0))
                for gi, tt in enumerate(tiles):
                    epilogue("run", kb, tt, acc[gi], ep)
            stg.drain()
            Gs_cur = Gs_next


def make_epilogue(I, MOD, src_tile, dst_tile, final=False):
    def ep(mode, kb, tt=None, acc=None, b=None):
        if mode == "alloc":
            b = {"M5": [kb.T([128, D], F32, f"M5_{i}") for i in range(2)],
                 "hres": kb.T([128, D], F32, "hres"), "o": kb.T([128, D], F32, "o")}
            for r in range(2):
                load_bcast(kb, b["M5"][r], MOD.ap[r, 5 * D:6 * D], [MOD])
            if final:
                b["fg"] = kb.T([128, D], F32, "fg")
                load_bcast(kb, b["fg"], I["norm_f_g"], [I["_norm_f_g"]])
                b["junk"] = kb.T([128, D], BF16, "junkf")
                b["ss"] = kb.T([128, 1], F32, "ssf")
                b["rstd"] = kb.T([128, 1], F32, "rstdf")
            return b
        isc = 1 if tt < 2 else 0
        sap, sbuf_ = src_tile(tt)
        hres, o, M5 = b["hres"], b["o"], b["M5"][isc]
        kb.dma(hres[:], sap, [sbuf_], [hres])
        for h in range(2):
            hs = slice(h * 512, (h + 1) * 512)
            kb.v("vector", "tensor_tensor", [acc[h], M5], [o], o[:, hs], acc[h][:], M5[:, hs], ALU.mult)
        kb.v("gpsimd", "tensor_tensor", [o, hres], [o], o[:], o[:], hres[:], ALU.add)
        dap, dbuf = dst_tile(tt)
        if final:
            kb.act(b["junk"][:], o[:], AF.Square, [o], [b["junk"], b["ss"]], accum_out=b["ss"][:])
            rstd_of(kb, b["ss"], b["rstd"], D)
            kb.v("vector", "scalar_tensor_tensor", [o, b["rstd"], b["fg"]], [o], o[:], o[:], b["rstd"][:, 0:1],
                 b["fg"][:], ALU.mult, ALU.mult)
        kb.dma(dap, o[:], [o], [dbuf], q="gpsimd")
    return ep


def tile_groups(tiles, n=3):
    return [tiles[i:i + n] for i in range(0, len(tiles), n)]


INPUT_SPECS = [
    ("x", [SEQ, D]), ("c", [D]), ("ctx", [NCTX, D]), ("c_ctx", [D]),
    ("norm1_g", [2, D]), ("norm2_g", [2, D]), ("w_mod", [2, D, 6 * D]), ("b_mod", [2, 6 * D]),
    ("even_w_in", [1, D, 1952]), ("mla_q_norm", [1, 256]), ("mla_kv_norm", [1, 128]),
    ("mla_w_uq", [1, 256, 768]), ("mla_w_ukv", [1, 128, 1024]), ("conv_w", [1, 3, 512]),
    ("even_w_out", [1, D, D]), ("odd_w_in", [1, D, 3088]), ("mlstm_gate_b", [1, 16]),
    ("mlstm_head_g", [1, D]), ("odd_w_out", [1, D, D]), ("peer_w_q", [2, D, 2048]),
    ("peer_subkeys", [2, 8, 2, 128, 128]), ("peer_u", [2, 16384, D]), ("peer_v", [2, 16384, D]),
    ("norm_f_g", [D]), ("rope", [SEQ, 32]),
]


def build_program(stop_after=None, dbg=()):
    nc = bass.Bass("TRN2", target_bir_lowering=False)
    kb = KB(nc, dbg)
    kb.stop_after = stop_after
    I = {}
    for name, shape in INPUT_SPECS:
        ap = nc.dram_tensor(name, shape, F32, kind="ExternalInput").ap()
        I[name] = ap
        I["_" + name] = Buf(ap, name)
    out = nc.dram_tensor("y", [SEQ, D], F32, kind="ExternalOutput").ap()
    kb.eps_t = kb.T([128, 1], F32, "eps")
    kb.v("gpsimd", "memset", [], [kb.eps_t], kb.eps_t[:], EPS)
    kb.one_t = kb.T([128, 1], F32, "one")
    kb.v("gpsimd", "memset", [], [kb.one_t], kb.one_t[:], 1.0)
    C = make_consts(kb)
    MOD = [Buf(kb.dram(f"MOD{l}", [2, 6 * D], F32), f"MOD{l}") for l in range(2)]
    H0 = kb.dram_tiles("H0", NT_ALL, [128, D], F32)

    def src0(tt):
        if tt < 2:
            return I["ctx"][tt * 128:(tt + 1) * 128, :], I["_ctx"]
        return I["x"][(tt - 2) * 128:(tt - 1) * 128, :], I["_x"]

    phase_mod(kb, C, I, 0, MOD[0])
    if stop_after == "mod0":
        kb.finish()
        return nc, kb
    layer_even(kb, C, I, 0, MOD[0], src0, H0)
    if stop_after in ("even", "E1"):
        kb.finish()
        return nc, kb
    UT = kb.dram_tiles("UT", 128, [128, 8, 128], BF16)
    VB = kb.dram_tiles("VB", 128, [128, D], BF16)
    NTd = kb.dram_tiles("NTd", NT_ALL, [128, 8, 128], BF16)
    RTd = kb.dram_tiles("RTd", NT_ALL, [128, 3, 128], F32)
    H1 = kb.dram_tiles("H1", NT_ALL, [128, D], F32)
    srcH0 = lambda tt: (H0[tt].ap, H0[tt])
    dstH1 = lambda tt: (H1[tt].ap, H1[tt])
    tiles0 = DBG_PTILES or list(range(NT_ALL))
    peer_prep(kb, C, I, 0, UT, VB)
    peer_route(kb, C, I, 0, MOD[0], tiles0, srcH0, NTd, RTd)
    if stop_after == "route0":
        kb.finish()
        return nc, kb
    peer_apply(kb, C, I, 0, MOD[0], tile_groups(tiles0), srcH0, NTd, RTd, UT, VB,
               make_epilogue(I, MOD[0], srcH0, dstH1))
    if stop_after == "peer0":
        kb.finish()
        return nc, kb
    phase_mod(kb, C, I, 1, MOD[1])
    H2 = kb.dram_tiles("H2", NT_ALL, [128, D], F32)
    srcH1 = lambda tt: (H1[tt].ap, H1[tt])
    layer_odd(kb, C, I, 1, MOD[1], srcH1, H2,
              bg=lambda ps: peer_prep_units(kb, C, I, 1, UT, VB, ps))
    if stop_after == "odd":
        kb.finish()
        return nc, kb
    srcH2 = lambda tt: (H2[tt].ap, H2[tt])
    outb = [Buf(out[(tt - 2) * 128:(tt - 1) * 128, :], f"y{tt}") for tt in range(NT_ALL)]
    dstY = lambda tt: (outb[tt].ap, outb[tt])
    tiles1 = list(range(2, NT_ALL))
    peer_route(kb, C, I, 1, MOD[1], tiles1, srcH2, NTd, RTd)
    peer_apply(kb, C, I, 1, MOD[1], tile_groups(tiles1), srcH2, NTd, RTd, UT, VB,
               make_epilogue(I, MOD[1], srcH2, dstY, final=True))
    kb.finish()
    return nc, kb


def rope_table():
    n_freq = 8
    inv = (10000.0 ** (-np.arange(n_freq, dtype=np.float32) / n_freq)).astype(np.float32)
    t = np.arange(SEQ)
    row = (t // 64).astype(np.float32)
    col = (t % 64).astype(np.float32)
    ang = np.concatenate([row[:, None] * inv, col[:, None] * inv], axis=-1).astype(np.float32)
    return np.concatenate([np.cos(ang), np.sin(ang)], axis=-1).astype(np.float32)


def make_in_maps(inputs, cores):
    shared = {k: np.ascontiguousarray(np.asarray(v, dtype=np.float32)) for k, v in inputs.items()
              if k not in ("x", "c", "ctx")}
    shared["rope"] = rope_table()
    maps = []
    for b in cores:
        m = dict(shared)
        m["x"] = np.ascontiguousarray(inputs["x"][b])
        m["c"] = np.ascontiguousarray(inputs["c"][b])
        m["ctx"] = np.ascontiguousarray(inputs["ctx"][b])
        maps.append(m)
    return maps


def kernel(**inputs):
    nc, kb = build_program()
    maps = make_in_maps(inputs, list(range(8)))
    res = run_bass_kernel_spmd(nc, maps, core_ids=list(range(8)))
    return np.stack([r["y"] for r in res.results], axis=0).astype(np.float32)
```
